# Optimizing a Trainium2 kernel written in Bass

```python
import math
import jax, jax.numpy as jnp
from jax import lax
import numpy as np

D_MODEL = 1024
BATCH = 32
SEQ = 2048
DEPTH = 1

RWKV_HEAD = 64
RWKV_HEADS = 8
RWKV_DIM = RWKV_HEADS * RWKV_HEAD
W_LORA = 64
A_LORA = 64
G_LORA = 128
DECAY_SCALE = math.exp(-0.5)
LNX_EPS = 64e-5
ATT_HEADS = 8
ATT_KV_HEADS = 2
ATT_HEAD = 64
ATT_DIM = ATT_HEADS * ATT_HEAD
KV_DIM = ATT_KV_HEADS * ATT_HEAD
WINDOW = 128
BLOCK = 128
NEG_INF = -1e30
N_EXPERTS = 16
CAPACITY_FACTOR = 2
EXPERT_FF = 1024
NORM_EPS = 1e-6

RWKV_SPLITS = (RWKV_DIM, 2 * RWKV_DIM, 3 * RWKV_DIM, 3 * RWKV_DIM + G_LORA,
               3 * RWKV_DIM + G_LORA + W_LORA, 3 * RWKV_DIM + G_LORA + 2 * W_LORA,
               3 * RWKV_DIM + G_LORA + 2 * W_LORA + A_LORA)
RWKV_COLS = 3 * RWKV_DIM + G_LORA + 2 * W_LORA + 2 * A_LORA
ATT_SPLITS = (ATT_DIM, ATT_DIM + KV_DIM, ATT_DIM + 2 * KV_DIM, ATT_DIM + 2 * KV_DIM + D_MODEL)
IN_COLS = RWKV_COLS + ATT_DIM + 2 * KV_DIM + 2 * D_MODEL

kernel_name = 'hybrid_rwkv7_swa_ec_moe'


def rms_norm(x, g):
    xf = x.astype(jnp.float32)
    y = xf * lax.rsqrt(jnp.mean(xf * xf, axis=-1, keepdims=True) + NORM_EPS)
    return (y * g.astype(jnp.float32)).astype(x.dtype)


def centred_token_shift(u, mu_prev, mu_next):
    prev = jnp.pad(u, ((0, 0), (1, 0), (0, 0)))[:, :-1]
    nxt = jnp.pad(u, ((0, 0), (0, 1), (0, 0)))[:, 1:]
    return u + mu_prev * (prev - u) + mu_next * (nxt - u)


def wkv7_scan(r, decay, k, v, kk, a, reverse):
    B, T, H, N = r.shape
    xs = tuple(jnp.moveaxis(t.astype(jnp.float32), 1, 0) for t in (r, decay, k, v, kk, a))

    def step(S, inp):
        r_t, w_t, k_t, v_t, kk_t, a_t = inp
        s_kk = jnp.einsum('bhij,bhj->bhi', S, kk_t)
        S = (S * w_t[:, :, None, :]
             - s_kk[..., None] * (a_t * kk_t)[:, :, None, :]
             + v_t[..., None] * k_t[:, :, None, :])
        return S, jnp.einsum('bhij,bhj->bhi', S, r_t)

    S0 = jnp.zeros((B, H, N, N), jnp.float32)
    _, y = lax.scan(step, S0, xs, reverse=reverse)
    return jnp.moveaxis(y, 0, 1)


def rwkv7_bidir(u, w0_f, w_up_f, w0_b, w_up_b, a0_f, a_up_f, a0_b, a_up_b, g_up, k_k, k_a, r_k, ln_w, ln_b):
    f32 = jnp.float32
    B, T, _ = u.shape
    r, k, v, g_lo, wf_lo, wb_lo, af_lo, ab_lo = jnp.split(u, RWKV_SPLITS, axis=-1)

    def heads(t):
        return t.reshape(B, T, RWKV_HEADS, RWKV_HEAD)

    def decay(w0, lo, up):
        return jnp.exp(-DECAY_SCALE * jax.nn.sigmoid((w0 + jnp.tanh(lo) @ up).astype(f32)))

    def icl_rate(a0, lo, up):
        return jax.nn.sigmoid((a0 + lo @ up).astype(f32))

    kf = k.astype(f32)
    kk = heads(kf * k_k.astype(f32))
    kk = kk / jnp.maximum(jnp.sqrt(jnp.sum(kk * kk, axis=-1, keepdims=True)), 1e-12)
    a_f = icl_rate(a0_f, af_lo, a_up_f)
    a_b = icl_rate(a0_b, ab_lo, a_up_b)
    k_f = heads(kf * (1.0 + (a_f - 1.0) * k_a.astype(f32)))
    k_b = heads(kf * (1.0 + (a_b - 1.0) * k_a.astype(f32)))
    r4 = heads(r.astype(f32))
    v4 = heads(v.astype(f32))

    wkv = (wkv7_scan(r4, heads(decay(w0_f, wf_lo, w_up_f)), k_f, v4, kk, heads(a_f), False)
           + wkv7_scan(r4, heads(decay(w0_b, wb_lo, w_up_b)), k_b, v4, kk, heads(a_b), True))

    mean = jnp.mean(wkv, axis=-1, keepdims=True)
    var = jnp.mean(jnp.square(wkv - mean), axis=-1, keepdims=True)
    normed = ((wkv - mean) * lax.rsqrt(var + LNX_EPS)).reshape(B, T, RWKV_DIM)
    normed = normed * ln_w.astype(f32) + ln_b.astype(f32)

    rk = r_k.astype(f32)
    bonus = ((jnp.sum(r4 * k_f * rk, axis=-1, keepdims=True)
              + jnp.sum(r4 * k_b * rk, axis=-1, keepdims=True)) * v4).reshape(B, T, RWKV_DIM)
    g = (jax.nn.sigmoid(g_lo) @ g_up).astype(f32)
    return ((normed + bonus) * g).astype(u.dtype)


def alibi_slopes(n_heads):
    return 2.0 ** (-8.0 * np.arange(1, n_heads + 1) / n_heads)


def banded_gqa_attention(q, k, v, sink):
    f32 = jnp.float32
    B, T, HQ, DH = q.shape
    G = HQ // ATT_KV_HEADS
    nb = T // BLOCK
    span = BLOCK + 2 * WINDOW
    q = q.reshape(B, T, ATT_KV_HEADS, G, DH) * (DH ** -0.5)
    kp = jnp.pad(k, ((0, 0), (WINDOW, WINDOW), (0, 0), (0, 0)))
    vp = jnp.pad(v, ((0, 0), (WINDOW, WINDOW), (0, 0), (0, 0)))
    slopes = jnp.asarray(alibi_slopes(HQ), f32).reshape(ATT_KV_HEADS, G)
    sink_l = sink.astype(f32).reshape(1, ATT_KV_HEADS, G, 1, 1)
    l_idx = jnp.arange(BLOCK)
    j_idx = jnp.arange(span)
    rel = j_idx[None, :] - WINDOW - l_idx[:, None]
    in_window = jnp.abs(rel) <= WINDOW
    alibi = -slopes[:, :, None, None] * jnp.abs(rel).astype(f32)

    def one_block(i):
        start = i * BLOCK
        qb = lax.dynamic_slice_in_dim(q, start, BLOCK, axis=1)
        kb = lax.dynamic_slice_in_dim(kp, start, span, axis=1)
        vb = lax.dynamic_slice_in_dim(vp, start, span, axis=1)
        key_pos = start - WINDOW + j_idx
        valid = in_window & ((key_pos >= 0) & (key_pos < T))[None, :]
        s = jnp.einsum('blkgd,bjkd->bkglj', qb, kb).astype(f32) + alibi
        s = jnp.where(valid, s, NEG_INF)
        m = jnp.maximum(jnp.max(s, axis=-1, keepdims=True), sink_l)
        p = jnp.exp(s - m)
        denom = jnp.sum(p, axis=-1, keepdims=True) + jnp.exp(sink_l - m)
        return jnp.einsum('bkglj,bjkd->blkgd', (p / denom).astype(vb.dtype), vb)

    out = lax.map(one_block, jnp.arange(nb))
    return jnp.moveaxis(out, 0, 1).reshape(B, T, HQ * DH)


def expert_choice_moe(h, w_router, w_gate, w_up, w_down):
    B, T, D = h.shape
    cap = CAPACITY_FACTOR * T // N_EXPERTS
    affinity = jax.nn.softmax((h @ w_router).astype(jnp.float32), axis=-1)
    top_vals, top_idx = lax.top_k(jnp.swapaxes(affinity, 1, 2), cap)
    b_idx = jnp.arange(B)[:, None, None]
    xs = h[b_idx, top_idx]
    hid = jax.nn.silu(jnp.einsum('becd,edf->becf', xs, w_gate)) * jnp.einsum('becd,edf->becf', xs, w_up)
    ys = jnp.einsum('becf,efd->becd', hid, w_down) * top_vals[..., None].astype(h.dtype)
    return jnp.zeros_like(h).at[b_idx, top_idx].add(ys)


def setup_inputs(seed: int = 0) -> dict:
    key = jax.random.key(seed)
    ks = jax.random.split(key, 32)
    L, D, f32 = DEPTH, D_MODEL, jnp.float32
    nrm = lambda k, shape, s: (jax.random.normal(k, shape, f32) * s)
    return {
        'x': nrm(ks[0], (BATCH, SEQ, D), 1.0),
        'norm_mix_g': 1.0 + nrm(ks[1], (L, D), 0.1),
        'w_in': nrm(ks[2], (L, D, IN_COLS), D ** -0.5),
        'mu_prev': jax.random.uniform(ks[3], (L, RWKV_COLS), f32, 0.05, 0.45),
        'mu_next': jax.random.uniform(ks[4], (L, RWKV_COLS), f32, 0.05, 0.45),
        'w0_f': nrm(ks[5], (L, RWKV_DIM), 1.0),
        'w_up_f': nrm(ks[6], (L, W_LORA, RWKV_DIM), 0.5 * W_LORA ** -0.5),
        'w0_b': nrm(ks[7], (L, RWKV_DIM), 1.0),
        'w_up_b': nrm(ks[8], (L, W_LORA, RWKV_DIM), 0.5 * W_LORA ** -0.5),
        'a0_f': nrm(ks[9], (L, RWKV_DIM), 0.5),
        'a_up_f': nrm(ks[10], (L, A_LORA, RWKV_DIM), 0.5 * A_LORA ** -0.5),
        'a0_b': nrm(ks[11], (L, RWKV_DIM), 0.5),
        'a_up_b': nrm(ks[12], (L, A_LORA, RWKV_DIM), 0.5 * A_LORA ** -0.5),
        'g_up': nrm(ks[13], (L, G_LORA, RWKV_DIM), G_LORA ** -0.5),
        'k_k': 0.85 + nrm(ks[14], (L, RWKV_DIM), 0.05),
        'k_a': 1.0 + nrm(ks[15], (L, RWKV_DIM), 0.05),
        'r_k': nrm(ks[16], (L, RWKV_HEADS, RWKV_HEAD), 0.1),
        'ln_x_w': 1.0 + nrm(ks[17], (L, RWKV_DIM), 0.1),
        'ln_x_b': nrm(ks[18], (L, RWKV_DIM), 0.02),
        'attn_sink': nrm(ks[19], (L, ATT_HEADS), 0.5),
        'w_proj_rwkv': nrm(ks[20], (L, RWKV_DIM, D), RWKV_DIM ** -0.5),
        'w_proj_attn': nrm(ks[21], (L, ATT_DIM, D), ATT_DIM ** -0.5),
        'w_out': nrm(ks[22], (L, D, D), D ** -0.5),
        'norm_ffn_g': 1.0 + nrm(ks[23], (L, D), 0.1),
        'w_router': nrm(ks[24], (L, D, N_EXPERTS), D ** -0.5),
        'exp_w_gate': nrm(ks[25], (L, N_EXPERTS, D, EXPERT_FF), D ** -0.5),
        'exp_w_up': nrm(ks[26], (L, N_EXPERTS, D, EXPERT_FF), D ** -0.5),
        'exp_w_down': nrm(ks[27], (L, N_EXPERTS, EXPERT_FF, D), EXPERT_FF ** -0.5),
        'norm_final_g': 1.0 + nrm(ks[28], (D,), 0.1),
    }


def reference(x, norm_mix_g, w_in, mu_prev, mu_next, w0_f, w_up_f, w0_b, w_up_b, a0_f, a_up_f, a0_b, a_up_b,
              g_up, k_k, k_a, r_k, ln_x_w, ln_x_b, attn_sink, w_proj_rwkv, w_proj_attn, w_out, norm_ffn_g,
              w_router, exp_w_gate, exp_w_up, exp_w_down, norm_final_g):
    B, T, _ = x.shape
    for layer in range(DEPTH):
        h = rms_norm(x, norm_mix_g[layer])
        u = h @ w_in[layer]
        u_rwkv = centred_token_shift(u[..., :RWKV_COLS], mu_prev[layer], mu_next[layer])
        q, k_att, v_att, gl_rwkv, gl_attn = jnp.split(u[..., RWKV_COLS:], ATT_SPLITS, axis=-1)
        y_rwkv = rwkv7_bidir(u_rwkv, w0_f[layer], w_up_f[layer], w0_b[layer], w_up_b[layer],
                             a0_f[layer], a_up_f[layer], a0_b[layer], a_up_b[layer], g_up[layer],
                             k_k[layer], k_a[layer], r_k[layer], ln_x_w[layer], ln_x_b[layer])
        y_attn = banded_gqa_attention(q.reshape(B, T, ATT_HEADS, ATT_HEAD),
                                      k_att.reshape(B, T, ATT_KV_HEADS, ATT_HEAD),
                                      v_att.reshape(B, T, ATT_KV_HEADS, ATT_HEAD), attn_sink[layer])
        merged = (jax.nn.sigmoid(gl_rwkv) * (y_rwkv @ w_proj_rwkv[layer])
                  + jax.nn.sigmoid(gl_attn) * (y_attn @ w_proj_attn[layer]))
        x = x + merged @ w_out[layer]
        x = x + expert_choice_moe(rms_norm(x, norm_ffn_g[layer]), w_router[layer],
                                  exp_w_gate[layer], exp_w_up[layer], exp_w_down[layer])
    return rms_norm(x, norm_final_g)
```

```python
import math
from contextlib import ExitStack
import numpy as np
import concourse.bass as bass
import concourse.mybir as mybir
from concourse.bass_utils import run_bass_kernel_spmd

F32 = mybir.dt.float32
BF16 = mybir.dt.bfloat16
U32 = mybir.dt.uint32
I32 = mybir.dt.int32
AF = mybir.ActivationFunctionType
ALU = mybir.AluOpType
AX = mybir.AxisListType

ENGS = ("pe", "dve", "act", "pool", "sp")
SEM_WRAP = 4000
NDMA_SEM = 12
NBANK = {'pe': 24, 'dve': 12, 'act': 12, 'pool': 6, 'sp': 2}

D = 1024
DS = math.exp(-0.5)
LNX_EPS = 64e-5
NORM_EPS = 1e-6
NEXP = 16
L = 64


class Buf:
    __slots__ = ("name", "w", "r")

    def __init__(self, name):
        self.name = name
        self.w = None
        self.r = []


class Op:
    __slots__ = ("eng", "fn", "waits", "signal", "idx", "dma", "tk", "sigval", "gid", "ninc")

    def __init__(self, eng, fn, dma):
        self.eng = eng
        self.fn = fn
        self.waits = []
        self.signal = False
        self.dma = dma
        self.tk = None
        self.sigval = None


class Prog:
    def __init__(self, nc, stack):
        self.nc = nc
        self.bufs = []
        self.sigcount = {e: 0 for e in ENGS}
        self.ndma = {e: 0 for e in ENGS}
        self.sems = {e: [stack.enter_context(nc.semaphore(f"s_{e}_{i}")) for i in range(NBANK[e])] for e in ENGS}
        self.dsems = {e: [stack.enter_context(nc.semaphore(f"d_{e}_{i}")) for i in range(NDMA_SEM)]
                      for e in ("sp", "pool", "act")}
        self.gid = 0
        self._reset()

    def _reset(self):
        self.q = {e: [] for e in ENGS}
        self.seen = {e: {} for e in ENGS}
        self.seen_dma = {e: set() for e in ENGS}
        self.dma_hist = {e: {} for e in ENGS}
        self.pending_dma = []
        for b in self.bufs:
            b.w = None
            b.r = []

    def buf(self, name="b"):
        b = Buf(name)
        self.bufs.append(b)
        return b

    def _dep(self, op, d):
        eng = op.eng
        if d is op:
            return
        if d.dma:
            if d.gid in self.seen_dma[eng]:
                return
            self.seen_dma[eng].add(d.gid)
            op.waits.append(d)
        else:
            if d.eng == eng and eng == "pe":
                return
            if self.seen[eng].get(d.eng, -1) >= d.idx:
                return
            self.seen[eng][d.eng] = d.idx
            d.signal = True
            op.waits.append(d)

    capture = None

    def emit(self, eng, fn, reads=(), writes=(), dma=False, ninc=1, est=None, hold=False):
        if self.capture is not None:
            self.capture.append((eng, fn, tuple(reads), tuple(writes), dma, est, hold))
            return None
        op = Op(eng, fn, dma)
        op.ninc = ninc
        op.gid = self.gid
        self.gid += 1
        op.idx = len(self.q[eng])
        deps = []
        for b in reads:
            if b.w is not None:
                deps.append(b.w)
        for b in writes:
            if b.w is not None:
                deps.append(b.w)
            deps.extend(b.r)
        for d in deps:
            self._dep(op, d)
        if dma:
            j = self.ndma[eng]
            self.ndma[eng] += 1
            op.tk = (j % NDMA_SEM, 16 * (j // NDMA_SEM + 1))
            hist = self.dma_hist[eng]
            if (j - NDMA_SEM) in hist:
                self._dep(op, hist[j - NDMA_SEM])
            hist[j] = op
            self.pending_dma.append(op)
        for b in reads:
            if not dma:
                b.r = [x for x in b.r if x.dma or x.eng != eng]
            b.r.append(op)
        for b in writes:
            b.w = op
            b.r = []
        self.q[eng].append(op)
        return op

    def barrier(self):
        pend = self.pending_dma
        self.pending_dma = []
        last = []
        for e in ENGS:
            for op in reversed(self.q[e]):
                if not op.dma and not getattr(op.fn, "_is_nop", False):
                    last.append(op)
                    break
        for e in ENGS:
            fn = lambda en: en.nop()
            op = self.emit(e, fn)
            for d in last:
                self._dep(op, d)
            for d in pend:
                self._dep(op, d)

    def stage_end(self):
        self.barrier()

    def flush(self):
        self.barrier()
        nc = self.nc
        base = dict(self.sigcount)
        for e in ENGS:
            c = base[e]
            for op in self.q[e]:
                if op.signal:
                    assert (c % SEM_WRAP) + op.ninc <= SEM_WRAP
                    c += op.ninc
                    op.sigval = c
            self.sigcount[e] = c
        sems, dsems = self.sems, self.dsems

        def resolve(d):
            if d.dma:
                return dsems[d.eng][d.tk[0]], d.tk[1]
            v = d.sigval - 1
            assert v // SEM_WRAP < NBANK[d.eng], (d.eng, v)
            return sems[d.eng][v // SEM_WRAP], v % SEM_WRAP + 1

        def run(e, engobj):
            for op in self.q[e]:
                for d in op.waits:
                    s, v = resolve(d)
                    engobj.wait_ge(s, v)
                ins = op.fn(engobj)
                if op.dma:
                    ins.then_inc(dsems[e][op.tk[0]], 16)
                elif op.signal:
                    v = op.sigval - 1
                    assert v // SEM_WRAP < NBANK[e], (e, v)
                    ins.then_inc(sems[e][v // SEM_WRAP], 1)

        with nc.Block() as block:
            @block.tensor
            def _(t):
                run("pe", t)

            @block.vector
            def _(v):
                run("dve", v)

            @block.scalar
            def _(s):
                run("act", s)

            @block.gpsimd
            def _(g):
                run("pool", g)

            @block.sync
            def _(sp):
                run("sp", sp)
        self._reset()


class Tl:
    def __init__(self, t, b):
        self.t = t
        self.b = b

    def __getitem__(self, k):
        return self.t[k]


class _Stop(Exception):
    pass


class Builder:
    def __init__(self, NS, T, stop=None, dbg=False):
        self.stop, self.dbg = stop, dbg
        self.NS, self.T = NS, T
        self.QS = min(512, T)
        self.NQ = T // self.QS
        self.TT = T // 128
        self.NCH = T // L
        self.CQ = self.QS // L
        self.CAP = 2 * T // NEXP
        self.SB = min(128, self.CAP)
        self.NB = self.CAP // self.SB
        self.nc = bass.Bass("TRN2", target_bir_lowering=False)
        self.stack = ExitStack()
        self.P = Prog(self.nc, self.stack)
        self.psi = 0

    def sb(self, st, name, shape, dt):
        self.nsb = getattr(self, "nsb", 0) + 1
        name = f"{name}__{self.nsb}"
        t = st.enter_context(self.nc.sbuf_tensor(name, list(shape), dt))
        return Tl(t, self.P.buf(name))

    def chk(self, k):
        if self.stop == k:
            self.P.flush()
            raise _Stop()

    def dram_in(self, name, shape, dt=F32):
        return self.nc.dram_tensor(name, list(shape), dt, kind="ExternalInput").ap()

    def nps(self, grp=None):
        if grp is None:
            p = self.ps[self.psi % 8]
            self.psi += 1
            return p
        banks = {"chain": (0, 1, 2, 3), "A": (4, 5), "B": (6, 7), "attO0": (0,), "attD0": (1,), "attO1": (2,), "attD1": (3,),
                 "attS": (4, 5, 6, 7)}[grp]
        self.psg = getattr(self, "psg", {})
        i = self.psg.get(grp, 0)
        self.psg[grp] = i + 1
        return self.ps[banks[i % len(banks)]]

    def mm(self, out, lhsT, rhs, start=True, stop=True, r=(), w=(), nohold=False):
        self.P.emit("pe", lambda e: e.matmul(out, lhsT=lhsT, rhs=rhs, start=start, stop=stop), r, w,
                    est=max(64, rhs.free_size()) / 2400.0 + 0.004, hold=(not stop) and not nohold)

    def tr(self, out, in_, ident, r=(), w=()):
        self.P.emit("pe", lambda e: e.transpose(out=out, in_=in_, identity=ident), r, w, est=0.06)

    def act(self, out, in_, func, bias=None, scale=None, accum=None, r=(), w=()):
        kw = {}
        if bias is not None:
            kw["bias"] = bias
        if scale is not None:
            kw["scale"] = scale
        if accum is not None:
            kw["accum_out"] = accum
        self.P.emit("act", lambda e: e.activation(out=out, in_=in_, func=func, **kw), r, w, ninc=1,
                    est=(224 + out.free_size()) / 1200.0)

    def tt(self, eng, out, in0, in1, op, r=(), w=()):
        self.P.emit(eng, lambda e: e.tensor_tensor(out=out, in0=in0, in1=in1, op=op), r, w, est=self._est(eng, out))

    def ts(self, eng, out, in0, s1, s2, op0, op1=None, r=(), w=()):
        if op1 is None:
            self.P.emit(eng, lambda e: e.tensor_scalar(out=out, in0=in0, scalar1=s1, scalar2=None, op0=op0), r, w,
                        est=self._est(eng, out))
        else:
            self.P.emit(eng, lambda e: e.tensor_scalar(out=out, in0=in0, scalar1=s1, scalar2=s2, op0=op0, op1=op1), r, w,
                        est=self._est(eng, out))

    def stt(self, out, in0, scalar, in1, op0, op1, r=(), w=()):
        self.P.emit("dve", lambda e: e.scalar_tensor_tensor(out=out, in0=in0, scalar=scalar, in1=in1, op0=op0, op1=op1), r, w,
                    est=self._est("dve", out))

    def cp(self, eng, out, in_, r=(), w=()):
        if eng == "act":
            self.P.emit("act", lambda e: e.activation(out=out, in_=in_, func=AF.Copy), r, w, est=(224 + out.free_size()) / 1200.0)
        else:
            self.P.emit(eng, lambda e: e.tensor_copy(out=out, in_=in_), r, w, est=self._est(eng, out))

    def ms(self, eng, ap, val, w=()):
        self.P.emit(eng, lambda e: e.memset(ap, val), (), w, est=self._est(eng, ap))

    def _est(self, eng, out):
        n = out.free_size()
        if eng == "pool":
            return (150 + 1.6 * n) / 1200.0
        return (100 + n) / 960.0

    def interleave(self, gens, window=None):
        P = self.P
        allg = [g for g in gens if g is not None]
        if window is None:
            window = len(allg)
        pending = allg[window:]
        streams = [{"g": g, "q": [], "done": False} for g in allg[:window]]
        eng_free = getattr(self, "_sim_eng", None)
        if eng_free is None:
            eng_free = self._sim_eng = {e: 0.0 for e in ENGS}
            self._sim_ready = {}
            self._sim_lastrd = {}
        ready, lastrd = self._sim_ready, self._sim_lastrd

        def fill(st_):
            while not st_["q"] and not st_["done"]:
                P.capture = st_["q"]
                try:
                    next(st_["g"])
                except StopIteration:
                    st_["done"] = True
                finally:
                    P.capture = None

        def start_time(o):
            eng, fn, rd, wr, dma, est, hold = o
            t = eng_free[eng]
            for b in rd:
                t = max(t, ready.get(b, (0.0, None))[0] + (0.15 if ready.get(b, (0.0, eng))[1] != eng else 0.05))
            for b in wr:
                t = max(t, ready.get(b, (0.0, None))[0] + 0.05, lastrd.get(b, 0.0) + 0.1)
            return t

        forced = None
        while True:
            for st_ in streams:
                fill(st_)
            for i_, st_ in enumerate(streams):
                if st_["done"] and not st_["q"] and pending:
                    streams[i_] = {"g": pending.pop(0), "q": [], "done": False}
                    fill(streams[i_])
            live = [st_ for st_ in streams if st_["q"]]
            if not live:
                break
            if forced is not None and forced["q"]:
                best = forced
            else:
                best = min(live, key=lambda st_: start_time(st_["q"][0]))
            o = best["q"].pop(0)
            eng, fn, rd, wr, dma, est, hold = o
            forced = best if hold else None
            t0 = start_time(o)
            dur = est if est is not None else 0.3
            if dma:
                eng_free[eng] = t0 + 0.06
                tend = t0 + 2.5
            else:
                eng_free[eng] = t0 + dur
                tend = t0 + dur
            for b in rd:
                lastrd[b] = max(lastrd.get(b, 0.0), tend)
            for b in wr:
                ready[b] = (tend, eng)
                lastrd[b] = 0.0
            P.emit(eng, fn, rd, wr, dma=dma)

    def dma(self, out, in_, r=(), w=(), eng="sp"):
        return self.P.emit(eng, lambda e: e.dma_start(out=out, in_=in_), r, w, dma=True)

    def load_w(self, dst, dst_b, src, kc=None, n=None, scale=None):
        assert scale is None
        self.dma(dst, src, w=[dst_b], eng="pool")

    def build(self):
        nc, P, NS, T = self.nc, self.P, self.NS, self.T
        QS, NQ, TT, NCH, CQ = self.QS, self.NQ, self.TT, self.NCH, self.CQ
        st0 = self.stack
        din = self.dram_in
        x = din("x", [NS * T, D])
        w_in = din("w_in", [D, 4736]).rearrange("(k p) n -> p k n", p=128)
        gmixr_d = din("gmixr", [1, D])
        g2r_d = din("g2r", [1, D])
        mu_d = din("mu", [128, 15, 2])
        pp_d = din("pp", [128, 4, 9])
        sink_d = din("sink", [1, 8])
        g2_d = din("g2", [128, 8])
        gF_d = din("gF", [1, D])
        wup_d = din("wup", [128, 512])
        aup_d = din("aup", [128, 512])
        gup_d = din("gup", [128, 512])
        wpr_d = din("wpr", [512, D]).rearrange("(k p) n -> p k n", p=128)
        wpa_d = din("wpa", [512, D]).rearrange("(k p) n -> p k n", p=128)
        wout_d = din("wout", [D, D]).rearrange("(k p) n -> p k n", p=128)
        wr_d = din("wr", [D, NEXP]).rearrange("(k p) n -> p k n", p=128)
        eg_d = din("eg", [NEXP, D, D])
        eu_d = din("eu", [NEXP, D, D])
        ed_d = din("ed", [NEXP, D, D])
        c_idf = din("c_idf", [128, 128])
        c_bo = din("c_bo", [128, 128])
        c_ba = din("c_ba", [128, 128])
        c_mT = din("c_mT", [128, 2, 512])
        c_mN = din("c_mN", [128, 2, 512])
        c_idr = din("c_idr", [128, 512])
        c_bias = din("c_bias", [128, 6, 512])
        c_offs = din("c_offs", [128, 128])
        y = nc.dram_tensor("y", [NS * T, D], F32, kind="ExternalOutput").ap()
        ik = "ExternalOutput" if self.dbg else "Internal"
        uS = nc.dram_tensor("uS", [15, 128, T], BF16, kind=ik).ap()
        acc = nc.dram_tensor("acc", [NS * T, D], F32, kind=ik).ap()
        h2d = nc.dram_tensor("h2d", [NS * T, D], BF16, kind="Internal").ap()
        if self.dbg:
            self.dbgR = nc.dram_tensor("dbgR", [128, 4, T], BF16, kind="ExternalOutput").ap()
            self.dbgA = nc.dram_tensor("dbgA", [128, 4, T], BF16, kind="ExternalOutput").ap()
        B_uS = [P.buf(f"uS{c}") for c in range(15)]
        B_acc = [P.buf(f"acc{s}") for s in range(NS)]
        B_h2d = [P.buf(f"h2d{s}") for s in range(NS)]

        self.ps = []
        for i in range(8):
            t = st0.enter_context(nc.psum_tensor(f"ps{i}", [128, 512], F32))
            self.ps.append(Tl(t, P.buf(f"ps{i}")))

        sb = self.sb
        idf = sb(st0, "idf", [128, 128], F32)
        idb = sb(st0, "idb", [128, 128], BF16)
        bo = sb(st0, "bo", [128, 128], F32)
        ba = sb(st0, "ba", [128, 128], F32)
        onesb = sb(st0, "onesb", [128, 128], BF16)
        bob = sb(st0, "bob", [128, 128], BF16)
        mu = sb(st0, "mu", [128, 15, 2], F32)
        mu0 = sb(st0, "mu0", [128, 15], F32)
        pp = sb(st0, "pp", [128, 4, 9], F32)
        omka = sb(st0, "omka", [128, 4], F32)
        g2 = sb(st0, "g2", [128, 8], F32)
        wrt = sb(st0, "wrt", [128, 8, NEXP], F32)
        affT = sb(st0, "affT", [128, T], F32)
        self.wst = [sb(st0, f"wst{i}", [128, 8 * NEXP], F32) for i in range(1)]
        self.wsi = 0
        PW0F, PW0B, PA0F, PA0B, PKK, PKA, PRK, PLW, PLB = range(9)

        for (dst, src) in ((idf, c_idf), (bo, c_bo), (ba, c_ba), (mu, mu_d), (pp, pp_d), (g2, g2_d)):
            self.dma(dst.t[:], src, w=[dst.b])
        self.cp("pool", idb[:], idf[:], r=[idf.b], w=[idb.b])
        self.ms("pool", onesb[:], 1.0, w=[onesb.b])
        self.cp("pool", bob[:], bo[:], r=[bo.b], w=[bob.b])
        self.tt("dve", mu0[:], mu[:, :, 0], mu[:, :, 1], ALU.add, r=[mu.b], w=[mu0.b])
        self.ts("dve", mu0[:], mu0[:], -1.0, 1.0, ALU.mult, ALU.add, r=[mu0.b], w=[mu0.b])
        self.ts("dve", omka[:], pp[:, :, PKA], -1.0, 1.0, ALU.mult, ALU.add, r=[pp.b], w=[omka.b])
        self.dma(self.wst[0].t[:, 0:8 * NEXP].rearrange("p (k n) -> p k n", k=8), wr_d, w=[self.wst[0].b])
        self.tt("dve", wrt[:], self.wst[0].t[:, 0:8 * NEXP].rearrange("p (k n) -> p k n", k=8),
                g2.t[:].unsqueeze(2).to_broadcast([128, 8, NEXP]),
                ALU.mult, r=[self.wst[0].b, g2.b], w=[wrt.b])
        self.ms("pool", affT[:], 0.0, w=[affT.b])
        self.yRk = sb(st0, "yRk", [128, 4, T], BF16)
        P.stage_end()

        env = locals()
        try:
            self.chk(0)
            for s in range(NS):
                self.phase_a(s, env)
            self.phase_b(env)
            self.chk(6)
            self.phase_c(env)
        except _Stop:
            return nc
        self.P.flush()
        print("signals", self.P.sigcount, "dmas", self.P.ndma)
        self.stack.close()
        return nc

    def norm_hT(self, st, s, env, hT):
        T, TT = self.T, self.TT
        x = env["x"]
        idb = env["idb"]
        gbc = self.sb(st, "gbc", [128, D], F32)
        self.dma(gbc.t[:], env["gmixr_d"].partition_broadcast(128), w=[gbc.b])
        xts = [self.sb(st, f"xt{i}", [128, D], F32) for i in range(2)]
        hns = [self.sb(st, f"hn{i}", [128, D], BF16) for i in range(2)]
        junk = self.sb(st, "junk", [128, D], F32)
        sst = [self.sb(st, f"ss{i}", [128, 4], F32) for i in range(2)]
        for tt_ in range(TT):
            xt, hn, ss = xts[tt_ % 2], hns[tt_ % 2], sst[tt_ % 2]
            r0 = s * T + tt_ * 128
            self.dma(xt.t[:], x[r0:r0 + 128, :], w=[xt.b])
            self.chk(20)
            self.act(junk[:], xt[:], AF.Square, accum=ss[:, 0:1], r=[xt.b], w=[junk.b, ss.b])
            self.chk(21)
            self.ts("dve", ss[:, 1:2], ss[:, 0:1], 1.0 / D, NORM_EPS, ALU.mult, ALU.add, r=[ss.b], w=[ss.b])
            self.act(ss[:, 2:3], ss[:, 1:2], AF.Sqrt, r=[ss.b], w=[ss.b])
            self.chk(22)
            self.P.emit("dve", lambda e, o=ss[:, 3:4], i=ss[:, 2:3]: e.reciprocal(out=o, in_=i), [ss.b], [ss.b])
            self.stt(hn[:], xt[:], ss[:, 3:4], gbc[:], ALU.mult, ALU.mult, r=[xt.b, ss.b, gbc.b], w=[hn.b])
            self.chk(23)
            ps = self.nps()
            psv = ps.t.bitcast(BF16)
            for kc in range(8):
                self.tr(psv[:, kc * 128:(kc + 1) * 128], hn[:, kc * 128:(kc + 1) * 128], idb[:], r=[hn.b, idb.b], w=[ps.b])
            self.chk(24)
            import os
            if os.environ.get("VARX") == "1":
                self.cp("dve", junk.t[:].bitcast(BF16)[:, 0:1024], psv[:, :], r=[ps.b], w=[junk.b])
            elif os.environ.get("VARX") == "2":
                self.cp("dve", hT[:, 0, 0:128], psv[:, 0:128], r=[ps.b], w=[hT.b])
            else:
                self.cp("dve", hT[:, :, tt_ * 128:(tt_ + 1) * 128], psv[:, :].rearrange("p (k n) -> p k n", k=8), r=[ps.b], w=[hT.b])
            self.chk(25)

    def inproj(self, hT, wb, col0, tq, reads, ps):
        QS = self.QS
        for kc in range(8):
            self.mm(ps[:, 0:QS], wb[:, kc, col0:col0 + 128], hT[:, kc, tq * QS:(tq + 1) * QS],
                    start=(kc == 0), stop=(kc == 7), r=reads, w=[ps.b])

    def phase_a(self, s, env):
        P, T, QS, NQ, TT, NCH, CQ = self.P, self.T, self.QS, self.NQ, self.TT, self.NCH, self.CQ
        w_in, mu, mu0, pp, omka = env["w_in"], env["mu"], env["mu0"], env["pp"], env["omka"]
        uS, B_uS = env["uS"], env["B_uS"]
        PW0F, PW0B, PA0F, PA0B, PKK, PKA, PRK, PLW, PLB = range(9)
        sb = self.sb

        with ExitStack() as st:
            hT = sb(st, "hT", [128, 8, T], BF16)
            self.norm_hT(st, s, env, hT)
            self.chk(10)
            wbs = [sb(st, f"wb{i}", [128, 8, 512], BF16) for i in range(2)]
            upad = sb(st, "upad", [128, T + 2], F32)
            tmp = sb(st, "tmpA", [128, T], F32)
            uss = [sb(st, f"us{i}", [128, T], F32) for i in range(2)]
            self.ms("pool", upad[:, 0:1], 0.0, w=[upad.b])
            self.ms("pool", upad[:, T + 1:T + 2], 0.0, w=[upad.b])
            for grp in range(4):
                c0 = grp * 4
                ncol = min(4, 15 - c0)
                wb = wbs[grp % 2]
                self.load_w(wb[:, :, 0:ncol * 128], wb.b, w_in[:, :, c0 * 128:(c0 + ncol) * 128], 8, ncol * 128)
                self.chk(11)
                for ci in range(ncol):
                    c = c0 + ci
                    us = uss[c % 2]
                    for tq in range(NQ):
                        ps = self.nps()
                        self.inproj(hT, wb, ci * 128, tq, [wb.b, hT.b], ps)
                        self.cp("act", upad[:, 1 + tq * QS:1 + (tq + 1) * QS], ps[:, 0:QS], r=[ps.b], w=[upad.b])
                    self.ts("dve", tmp[:], upad[:, 1:T + 1], mu0[:, c:c + 1], None, ALU.mult, r=[upad.b, mu0.b], w=[tmp.b])
                    self.stt(tmp[:], upad[:, 0:T], mu[:, c, 0:1], tmp[:], ALU.mult, ALU.add, r=[upad.b, mu.b, tmp.b], w=[tmp.b])
                    self.stt(us[:], upad[:, 2:T + 2], mu[:, c, 1:2], tmp[:], ALU.mult, ALU.add, r=[upad.b, mu.b, tmp.b], w=[us.b])
                    if c == 12:
                        self.act(us[:], us[:], AF.Sigmoid, r=[us.b], w=[us.b])
                    elif c == 13:
                        self.act(us[:], us[:], AF.Tanh, r=[us.b], w=[us.b])
                    self.dma(uS[c], us[:], r=[us.b], w=[B_uS[c]], eng="pool")
                    self.chk(12)
            P.stage_end()
        self.chk(1)

        with ExitStack() as st:
            self.rwkv(st, s, env, self.yRk)
            if self.dbg and s == 0:
                self.dma(self.dbgR, self.yRk[:], r=[self.yRk.b])
            P.stage_end()
        self.chk(2)

        with ExitStack() as st:
            yA = sb(st, "yA", [128, 4, T], BF16)
            mrg = sb(st, "mrg", [128, 8, T], BF16)
            with ExitStack() as st1:
                hT = sb(st1, "hT", [128, 8, T], BF16)
                self.norm_hT(st1, s, env, hT)
                with ExitStack() as st2:
                    self.attention(st2, s, env, hT, yA)
                    if self.dbg and s == 0:
                        self.dma(self.dbgA, yA[:], r=[yA.b])
                    P.stage_end()
                self.chk(3)
                self.merge_gates(st1, s, env, hT, yA, mrg)
                P.stage_end()
                self.chk(4)
            with ExitStack() as st1:
                self.out_proj(st1, s, env, mrg)
                P.stage_end()
            self.chk(5)

    def rwkv(self, st, s, env, yR):
        P, T, NCH = self.P, self.T, self.NCH
        QS = min(512, T)
        NQ = T // QS
        CQ = QS // L
        pp, omka = env["pp"], env["omka"]
        uS, B_uS = env["uS"], env["B_uS"]
        bo, ba, idb, bob = env["bo"], env["ba"], env["idb"], env["bob"]
        PW0F, PW0B, PA0F, PA0B, PKK, PKA, PRK, PLW, PLB = range(9)
        sb = self.sb
        mT = sb(st, "mT", [128, 2, 512], F32)
        mN = sb(st, "mN", [128, 2, 512], F32)
        idr = sb(st, "idr", [128, 512], F32)
        self.dma(mT.t[:], env["c_mT"], w=[mT.b])
        self.dma(mN.t[:], env["c_mN"], w=[mN.b])
        self.dma(idr.t[:], env["c_idr"], w=[idr.b])
        wup = sb(st, "wup", [128, 512], BF16)
        aup = sb(st, "aup", [128, 512], BF16)
        gup = sb(st, "gup", [128, 512], BF16)
        for dst, src in ((wup, env["wup_d"]), (aup, env["aup_d"]), (gup, env["gup_d"])):
            self.load_w(dst[:], dst.b, src)
        NQ_ = NQ
        bonus = [sb(st, f"bonus{i}", [128, T], BF16) for i in range(2)]
        wkv = sb(st, "wkv", [128, T], F32)
        vtok = [sb(st, f"vtok{i}", [128, NCH, L], BF16) for i in range(2)]
        for t_ in vtok:
            t_.bq = [P.buf("vtokq") for _ in range(NQ_)]
        R = []
        for d in range(2):
            Rd = dict(
                ar=sb(st, f"ar{d}", [128, NCH, 2, L], BF16),
                SB=sb(st, f"SB{d}", [128, NCH, L], BF16),
                SK=sb(st, f"SK{d}", [128, NCH, 2, L], BF16),
                TTm=sb(st, f"TT{d}", [128, NCH, L], BF16),
                BB=sb(st, f"BB{d}", [128, NCH, L], BF16),
                KB=sb(st, f"KB{d}", [128, NCH, L], BF16),
                Wtot=sb(st, f"Wtot{d}", [128, NCH], F32),
                ST=sb(st, f"ST{d}", [128, L], BF16),
                Xs=sb(st, f"Xs{d}", [128, L], BF16),
                Us=sb(st, f"Us{d}", [128, L], BF16),
            )
            for k_ in ("ar", "SB", "SK", "TTm", "BB", "KB", "Wtot"):
                Rd[k_].bq = [P.buf(k_ + "q") for _ in range(NQ_)]
            R.append(Rd)

        def q(name, dt=F32):
            return sb(st, name, [128, QS], dt)
        def mk_temps(sfx):
            names_f = ["t2", "sgw", "cs"] + (["X1", "t1f", "sqf"] if sfx == "A" else ["X2", "X3"])
            names_b = ["rq", "kq", "vb", "twb", "alb", "kkn", "sq", "t1", "t2b", "t3", "kft", "aqt", "E", "akk", "p1", "p2",
                       "bT", "kT", "Pm", "PTm", "Pm2", "PTm2"] + (["sgb"] if sfx == "A" else [])
            d_ = {n: q(n + sfx) for n in names_f}
            d_.update({n: q(n + sfx, BF16) for n in names_b})
            for n in ("X1", "X2", "X3"):
                d_.setdefault(n, None)
            return d_
        TA, TB = mk_temps("A"), mk_temps("B")
        TA["grp"], TB["grp"] = "A", "B"
        sgb, t1, t2, sq = (TA[n] for n in ("sgb", "t1f", "t2", "sqf"))
        scm = sb(st, "scm", [128, QS], F32)
        self.ms("pool", scm[:], 1.0, w=[scm.b])
        self.ms("pool", scm.t[:].rearrange("p (c l) -> p c l", l=L)[:, :, 0:1], 0.0, w=[scm.b])

        def v3(ap):
            return ap.rearrange("p (c l) -> p c l", l=L)

        def pre_pass(p, d, tq, first, tmp):
            (rq, kq, vb, twb, alb, kkn, sq, t1, t2, t2b, t3, kft, aqt, sgw, cs, X1, X2, X3, E, akk, p1, p2,
             bT, kT, Pm, PTm, Pm2, PTm2) = (tmp[n] for n in (
                "rq", "kq", "vb", "twb", "alb", "kkn", "sq", "t1", "t2", "t2b", "t3", "kft", "aqt", "sgw", "cs", "X1", "X2", "X3",
                "E", "akk", "p1", "p2", "bT", "kT", "Pm", "PTm", "Pm2", "PTm2"))
            par = p % 2
            pc = slice(p * 128, (p + 1) * 128)
            ts_ = slice(tq * QS, (tq + 1) * QS)
            cq0 = tq * CQ
            csl = slice(cq0, cq0 + CQ)
            Rd = R[d]
            hs_lo = slice(64 * d, 64 * d + 64)
            for dst, c in ((rq, p), (kq, 4 + p), (vb, 8 + p), (twb, 13), (alb, 14)):
                self.dma(dst.t[:], uS[c][:, ts_], r=[B_uS[c]], w=[dst.b])
            yield
            self.ts("dve", t1[:], kq[:], pp[:, p, PKK:PKK + 1], None, ALU.mult, r=[kq.b, pp.b], w=[t1.b])
            self.tt("pool", sq[:], t1[:], t1[:], ALU.mult, r=[t1.b], w=[sq.b])
            ps = self.nps(tmp["grp"])
            self.mm(ps[:, 0:QS], bob[:], sq[:], r=[bob.b, sq.b], w=[ps.b])
            self.ts("dve", t2[:], ps[:, 0:QS], 1e-24, None, ALU.max, r=[ps.b], w=[t2.b])
            self.act(t2[:], t2[:], AF.Ln, r=[t2.b], w=[t2.b])
            self.act(t2b[:], t2[:], AF.Exp, scale=-0.5, r=[t2.b], w=[t2b.b])
            self.tt("dve", kkn[:], t1[:], t2b[:], ALU.mult, r=[t1.b, t2b.b], w=[kkn.b])
            yield
            ps = self.nps(tmp["grp"])
            self.mm(ps[:, 0:QS], aup[hs_lo, pc], alb[hs_lo, :], r=[aup.b, alb.b], w=[ps.b])
            self.act(aqt[:], ps[:, 0:QS], AF.Sigmoid, bias=pp[:, p, PA0F + d:PA0F + d + 1], r=[ps.b, pp.b], w=[aqt.b])
            self.ts("dve", t3[:], aqt[:], pp[:, p, PKA:PKA + 1], omka[:, p:p + 1], ALU.mult, ALU.add,
                    r=[aqt.b, pp.b, omka.b], w=[t3.b])
            self.tt("dve", kft[:], kq[:], t3[:], ALU.mult, r=[kq.b, t3.b], w=[kft.b])
            yield
            self.stt(t3[:], kft[:], pp[:, p, PRK:PRK + 1], rq[:], ALU.mult, ALU.mult, r=[kft.b, pp.b, rq.b], w=[t3.b])
            ps = self.nps(tmp["grp"])
            self.mm(ps[:, 0:QS], bob[:], t3[:], r=[bob.b, t3.b], w=[ps.b])
            if first:
                self.tt("dve", bonus[par][:, ts_], ps[:, 0:QS], vb[:], ALU.mult, r=[ps.b, vb.b], w=[bonus[par].b])
            else:
                self.tt("dve", t3[:], ps[:, 0:QS], vb[:], ALU.mult, r=[ps.b, vb.b], w=[t3.b])
                self.tt("pool", bonus[par][:, ts_], bonus[par][:, ts_], t3[:], ALU.add, r=[t3.b, bonus[par].b], w=[bonus[par].b])
            yield
            if first:
                ps = self.nps(tmp["grp"])
                psv = ps.t.bitcast(BF16)
                for c in range(CQ):
                    for h in range(2):
                        hs = slice(64 * h, 64 * h + 64)
                        self.tr(psv[hs, c * L:(c + 1) * L], vb[hs, c * L:(c + 1) * L], idb[hs, hs], r=[vb.b, idb.b], w=[ps.b])
                self.cp("act", vtok[par][:, csl, :], v3(psv[:, 0:QS]), r=[ps.b], w=[vtok[par].bq[tq]])
                yield
            ps = self.nps(tmp["grp"])
            self.mm(ps[:, 0:QS], wup[hs_lo, pc], twb[hs_lo, :], r=[wup.b, twb.b], w=[ps.b])
            self.act(sgw[:], ps[:, 0:QS], AF.Sigmoid, bias=pp[:, p, PW0F + d:PW0F + d + 1], r=[ps.b, pp.b], w=[sgw.b])
            self.P.emit("dve", lambda e, o=cs[:], a=scm[:], b=sgw[:]: e.tensor_tensor_scan(
                out=o, data0=a, data1=b, initial=0.0, op0=ALU.mult, op1=ALU.add), [scm.b, sgw.b], [cs.b], est=(100 + 2 * QS) / 960.0)
            tot = v3(cs[:])[:, :, L - 1:L]
            self.act(Rd["Wtot"][:, csl], tot.rearrange("p c l -> p (c l)"), AF.Exp, scale=-DS, r=[cs.b], w=[Rd["Wtot"].bq[tq]])
            if d == 0:
                self.tt("pool", X1[:], cs[:], sgw[:], ALU.subtract, r=[cs.b, sgw.b], w=[X1.b])
                ce, ci = X1, cs
            else:
                self.tt("dve", v3(X2[:]), tot.to_broadcast([128, CQ, L]), v3(cs[:]), ALU.subtract, r=[cs.b], w=[X2.b])
                self.tt("pool", X3[:], X2[:], sgw[:], ALU.add, r=[X2.b, sgw.b], w=[X3.b])
                ce, ci = X2, X3
            yield
            ar = Rd["ar"]
            arb = ar.bq[tq]
            self.act(E[:], ce[:], AF.Exp, scale=-DS, r=[ce.b], w=[E.b])
            self.stt(ar[:, csl, 0, :], v3(kkn[:]), -1.0, v3(E[:]), ALU.mult, ALU.mult, r=[kkn.b, E.b], w=[arb])
            self.act(p1[:], ci[:], AF.Exp, scale=-DS, r=[ci.b], w=[p1.b])
            self.tt("dve", ar[:, csl, 1, :], v3(rq[:]), v3(p1[:]), ALU.mult, r=[rq.b, p1.b], w=[arb])
            yield
            self.tt("pool", akk[:], aqt[:], kkn[:], ALU.mult, r=[aqt.b, kkn.b], w=[akk.b])
            self.act(p2[:], ci[:], AF.Exp, scale=DS, r=[ci.b], w=[p2.b])
            self.tt("dve", bT[:], akk[:], p2[:], ALU.mult, r=[akk.b, p2.b], w=[bT.b])
            self.tt("dve", kT[:], kft[:], p2[:], ALU.mult, r=[kft.b, p2.b], w=[kT.b])
            yield
            for src, dstk in ((bT, "BB"), (kT, "KB")):
                ps = self.nps(tmp["grp"])
                psv = ps.t.bitcast(BF16)
                for c in range(CQ):
                    for h in range(2):
                        hs = slice(64 * h, 64 * h + 64)
                        self.tr(psv[hs, c * L:(c + 1) * L], src[hs, c * L:(c + 1) * L], idb[hs, hs],
                                r=[src.b, idb.b], w=[ps.b])
                self.cp("act", Rd[dstk][:, csl, :], v3(psv[:, 0:QS]), r=[ps.b], w=[Rd[dstk].bq[tq]])
                yield
            for lhs, dstk in ((bT, "SB"), (kT, "SK")):
                for c4 in range(0, CQ, 4):
                    ps = self.nps(tmp["grp"])
                    for cc in range(4):
                        c = c4 + cc
                        for h in range(2):
                            hs = slice(64 * h, 64 * h + 64)
                            self.mm(ps[hs, cc * 128:(cc + 1) * 128], lhs[hs, c * L:(c + 1) * L],
                                    ar[hs, cq0 + c, :, :].rearrange("p a l -> p (a l)"),
                                    r=[lhs.b, arb], w=[ps.b])
                    if dstk == "SK":
                        self.tt("dve", Rd[dstk][:, cq0 + c4:cq0 + c4 + 4, :, :].rearrange("p c a l -> p (c a l)"),
                                ps[:, :], mT[:, d, :], ALU.mult, r=[ps.b, mT.b], w=[Rd[dstk].bq[tq]])
                    else:
                        ps4 = ps[:, :].rearrange("p (c a l) -> p c a l", c=4, a=2)
                        m4 = mT[:, d, :].rearrange("p (c a l) -> p c a l", c=4, a=2)
                        self.tt("dve", v3(PTm[:])[:, c4:c4 + 4, :], ps4[:, :, 0, :], m4[:, :, 0, :], ALU.mult,
                                r=[ps.b, mT.b], w=[PTm.b])
                        self.tt("dve", Rd["SB"][:, cq0 + c4:cq0 + c4 + 4, :], ps4[:, :, 1, :], m4[:, :, 1, :], ALU.mult,
                                r=[ps.b, mT.b], w=[Rd["SB"].bq[tq]])
                    yield
            ps = self.nps(tmp["grp"])
            for c in range(CQ):
                for h in range(2):
                    hs = slice(64 * h, 64 * h + 64)
                    self.mm(ps[hs, c * L:(c + 1) * L], ar[hs, cq0 + c, 0, :], bT[hs, c * L:(c + 1) * L],
                            r=[arb, bT.b], w=[ps.b])
            self.tt("dve", Pm[:], ps[:, 0:QS], mN[:, d, 0:QS], ALU.mult, r=[ps.b, mN.b], w=[Pm.b])
            TTq = Rd["TTm"][:, csl, :]
            TTb = Rd["TTm"].bq[tq]
            self.tt("pool", TTq, v3(PTm[:]), v3(idr[:, 0:QS]), ALU.add, r=[PTm.b, idr.b], w=[TTb])
            yield
            Pc, PTc, Pn, PTn = Pm, PTm, Pm2, PTm2
            for lvl in range(1, 6):
                psA = self.nps(tmp["grp"])
                for c in range(CQ):
                    for h in range(2):
                        hs = slice(64 * h, 64 * h + 64)
                        cl = slice(c * L, (c + 1) * L)
                        self.mm(psA[hs, cl], PTc[hs, cl], Pc[hs, cl], r=[PTc.b, Pc.b], w=[psA.b])
                self.cp("act", Pn[:], psA[:, 0:QS], r=[psA.b], w=[Pn.b])
                if lvl < 5:
                    psB = self.nps(tmp["grp"])
                    for c in range(CQ):
                        for h in range(2):
                            hs = slice(64 * h, 64 * h + 64)
                            cl = slice(c * L, (c + 1) * L)
                            self.mm(psB[hs, cl], Pc[hs, cl], PTc[hs, cl], r=[PTc.b, Pc.b], w=[psB.b])
                    self.cp("act", PTn[:], psB[:, 0:QS], r=[psB.b], w=[PTn.b])
                yield
                psC = self.nps(tmp["grp"])
                for c in range(CQ):
                    for h in range(2):
                        hs = slice(64 * h, 64 * h + 64)
                        cl = slice(c * L, (c + 1) * L)
                        self.mm(psC[hs, cl], Pn[hs, cl], Rd["TTm"][hs, cq0 + c, :], r=[Pn.b, TTb], w=[psC.b])
                self.tt("dve", TTq, v3(psC[:, 0:QS]), TTq, ALU.add, r=[psC.b, TTb], w=[TTb])
                Pc, PTc, Pn, PTn = Pn, PTn, Pc, PTc
                yield

        def pre_stage(p, k):
            gens = []
            for d, tq, tmp in ((0, k, TA), (1, NQ_ - 1 - k, TB)):
                fstage, bstage = tq, NQ_ - 1 - tq
                first = (fstage <= bstage) if d == 0 else (bstage < fstage)
                gens.append(pre_pass(p, d, tq, first, tmp))
            return gens

        def chain_group(p, k):
            par = p % 2
            if k == 0:
                self.ms("pool", wkv[:], 0.0, w=[wkv.b])
                for d in range(2):
                    self.ms("pool", R[d]["ST"][:], 0.0, w=[R[d]["ST"].b])
            for step in range(k * CQ, (k + 1) * CQ):
                for d in range(2):
                    Rd = R[d]
                    c = step if d == 0 else NCH - 1 - step
                    qi = c // CQ
                    ar, SBm, SKm, TTm, BB, KB, ST, Xs, Us = (Rd[k_] for k_ in ("ar", "SB", "SK", "TTm", "BB", "KB", "ST", "Xs", "Us"))
                    vt = vtok[par]
                    vtb = vt.bq[qi]
                    psX = self.nps("chain")
                    for h in range(2):
                        hs = slice(64 * h, 64 * h + 64)
                        self.mm(psX[hs, 0:L], ar[hs, c, 0, :], ST[hs, :], start=True, stop=False, r=[ar.bq[qi], ST.b], w=[psX.b])
                        self.mm(psX[hs, 0:L], SKm[hs, c, 0, :], vt[hs, c, :], start=False, stop=True, r=[SKm.bq[qi], vtb], w=[psX.b])
                    self.cp("act", Xs[:], psX[:, 0:L], r=[psX.b], w=[Xs.b])
                    yield
                    psU = self.nps("chain")
                    for h in range(2):
                        hs = slice(64 * h, 64 * h + 64)
                        self.mm(psU[hs, 0:L], TTm[hs, c, :], Xs[hs, :], r=[TTm.bq[qi], Xs.b], w=[psU.b])
                    self.cp("dve", Us[:], psU[:, 0:L], r=[psU.b], w=[Us.b])
                    yield
                    psY = self.nps("chain")
                    for h in range(2):
                        hs = slice(64 * h, 64 * h + 64)
                        self.mm(psY[hs, 0:L], ST[hs, :], ar[hs, c, 1, :], start=True, stop=False, r=[ar.bq[qi], ST.b], w=[psY.b])
                        self.mm(psY[hs, 0:L], Us[hs, :], SBm[hs, c, :], start=False, stop=False, r=[Us.b, SBm.bq[qi]], w=[psY.b])
                        self.mm(psY[hs, 0:L], vt[hs, c, :], SKm[hs, c, 1, :], start=False, stop=True, r=[vtb, SKm.bq[qi]], w=[psY.b])
                    psS = self.nps("chain")
                    for h in range(2):
                        hs = slice(64 * h, 64 * h + 64)
                        self.mm(psS[hs, 0:L], idb[hs, hs], ST[hs, :], start=True, stop=False, r=[idb.b, ST.b], w=[psS.b])
                        self.mm(psS[hs, 0:L], BB[hs, c, :], Us[hs, :], start=False, stop=False, r=[BB.bq[qi], Us.b], w=[psS.b])
                        self.mm(psS[hs, 0:L], KB[hs, c, :], vt[hs, c, :], start=False, stop=True, r=[KB.bq[qi], vtb], w=[psS.b])
                    self.ts("dve", ST[:], psS[:, 0:L], Rd["Wtot"][:, c:c + 1], None, ALU.mult,
                            r=[Rd["Wtot"].bq[qi], psS.b], w=[ST.b])
                    wsl = wkv[:, c * L:(c + 1) * L]
                    self.tt("dve", wsl, psY[:, 0:L], wsl, ALU.add, r=[psY.b, wkv.b], w=[wkv.b])
                    yield

        def post(p):
            par = p % 2
            pc = slice(p * 128, (p + 1) * 128)
            for tq in range(NQ):
                ts_ = slice(tq * QS, (tq + 1) * QS)
                self.dma(sgb.t[:], uS[12][:, ts_], r=[B_uS[12]], w=[sgb.b])
                ps = self.nps()
                self.mm(ps[:, 0:QS], ba[:], wkv[:, ts_], r=[ba.b, wkv.b], w=[ps.b])
                self.tt("dve", t1[:], wkv[:, ts_], ps[:, 0:QS], ALU.subtract, r=[wkv.b, ps.b], w=[t1.b])
                self.tt("pool", sq[:], t1[:], t1[:], ALU.mult, r=[t1.b], w=[sq.b])
                yield
                ps = self.nps()
                self.mm(ps[:, 0:QS], ba[:], sq[:], r=[ba.b, sq.b], w=[ps.b])
                self.ts("dve", t2[:], ps[:, 0:QS], LNX_EPS, None, ALU.add, r=[ps.b], w=[t2.b])
                self.act(t2[:], t2[:], AF.Ln, r=[t2.b], w=[t2.b])
                self.act(t2[:], t2[:], AF.Exp, scale=-0.5, r=[t2.b], w=[t2.b])
                self.tt("dve", t1[:], t1[:], t2[:], ALU.mult, r=[t1.b, t2.b], w=[t1.b])
                yield
                self.ts("dve", t1[:], t1[:], pp[:, p, PLW:PLW + 1], pp[:, p, PLB:PLB + 1], ALU.mult, ALU.add,
                        r=[t1.b, pp.b], w=[t1.b])
                self.tt("pool", t1[:], t1[:], bonus[par][:, ts_], ALU.add, r=[t1.b, bonus[par].b], w=[t1.b])
                ps = self.nps()
                self.mm(ps[:, 0:QS], gup[:, pc], sgb[:], r=[gup.b, sgb.b], w=[ps.b])
                self.tt("dve", yR[:, p, ts_], t1[:], ps[:, 0:QS], ALU.mult, r=[t1.b, ps.b], w=[yR.b])
                yield

        def run(g):
            for _ in g:
                pass

        interleave = self.interleave

        interleave(pre_stage(0, 0))
        for p in range(4):
            for k in range(NQ_):
                nxt = None
                if k + 1 < NQ_:
                    nxt = pre_stage(p, k + 1)
                elif p + 1 < 4:
                    nxt = pre_stage(p + 1, 0)
                if nxt is not None and NQ_ > 1:
                    interleave([chain_group(p, k)] + nxt)
                else:
                    run(chain_group(p, k))
                    if nxt is not None:
                        interleave(nxt)
            run(post(p))

    def attention(self, st, s, env, hT, yA):
        P, T, QS, NQ, TT = self.P, self.T, self.QS, self.NQ, self.TT
        w_in, idb, onesb = env["w_in"], env["idb"], env["onesb"]
        sb = self.sb
        bias = sb(st, "bias", [128, 6, 512], BF16)
        self.load_w(bias[:], bias.b, env["c_bias"])
        snk = sb(st, "snk", [128, 8], F32)
        esk = sb(st, "esk", [128, 2, 2, 128], F32)
        self.dma(snk.t[:], env["sink_d"].partition_broadcast(128), w=[snk.b])
        self.act(snk[:], snk[:], AF.Exp, r=[snk.b], w=[snk.b])
        for kv in range(2):
            for gh in range(2):
                for gl in range(2):
                    hs = slice(64 * gl, 64 * gl + 64)
                    col = kv * 4 + 2 * gh + gl
                    self.cp("dve", esk[hs, kv, gh, :], snk[hs, col:col + 1].to_broadcast([64, 128]), r=[snk.b], w=[esk.b])
        qT = sb(st, "qT", [128, TT, 4, 128], BF16)
        kTz = [sb(st, f"kTz{i}", [128, T], BF16) for i in range(2)]
        vtk = sb(st, "vtk", [128, TT, 128], BF16)
        wq = sb(st, "wq", [128, 8, 512], BF16)
        wkv_ = sb(st, "wkvw", [128, 8, 256], BF16)
        c0 = 1920
        wq5 = wq.t[:].rearrange("p k (g kv d) -> p k g kv d", g=4, kv=2)
        for i in range(2):
            for kc in range(8):
                self.load_w(wq5[:, kc, :, i, :], wq.b,
                            w_in[:, kc, c0 + i * 256:c0 + (i + 1) * 256].rearrange("p (g d) -> p g d", g=4))
        self.load_w(wkv_[:], wkv_.b, w_in[:, :, c0 + 512:c0 + 768], 8, 256)
        for i in range(2):
            self.ms("pool", kTz[i][:], 0.0, w=[kTz[i].b])
        for tq in range(NQ):
            ts_ = slice(tq * QS, (tq + 1) * QS)
            for g in range(4):
                ps = self.nps()
                for kc in range(8):
                    self.mm(ps[:, 0:QS], wq[:, kc, g * 128:(g + 1) * 128], hT[:, kc, ts_], start=(kc == 0), stop=(kc == 7),
                            r=[wq.b, hT.b], w=[ps.b])
                sg_ = (g % 2) * 2 + g // 2
                nb_ = QS // 128
                self.act(qT[:, tq * nb_:(tq + 1) * nb_, sg_, :], ps[:, 0:QS].rearrange("p (b q) -> p b q", q=128), AF.Copy, scale=0.125,
                         r=[ps.b], w=[qT.b])
            ps = self.nps()
            self.inproj(hT, wkv_, 0, tq, [wkv_.b, hT.b], ps)
            for kv in range(2):
                hs = slice(64 * kv, 64 * kv + 64)
                self.cp("act", kTz[kv][hs, ts_], ps[hs, 0:QS], r=[ps.b], w=[kTz[kv].b])
        for tt_ in range(TT):
            ps = self.nps()
            for kc in range(8):
                self.mm(ps[:, 0:128], hT[:, kc, tt_ * 128:(tt_ + 1) * 128], wkv_[:, kc, 128:256], start=(kc == 0), stop=(kc == 7),
                        r=[wkv_.b, hT.b], w=[ps.b])
            self.cp("act", vtk[:, tt_, :], ps[:, 0:128], r=[ps.b], w=[vtk.b])
        pTs = [[sb(st, f"pT{j}_{i}", [128, 512], BF16) for i in range(3)] for j in range(2)]
        dens = [sb(st, f"den{j}", [128, 256], F32) for j in range(2)]

        def att_iter(kv, qb, par):
            den = dens[par]
            kbs = [kb for kb in (qb - 1, qb, qb + 1) if 0 <= kb < TT]
            psO = self.nps(f"attO{par}")
            psD = self.nps(f"attD{par}")
            for ki, kb in enumerate(kbs):
                rel = kb - qb + 1
                psS = self.nps("attS")
                pT = pTs[par][ki]
                self.mm(psS[:, :], kTz[kv][:, kb * 128:(kb + 1) * 128], qT[:, qb, :, :].rearrange("p g q -> p (g q)"),
                        start=True, stop=False, r=[kTz[kv].b, qT.b], w=[psS.b])
                self.mm(psS[:, :], idb[:], bias[:, rel * 2 + kv, :], start=False, stop=True, r=[idb.b, bias.b], w=[psS.b])
                self.act(pT[:], psS[:, :], AF.Exp, r=[psS.b], w=[pT.b])
                yield
                for gl in range(2):
                    hs = slice(64 * gl, 64 * gl + 64)
                    self.mm(psO[hs, 0:256], vtk[:, kb, 64 * kv:64 * kv + 64], pT[:, gl * 256:(gl + 1) * 256],
                            start=(ki == 0), stop=(ki == len(kbs) - 1), r=[vtk.b, pT.b], w=[psO.b], nohold=True)
                    self.mm(psD[hs, 0:256], onesb[:, 0:64], pT[:, gl * 256:(gl + 1) * 256],
                            start=(ki == 0), stop=(ki == len(kbs) - 1), r=[onesb.b, pT.b], w=[psD.b], nohold=True)
                yield
            self.tt("dve", den[:], psD[:, 0:256], esk[:, kv, :, :].rearrange("p a q -> p (a q)"), ALU.add,
                    r=[psD.b, esk.b], w=[den.b])
            self.act(den[:], den[:], AF.Ln, r=[den.b], w=[den.b])
            self.act(den[:], den[:], AF.Exp, scale=-1.0, r=[den.b], w=[den.b])
            yield
            self.tt("dve", yA[:, 2 * kv:2 * kv + 2, qb * 128:(qb + 1) * 128],
                    psO[:, 0:256].rearrange("p (a q) -> p a q", a=2), den[:].rearrange("p (a q) -> p a q", a=2),
                    ALU.mult, r=[psO.b, den.b], w=[yA.b])
            yield

        its = [(kv, qb) for kv in range(2) for qb in range(TT)]
        self.interleave([att_iter(kv, qb, i % 2) for i, (kv, qb) in enumerate(its)], window=2)

    def merge_gates(self, st, s, env, hT, yA, mrg):
        P, T, QS, NQ, TT = self.P, self.T, self.QS, self.NQ, self.TT
        NS = self.NS
        w_in, idf = env["w_in"], env["idf"]
        x, acc, h2d, B_acc, B_h2d = env["x"], env["acc"], env["h2d"], env["B_acc"], env["B_h2d"]
        wrt, affT = env["wrt"], env["affT"]
        yR = self.yRk
        sb = self.sb
        wpr = sb(st, "wpr", [128, 4, D], BF16)
        wpa = sb(st, "wpa", [128, 4, D], BF16)
        self.load_w(wpr[:], wpr.b, env["wpr_d"])
        self.load_w(wpa[:], wpa.b, env["wpa_d"])
        wgs = [sb(st, f"wg{i}", [128, 8, 1024], BF16) for i in range(2)]
        sg1s = [sb(st, f"sg1_{i}", [128, QS], F32) for i in range(2)]
        sg2s = [sb(st, f"sg2_{i}", [128, QS], F32) for i in range(2)]
        m1s = [sb(st, f"m1_{i}", [128, QS], F32) for i in range(2)]
        m2s = [sb(st, f"m2_{i}", [128, QS], F32) for i in range(2)]
        cg = 1920 + 768

        def gate_iter(oc, tq, par):
            sg1, sg2, m1, m2 = sg1s[par], sg2s[par], m1s[par], m2s[par]
            wg = wgs[oc // 4]
            ol = (oc % 4) * 128
            if oc % 4 == 0 and tq == 0:
                self.load_w(wg[:, :, 0:512], wg.b, w_in[:, :, cg + oc * 128:cg + oc * 128 + 512])
                self.load_w(wg[:, :, 512:1024], wg.b, w_in[:, :, cg + 1024 + oc * 128:cg + 1024 + oc * 128 + 512])
            ts_ = slice(tq * QS, (tq + 1) * QS)
            ps1 = self.nps()
            self.inproj(hT, wg, ol, tq, [wg.b, hT.b], ps1)
            self.act(sg1[:], ps1[:, 0:QS], AF.Sigmoid, r=[ps1.b], w=[sg1.b])
            yield
            ps2 = self.nps()
            self.inproj(hT, wg, 512 + ol, tq, [wg.b, hT.b], ps2)
            self.act(sg2[:], ps2[:, 0:QS], AF.Sigmoid, r=[ps2.b], w=[sg2.b])
            yield
            ps3 = self.nps()
            for kc in range(4):
                self.mm(ps3[:, 0:QS], wpr[:, kc, oc * 128:(oc + 1) * 128], yR[:, kc, ts_], start=(kc == 0), stop=(kc == 3),
                        r=[wpr.b, yR.b], w=[ps3.b])
            self.tt("dve", m1[:], sg1[:], ps3[:, 0:QS], ALU.mult, r=[sg1.b, ps3.b], w=[m1.b])
            yield
            ps4 = self.nps()
            for kc in range(4):
                self.mm(ps4[:, 0:QS], wpa[:, kc, oc * 128:(oc + 1) * 128], yA[:, kc, ts_], start=(kc == 0), stop=(kc == 3),
                        r=[wpa.b, yA.b], w=[ps4.b])
            self.tt("dve", m2[:], sg2[:], ps4[:, 0:QS], ALU.mult, r=[sg2.b, ps4.b], w=[m2.b])
            yield
            self.tt("pool", mrg[:, oc, ts_], m1[:], m2[:], ALU.add, r=[m1.b, m2.b], w=[mrg.b])
            yield

        its = [(oc, tq) for oc in range(8) for tq in range(NQ)]
        self.interleave([gate_iter(oc, tq, i % 2) for i, (oc, tq) in enumerate(its)], window=2)

    def out_proj(self, st, s, env, mrg):
        P, T, QS, NQ, TT = self.P, self.T, self.QS, self.NQ, self.TT
        idf = env["idf"]
        x, acc, h2d, B_acc, B_h2d = env["x"], env["acc"], env["h2d"], env["B_acc"], env["B_h2d"]
        wrt, affT = env["wrt"], env["affT"]
        sb = self.sb
        wout = sb(st, "wout", [128, 8, D], BF16)
        self.load_w(wout[:], wout.b, env["wout_d"])
        g2bc = sb(st, "g2bc", [128, D], F32)
        self.dma(g2bc.t[:], env["g2r_d"].partition_broadcast(128), w=[g2bc.b])
        xts = [sb(st, f"xm{i}", [128, D], F32) for i in range(2)]
        x1s = [sb(st, f"x1{i}", [128, D], F32) for i in range(2)]
        h2s = [sb(st, f"h2{i}", [128, D], F32) for i in range(2)]
        h2bs = [sb(st, f"h2b{i}", [128, D], BF16) for i in range(2)]
        h2T = sb(st, "h2T", [128, 8, 128], F32)
        junk = sb(st, "junk2", [128, D], F32)
        sst = [sb(st, f"sm{i}", [128, 8], F32) for i in range(2)]
        lg = sb(st, "lg", [128, NEXP], F32)
        afft = sb(st, "afft", [128, 128], F32)
        self.ms("pool", afft[:], 0.0, w=[afft.b])
        def tile_iter(tt_):
                xt, x1, h2, h2b, ss = xts[tt_ % 2], x1s[tt_ % 2], h2s[tt_ % 2], h2bs[tt_ % 2], sst[tt_ % 2]
                r0 = s * T + tt_ * 128
                tl = slice(tt_ * 128, (tt_ + 1) * 128)
                self.dma(xt.t[:], x[r0:r0 + 128, :], w=[xt.b])
                for half in range(2):
                    ps = self.nps()
                    for kc in range(8):
                        self.mm(ps[:, :], mrg[:, kc, tl], wout[:, kc, half * 512:(half + 1) * 512], start=(kc == 0), stop=(kc == 7),
                                r=[mrg.b, wout.b], w=[ps.b])
                    self.tt("dve", x1[:, half * 512:(half + 1) * 512], xt[:, half * 512:(half + 1) * 512], ps[:, :], ALU.add,
                            r=[xt.b, ps.b], w=[x1.b])
                yield
                self.dma(acc[r0:r0 + 128, :], x1[:], r=[x1.b], w=[B_acc[s]])
                self.act(junk[:], x1[:], AF.Square, accum=ss[:, 0:1], r=[x1.b], w=[junk.b, ss.b])
                self.ts("dve", ss[:, 1:2], ss[:, 0:1], 1.0 / D, NORM_EPS, ALU.mult, ALU.add, r=[ss.b], w=[ss.b])
                self.act(ss[:, 2:3], ss[:, 1:2], AF.Sqrt, r=[ss.b], w=[ss.b])
                self.P.emit("dve", lambda e, o=ss[:, 3:4], i=ss[:, 2:3]: e.reciprocal(out=o, in_=i), [ss.b], [ss.b])
                self.ts("dve", h2[:], x1[:], ss[:, 3:4], None, ALU.mult, r=[x1.b, ss.b], w=[h2.b])
                self.tt("pool", h2b[:], h2[:], g2bc[:], ALU.mult, r=[h2.b, g2bc.b], w=[h2b.b])
                self.dma(h2d[r0:r0 + 128, :], h2b[:], r=[h2b.b], w=[B_h2d[s]])
                yield
                for k2 in range(2):
                    ps = self.nps()
                    for kk in range(4):
                        kc = k2 * 4 + kk
                        self.tr(ps[:, kk * 128:(kk + 1) * 128], h2[:, kc * 128:(kc + 1) * 128], idf[:], r=[h2.b, idf.b], w=[ps.b])
                    self.cp("act", h2T[:, k2 * 4:(k2 + 1) * 4, :], ps[:, :].rearrange("p (k n) -> p k n", k=4), r=[ps.b], w=[h2T.b])
                yield
                ps = self.nps()
                for kc in range(8):
                    self.mm(ps[:, 0:NEXP], h2T[:, kc, :], wrt[:, kc, :], start=(kc == 0), stop=(kc == 7), r=[h2T.b, wrt.b], w=[ps.b])
                self.P.emit("dve", lambda e, o=ss[:, 4:5], i=ps[:, 0:NEXP]: e.reduce_max(out=o, in_=i, axis=AX.X), [ps.b], [ss.b])
                self.ts("dve", ss[:, 5:6], ss[:, 4:5], -1.0, None, ALU.mult, r=[ss.b], w=[ss.b])
                self.act(lg[:], ps[:, 0:NEXP], AF.Exp, bias=ss[:, 5:6], accum=ss[:, 6:7], r=[ps.b, ss.b], w=[lg.b, ss.b])
                self.P.emit("dve", lambda e, o=ss[:, 7:8], i=ss[:, 6:7]: e.reciprocal(out=o, in_=i), [ss.b], [ss.b])
                self.ts("dve", afft[:, 32 * s:32 * s + NEXP], lg[:], ss[:, 7:8], None, ALU.mult, r=[lg.b, ss.b], w=[afft.b])
                ps = self.nps()
                self.tr(ps[:, 0:128], afft[:], idf[:], r=[afft.b, idf.b], w=[ps.b])
                self.tt("dve", affT[:, tl], affT[:, tl], ps[:, 0:128], ALU.add, r=[ps.b, affT.b], w=[affT.b])


        self.interleave([tile_iter(tt_) for tt_ in range(TT)], window=2)

    def phase_b(self, env):
        P, NS, T, CAP, SB_, NB = self.P, self.NS, self.T, self.CAP, self.SB, self.NB
        affT, idf, idb, g2 = env["affT"], env["idf"], env["idb"], env["g2"]
        acc, h2d, B_acc, B_h2d = env["acc"], env["h2d"], env["B_acc"], env["B_h2d"]
        sb = self.sb
        with ExitStack() as st:
            wk = sb(st, "wk", [128, T], F32)
            mv = sb(st, "mv", [128, CAP], F32)
            mi = sb(st, "mi", [128, CAP], U32)
            mif = sb(st, "mif", [128, CAP], F32)
            offs = sb(st, "offs", [128, 128], F32)
            idxT = sb(st, "idxT", [128, NB, 128], I32)
            valT = sb(st, "valT", [128, NB, 128], F32)
            self.dma(offs.t[:], env["c_offs"], w=[offs.b])
            self.cp("dve", wk[:], affT[:], r=[affT.b], w=[wk.b])
            for r_ in range(CAP // 8):
                sl = slice(r_ * 8, r_ * 8 + 8)
                self.P.emit("dve", lambda e, o=mv[:, sl], i=wk[:]: e.max(out=o, in_=i), [wk.b], [mv.b])
                self.P.emit("dve", lambda e, o=mi[:, sl], m=mv[:, sl], i=wk[:]: e.max_index(out=o, in_max=m, in_values=i),
                            [wk.b, mv.b], [mi.b])
                self.P.emit("dve", lambda e, o=wk[:], m=mv[:, sl], i=wk[:]: e.match_replace(
                    out=o, in_to_replace=m, in_values=i, imm_value=-1.0), [wk.b, mv.b], [wk.b])
            self.cp("dve", mif[:], mi[:], r=[mi.b], w=[mif.b])
            for blk in range(NB):
                bs = slice(blk * SB_, (blk + 1) * SB_)
                ps = self.nps()
                self.tr(ps[0:SB_, 0:128], mif[:, bs], idf[:], r=[mif.b, idf.b], w=[ps.b])
                self.tt("dve", idxT[0:SB_, blk, :], ps[0:SB_, 0:128], offs[0:SB_, :], ALU.add, r=[ps.b, offs.b], w=[idxT.b])
                ps = self.nps()
                self.tr(ps[0:SB_, 0:128], mv[:, bs], idf[:], r=[mv.b, idf.b], w=[ps.b])
                self.cp("act", valT[0:SB_, blk, :], ps[0:SB_, 0:128], r=[ps.b], w=[valT.b])
            wgs = [sb(st, f"ewg{i}", [128, 8, D], BF16) for i in range(2)]
            wus = [sb(st, f"ewu{i}", [128, 8, D], BF16) for i in range(2)]
            wds = [sb(st, f"ewd{i}", [128, 8, D], BF16) for i in range(2)]
            NPF = 2
            xss = [sb(st, f"xs{i}", [128, D], BF16) for i in range((NPF + 1) * NB)]
            xsT = sb(st, "xsT", [128, 8, CAP], BF16)
            hid = sb(st, "hid", [128, 8, CAP], BF16)
            sl_ = sb(st, "silu", [128, CAP], F32)
            yss = [sb(st, f"ys{i}", [128, D], F32) for i in range(2 * NB)]
            its = [(e, s) for e in range(NEXP) for s in range(NS)]
            last_sc = {s: [] for s in range(NS)}

            def load_expert(e):
                for dst, src in ((wgs[e % 2], env["eg_d"]), (wus[e % 2], env["eu_d"]), (wds[e % 2], env["ed_d"])):
                    self.load_w(dst[:], dst.b, src[e].rearrange("(k p) n -> p k n", p=128))

            def gather(i):
                e, s = its[i]
                pcol = 32 * s + e
                for blk in range(NB):
                    xs = xss[(i % (NPF + 1)) * NB + blk]
                    ia = idxT[0:SB_, blk, pcol:pcol + 1]
                    self.P.emit("pool", lambda en, o=xs[0:SB_, :], ia=ia: en.indirect_dma_start(
                        out=o, out_offset=None, in_=h2d, in_offset=bass.IndirectOffsetOnAxis(ap=ia, axis=0)),
                        [idxT.b, B_h2d[s]], [xs.b], dma=True)

            load_expert(0)
            for i in range(min(NPF, len(its))):
                gather(i)
            for i, (e, s) in enumerate(its):
                wg, wu, wd = wgs[e % 2], wus[e % 2], wds[e % 2]
                pcol = 32 * s + e
                if s == 0 and e + 1 < NEXP:
                    load_expert(e + 1)
                if i + NPF < len(its):
                    gather(i + NPF)
                for blk in range(NB):
                    xs = xss[(i % (NPF + 1)) * NB + blk]
                    ps = self.nps()
                    psv = ps.t.bitcast(BF16)
                    for kc in range(8):
                        self.tr(psv[:, kc * 128:kc * 128 + SB_], xs[0:SB_, kc * 128:(kc + 1) * 128], idb[0:SB_, 0:SB_],
                                r=[xs.b, idb.b], w=[ps.b])
                    self.cp("act", xsT[:, :, blk * SB_:(blk + 1) * SB_],
                            psv[:, :].rearrange("p (k n) -> p k n", k=8)[:, :, 0:SB_], r=[ps.b], w=[xsT.b])
                for fc in range(8):
                    psg = self.nps()
                    psu = self.nps()
                    for kc in range(8):
                        self.mm(psg[:, 0:CAP], wg[:, kc, fc * 128:(fc + 1) * 128], xsT[:, kc, :], start=(kc == 0), stop=(kc == 7),
                                r=[wg.b, xsT.b], w=[psg.b])
                    for kc in range(8):
                        self.mm(psu[:, 0:CAP], wu[:, kc, fc * 128:(fc + 1) * 128], xsT[:, kc, :], start=(kc == 0), stop=(kc == 7),
                                r=[wu.b, xsT.b], w=[psu.b])
                    self.act(sl_[:], psg[:, 0:CAP], AF.Silu, r=[psg.b], w=[sl_.b])
                    self.tt("dve", hid[:, fc, :], sl_[:], psu[:, 0:CAP], ALU.mult, r=[sl_.b, psu.b], w=[hid.b])
                new_sc = []
                for blk in range(NB):
                    ys = yss[(i % 2) * NB + blk]
                    for half in range(2):
                        ps = self.nps()
                        for fc in range(8):
                            self.mm(ps[0:SB_, :], hid[:, fc, blk * SB_:(blk + 1) * SB_], wd[:, fc, half * 512:(half + 1) * 512],
                                    start=(fc == 0), stop=(fc == 7), r=[hid.b, wd.b], w=[ps.b])
                        self.act(ys[0:SB_, half * 512:(half + 1) * 512], ps[0:SB_, :], AF.Copy,
                                 scale=valT[0:SB_, blk, pcol:pcol + 1], r=[ps.b, valT.b], w=[ys.b])
                    ia = idxT[0:SB_, blk, pcol:pcol + 1]
                    op = self.P.emit("pool", lambda en, i_=ys[0:SB_, :], ia=ia: en.indirect_dma_start(
                        out=acc, out_offset=bass.IndirectOffsetOnAxis(ap=ia, axis=0), in_=i_, in_offset=None,
                        compute_op=ALU.add), [idxT.b, ys.b, B_acc[s]], [], dma=True)
                    for d_ in last_sc[s]:
                        self.P._dep(op, d_)
                    new_sc.append(op)
                last_sc[s] = new_sc
            P.stage_end()

    def phase_c(self, env):
        P, NS, T, TT = self.P, self.NS, self.T, self.TT
        acc, y, B_acc = env["acc"], env["y"], env["B_acc"]
        sb = self.sb
        outs = []
        with ExitStack() as st:
            gF = sb(st, "gF", [128, D], F32)
            self.dma(gF.t[:], env["gF_d"].partition_broadcast(128), w=[gF.b])
            ats = [sb(st, f"at{i}", [128, D], F32) for i in range(2)]
            ots = [sb(st, f"ot{i}", [128, D], F32) for i in range(2)]
            junk = sb(st, "junk3", [128, D], F32)
            sst = [sb(st, f"sc{i}", [128, 4], F32) for i in range(2)]
            n = 0
            for s in range(NS):
                for tt_ in range(TT):
                    at, ot, ss = ats[n % 2], ots[n % 2], sst[n % 2]
                    n += 1
                    r0 = s * T + tt_ * 128
                    self.dma(at.t[:], acc[r0:r0 + 128, :], r=[B_acc[s]], w=[at.b])
                    self.act(junk[:], at[:], AF.Square, accum=ss[:, 0:1], r=[at.b], w=[junk.b, ss.b])
                    self.ts("dve", ss[:, 1:2], ss[:, 0:1], 1.0 / D, NORM_EPS, ALU.mult, ALU.add, r=[ss.b], w=[ss.b])
                    self.act(ss[:, 2:3], ss[:, 1:2], AF.Sqrt, r=[ss.b], w=[ss.b])
                    self.P.emit("dve", lambda e, o=ss[:, 3:4], i=ss[:, 2:3]: e.reciprocal(out=o, in_=i), [ss.b], [ss.b])
                    self.stt(ot[:], at[:], ss[:, 3:4], gF[:], ALU.mult, ALU.mult, r=[at.b, ss.b, gF.b], w=[ot.b])
                    outs.append(self.dma(y[r0:r0 + 128, :], ot[:], r=[ot.b]))
            fin = P.buf("fin")
            op = P.emit("sp", lambda en: en.nop(), writes=[fin])
            for d_ in outs:
                P._dep(op, d_)
            P.stage_end()


def host_consts(T):
    c = {}
    c["c_idf"] = np.eye(128, dtype=np.float32)
    blk = (np.arange(128)[:, None] // 64 == np.arange(128)[None, :] // 64).astype(np.float32)
    c["c_bo"] = blk
    c["c_ba"] = blk / 64.0
    s = (np.arange(128) % 64)[:, None]
    j = np.arange(128)[None, :]
    t = j % 64
    isr = j >= 64
    mT = np.zeros((128, 2, 4, 128), np.float32)
    mT[:, 0] = np.where(isr, s <= t, s < t)[:, None, :]
    mT[:, 1] = np.where(isr, s >= t, s > t)[:, None, :]
    c["c_mT"] = mT.reshape(128, 2, 512)
    tt = (np.arange(128) % 64)[:, None]
    ss = np.arange(64)[None, :]
    mN = np.zeros((128, 2, 8, 64), np.float32)
    mN[:, 0] = (ss < tt)[:, None, :]
    mN[:, 1] = (ss > tt)[:, None, :]
    c["c_mN"] = mN.reshape(128, 2, 512)
    idr = np.zeros((128, 8, 64), np.float32)
    idr[:] = (ss == tt)[:, None, :]
    c["c_idr"] = idr.reshape(128, 512)
    slopes = 2.0 ** (-8.0 * np.arange(1, 9) / 8)
    key = np.arange(128)[:, None]
    qq = np.arange(128)[None, :]
    bias = np.zeros((128, 3, 2, 4, 128), np.float32)
    for rel in (-1, 0, 1):
        dist = np.abs(rel * 128 + key - qq)
        for kv in range(2):
            for sg in range(4):
                g = 2 * (sg % 2) + sg // 2
                bias[:, rel + 1, kv, sg, :] = np.where(dist <= 128, -slopes[kv * 4 + g] * dist, -1e30)
    c["c_bias"] = bias.reshape(128, 6, 512)
    offs = np.zeros((128, 128), np.float32)
    offs[:] = ((np.arange(128) // 32) * T)[None, :]
    c["c_offs"] = offs
    return c


def host_params(inp):
    f = lambda a: np.ascontiguousarray(np.asarray(a, dtype=np.float32))
    m = {}
    m["w_in"] = f(inp["w_in"][0])
    m["gmixr"] = f(inp["norm_mix_g"][0].reshape(1, D))
    m["g2r"] = f(inp["norm_ffn_g"][0].reshape(1, D))
    m["mu"] = f(np.stack([inp["mu_prev"][0].reshape(15, 128).T, inp["mu_next"][0].reshape(15, 128).T], axis=-1))
    names = ["w0_f", "w0_b", "a0_f", "a0_b", "k_k", "k_a", "r_k", "ln_x_w", "ln_x_b"]
    m["pp"] = f(np.stack([np.asarray(inp[n][0]).reshape(4, 128).T for n in names], axis=-1))
    m["sink"] = f(inp["attn_sink"][0].reshape(1, 8))
    m["g2"] = f(inp["norm_ffn_g"][0].reshape(8, 128).T)
    m["gF"] = f(np.asarray(inp["norm_final_g"]).reshape(1, D))
    m["wup"] = f(np.concatenate([inp["w_up_f"][0], inp["w_up_b"][0]], axis=0))
    m["aup"] = f(np.concatenate([inp["a_up_f"][0], inp["a_up_b"][0]], axis=0))
    m["gup"] = f(inp["g_up"][0])
    m["wpr"] = f(inp["w_proj_rwkv"][0])
    m["wpa"] = f(inp["w_proj_attn"][0])
    m["wout"] = f(inp["w_out"][0])
    m["wr"] = f(inp["w_router"][0])
    m["eg"] = f(inp["exp_w_gate"][0])
    m["eu"] = f(inp["exp_w_up"][0])
    m["ed"] = f(inp["exp_w_down"][0])
    return m


_NC_CACHE = {}


def run(inp, n_cores=8, stop=None, dbg=False, raw=False):
    x = np.asarray(inp["x"], dtype=np.float32)
    B, T, _ = x.shape
    NS = B // n_cores
    key = (NS, T, stop, dbg)
    if key not in _NC_CACHE:
        _NC_CACHE[key] = Builder(NS, T, stop, dbg).build()
    nc = _NC_CACHE[key]
    shared = host_params(inp)
    shared.update(host_consts(T))
    in_maps = []
    for c in range(n_cores):
        m = dict(shared)
        m["x"] = np.ascontiguousarray(x[c * NS:(c + 1) * NS].reshape(NS * T, D))
        in_maps.append(m)
    res = run_bass_kernel_spmd(nc, in_maps, core_ids=list(range(n_cores)))
    if raw:
        return res.results
    out = np.concatenate([r["y"].reshape(NS, T, D) for r in res.results], axis=0)
    return out.astype(np.float32)


def kernel(**inputs):
    return run(inputs, 8)
```

```python
import math
from contextlib import ExitStack
import numpy as np
import concourse.bass as bass
import concourse.mybir as mybir
from concourse.bass_utils import run_bass_kernel_spmd

F32 = mybir.dt.float32
BF16 = mybir.dt.bfloat16
U32 = mybir.dt.uint32
I32 = mybir.dt.int32
AF = mybir.ActivationFunctionType
ALU = mybir.AluOpType
AX = mybir.AxisListType

ENGS = ("pe", "dve", "act", "pool", "sp")
SEM_WRAP = 4000
NDMA_SEM = 12
NBANK = {'pe': 24, 'dve': 12, 'act': 12, 'pool': 6, 'sp': 2}

D = 1024
DS = math.exp(-0.5)
LNX_EPS = 64e-5
NORM_EPS = 1e-6
NEXP = 16
L = 64


class Buf:
    __slots__ = ("name", "w", "r")

    def __init__(self, name):
        self.name = name
        self.w = None
        self.r = []


class Op:
    __slots__ = ("eng", "fn", "waits", "signal", "idx", "dma", "tk", "sigval", "gid", "ninc")

    def __init__(self, eng, fn, dma):
        self.eng = eng
        self.fn = fn
        self.waits = []
        self.signal = False
        self.dma = dma
        self.tk = None
        self.sigval = None


class Prog:
    def __init__(self, nc, stack):
        self.nc = nc
        self.bufs = []
        self.sigcount = {e: 0 for e in ENGS}
        self.ndma = {e: 0 for e in ENGS}
        self.sems = {e: [stack.enter_context(nc.semaphore(f"s_{e}_{i}")) for i in range(NBANK[e])] for e in ENGS}
        self.dsems = {e: [stack.enter_context(nc.semaphore(f"d_{e}_{i}")) for i in range(NDMA_SEM)]
                      for e in ("sp", "pool", "act")}
        self.gid = 0
        self._reset()

    def _reset(self):
        self.q = {e: [] for e in ENGS}
        self.seen = {e: {} for e in ENGS}
        self.seen_dma = {e: set() for e in ENGS}
        self.dma_hist = {e: {} for e in ENGS}
        self.pending_dma = []
        for b in self.bufs:
            b.w = None
            b.r = []

    def buf(self, name="b"):
        b = Buf(name)
        self.bufs.append(b)
        return b

    def _dep(self, op, d):
        eng = op.eng
        if d is op:
            return
        if d.dma:
            if d.gid in self.seen_dma[eng]:
                return
            self.seen_dma[eng].add(d.gid)
            op.waits.append(d)
        else:
            if d.eng == eng and eng == "pe":
                return
            if self.seen[eng].get(d.eng, -1) >= d.idx:
                return
            self.seen[eng][d.eng] = d.idx
            d.signal = True
            op.waits.append(d)

    capture = None

    def emit(self, eng, fn, reads=(), writes=(), dma=False, ninc=1, est=None, hold=False):
        if self.capture is not None:
            self.capture.append((eng, fn, tuple(reads), tuple(writes), dma, est, hold))
            return None
        op = Op(eng, fn, dma)
        op.ninc = ninc
        op.gid = self.gid
        self.gid += 1
        op.idx = len(self.q[eng])
        deps = []
        for b in reads:
            if b.w is not None:
                deps.append(b.w)
        for b in writes:
            if b.w is not None:
                deps.append(b.w)
            deps.extend(b.r)
        for d in deps:
            self._dep(op, d)
        if dma:
            j = self.ndma[eng]
            self.ndma[eng] += 1
            op.tk = (j % NDMA_SEM, 16 * (j // NDMA_SEM + 1))
            hist = self.dma_hist[eng]
            if (j - NDMA_SEM) in hist:
                self._dep(op, hist[j - NDMA_SEM])
            hist[j] = op
            self.pending_dma.append(op)
        for b in reads:
            if not dma:
                b.r = [x for x in b.r if x.dma or x.eng != eng]
            b.r.append(op)
        for b in writes:
            b.w = op
            b.r = []
        self.q[eng].append(op)
        return op

    def barrier(self):
        pend = self.pending_dma
        self.pending_dma = []
        last = []
        for e in ENGS:
            for op in reversed(self.q[e]):
                if not op.dma and not getattr(op.fn, "_is_nop", False):
                    last.append(op)
                    break
        for e in ENGS:
            fn = lambda en: en.nop()
            op = self.emit(e, fn)
            for d in last:
                self._dep(op, d)
            for d in pend:
                self._dep(op, d)

    def stage_end(self):
        self.barrier()

    def flush(self):
        self.barrier()
        nc = self.nc
        base = dict(self.sigcount)
        for e in ENGS:
            c = base[e]
            for op in self.q[e]:
                if op.signal:
                    assert (c % SEM_WRAP) + op.ninc <= SEM_WRAP
                    c += op.ninc
                    op.sigval = c
            self.sigcount[e] = c
        sems, dsems = self.sems, self.dsems

        def resolve(d):
            if d.dma:
                return dsems[d.eng][d.tk[0]], d.tk[1]
            v = d.sigval - 1
            assert v // SEM_WRAP < NBANK[d.eng], (d.eng, v)
            return sems[d.eng][v // SEM_WRAP], v % SEM_WRAP + 1

        def run(e, engobj):
            for op in self.q[e]:
                for d in op.waits:
                    s, v = resolve(d)
                    engobj.wait_ge(s, v)
                ins = op.fn(engobj)
                if op.dma:
                    ins.then_inc(dsems[e][op.tk[0]], 16)
                elif op.signal:
                    v = op.sigval - 1
                    assert v // SEM_WRAP < NBANK[e], (e, v)
                    ins.then_inc(sems[e][v // SEM_WRAP], 1)

        with nc.Block() as block:
            @block.tensor
            def _(t):
                run("pe", t)

            @block.vector
            def _(v):
                run("dve", v)

            @block.scalar
            def _(s):
                run("act", s)

            @block.gpsimd
            def _(g):
                run("pool", g)

            @block.sync
            def _(sp):
                run("sp", sp)
        self._reset()


class Tl:
    def __init__(self, t, b):
        self.t = t
        self.b = b

    def __getitem__(self, k):
        return self.t[k]


class _Stop(Exception):
    pass


class Builder:
    def __init__(self, NS, T, stop=None, dbg=False):
        self.stop, self.dbg = stop, dbg
        self.NS, self.T = NS, T
        self.QS = min(512, T)
        self.NQ = T // self.QS
        self.TT = T // 128
        self.NCH = T // L
        self.CQ = self.QS // L
        self.CAP = 2 * T // NEXP
        self.SB = min(128, self.CAP)
        self.NB = self.CAP // self.SB
        self.nc = bass.Bass("TRN2", target_bir_lowering=False)
        self.stack = ExitStack()
        self.P = Prog(self.nc, self.stack)
        self.psi = 0

    def sb(self, st, name, shape, dt):
        self.nsb = getattr(self, "nsb", 0) + 1
        name = f"{name}__{self.nsb}"
        t = st.enter_context(self.nc.sbuf_tensor(name, list(shape), dt))
        return Tl(t, self.P.buf(name))

    def chk(self, k):
        if self.stop == k:
            self.P.flush()
            raise _Stop()

    def dram_in(self, name, shape, dt=F32):
        return self.nc.dram_tensor(name, list(shape), dt, kind="ExternalInput").ap()

    def nps(self, grp=None):
        if grp is None:
            p = self.ps[self.psi % 8]
            self.psi += 1
            return p
        banks = {"chain": (0, 1, 2, 3), "A": (4, 5), "B": (6, 7), "attO0": (0,), "attD0": (1,), "attO1": (2,), "attD1": (3,),
                 "attS": (4, 5, 6, 7)}[grp]
        self.psg = getattr(self, "psg", {})
        i = self.psg.get(grp, 0)
        self.psg[grp] = i + 1
        return self.ps[banks[i % len(banks)]]

    def mm(self, out, lhsT, rhs, start=True, stop=True, r=(), w=(), nohold=False):
        self.P.emit("pe", lambda e: e.matmul(out, lhsT=lhsT, rhs=rhs, start=start, stop=stop), r, w,
                    est=max(64, rhs.free_size()) / 2400.0 + 0.004, hold=(not stop) and not nohold)

    def tr(self, out, in_, ident, r=(), w=()):
        self.P.emit("pe", lambda e: e.transpose(out=out, in_=in_, identity=ident), r, w, est=0.06)

    def act(self, out, in_, func, bias=None, scale=None, accum=None, r=(), w=()):
        kw = {}
        if bias is not None:
            kw["bias"] = bias
        if scale is not None:
            kw["scale"] = scale
        if accum is not None:
            kw["accum_out"] = accum
        self.P.emit("act", lambda e: e.activation(out=out, in_=in_, func=func, **kw), r, w, ninc=1,
                    est=(224 + out.free_size()) / 1200.0)

    def tt(self, eng, out, in0, in1, op, r=(), w=()):
        self.P.emit(eng, lambda e: e.tensor_tensor(out=out, in0=in0, in1=in1, op=op), r, w, est=self._est(eng, out))

    def ts(self, eng, out, in0, s1, s2, op0, op1=None, r=(), w=()):
        if op1 is None:
            self.P.emit(eng, lambda e: e.tensor_scalar(out=out, in0=in0, scalar1=s1, scalar2=None, op0=op0), r, w,
                        est=self._est(eng, out))
        else:
            self.P.emit(eng, lambda e: e.tensor_scalar(out=out, in0=in0, scalar1=s1, scalar2=s2, op0=op0, op1=op1), r, w,
                        est=self._est(eng, out))

    def stt(self, out, in0, scalar, in1, op0, op1, r=(), w=()):
        self.P.emit("dve", lambda e: e.scalar_tensor_tensor(out=out, in0=in0, scalar=scalar, in1=in1, op0=op0, op1=op1), r, w,
                    est=self._est("dve", out))

    def cp(self, eng, out, in_, r=(), w=()):
        if eng == "act":
            self.P.emit("act", lambda e: e.activation(out=out, in_=in_, func=AF.Copy), r, w, est=(224 + out.free_size()) / 1200.0)
        else:
            self.P.emit(eng, lambda e: e.tensor_copy(out=out, in_=in_), r, w, est=self._est(eng, out))

    def ms(self, eng, ap, val, w=()):
        self.P.emit(eng, lambda e: e.memset(ap, val), (), w, est=self._est(eng, ap))

    def _est(self, eng, out):
        n = out.free_size()
        if eng == "pool":
            return (150 + 1.6 * n) / 1200.0
        return (100 + n) / 960.0

    def interleave(self, gens, window=None):
        P = self.P
        allg = [g for g in gens if g is not None]
        if window is None:
            window = len(allg)
        pending = allg[window:]
        streams = [{"g": g, "q": [], "done": False} for g in allg[:window]]
        eng_free = getattr(self, "_sim_eng", None)
        if eng_free is None:
            eng_free = self._sim_eng = {e: 0.0 for e in ENGS}
            self._sim_ready = {}
            self._sim_lastrd = {}
        ready, lastrd = self._sim_ready, self._sim_lastrd

        def fill(st_):
            while not st_["q"] and not st_["done"]:
                P.capture = st_["q"]
                try:
                    next(st_["g"])
                except StopIteration:
                    st_["done"] = True
                finally:
                    P.capture = None

        def start_time(o):
            eng, fn, rd, wr, dma, est, hold = o
            t = eng_free[eng]
            for b in rd:
                t = max(t, ready.get(b, (0.0, None))[0] + (0.15 if ready.get(b, (0.0, eng))[1] != eng else 0.05))
            for b in wr:
                t = max(t, ready.get(b, (0.0, None))[0] + 0.05, lastrd.get(b, 0.0) + 0.1)
            return t

        forced = None
        while True:
            for st_ in streams:
                fill(st_)
            for i_, st_ in enumerate(streams):
                if st_["done"] and not st_["q"] and pending:
                    streams[i_] = {"g": pending.pop(0), "q": [], "done": False}
                    fill(streams[i_])
            live = [st_ for st_ in streams if st_["q"]]
            if not live:
                break
            if forced is not None and forced["q"]:
                best = forced
            else:
                best = min(live, key=lambda st_: start_time(st_["q"][0]))
            o = best["q"].pop(0)
            eng, fn, rd, wr, dma, est, hold = o
            forced = best if hold else None
            t0 = start_time(o)
            dur = est if est is not None else 0.3
            if dma:
                eng_free[eng] = t0 + 0.06
                tend = t0 + 2.5
            else:
                eng_free[eng] = t0 + dur
                tend = t0 + dur
            for b in rd:
                lastrd[b] = max(lastrd.get(b, 0.0), tend)
            for b in wr:
                ready[b] = (tend, eng)
                lastrd[b] = 0.0
            P.emit(eng, fn, rd, wr, dma=dma)

    def dma(self, out, in_, r=(), w=(), eng="sp"):
        return self.P.emit(eng, lambda e: e.dma_start(out=out, in_=in_), r, w, dma=True)

    def load_w(self, dst, dst_b, src, kc=None, n=None, scale=None):
        assert scale is None
        self.dma(dst, src, w=[dst_b], eng="pool")

    def build(self):
        nc, P, NS, T = self.nc, self.P, self.NS, self.T
        QS, NQ, TT, NCH, CQ = self.QS, self.NQ, self.TT, self.NCH, self.CQ
        st0 = self.stack
        din = self.dram_in
        x = din("x", [NS * T, D])
        w_in = din("w_in", [D, 4736]).rearrange("(k p) n -> p k n", p=128)
        gmixr_d = din("gmixr", [1, D])
        g2r_d = din("g2r", [1, D])
        mu_d = din("mu", [128, 15, 2])
        pp_d = din("pp", [128, 4, 9])
        sink_d = din("sink", [1, 8])
        g2_d = din("g2", [128, 8])
        gF_d = din("gF", [1, D])
        wup_d = din("wup", [128, 512])
        aup_d = din("aup", [128, 512])
        gup_d = din("gup", [128, 512])
        wpr_d = din("wpr", [512, D]).rearrange("(k p) n -> p k n", p=128)
        wpa_d = din("wpa", [512, D]).rearrange("(k p) n -> p k n", p=128)
        wout_d = din("wout", [D, D]).rearrange("(k p) n -> p k n", p=128)
        wr_d = din("wr", [D, NEXP]).rearrange("(k p) n -> p k n", p=128)
        eg_d = din("eg", [NEXP, D, D])
        eu_d = din("eu", [NEXP, D, D])
        ed_d = din("ed", [NEXP, D, D])
        c_idf = din("c_idf", [128, 128])
        c_bo = din("c_bo", [128, 128])
        c_ba = din("c_ba", [128, 128])
        c_mT = din("c_mT", [128, 2, 512])
        c_mN = din("c_mN", [128, 2, 512])
        c_idr = din("c_idr", [128, 512])
        c_bias = din("c_bias", [128, 6, 512])
        c_offs = din("c_offs", [128, 128])
        y = nc.dram_tensor("y", [NS * T, D], F32, kind="ExternalOutput").ap()
        ik = "ExternalOutput" if self.dbg else "Internal"
        uS = nc.dram_tensor("uS", [15, 128, T], BF16, kind=ik).ap()
        acc = nc.dram_tensor("acc", [NS * T, D], F32, kind=ik).ap()
        h2d = nc.dram_tensor("h2d", [NS * T, D], BF16, kind="Internal").ap()
        hTd = nc.dram_tensor("hTd", [128, 8, T], BF16, kind="Internal").ap()
        B_hTd = P.buf("hTd")
        if self.dbg:
            self.dbgR = nc.dram_tensor("dbgR", [128, 4, T], BF16, kind="ExternalOutput").ap()
            self.dbgA = nc.dram_tensor("dbgA", [128, 4, T], BF16, kind="ExternalOutput").ap()
        B_uS = [P.buf(f"uS{c}") for c in range(15)]
        B_acc = [P.buf(f"acc{s}") for s in range(NS)]
        B_h2d = [P.buf(f"h2d{s}") for s in range(NS)]

        self.ps = []
        for i in range(8):
            t = st0.enter_context(nc.psum_tensor(f"ps{i}", [128, 512], F32))
            self.ps.append(Tl(t, P.buf(f"ps{i}")))

        sb = self.sb
        idf = sb(st0, "idf", [128, 128], F32)
        idb = sb(st0, "idb", [128, 128], BF16)
        bo = sb(st0, "bo", [128, 128], F32)
        ba = sb(st0, "ba", [128, 128], F32)
        onesb = sb(st0, "onesb", [128, 128], BF16)
        bob = sb(st0, "bob", [128, 128], BF16)
        mu = sb(st0, "mu", [128, 15, 2], F32)
        mu0 = sb(st0, "mu0", [128, 15], F32)
        pp = sb(st0, "pp", [128, 4, 9], F32)
        omka = sb(st0, "omka", [128, 4], F32)
        g2 = sb(st0, "g2", [128, 8], F32)
        wrt = sb(st0, "wrt", [128, 8, NEXP], F32)
        affT = sb(st0, "affT", [128, T], F32)
        self.wst = [sb(st0, f"wst{i}", [128, 8 * NEXP], F32) for i in range(1)]
        self.wsi = 0
        PW0F, PW0B, PA0F, PA0B, PKK, PKA, PRK, PLW, PLB = range(9)

        for (dst, src) in ((idf, c_idf), (bo, c_bo), (ba, c_ba), (mu, mu_d), (pp, pp_d), (g2, g2_d)):
            self.dma(dst.t[:], src, w=[dst.b])
        self.cp("pool", idb[:], idf[:], r=[idf.b], w=[idb.b])
        self.ms("pool", onesb[:], 1.0, w=[onesb.b])
        self.cp("pool", bob[:], bo[:], r=[bo.b], w=[bob.b])
        self.tt("dve", mu0[:], mu[:, :, 0], mu[:, :, 1], ALU.add, r=[mu.b], w=[mu0.b])
        self.ts("dve", mu0[:], mu0[:], -1.0, 1.0, ALU.mult, ALU.add, r=[mu0.b], w=[mu0.b])
        self.ts("dve", omka[:], pp[:, :, PKA], -1.0, 1.0, ALU.mult, ALU.add, r=[pp.b], w=[omka.b])
        self.dma(self.wst[0].t[:, 0:8 * NEXP].rearrange("p (k n) -> p k n", k=8), wr_d, w=[self.wst[0].b])
        self.tt("dve", wrt[:], self.wst[0].t[:, 0:8 * NEXP].rearrange("p (k n) -> p k n", k=8),
                g2.t[:].unsqueeze(2).to_broadcast([128, 8, NEXP]),
                ALU.mult, r=[self.wst[0].b, g2.b], w=[wrt.b])
        self.ms("pool", affT[:], 0.0, w=[affT.b])
        self.yRk = sb(st0, "yRk", [128, 4, T], BF16)
        P.stage_end()

        env = locals()
        try:
            self.chk(0)
            for s in range(NS):
                self.phase_a(s, env)
            self.phase_b(env)
            self.chk(6)
            self.phase_c(env)
        except _Stop:
            return nc
        self.P.flush()
        print("signals", self.P.sigcount, "dmas", self.P.ndma)
        self.stack.close()
        return nc

    def norm_hT(self, st, s, env, hT):
        T, TT = self.T, self.TT
        x = env["x"]
        idb = env["idb"]
        gbc = self.sb(st, "gbc", [128, D], F32)
        self.dma(gbc.t[:], env["gmixr_d"].partition_broadcast(128), w=[gbc.b])
        xts = [self.sb(st, f"xt{i}", [128, D], F32) for i in range(2)]
        hns = [self.sb(st, f"hn{i}", [128, D], BF16) for i in range(2)]
        junk = self.sb(st, "junk", [128, D], F32)
        sst = [self.sb(st, f"ss{i}", [128, 4], F32) for i in range(2)]
        for tt_ in range(TT):
            xt, hn, ss = xts[tt_ % 2], hns[tt_ % 2], sst[tt_ % 2]
            r0 = s * T + tt_ * 128
            self.dma(xt.t[:], x[r0:r0 + 128, :], w=[xt.b])
            self.chk(20)
            self.act(junk[:], xt[:], AF.Square, accum=ss[:, 0:1], r=[xt.b], w=[junk.b, ss.b])
            self.chk(21)
            self.ts("dve", ss[:, 1:2], ss[:, 0:1], 1.0 / D, NORM_EPS, ALU.mult, ALU.add, r=[ss.b], w=[ss.b])
            self.act(ss[:, 2:3], ss[:, 1:2], AF.Sqrt, r=[ss.b], w=[ss.b])
            self.chk(22)
            self.P.emit("dve", lambda e, o=ss[:, 3:4], i=ss[:, 2:3]: e.reciprocal(out=o, in_=i), [ss.b], [ss.b])
            self.stt(hn[:], xt[:], ss[:, 3:4], gbc[:], ALU.mult, ALU.mult, r=[xt.b, ss.b, gbc.b], w=[hn.b])
            self.chk(23)
            ps = self.nps()
            psv = ps.t.bitcast(BF16)
            for kc in range(8):
                self.tr(psv[:, kc * 128:(kc + 1) * 128], hn[:, kc * 128:(kc + 1) * 128], idb[:], r=[hn.b, idb.b], w=[ps.b])
            self.chk(24)
            import os
            if os.environ.get("VARX") == "1":
                self.cp("dve", junk.t[:].bitcast(BF16)[:, 0:1024], psv[:, :], r=[ps.b], w=[junk.b])
            elif os.environ.get("VARX") == "2":
                self.cp("dve", hT[:, 0, 0:128], psv[:, 0:128], r=[ps.b], w=[hT.b])
            else:
                self.cp("dve", hT[:, :, tt_ * 128:(tt_ + 1) * 128], psv[:, :].rearrange("p (k n) -> p k n", k=8), r=[ps.b], w=[hT.b])
            self.chk(25)

    def inproj(self, hT, wb, col0, tq, reads, ps):
        QS = self.QS
        for kc in range(8):
            self.mm(ps[:, 0:QS], wb[:, kc, col0:col0 + 128], hT[:, kc, tq * QS:(tq + 1) * QS],
                    start=(kc == 0), stop=(kc == 7), r=reads, w=[ps.b])

    def phase_a(self, s, env):
        P, T, QS, NQ, TT, NCH, CQ = self.P, self.T, self.QS, self.NQ, self.TT, self.NCH, self.CQ
        w_in, mu, mu0, pp, omka = env["w_in"], env["mu"], env["mu0"], env["pp"], env["omka"]
        uS, B_uS = env["uS"], env["B_uS"]
        PW0F, PW0B, PA0F, PA0B, PKK, PKA, PRK, PLW, PLB = range(9)
        sb = self.sb

        with ExitStack() as st:
            hT = sb(st, "hT", [128, 8, T], BF16)
            self.norm_hT(st, s, env, hT)
            self.dma(env["hTd"], hT[:], r=[hT.b], w=[env["B_hTd"]])
            self.chk(10)
            wbs = [sb(st, f"wb{i}", [128, 8, 512], BF16) for i in range(2)]
            upad = sb(st, "upad", [128, T + 2], BF16)
            tmp = sb(st, "tmpA", [128, T], BF16)
            uss = [sb(st, f"us{i}", [128, T], BF16) for i in range(2)]
            self.ms("pool", upad[:, 0:1], 0.0, w=[upad.b])
            self.ms("pool", upad[:, T + 1:T + 2], 0.0, w=[upad.b])
            for grp in range(4):
                c0 = grp * 4
                ncol = min(4, 15 - c0)
                wb = wbs[grp % 2]
                self.load_w(wb[:, :, 0:ncol * 128], wb.b, w_in[:, :, c0 * 128:(c0 + ncol) * 128], 8, ncol * 128)
                self.chk(11)
                for ci in range(ncol):
                    c = c0 + ci
                    us = uss[c % 2]
                    for tq in range(NQ):
                        ps = self.nps()
                        self.inproj(hT, wb, ci * 128, tq, [wb.b, hT.b], ps)
                        self.cp("act", upad[:, 1 + tq * QS:1 + (tq + 1) * QS], ps[:, 0:QS], r=[ps.b], w=[upad.b])
                    self.ts("dve", tmp[:], upad[:, 1:T + 1], mu0[:, c:c + 1], None, ALU.mult, r=[upad.b, mu0.b], w=[tmp.b])
                    self.stt(tmp[:], upad[:, 0:T], mu[:, c, 0:1], tmp[:], ALU.mult, ALU.add, r=[upad.b, mu.b, tmp.b], w=[tmp.b])
                    self.stt(us[:], upad[:, 2:T + 2], mu[:, c, 1:2], tmp[:], ALU.mult, ALU.add, r=[upad.b, mu.b, tmp.b], w=[us.b])
                    if c == 12:
                        self.act(us[:], us[:], AF.Sigmoid, r=[us.b], w=[us.b])
                    elif c == 13:
                        self.act(us[:], us[:], AF.Tanh, r=[us.b], w=[us.b])
                    self.dma(uS[c], us[:], r=[us.b], w=[B_uS[c]])
                    self.chk(12)
            P.stage_end()
        self.chk(1)

        with ExitStack() as st:
            self.rwkv(st, s, env, self.yRk)
            if self.dbg and s == 0:
                self.dma(self.dbgR, self.yRk[:], r=[self.yRk.b])
            P.stage_end()
        self.chk(2)

        with ExitStack() as st:
            yA = sb(st, "yA", [128, 4, T], BF16)
            mrg = sb(st, "mrg", [128, 8, T], BF16)
            with ExitStack() as st1:
                hT = sb(st1, "hT", [128, 8, T], BF16)
                self.dma(hT.t[:], env["hTd"], r=[env["B_hTd"]], w=[hT.b])
                with ExitStack() as st2:
                    self.attention(st2, s, env, hT, yA)
                    if self.dbg and s == 0:
                        self.dma(self.dbgA, yA[:], r=[yA.b])
                    P.stage_end()
                self.chk(3)
                self.merge_gates(st1, s, env, hT, yA, mrg)
                P.stage_end()
                self.chk(4)
            with ExitStack() as st1:
                self.out_proj(st1, s, env, mrg)
                P.stage_end()
            self.chk(5)

    def rwkv(self, st, s, env, yR):
        P, T, NCH = self.P, self.T, self.NCH
        QS = min(512, T)
        NQ = T // QS
        CQ = QS // L
        pp, omka = env["pp"], env["omka"]
        uS, B_uS = env["uS"], env["B_uS"]
        bo, ba, idb, bob = env["bo"], env["ba"], env["idb"], env["bob"]
        PW0F, PW0B, PA0F, PA0B, PKK, PKA, PRK, PLW, PLB = range(9)
        sb = self.sb
        mT = sb(st, "mT", [128, 2, 512], F32)
        mN = sb(st, "mN", [128, 2, 512], F32)
        idr = sb(st, "idr", [128, 512], F32)
        self.dma(mT.t[:], env["c_mT"], w=[mT.b])
        self.dma(mN.t[:], env["c_mN"], w=[mN.b])
        self.dma(idr.t[:], env["c_idr"], w=[idr.b])
        wup = sb(st, "wup", [128, 512], BF16)
        aup = sb(st, "aup", [128, 512], BF16)
        gup = sb(st, "gup", [128, 512], BF16)
        for dst, src in ((wup, env["wup_d"]), (aup, env["aup_d"]), (gup, env["gup_d"])):
            self.load_w(dst[:], dst.b, src)
        NQ_ = NQ
        bonus = [sb(st, f"bonus{i}", [128, T], BF16) for i in range(2)]
        wkv = sb(st, "wkv", [128, T], F32)
        vtok = [sb(st, f"vtok{i}", [128, NCH, L], BF16) for i in range(2)]
        for t_ in vtok:
            t_.bq = [P.buf("vtokq") for _ in range(NQ_)]
        R = []
        for d in range(2):
            Rd = dict(
                ar=sb(st, f"ar{d}", [128, NCH, 2, L], BF16),
                SB=sb(st, f"SB{d}", [128, NCH, L], BF16),
                SK=sb(st, f"SK{d}", [128, NCH, 2, L], BF16),
                TTm=sb(st, f"TT{d}", [128, NCH, L], BF16),
                BB=sb(st, f"BB{d}", [128, NCH, L], BF16),
                KB=sb(st, f"KB{d}", [128, NCH, L], BF16),
                Wtot=sb(st, f"Wtot{d}", [128, NCH], F32),
                ST=sb(st, f"ST{d}", [128, L], BF16),
                Xs=sb(st, f"Xs{d}", [128, L], BF16),
                Us=sb(st, f"Us{d}", [128, L], BF16),
            )
            for k_ in ("ar", "SB", "SK", "TTm", "BB", "KB", "Wtot"):
                Rd[k_].bq = [P.buf(k_ + "q") for _ in range(NQ_)]
            R.append(Rd)

        def q(name, dt=F32):
            return sb(st, name, [128, QS], dt)
        def mk_temps(sfx):
            names_f = ["t2", "sgw", "cs"] + (["X1", "t1f", "sqf"] if sfx == "A" else ["X2", "X3"])
            names_b = ["rq", "kq", "vb", "twb", "alb", "kkn", "sq", "t1", "t2b", "t3", "kft", "aqt", "E", "akk", "p1", "p2",
                       "bT", "kT", "Pm", "PTm", "Pm2", "PTm2"] + (["sgb"] if sfx == "A" else [])
            d_ = {n: q(n + sfx) for n in names_f}
            d_.update({n: q(n + sfx, BF16) for n in names_b})
            for n in ("X1", "X2", "X3"):
                d_.setdefault(n, None)
            return d_
        TA, TB = mk_temps("A"), mk_temps("B")
        TA["grp"], TB["grp"] = "A", "B"
        sgb, t1, t2, sq = (TA[n] for n in ("sgb", "t1f", "t2", "sqf"))
        scm = sb(st, "scm", [128, QS], F32)
        self.ms("pool", scm[:], 1.0, w=[scm.b])
        self.ms("pool", scm.t[:].rearrange("p (c l) -> p c l", l=L)[:, :, 0:1], 0.0, w=[scm.b])

        def v3(ap):
            return ap.rearrange("p (c l) -> p c l", l=L)

        def pre_pass(p, d, tq, first, tmp):
            (rq, kq, vb, twb, alb, kkn, sq, t1, t2, t2b, t3, kft, aqt, sgw, cs, X1, X2, X3, E, akk, p1, p2,
             bT, kT, Pm, PTm, Pm2, PTm2) = (tmp[n] for n in (
                "rq", "kq", "vb", "twb", "alb", "kkn", "sq", "t1", "t2", "t2b", "t3", "kft", "aqt", "sgw", "cs", "X1", "X2", "X3",
                "E", "akk", "p1", "p2", "bT", "kT", "Pm", "PTm", "Pm2", "PTm2"))
            par = p % 2
            pc = slice(p * 128, (p + 1) * 128)
            ts_ = slice(tq * QS, (tq + 1) * QS)
            cq0 = tq * CQ
            csl = slice(cq0, cq0 + CQ)
            Rd = R[d]
            hs_lo = slice(64 * d, 64 * d + 64)
            for dst, c in ((rq, p), (kq, 4 + p), (vb, 8 + p), (twb, 13), (alb, 14)):
                self.dma(dst.t[:], uS[c][:, ts_], r=[B_uS[c]], w=[dst.b])
            yield
            self.ts("dve", t1[:], kq[:], pp[:, p, PKK:PKK + 1], None, ALU.mult, r=[kq.b, pp.b], w=[t1.b])
            self.tt("pool", sq[:], t1[:], t1[:], ALU.mult, r=[t1.b], w=[sq.b])
            ps = self.nps(tmp["grp"])
            self.mm(ps[:, 0:QS], bob[:], sq[:], r=[bob.b, sq.b], w=[ps.b])
            self.ts("dve", t2[:], ps[:, 0:QS], 1e-24, None, ALU.max, r=[ps.b], w=[t2.b])
            self.act(t2[:], t2[:], AF.Ln, r=[t2.b], w=[t2.b])
            self.act(t2b[:], t2[:], AF.Exp, scale=-0.5, r=[t2.b], w=[t2b.b])
            self.tt("dve", kkn[:], t1[:], t2b[:], ALU.mult, r=[t1.b, t2b.b], w=[kkn.b])
            yield
            ps = self.nps(tmp["grp"])
            self.mm(ps[:, 0:QS], aup[hs_lo, pc], alb[hs_lo, :], r=[aup.b, alb.b], w=[ps.b])
            self.act(aqt[:], ps[:, 0:QS], AF.Sigmoid, bias=pp[:, p, PA0F + d:PA0F + d + 1], r=[ps.b, pp.b], w=[aqt.b])
            self.ts("dve", t3[:], aqt[:], pp[:, p, PKA:PKA + 1], omka[:, p:p + 1], ALU.mult, ALU.add,
                    r=[aqt.b, pp.b, omka.b], w=[t3.b])
            self.tt("dve", kft[:], kq[:], t3[:], ALU.mult, r=[kq.b, t3.b], w=[kft.b])
            yield
            self.stt(t3[:], kft[:], pp[:, p, PRK:PRK + 1], rq[:], ALU.mult, ALU.mult, r=[kft.b, pp.b, rq.b], w=[t3.b])
            ps = self.nps(tmp["grp"])
            self.mm(ps[:, 0:QS], bob[:], t3[:], r=[bob.b, t3.b], w=[ps.b])
            if first:
                self.tt("dve", bonus[par][:, ts_], ps[:, 0:QS], vb[:], ALU.mult, r=[ps.b, vb.b], w=[bonus[par].b])
            else:
                self.tt("dve", t3[:], ps[:, 0:QS], vb[:], ALU.mult, r=[ps.b, vb.b], w=[t3.b])
                self.tt("pool", bonus[par][:, ts_], bonus[par][:, ts_], t3[:], ALU.add, r=[t3.b, bonus[par].b], w=[bonus[par].b])
            yield
            if first:
                ps = self.nps(tmp["grp"])
                psv = ps.t.bitcast(BF16)
                for c in range(CQ):
                    for h in range(2):
                        hs = slice(64 * h, 64 * h + 64)
                        self.tr(psv[hs, c * L:(c + 1) * L], vb[hs, c * L:(c + 1) * L], idb[hs, hs], r=[vb.b, idb.b], w=[ps.b])
                self.cp("act", vtok[par][:, csl, :], v3(psv[:, 0:QS]), r=[ps.b], w=[vtok[par].bq[tq]])
                yield
            ps = self.nps(tmp["grp"])
            self.mm(ps[:, 0:QS], wup[hs_lo, pc], twb[hs_lo, :], r=[wup.b, twb.b], w=[ps.b])
            self.act(sgw[:], ps[:, 0:QS], AF.Sigmoid, bias=pp[:, p, PW0F + d:PW0F + d + 1], r=[ps.b, pp.b], w=[sgw.b])
            self.P.emit("dve", lambda e, o=cs[:], a=scm[:], b=sgw[:]: e.tensor_tensor_scan(
                out=o, data0=a, data1=b, initial=0.0, op0=ALU.mult, op1=ALU.add), [scm.b, sgw.b], [cs.b], est=(100 + 2 * QS) / 960.0)
            tot = v3(cs[:])[:, :, L - 1:L]
            self.act(Rd["Wtot"][:, csl], tot.rearrange("p c l -> p (c l)"), AF.Exp, scale=-DS, r=[cs.b], w=[Rd["Wtot"].bq[tq]])
            if d == 0:
                self.tt("pool", X1[:], cs[:], sgw[:], ALU.subtract, r=[cs.b, sgw.b], w=[X1.b])
                ce, ci = X1, cs
            else:
                self.tt("dve", v3(X2[:]), tot.to_broadcast([128, CQ, L]), v3(cs[:]), ALU.subtract, r=[cs.b], w=[X2.b])
                self.tt("pool", X3[:], X2[:], sgw[:], ALU.add, r=[X2.b, sgw.b], w=[X3.b])
                ce, ci = X2, X3
            yield
            ar = Rd["ar"]
            arb = ar.bq[tq]
            self.act(E[:], ce[:], AF.Exp, scale=-DS, r=[ce.b], w=[E.b])
            self.stt(ar[:, csl, 0, :], v3(kkn[:]), -1.0, v3(E[:]), ALU.mult, ALU.mult, r=[kkn.b, E.b], w=[arb])
            self.act(p1[:], ci[:], AF.Exp, scale=-DS, r=[ci.b], w=[p1.b])
            self.tt("dve", ar[:, csl, 1, :], v3(rq[:]), v3(p1[:]), ALU.mult, r=[rq.b, p1.b], w=[arb])
            yield
            self.tt("pool", akk[:], aqt[:], kkn[:], ALU.mult, r=[aqt.b, kkn.b], w=[akk.b])
            self.act(p2[:], ci[:], AF.Exp, scale=DS, r=[ci.b], w=[p2.b])
            self.tt("dve", bT[:], akk[:], p2[:], ALU.mult, r=[akk.b, p2.b], w=[bT.b])
            self.tt("dve", kT[:], kft[:], p2[:], ALU.mult, r=[kft.b, p2.b], w=[kT.b])
            yield
            for src, dstk in ((bT, "BB"), (kT, "KB")):
                ps = self.nps(tmp["grp"])
                psv = ps.t.bitcast(BF16)
                for c in range(CQ):
                    for h in range(2):
                        hs = slice(64 * h, 64 * h + 64)
                        self.tr(psv[hs, c * L:(c + 1) * L], src[hs, c * L:(c + 1) * L], idb[hs, hs],
                                r=[src.b, idb.b], w=[ps.b])
                self.cp("act", Rd[dstk][:, csl, :], v3(psv[:, 0:QS]), r=[ps.b], w=[Rd[dstk].bq[tq]])
                yield
            for lhs, dstk in ((bT, "SB"), (kT, "SK")):
                for c4 in range(0, CQ, 4):
                    ps = self.nps(tmp["grp"])
                    for cc in range(4):
                        c = c4 + cc
                        for h in range(2):
                            hs = slice(64 * h, 64 * h + 64)
                            self.mm(ps[hs, cc * 128:(cc + 1) * 128], lhs[hs, c * L:(c + 1) * L],
                                    ar[hs, cq0 + c, :, :].rearrange("p a l -> p (a l)"),
                                    r=[lhs.b, arb], w=[ps.b])
                    if dstk == "SK":
                        self.tt("dve", Rd[dstk][:, cq0 + c4:cq0 + c4 + 4, :, :].rearrange("p c a l -> p (c a l)"),
                                ps[:, :], mT[:, d, :], ALU.mult, r=[ps.b, mT.b], w=[Rd[dstk].bq[tq]])
                    else:
                        ps4 = ps[:, :].rearrange("p (c a l) -> p c a l", c=4, a=2)
                        m4 = mT[:, d, :].rearrange("p (c a l) -> p c a l", c=4, a=2)
                        self.tt("dve", v3(PTm[:])[:, c4:c4 + 4, :], ps4[:, :, 0, :], m4[:, :, 0, :], ALU.mult,
                                r=[ps.b, mT.b], w=[PTm.b])
                        self.tt("dve", Rd["SB"][:, cq0 + c4:cq0 + c4 + 4, :], ps4[:, :, 1, :], m4[:, :, 1, :], ALU.mult,
                                r=[ps.b, mT.b], w=[Rd["SB"].bq[tq]])
                    yield
            ps = self.nps(tmp["grp"])
            for c in range(CQ):
                for h in range(2):
                    hs = slice(64 * h, 64 * h + 64)
                    self.mm(ps[hs, c * L:(c + 1) * L], ar[hs, cq0 + c, 0, :], bT[hs, c * L:(c + 1) * L],
                            r=[arb, bT.b], w=[ps.b])
            self.tt("dve", Pm[:], ps[:, 0:QS], mN[:, d, 0:QS], ALU.mult, r=[ps.b, mN.b], w=[Pm.b])
            TTq = Rd["TTm"][:, csl, :]
            TTb = Rd["TTm"].bq[tq]
            self.tt("pool", TTq, v3(PTm[:]), v3(idr[:, 0:QS]), ALU.add, r=[PTm.b, idr.b], w=[TTb])
            yield
            Pc, PTc, Pn, PTn = Pm, PTm, Pm2, PTm2
            for lvl in range(1, 6):
                psA = self.nps(tmp["grp"])
                for c in range(CQ):
                    for h in range(2):
                        hs = slice(64 * h, 64 * h + 64)
                        cl = slice(c * L, (c + 1) * L)
                        self.mm(psA[hs, cl], PTc[hs, cl], Pc[hs, cl], r=[PTc.b, Pc.b], w=[psA.b])
                self.cp("act", Pn[:], psA[:, 0:QS], r=[psA.b], w=[Pn.b])
                if lvl < 5:
                    psB = self.nps(tmp["grp"])
                    for c in range(CQ):
                        for h in range(2):
                            hs = slice(64 * h, 64 * h + 64)
                            cl = slice(c * L, (c + 1) * L)
                            self.mm(psB[hs, cl], Pc[hs, cl], PTc[hs, cl], r=[PTc.b, Pc.b], w=[psB.b])
                    self.cp("act", PTn[:], psB[:, 0:QS], r=[psB.b], w=[PTn.b])
                yield
                psC = self.nps(tmp["grp"])
                for c in range(CQ):
                    for h in range(2):
                        hs = slice(64 * h, 64 * h + 64)
                        cl = slice(c * L, (c + 1) * L)
                        self.mm(psC[hs, cl], Pn[hs, cl], Rd["TTm"][hs, cq0 + c, :], r=[Pn.b, TTb], w=[psC.b])
                self.tt("dve", TTq, v3(psC[:, 0:QS]), TTq, ALU.add, r=[psC.b, TTb], w=[TTb])
                Pc, PTc, Pn, PTn = Pn, PTn, Pc, PTc
                yield

        def pre_stage(p, k):
            gens = []
            for d, tq, tmp in ((0, k, TA), (1, NQ_ - 1 - k, TB)):
                fstage, bstage = tq, NQ_ - 1 - tq
                first = (fstage <= bstage) if d == 0 else (bstage < fstage)
                gens.append(pre_pass(p, d, tq, first, tmp))
            return gens

        def chain_group(p, k):
            par = p % 2
            if k == 0:
                self.ms("pool", wkv[:], 0.0, w=[wkv.b])
                for d in range(2):
                    self.ms("pool", R[d]["ST"][:], 0.0, w=[R[d]["ST"].b])
            for step in range(k * CQ, (k + 1) * CQ):
                for d in range(2):
                    Rd = R[d]
                    c = step if d == 0 else NCH - 1 - step
                    qi = c // CQ
                    ar, SBm, SKm, TTm, BB, KB, ST, Xs, Us = (Rd[k_] for k_ in ("ar", "SB", "SK", "TTm", "BB", "KB", "ST", "Xs", "Us"))
                    vt = vtok[par]
                    vtb = vt.bq[qi]
                    psX = self.nps("chain")
                    for h in range(2):
                        hs = slice(64 * h, 64 * h + 64)
                        self.mm(psX[hs, 0:L], ar[hs, c, 0, :], ST[hs, :], start=True, stop=False, r=[ar.bq[qi], ST.b], w=[psX.b])
                        self.mm(psX[hs, 0:L], SKm[hs, c, 0, :], vt[hs, c, :], start=False, stop=True, r=[SKm.bq[qi], vtb], w=[psX.b])
                    self.cp("act", Xs[:], psX[:, 0:L], r=[psX.b], w=[Xs.b])
                    yield
                    psU = self.nps("chain")
                    for h in range(2):
                        hs = slice(64 * h, 64 * h + 64)
                        self.mm(psU[hs, 0:L], TTm[hs, c, :], Xs[hs, :], r=[TTm.bq[qi], Xs.b], w=[psU.b])
                    self.cp("dve", Us[:], psU[:, 0:L], r=[psU.b], w=[Us.b])
                    yield
                    psY = self.nps("chain")
                    for h in range(2):
                        hs = slice(64 * h, 64 * h + 64)
                        self.mm(psY[hs, 0:L], ST[hs, :], ar[hs, c, 1, :], start=True, stop=False, r=[ar.bq[qi], ST.b], w=[psY.b])
                        self.mm(psY[hs, 0:L], Us[hs, :], SBm[hs, c, :], start=False, stop=False, r=[Us.b, SBm.bq[qi]], w=[psY.b])
                        self.mm(psY[hs, 0:L], vt[hs, c, :], SKm[hs, c, 1, :], start=False, stop=True, r=[vtb, SKm.bq[qi]], w=[psY.b])
                    psS = self.nps("chain")
                    for h in range(2):
                        hs = slice(64 * h, 64 * h + 64)
                        self.mm(psS[hs, 0:L], idb[hs, hs], ST[hs, :], start=True, stop=False, r=[idb.b, ST.b], w=[psS.b])
                        self.mm(psS[hs, 0:L], BB[hs, c, :], Us[hs, :], start=False, stop=False, r=[BB.bq[qi], Us.b], w=[psS.b])
                        self.mm(psS[hs, 0:L], KB[hs, c, :], vt[hs, c, :], start=False, stop=True, r=[KB.bq[qi], vtb], w=[psS.b])
                    self.ts("dve", ST[:], psS[:, 0:L], Rd["Wtot"][:, c:c + 1], None, ALU.mult,
                            r=[Rd["Wtot"].bq[qi], psS.b], w=[ST.b])
                    wsl = wkv[:, c * L:(c + 1) * L]
                    self.tt("dve", wsl, psY[:, 0:L], wsl, ALU.add, r=[psY.b, wkv.b], w=[wkv.b])
                    yield

        def post(p):
            par = p % 2
            pc = slice(p * 128, (p + 1) * 128)
            for tq in range(NQ):
                ts_ = slice(tq * QS, (tq + 1) * QS)
                self.dma(sgb.t[:], uS[12][:, ts_], r=[B_uS[12]], w=[sgb.b])
                ps = self.nps()
                self.mm(ps[:, 0:QS], ba[:], wkv[:, ts_], r=[ba.b, wkv.b], w=[ps.b])
                self.tt("dve", t1[:], wkv[:, ts_], ps[:, 0:QS], ALU.subtract, r=[wkv.b, ps.b], w=[t1.b])
                self.tt("pool", sq[:], t1[:], t1[:], ALU.mult, r=[t1.b], w=[sq.b])
                yield
                ps = self.nps()
                self.mm(ps[:, 0:QS], ba[:], sq[:], r=[ba.b, sq.b], w=[ps.b])
                self.ts("dve", t2[:], ps[:, 0:QS], LNX_EPS, None, ALU.add, r=[ps.b], w=[t2.b])
                self.act(t2[:], t2[:], AF.Ln, r=[t2.b], w=[t2.b])
                self.act(t2[:], t2[:], AF.Exp, scale=-0.5, r=[t2.b], w=[t2.b])
                self.tt("dve", t1[:], t1[:], t2[:], ALU.mult, r=[t1.b, t2.b], w=[t1.b])
                yield
                self.ts("dve", t1[:], t1[:], pp[:, p, PLW:PLW + 1], pp[:, p, PLB:PLB + 1], ALU.mult, ALU.add,
                        r=[t1.b, pp.b], w=[t1.b])
                self.tt("pool", t1[:], t1[:], bonus[par][:, ts_], ALU.add, r=[t1.b, bonus[par].b], w=[t1.b])
                ps = self.nps()
                self.mm(ps[:, 0:QS], gup[:, pc], sgb[:], r=[gup.b, sgb.b], w=[ps.b])
                self.tt("dve", yR[:, p, ts_], t1[:], ps[:, 0:QS], ALU.mult, r=[t1.b, ps.b], w=[yR.b])
                yield

        def run(g):
            for _ in g:
                pass

        interleave = self.interleave

        interleave(pre_stage(0, 0))
        for p in range(4):
            for k in range(NQ_):
                nxt = None
                if k + 1 < NQ_:
                    nxt = pre_stage(p, k + 1)
                elif p + 1 < 4:
                    nxt = pre_stage(p + 1, 0)
                if nxt is not None and NQ_ > 1:
                    interleave([chain_group(p, k)] + nxt)
                else:
                    run(chain_group(p, k))
                    if nxt is not None:
                        interleave(nxt)
            run(post(p))

    def attention(self, st, s, env, hT, yA):
        P, T, QS, NQ, TT = self.P, self.T, self.QS, self.NQ, self.TT
        w_in, idb, onesb = env["w_in"], env["idb"], env["onesb"]
        sb = self.sb
        bias = sb(st, "bias", [128, 6, 512], BF16)
        self.load_w(bias[:], bias.b, env["c_bias"])
        snk = sb(st, "snk", [128, 8], F32)
        esk = sb(st, "esk", [128, 2, 2, 128], F32)
        self.dma(snk.t[:], env["sink_d"].partition_broadcast(128), w=[snk.b])
        self.act(snk[:], snk[:], AF.Exp, r=[snk.b], w=[snk.b])
        for kv in range(2):
            for gh in range(2):
                for gl in range(2):
                    hs = slice(64 * gl, 64 * gl + 64)
                    col = kv * 4 + 2 * gh + gl
                    self.cp("dve", esk[hs, kv, gh, :], snk[hs, col:col + 1].to_broadcast([64, 128]), r=[snk.b], w=[esk.b])
        qT = sb(st, "qT", [128, TT, 4, 128], BF16)
        kTz = [sb(st, f"kTz{i}", [128, T], BF16) for i in range(2)]
        vtk = sb(st, "vtk", [128, TT, 128], BF16)
        wq = sb(st, "wq", [128, 8, 512], BF16)
        wkv_ = sb(st, "wkvw", [128, 8, 256], BF16)
        c0 = 1920
        wq5 = wq.t[:].rearrange("p k (g kv d) -> p k g kv d", g=4, kv=2)
        for i in range(2):
            for kc in range(8):
                self.load_w(wq5[:, kc, :, i, :], wq.b,
                            w_in[:, kc, c0 + i * 256:c0 + (i + 1) * 256].rearrange("p (g d) -> p g d", g=4))
        self.load_w(wkv_[:], wkv_.b, w_in[:, :, c0 + 512:c0 + 768], 8, 256)
        for i in range(2):
            self.ms("pool", kTz[i][:], 0.0, w=[kTz[i].b])
        for tq in range(NQ):
            ts_ = slice(tq * QS, (tq + 1) * QS)
            for g in range(4):
                ps = self.nps()
                for kc in range(8):
                    self.mm(ps[:, 0:QS], wq[:, kc, g * 128:(g + 1) * 128], hT[:, kc, ts_], start=(kc == 0), stop=(kc == 7),
                            r=[wq.b, hT.b], w=[ps.b])
                sg_ = (g % 2) * 2 + g // 2
                nb_ = QS // 128
                self.act(qT[:, tq * nb_:(tq + 1) * nb_, sg_, :], ps[:, 0:QS].rearrange("p (b q) -> p b q", q=128), AF.Copy, scale=0.125,
                         r=[ps.b], w=[qT.b])
            ps = self.nps()
            self.inproj(hT, wkv_, 0, tq, [wkv_.b, hT.b], ps)
            for kv in range(2):
                hs = slice(64 * kv, 64 * kv + 64)
                self.cp("act", kTz[kv][hs, ts_], ps[hs, 0:QS], r=[ps.b], w=[kTz[kv].b])
        for tt_ in range(TT):
            ps = self.nps()
            for kc in range(8):
                self.mm(ps[:, 0:128], hT[:, kc, tt_ * 128:(tt_ + 1) * 128], wkv_[:, kc, 128:256], start=(kc == 0), stop=(kc == 7),
                        r=[wkv_.b, hT.b], w=[ps.b])
            self.cp("act", vtk[:, tt_, :], ps[:, 0:128], r=[ps.b], w=[vtk.b])
        pTs = [[sb(st, f"pT{j}_{i}", [128, 512], BF16) for i in range(3)] for j in range(2)]
        dens = [sb(st, f"den{j}", [128, 256], F32) for j in range(2)]

        def att_iter(kv, qb, par):
            den = dens[par]
            kbs = [kb for kb in (qb - 1, qb, qb + 1) if 0 <= kb < TT]
            psO = self.nps(f"attO{par}")
            psD = self.nps(f"attD{par}")
            for ki, kb in enumerate(kbs):
                rel = kb - qb + 1
                psS = self.nps("attS")
                pT = pTs[par][ki]
                self.mm(psS[:, :], kTz[kv][:, kb * 128:(kb + 1) * 128], qT[:, qb, :, :].rearrange("p g q -> p (g q)"),
                        start=True, stop=False, r=[kTz[kv].b, qT.b], w=[psS.b])
                self.mm(psS[:, :], idb[:], bias[:, rel * 2 + kv, :], start=False, stop=True, r=[idb.b, bias.b], w=[psS.b])
                self.act(pT[:], psS[:, :], AF.Exp, r=[psS.b], w=[pT.b])
                yield
                for gl in range(2):
                    hs = slice(64 * gl, 64 * gl + 64)
                    self.mm(psO[hs, 0:256], vtk[:, kb, 64 * kv:64 * kv + 64], pT[:, gl * 256:(gl + 1) * 256],
                            start=(ki == 0), stop=(ki == len(kbs) - 1), r=[vtk.b, pT.b], w=[psO.b], nohold=True)
                    self.mm(psD[hs, 0:256], onesb[:, 0:64], pT[:, gl * 256:(gl + 1) * 256],
                            start=(ki == 0), stop=(ki == len(kbs) - 1), r=[onesb.b, pT.b], w=[psD.b], nohold=True)
                yield
            self.tt("dve", den[:], psD[:, 0:256], esk[:, kv, :, :].rearrange("p a q -> p (a q)"), ALU.add,
                    r=[psD.b, esk.b], w=[den.b])
            self.act(den[:], den[:], AF.Ln, r=[den.b], w=[den.b])
            self.act(den[:], den[:], AF.Exp, scale=-1.0, r=[den.b], w=[den.b])
            yield
            self.tt("dve", yA[:, 2 * kv:2 * kv + 2, qb * 128:(qb + 1) * 128],
                    psO[:, 0:256].rearrange("p (a q) -> p a q", a=2), den[:].rearrange("p (a q) -> p a q", a=2),
                    ALU.mult, r=[psO.b, den.b], w=[yA.b])
            yield

        its = [(kv, qb) for kv in range(2) for qb in range(TT)]
        self.interleave([att_iter(kv, qb, i % 2) for i, (kv, qb) in enumerate(its)], window=2)

    def merge_gates(self, st, s, env, hT, yA, mrg):
        P, T, QS, NQ, TT = self.P, self.T, self.QS, self.NQ, self.TT
        NS = self.NS
        w_in, idf = env["w_in"], env["idf"]
        x, acc, h2d, B_acc, B_h2d = env["x"], env["acc"], env["h2d"], env["B_acc"], env["B_h2d"]
        wrt, affT = env["wrt"], env["affT"]
        yR = self.yRk
        sb = self.sb
        wpr = sb(st, "wpr", [128, 4, D], BF16)
        wpa = sb(st, "wpa", [128, 4, D], BF16)
        self.load_w(wpr[:], wpr.b, env["wpr_d"])
        self.load_w(wpa[:], wpa.b, env["wpa_d"])
        wgs = [sb(st, f"wg{i}", [128, 8, 1024], BF16) for i in range(2)]
        sg1s = [sb(st, f"sg1_{i}", [128, QS], F32) for i in range(2)]
        sg2s = [sb(st, f"sg2_{i}", [128, QS], F32) for i in range(2)]
        m1s = [sb(st, f"m1_{i}", [128, QS], F32) for i in range(2)]
        m2s = [sb(st, f"m2_{i}", [128, QS], F32) for i in range(2)]
        cg = 1920 + 768

        def gate_iter(oc, tq, par):
            sg1, sg2, m1, m2 = sg1s[par], sg2s[par], m1s[par], m2s[par]
            wg = wgs[oc // 4]
            ol = (oc % 4) * 128
            if oc % 4 == 0 and tq == 0:
                self.load_w(wg[:, :, 0:512], wg.b, w_in[:, :, cg + oc * 128:cg + oc * 128 + 512])
                self.load_w(wg[:, :, 512:1024], wg.b, w_in[:, :, cg + 1024 + oc * 128:cg + 1024 + oc * 128 + 512])
            ts_ = slice(tq * QS, (tq + 1) * QS)
            ps1 = self.nps()
            self.inproj(hT, wg, ol, tq, [wg.b, hT.b], ps1)
            self.act(sg1[:], ps1[:, 0:QS], AF.Sigmoid, r=[ps1.b], w=[sg1.b])
            yield
            ps2 = self.nps()
            self.inproj(hT, wg, 512 + ol, tq, [wg.b, hT.b], ps2)
            self.act(sg2[:], ps2[:, 0:QS], AF.Sigmoid, r=[ps2.b], w=[sg2.b])
            yield
            ps3 = self.nps()
            for kc in range(4):
                self.mm(ps3[:, 0:QS], wpr[:, kc, oc * 128:(oc + 1) * 128], yR[:, kc, ts_], start=(kc == 0), stop=(kc == 3),
                        r=[wpr.b, yR.b], w=[ps3.b])
            self.tt("dve", m1[:], sg1[:], ps3[:, 0:QS], ALU.mult, r=[sg1.b, ps3.b], w=[m1.b])
            yield
            ps4 = self.nps()
            for kc in range(4):
                self.mm(ps4[:, 0:QS], wpa[:, kc, oc * 128:(oc + 1) * 128], yA[:, kc, ts_], start=(kc == 0), stop=(kc == 3),
                        r=[wpa.b, yA.b], w=[ps4.b])
            self.tt("dve", m2[:], sg2[:], ps4[:, 0:QS], ALU.mult, r=[sg2.b, ps4.b], w=[m2.b])
            yield
            self.tt("pool", mrg[:, oc, ts_], m1[:], m2[:], ALU.add, r=[m1.b, m2.b], w=[mrg.b])
            yield

        its = [(oc, tq) for oc in range(8) for tq in range(NQ)]
        self.interleave([gate_iter(oc, tq, i % 2) for i, (oc, tq) in enumerate(its)], window=2)

    def out_proj(self, st, s, env, mrg):
        P, T, QS, NQ, TT = self.P, self.T, self.QS, self.NQ, self.TT
        idf = env["idf"]
        x, acc, h2d, B_acc, B_h2d = env["x"], env["acc"], env["h2d"], env["B_acc"], env["B_h2d"]
        wrt, affT = env["wrt"], env["affT"]
        sb = self.sb
        wout = sb(st, "wout", [128, 8, D], BF16)
        self.load_w(wout[:], wout.b, env["wout_d"])
        g2bc = sb(st, "g2bc", [128, D], F32)
        self.dma(g2bc.t[:], env["g2r_d"].partition_broadcast(128), w=[g2bc.b])
        xts = [sb(st, f"xm{i}", [128, D], F32) for i in range(2)]
        x1s = [sb(st, f"x1{i}", [128, D], F32) for i in range(2)]
        h2s = [sb(st, f"h2{i}", [128, D], F32) for i in range(2)]
        h2bs = [sb(st, f"h2b{i}", [128, D], BF16) for i in range(2)]
        h2T = sb(st, "h2T", [128, 8, 128], F32)
        junk = sb(st, "junk2", [128, D], F32)
        sst = [sb(st, f"sm{i}", [128, 8], F32) for i in range(2)]
        lg = sb(st, "lg", [128, NEXP], F32)
        afft = sb(st, "afft", [128, 128], F32)
        self.ms("pool", afft[:], 0.0, w=[afft.b])
        def tile_iter(tt_):
                xt, x1, h2, h2b, ss = xts[tt_ % 2], x1s[tt_ % 2], h2s[tt_ % 2], h2bs[tt_ % 2], sst[tt_ % 2]
                r0 = s * T + tt_ * 128
                tl = slice(tt_ * 128, (tt_ + 1) * 128)
                self.dma(xt.t[:], x[r0:r0 + 128, :], w=[xt.b])
                for half in range(2):
                    ps = self.nps()
                    for kc in range(8):
                        self.mm(ps[:, :], mrg[:, kc, tl], wout[:, kc, half * 512:(half + 1) * 512], start=(kc == 0), stop=(kc == 7),
                                r=[mrg.b, wout.b], w=[ps.b])
                    self.tt("dve", x1[:, half * 512:(half + 1) * 512], xt[:, half * 512:(half + 1) * 512], ps[:, :], ALU.add,
                            r=[xt.b, ps.b], w=[x1.b])
                yield
                self.dma(acc[r0:r0 + 128, :], x1[:], r=[x1.b], w=[B_acc[s]])
                self.act(junk[:], x1[:], AF.Square, accum=ss[:, 0:1], r=[x1.b], w=[junk.b, ss.b])
                self.ts("dve", ss[:, 1:2], ss[:, 0:1], 1.0 / D, NORM_EPS, ALU.mult, ALU.add, r=[ss.b], w=[ss.b])
                self.act(ss[:, 2:3], ss[:, 1:2], AF.Sqrt, r=[ss.b], w=[ss.b])
                self.P.emit("dve", lambda e, o=ss[:, 3:4], i=ss[:, 2:3]: e.reciprocal(out=o, in_=i), [ss.b], [ss.b])
                self.ts("dve", h2[:], x1[:], ss[:, 3:4], None, ALU.mult, r=[x1.b, ss.b], w=[h2.b])
                self.tt("pool", h2b[:], h2[:], g2bc[:], ALU.mult, r=[h2.b, g2bc.b], w=[h2b.b])
                self.dma(h2d[r0:r0 + 128, :], h2b[:], r=[h2b.b], w=[B_h2d[s]])
                yield
                for k2 in range(2):
                    ps = self.nps()
                    for kk in range(4):
                        kc = k2 * 4 + kk
                        self.tr(ps[:, kk * 128:(kk + 1) * 128], h2[:, kc * 128:(kc + 1) * 128], idf[:], r=[h2.b, idf.b], w=[ps.b])
                    self.cp("act", h2T[:, k2 * 4:(k2 + 1) * 4, :], ps[:, :].rearrange("p (k n) -> p k n", k=4), r=[ps.b], w=[h2T.b])
                yield
                ps = self.nps()
                for kc in range(8):
                    self.mm(ps[:, 0:NEXP], h2T[:, kc, :], wrt[:, kc, :], start=(kc == 0), stop=(kc == 7), r=[h2T.b, wrt.b], w=[ps.b])
                self.P.emit("dve", lambda e, o=ss[:, 4:5], i=ps[:, 0:NEXP]: e.reduce_max(out=o, in_=i, axis=AX.X), [ps.b], [ss.b])
                self.ts("dve", ss[:, 5:6], ss[:, 4:5], -1.0, None, ALU.mult, r=[ss.b], w=[ss.b])
                self.act(lg[:], ps[:, 0:NEXP], AF.Exp, bias=ss[:, 5:6], accum=ss[:, 6:7], r=[ps.b, ss.b], w=[lg.b, ss.b])
                self.P.emit("dve", lambda e, o=ss[:, 7:8], i=ss[:, 6:7]: e.reciprocal(out=o, in_=i), [ss.b], [ss.b])
                self.ts("dve", afft[:, 32 * s:32 * s + NEXP], lg[:], ss[:, 7:8], None, ALU.mult, r=[lg.b, ss.b], w=[afft.b])
                ps = self.nps()
                self.tr(ps[:, 0:128], afft[:], idf[:], r=[afft.b, idf.b], w=[ps.b])
                self.tt("dve", affT[:, tl], affT[:, tl], ps[:, 0:128], ALU.add, r=[ps.b, affT.b], w=[affT.b])


        self.interleave([tile_iter(tt_) for tt_ in range(TT)], window=2)

    def phase_b(self, env):
        P, NS, T, CAP, SB_, NB = self.P, self.NS, self.T, self.CAP, self.SB, self.NB
        affT, idf, idb, g2 = env["affT"], env["idf"], env["idb"], env["g2"]
        acc, h2d, B_acc, B_h2d = env["acc"], env["h2d"], env["B_acc"], env["B_h2d"]
        sb = self.sb
        with ExitStack() as st:
            wk = sb(st, "wk", [128, T], F32)
            mv = sb(st, "mv", [128, CAP], F32)
            mi = sb(st, "mi", [128, CAP], U32)
            mif = sb(st, "mif", [128, CAP], F32)
            offs = sb(st, "offs", [128, 128], F32)
            idxT = sb(st, "idxT", [128, NB, 128], I32)
            valT = sb(st, "valT", [128, NB, 128], F32)
            self.dma(offs.t[:], env["c_offs"], w=[offs.b])
            self.cp("dve", wk[:], affT[:], r=[affT.b], w=[wk.b])
            for r_ in range(CAP // 8):
                sl = slice(r_ * 8, r_ * 8 + 8)
                self.P.emit("dve", lambda e, o=mv[:, sl], i=wk[:]: e.max(out=o, in_=i), [wk.b], [mv.b])
                self.P.emit("dve", lambda e, o=mi[:, sl], m=mv[:, sl], i=wk[:]: e.max_index(out=o, in_max=m, in_values=i),
                            [wk.b, mv.b], [mi.b])
                self.P.emit("dve", lambda e, o=wk[:], m=mv[:, sl], i=wk[:]: e.match_replace(
                    out=o, in_to_replace=m, in_values=i, imm_value=-1.0), [wk.b, mv.b], [wk.b])
            self.cp("dve", mif[:], mi[:], r=[mi.b], w=[mif.b])
            for blk in range(NB):
                bs = slice(blk * SB_, (blk + 1) * SB_)
                ps = self.nps()
                self.tr(ps[0:SB_, 0:128], mif[:, bs], idf[:], r=[mif.b, idf.b], w=[ps.b])
                self.tt("dve", idxT[0:SB_, blk, :], ps[0:SB_, 0:128], offs[0:SB_, :], ALU.add, r=[ps.b, offs.b], w=[idxT.b])
                ps = self.nps()
                self.tr(ps[0:SB_, 0:128], mv[:, bs], idf[:], r=[mv.b, idf.b], w=[ps.b])
                self.cp("act", valT[0:SB_, blk, :], ps[0:SB_, 0:128], r=[ps.b], w=[valT.b])
            wgs = [sb(st, f"ewg{i}", [128, 8, D], BF16) for i in range(2)]
            wus = [sb(st, f"ewu{i}", [128, 8, D], BF16) for i in range(2)]
            wds = [sb(st, f"ewd{i}", [128, 8, D], BF16) for i in range(2)]
            NPF = 2
            xss = [sb(st, f"xs{i}", [128, D], BF16) for i in range((NPF + 1) * NB)]
            xsT = sb(st, "xsT", [128, 8, CAP], BF16)
            hid = sb(st, "hid", [128, 8, CAP], BF16)
            sl_ = sb(st, "silu", [128, CAP], F32)
            yss = [sb(st, f"ys{i}", [128, D], F32) for i in range(2 * NB)]
            its = [(e, s) for e in range(NEXP) for s in range(NS)]
            last_sc = {s: [] for s in range(NS)}

            def load_expert(e):
                for dst, src in ((wgs[e % 2], env["eg_d"]), (wus[e % 2], env["eu_d"]), (wds[e % 2], env["ed_d"])):
                    self.load_w(dst[:], dst.b, src[e].rearrange("(k p) n -> p k n", p=128))

            def gather(i):
                e, s = its[i]
                pcol = 32 * s + e
                for blk in range(NB):
                    xs = xss[(i % (NPF + 1)) * NB + blk]
                    ia = idxT[0:SB_, blk, pcol:pcol + 1]
                    self.P.emit("pool", lambda en, o=xs[0:SB_, :], ia=ia: en.indirect_dma_start(
                        out=o, out_offset=None, in_=h2d, in_offset=bass.IndirectOffsetOnAxis(ap=ia, axis=0)),
                        [idxT.b, B_h2d[s]], [xs.b], dma=True)

            load_expert(0)
            for i in range(min(NPF, len(its))):
                gather(i)
            for i, (e, s) in enumerate(its):
                wg, wu, wd = wgs[e % 2], wus[e % 2], wds[e % 2]
                pcol = 32 * s + e
                if s == 0 and e + 1 < NEXP:
                    load_expert(e + 1)
                if i + NPF < len(its):
                    gather(i + NPF)
                for blk in range(NB):
                    xs = xss[(i % (NPF + 1)) * NB + blk]
                    ps = self.nps()
                    psv = ps.t.bitcast(BF16)
                    for kc in range(8):
                        self.tr(psv[:, kc * 128:kc * 128 + SB_], xs[0:SB_, kc * 128:(kc + 1) * 128], idb[0:SB_, 0:SB_],
                                r=[xs.b, idb.b], w=[ps.b])
                    self.cp("act", xsT[:, :, blk * SB_:(blk + 1) * SB_],
                            psv[:, :].rearrange("p (k n) -> p k n", k=8)[:, :, 0:SB_], r=[ps.b], w=[xsT.b])
                for fc in range(8):
                    psg = self.nps()
                    psu = self.nps()
                    for kc in range(8):
                        self.mm(psg[:, 0:CAP], wg[:, kc, fc * 128:(fc + 1) * 128], xsT[:, kc, :], start=(kc == 0), stop=(kc == 7),
                                r=[wg.b, xsT.b], w=[psg.b])
                    for kc in range(8):
                        self.mm(psu[:, 0:CAP], wu[:, kc, fc * 128:(fc + 1) * 128], xsT[:, kc, :], start=(kc == 0), stop=(kc == 7),
                                r=[wu.b, xsT.b], w=[psu.b])
                    self.act(sl_[:], psg[:, 0:CAP], AF.Silu, r=[psg.b], w=[sl_.b])
                    self.tt("dve", hid[:, fc, :], sl_[:], psu[:, 0:CAP], ALU.mult, r=[sl_.b, psu.b], w=[hid.b])
                new_sc = []
                for blk in range(NB):
                    ys = yss[(i % 2) * NB + blk]
                    for half in range(2):
                        ps = self.nps()
                        for fc in range(8):
                            self.mm(ps[0:SB_, :], hid[:, fc, blk * SB_:(blk + 1) * SB_], wd[:, fc, half * 512:(half + 1) * 512],
                                    start=(fc == 0), stop=(fc == 7), r=[hid.b, wd.b], w=[ps.b])
                        self.act(ys[0:SB_, half * 512:(half + 1) * 512], ps[0:SB_, :], AF.Copy,
                                 scale=valT[0:SB_, blk, pcol:pcol + 1], r=[ps.b, valT.b], w=[ys.b])
                    ia = idxT[0:SB_, blk, pcol:pcol + 1]
                    op = self.P.emit("pool", lambda en, i_=ys[0:SB_, :], ia=ia: en.indirect_dma_start(
                        out=acc, out_offset=bass.IndirectOffsetOnAxis(ap=ia, axis=0), in_=i_, in_offset=None,
                        compute_op=ALU.add), [idxT.b, ys.b, B_acc[s]], [], dma=True)
                    for d_ in last_sc[s]:
                        self.P._dep(op, d_)
                    new_sc.append(op)
                last_sc[s] = new_sc
            P.stage_end()

    def phase_c(self, env):
        P, NS, T, TT = self.P, self.NS, self.T, self.TT
        acc, y, B_acc = env["acc"], env["y"], env["B_acc"]
        sb = self.sb
        outs = []
        with ExitStack() as st:
            gF = sb(st, "gF", [128, D], F32)
            self.dma(gF.t[:], env["gF_d"].partition_broadcast(128), w=[gF.b])
            NW = 4
            ats = [sb(st, f"at{i}", [128, D], F32) for i in range(NW)]
            ots = [sb(st, f"ot{i}", [128, D], F32) for i in range(NW)]
            junks = [sb(st, f"junk3_{i}", [128, D], F32) for i in range(2)]
            sst = [sb(st, f"sc{i}", [128, 4], F32) for i in range(NW)]

            def c_iter(n, s, tt_):
                at, ot, ss, junk = ats[n % NW], ots[n % NW], sst[n % NW], junks[n % 2]
                r0 = s * T + tt_ * 128
                self.dma(at.t[:], acc[r0:r0 + 128, :], r=[B_acc[s]], w=[at.b])
                yield
                self.act(junk[:], at[:], AF.Square, accum=ss[:, 0:1], r=[at.b], w=[junk.b, ss.b])
                self.ts("dve", ss[:, 1:2], ss[:, 0:1], 1.0 / D, NORM_EPS, ALU.mult, ALU.add, r=[ss.b], w=[ss.b])
                self.act(ss[:, 2:3], ss[:, 1:2], AF.Sqrt, r=[ss.b], w=[ss.b])
                self.P.emit("dve", lambda e, o=ss[:, 3:4], i=ss[:, 2:3]: e.reciprocal(out=o, in_=i), [ss.b], [ss.b])
                yield
                self.stt(ot[:], at[:], ss[:, 3:4], gF[:], ALU.mult, ALU.mult, r=[at.b, ss.b, gF.b], w=[ot.b])
                self.P.emit("sp", lambda e, o=y[r0:r0 + 128, :], i=ot[:]: e.dma_start(out=o, in_=i), [ot.b], [], dma=True)
                yield

            fin_b = P.buf("yout")
            gens = []
            n = 0
            for s in range(NS):
                for tt_ in range(TT):
                    gens.append(c_iter(n, s, tt_))
                    n += 1
            self.interleave(gens, window=NW)
            outs = []
            fin = P.buf("fin")
            op = P.emit("sp", lambda en: en.nop(), writes=[fin])
            for d_ in outs:
                P._dep(op, d_)
            P.stage_end()


def host_consts(T):
    c = {}
    c["c_idf"] = np.eye(128, dtype=np.float32)
    blk = (np.arange(128)[:, None] // 64 == np.arange(128)[None, :] // 64).astype(np.float32)
    c["c_bo"] = blk
    c["c_ba"] = blk / 64.0
    s = (np.arange(128) % 64)[:, None]
    j = np.arange(128)[None, :]
    t = j % 64
    isr = j >= 64
    mT = np.zeros((128, 2, 4, 128), np.float32)
    mT[:, 0] = np.where(isr, s <= t, s < t)[:, None, :]
    mT[:, 1] = np.where(isr, s >= t, s > t)[:, None, :]
    c["c_mT"] = mT.reshape(128, 2, 512)
    tt = (np.arange(128) % 64)[:, None]
    ss = np.arange(64)[None, :]
    mN = np.zeros((128, 2, 8, 64), np.float32)
    mN[:, 0] = (ss < tt)[:, None, :]
    mN[:, 1] = (ss > tt)[:, None, :]
    c["c_mN"] = mN.reshape(128, 2, 512)
    idr = np.zeros((128, 8, 64), np.float32)
    idr[:] = (ss == tt)[:, None, :]
    c["c_idr"] = idr.reshape(128, 512)
    slopes = 2.0 ** (-8.0 * np.arange(1, 9) / 8)
    key = np.arange(128)[:, None]
    qq = np.arange(128)[None, :]
    bias = np.zeros((128, 3, 2, 4, 128), np.float32)
    for rel in (-1, 0, 1):
        dist = np.abs(rel * 128 + key - qq)
        for kv in range(2):
            for sg in range(4):
                g = 2 * (sg % 2) + sg // 2
                bias[:, rel + 1, kv, sg, :] = np.where(dist <= 128, -slopes[kv * 4 + g] * dist, -1e30)
    c["c_bias"] = bias.reshape(128, 6, 512)
    offs = np.zeros((128, 128), np.float32)
    offs[:] = ((np.arange(128) // 32) * T)[None, :]
    c["c_offs"] = offs
    return c


def host_params(inp):
    f = lambda a: np.ascontiguousarray(np.asarray(a, dtype=np.float32))
    m = {}
    m["w_in"] = f(inp["w_in"][0])
    m["gmixr"] = f(inp["norm_mix_g"][0].reshape(1, D))
    m["g2r"] = f(inp["norm_ffn_g"][0].reshape(1, D))
    m["mu"] = f(np.stack([inp["mu_prev"][0].reshape(15, 128).T, inp["mu_next"][0].reshape(15, 128).T], axis=-1))
    names = ["w0_f", "w0_b", "a0_f", "a0_b", "k_k", "k_a", "r_k", "ln_x_w", "ln_x_b"]
    m["pp"] = f(np.stack([np.asarray(inp[n][0]).reshape(4, 128).T for n in names], axis=-1))
    m["sink"] = f(inp["attn_sink"][0].reshape(1, 8))
    m["g2"] = f(inp["norm_ffn_g"][0].reshape(8, 128).T)
    m["gF"] = f(np.asarray(inp["norm_final_g"]).reshape(1, D))
    m["wup"] = f(np.concatenate([inp["w_up_f"][0], inp["w_up_b"][0]], axis=0))
    m["aup"] = f(np.concatenate([inp["a_up_f"][0], inp["a_up_b"][0]], axis=0))
    m["gup"] = f(inp["g_up"][0])
    m["wpr"] = f(inp["w_proj_rwkv"][0])
    m["wpa"] = f(inp["w_proj_attn"][0])
    m["wout"] = f(inp["w_out"][0])
    m["wr"] = f(inp["w_router"][0])
    m["eg"] = f(inp["exp_w_gate"][0])
    m["eu"] = f(inp["exp_w_up"][0])
    m["ed"] = f(inp["exp_w_down"][0])
    return m


_NC_CACHE = {}


def run(inp, n_cores=8, stop=None, dbg=False, raw=False):
    x = np.asarray(inp["x"], dtype=np.float32)
    B, T, _ = x.shape
    NS = B // n_cores
    key = (NS, T, stop, dbg)
    if key not in _NC_CACHE:
        _NC_CACHE[key] = Builder(NS, T, stop, dbg).build()
    nc = _NC_CACHE[key]
    shared = host_params(inp)
    shared.update(host_consts(T))
    in_maps = []
    for c in range(n_cores):
        m = dict(shared)
        m["x"] = np.ascontiguousarray(x[c * NS:(c + 1) * NS].reshape(NS * T, D))
        in_maps.append(m)
    res = run_bass_kernel_spmd(nc, in_maps, core_ids=list(range(n_cores)))
    if raw:
        return res.results
    out = np.concatenate([r["y"].reshape(NS, T, D) for r in res.results], axis=0)
    return out.astype(np.float32)


def kernel(**inputs):
    return run(inputs, 8)
```

```python
import math
from contextlib import ExitStack
import numpy as np
import concourse.bass as bass
import concourse.mybir as mybir
from concourse.bass_utils import run_bass_kernel_spmd

F32 = mybir.dt.float32
BF16 = mybir.dt.bfloat16
U32 = mybir.dt.uint32
I32 = mybir.dt.int32
AF = mybir.ActivationFunctionType
ALU = mybir.AluOpType
AX = mybir.AxisListType

ENGS = ("pe", "dve", "act", "pool", "sp")
SEM_WRAP = 4000
NDMA_SEM = 12
NBANK = {'pe': 24, 'dve': 12, 'act': 12, 'pool': 6, 'sp': 2}

D = 1024
DS = math.exp(-0.5)
LNX_EPS = 64e-5
NORM_EPS = 1e-6
NEXP = 16
L = 64


class Buf:
    __slots__ = ("name", "w", "r")

    def __init__(self, name):
        self.name = name
        self.w = None
        self.r = []


class Op:
    __slots__ = ("eng", "fn", "waits", "signal", "idx", "dma", "tk", "sigval", "gid", "ninc")

    def __init__(self, eng, fn, dma):
        self.eng = eng
        self.fn = fn
        self.waits = []
        self.signal = False
        self.dma = dma
        self.tk = None
        self.sigval = None


class Prog:
    def __init__(self, nc, stack):
        self.nc = nc
        self.bufs = []
        self.sigcount = {e: 0 for e in ENGS}
        self.ndma = {e: 0 for e in ENGS}
        self.sems = {e: [stack.enter_context(nc.semaphore(f"s_{e}_{i}")) for i in range(NBANK[e])] for e in ENGS}
        self.dsems = {e: [stack.enter_context(nc.semaphore(f"d_{e}_{i}")) for i in range(NDMA_SEM)]
                      for e in ("sp", "pool", "act")}
        self.gid = 0
        self._reset()

    def _reset(self):
        self.q = {e: [] for e in ENGS}
        self.seen = {e: {} for e in ENGS}
        self.seen_dma = {e: set() for e in ENGS}
        self.dma_hist = {e: {} for e in ENGS}
        self.pending_dma = []
        for b in self.bufs:
            b.w = None
            b.r = []

    def buf(self, name="b"):
        b = Buf(name)
        self.bufs.append(b)
        return b

    def _dep(self, op, d):
        eng = op.eng
        if d is op:
            return
        if d.dma:
            if d.gid in self.seen_dma[eng]:
                return
            self.seen_dma[eng].add(d.gid)
            op.waits.append(d)
        else:
            if d.eng == eng and eng == "pe":
                return
            if self.seen[eng].get(d.eng, -1) >= d.idx:
                return
            self.seen[eng][d.eng] = d.idx
            d.signal = True
            op.waits.append(d)

    capture = None

    def emit(self, eng, fn, reads=(), writes=(), dma=False, ninc=1, est=None, hold=False):
        if self.capture is not None:
            self.capture.append((eng, fn, tuple(reads), tuple(writes), dma, est, hold))
            return None
        op = Op(eng, fn, dma)
        op.ninc = ninc
        op.gid = self.gid
        self.gid += 1
        op.idx = len(self.q[eng])
        deps = []
        for b in reads:
            if b.w is not None:
                deps.append(b.w)
        for b in writes:
            if b.w is not None:
                deps.append(b.w)
            deps.extend(b.r)
        for d in deps:
            self._dep(op, d)
        if dma:
            j = self.ndma[eng]
            self.ndma[eng] += 1
            op.tk = (j % NDMA_SEM, 16 * (j // NDMA_SEM + 1))
            hist = self.dma_hist[eng]
            if (j - NDMA_SEM) in hist:
                self._dep(op, hist[j - NDMA_SEM])
            hist[j] = op
            self.pending_dma.append(op)
        for b in reads:
            if not dma:
                b.r = [x for x in b.r if x.dma or x.eng != eng]
            b.r.append(op)
        for b in writes:
            b.w = op
            b.r = []
        self.q[eng].append(op)
        return op

    def barrier(self):
        pend = self.pending_dma
        self.pending_dma = []
        last = []
        for e in ENGS:
            for op in reversed(self.q[e]):
                if not op.dma and not getattr(op.fn, "_is_nop", False):
                    last.append(op)
                    break
        for e in ENGS:
            fn = lambda en: en.nop()
            op = self.emit(e, fn)
            for d in last:
                self._dep(op, d)
            for d in pend:
                self._dep(op, d)

    def stage_end(self):
        self.barrier()

    def flush(self):
        self.barrier()
        nc = self.nc
        base = dict(self.sigcount)
        for e in ENGS:
            c = base[e]
            for op in self.q[e]:
                if op.signal:
                    assert (c % SEM_WRAP) + op.ninc <= SEM_WRAP
                    c += op.ninc
                    op.sigval = c
            self.sigcount[e] = c
        sems, dsems = self.sems, self.dsems

        def resolve(d):
            if d.dma:
                return dsems[d.eng][d.tk[0]], d.tk[1]
            v = d.sigval - 1
            assert v // SEM_WRAP < NBANK[d.eng], (d.eng, v)
            return sems[d.eng][v // SEM_WRAP], v % SEM_WRAP + 1

        def run(e, engobj):
            for op in self.q[e]:
                for d in op.waits:
                    s, v = resolve(d)
                    engobj.wait_ge(s, v)
                ins = op.fn(engobj)
                if op.dma:
                    ins.then_inc(dsems[e][op.tk[0]], 16)
                elif op.signal:
                    v = op.sigval - 1
                    assert v // SEM_WRAP < NBANK[e], (e, v)
                    ins.then_inc(sems[e][v // SEM_WRAP], 1)

        with nc.Block() as block:
            @block.tensor
            def _(t):
                run("pe", t)

            @block.vector
            def _(v):
                run("dve", v)

            @block.scalar
            def _(s):
                run("act", s)

            @block.gpsimd
            def _(g):
                run("pool", g)

            @block.sync
            def _(sp):
                run("sp", sp)
        self._reset()


class Tl:
    def __init__(self, t, b):
        self.t = t
        self.b = b

    def __getitem__(self, k):
        return self.t[k]


class _Stop(Exception):
    pass


class Builder:
    def __init__(self, NS, T, stop=None, dbg=False):
        self.stop, self.dbg = stop, dbg
        self.NS, self.T = NS, T
        self.QS = min(512, T)
        self.NQ = T // self.QS
        self.TT = T // 128
        self.NCH = T // L
        self.CQ = self.QS // L
        self.CAP = 2 * T // NEXP
        self.SB = min(128, self.CAP)
        self.NB = self.CAP // self.SB
        self.nc = bass.Bass("TRN2", target_bir_lowering=False)
        self.stack = ExitStack()
        self.P = Prog(self.nc, self.stack)
        self.psi = 0

    def sb(self, st, name, shape, dt):
        self.nsb = getattr(self, "nsb", 0) + 1
        name = f"{name}__{self.nsb}"
        t = st.enter_context(self.nc.sbuf_tensor(name, list(shape), dt))
        return Tl(t, self.P.buf(name))

    def chk(self, k):
        if self.stop == k:
            self.P.flush()
            raise _Stop()

    def dram_in(self, name, shape, dt=F32):
        return self.nc.dram_tensor(name, list(shape), dt, kind="ExternalInput").ap()

    def nps(self, grp=None):
        if grp is None:
            p = self.ps[self.psi % 8]
            self.psi += 1
            return p
        banks = {"chain": (0, 1, 2), "post": (3,), "A": (4, 5), "B": (6, 7), "attO0": (0,), "attD0": (1,), "attO1": (2,), "attD1": (3,),
                 "attS0": (4, 5), "attS1": (6, 7), "h0": (0, 1, 2, 3), "h1": (4, 5, 6, 7)}[grp]
        self.psg = getattr(self, "psg", {})
        i = self.psg.get(grp, 0)
        self.psg[grp] = i + 1
        return self.ps[banks[i % len(banks)]]

    def mm(self, out, lhsT, rhs, start=True, stop=True, r=(), w=(), nohold=False):
        self.P.emit("pe", lambda e: e.matmul(out, lhsT=lhsT, rhs=rhs, start=start, stop=stop), r, w,
                    est=max(64, rhs.free_size()) / 2400.0 + 0.004, hold=(not stop) and not nohold)

    def tr(self, out, in_, ident, r=(), w=()):
        self.P.emit("pe", lambda e: e.transpose(out=out, in_=in_, identity=ident), r, w, est=0.06)

    def act(self, out, in_, func, bias=None, scale=None, accum=None, r=(), w=()):
        kw = {}
        if bias is not None:
            kw["bias"] = bias
        if scale is not None:
            kw["scale"] = scale
        if accum is not None:
            kw["accum_out"] = accum
        self.P.emit("act", lambda e: e.activation(out=out, in_=in_, func=func, **kw), r, w, ninc=1,
                    est=(224 + out.free_size()) / 1200.0)

    def tt(self, eng, out, in0, in1, op, r=(), w=()):
        self.P.emit(eng, lambda e: e.tensor_tensor(out=out, in0=in0, in1=in1, op=op), r, w, est=self._est(eng, out))

    def ts(self, eng, out, in0, s1, s2, op0, op1=None, r=(), w=()):
        if op1 is None:
            self.P.emit(eng, lambda e: e.tensor_scalar(out=out, in0=in0, scalar1=s1, scalar2=None, op0=op0), r, w,
                        est=self._est(eng, out))
        else:
            self.P.emit(eng, lambda e: e.tensor_scalar(out=out, in0=in0, scalar1=s1, scalar2=s2, op0=op0, op1=op1), r, w,
                        est=self._est(eng, out))

    def stt(self, out, in0, scalar, in1, op0, op1, r=(), w=()):
        self.P.emit("dve", lambda e: e.scalar_tensor_tensor(out=out, in0=in0, scalar=scalar, in1=in1, op0=op0, op1=op1), r, w,
                    est=self._est("dve", out))

    def cp(self, eng, out, in_, r=(), w=()):
        if eng == "act":
            self.P.emit("act", lambda e: e.activation(out=out, in_=in_, func=AF.Copy), r, w, est=(224 + out.free_size()) / 1200.0)
        else:
            self.P.emit(eng, lambda e: e.tensor_copy(out=out, in_=in_), r, w, est=self._est(eng, out))

    def ms(self, eng, ap, val, w=()):
        self.P.emit(eng, lambda e: e.memset(ap, val), (), w, est=self._est(eng, ap))

    def _est(self, eng, out):
        n = out.free_size()
        if eng == "pool":
            return (150 + 1.6 * n) / 1200.0
        return (100 + n) / 960.0

    def interleave(self, gens, window=None):
        P = self.P
        allg = [g for g in gens if g is not None]
        if window is None:
            window = len(allg)
        pending = allg[window:]

        def mk(f, slot):
            return {"g": (f(slot) if callable(f) else f), "q": [], "done": False}
        streams = [mk(g, i_) for i_, g in enumerate(allg[:window])]
        eng_free = getattr(self, "_sim_eng", None)
        if eng_free is None:
            eng_free = self._sim_eng = {e: 0.0 for e in ENGS}
            self._sim_ready = {}
            self._sim_lastrd = {}
        ready, lastrd = self._sim_ready, self._sim_lastrd

        def fill(st_):
            while not st_["q"] and not st_["done"]:
                P.capture = st_["q"]
                try:
                    next(st_["g"])
                except StopIteration:
                    st_["done"] = True
                finally:
                    P.capture = None

        def start_time(o):
            eng, fn, rd, wr, dma, est, hold = o
            t = eng_free[eng]
            for b in rd:
                t = max(t, ready.get(b, (0.0, None))[0] + (0.15 if ready.get(b, (0.0, eng))[1] != eng else 0.05))
            for b in wr:
                t = max(t, ready.get(b, (0.0, None))[0] + 0.05, lastrd.get(b, 0.0) + 0.1)
            return t

        forced = None
        while True:
            for st_ in streams:
                fill(st_)
            for i_, st_ in enumerate(streams):
                if st_["done"] and not st_["q"] and pending:
                    streams[i_] = mk(pending.pop(0), i_)
                    fill(streams[i_])
            live = [st_ for st_ in streams if st_["q"]]
            if not live:
                break
            if forced is not None and forced["q"]:
                best = forced
            else:
                best = min(live, key=lambda st_: start_time(st_["q"][0]))
            o = best["q"].pop(0)
            eng, fn, rd, wr, dma, est, hold = o
            forced = best if hold else None
            t0 = start_time(o)
            dur = est if est is not None else 0.3
            if dma:
                eng_free[eng] = t0 + 0.06
                tend = t0 + 2.5
            else:
                eng_free[eng] = t0 + dur
                tend = t0 + dur
            for b in rd:
                lastrd[b] = max(lastrd.get(b, 0.0), tend)
            for b in wr:
                ready[b] = (tend, eng)
                lastrd[b] = 0.0
            P.emit(eng, fn, rd, wr, dma=dma)

    def dma(self, out, in_, r=(), w=(), eng="sp"):
        return self.P.emit(eng, lambda e: e.dma_start(out=out, in_=in_), r, w, dma=True)

    def load_w(self, dst, dst_b, src, kc=None, n=None, scale=None):
        assert scale is None
        self.dma(dst, src, w=[dst_b], eng="pool")

    def build(self):
        nc, P, NS, T = self.nc, self.P, self.NS, self.T
        QS, NQ, TT, NCH, CQ = self.QS, self.NQ, self.TT, self.NCH, self.CQ
        st0 = self.stack
        din = self.dram_in
        x = din("x", [NS * T, D])
        w_in = din("w_in", [D, 4736]).rearrange("(k p) n -> p k n", p=128)
        gmixr_d = din("gmixr", [1, D])
        g2r_d = din("g2r", [1, D])
        mu_d = din("mu", [128, 15, 2])
        pp_d = din("pp", [128, 4, 9])
        sink_d = din("sink", [1, 8])
        g2_d = din("g2", [128, 8])
        gF_d = din("gF", [1, D])
        wup_d = din("wup", [128, 512])
        aup_d = din("aup", [128, 512])
        gup_d = din("gup", [128, 512])
        wpr_d = din("wpr", [512, D]).rearrange("(k p) n -> p k n", p=128)
        wpa_d = din("wpa", [512, D]).rearrange("(k p) n -> p k n", p=128)
        wout_d = din("wout", [D, D]).rearrange("(k p) n -> p k n", p=128)
        wr_d = din("wr", [D, NEXP]).rearrange("(k p) n -> p k n", p=128)
        eg_d = din("eg", [NEXP, D, D])
        eu_d = din("eu", [NEXP, D, D])
        ed_d = din("ed", [NEXP, D, D])
        c_idf = din("c_idf", [128, 128])
        c_bo = din("c_bo", [128, 128])
        c_ba = din("c_ba", [128, 128])
        c_mT = din("c_mT", [128, 2, 512])
        c_mN = din("c_mN", [128, 2, 512])
        c_idr = din("c_idr", [128, 512])
        c_bias = din("c_bias", [128, 6, 512])
        c_offs = din("c_offs", [128, 128])
        y = nc.dram_tensor("y", [NS * T, D], F32, kind="ExternalOutput").ap()
        ik = "ExternalOutput" if self.dbg else "Internal"
        uS = nc.dram_tensor("uS", [15, 128, T], BF16, kind=ik).ap()
        acc = nc.dram_tensor("acc", [NS * T, D], F32, kind=ik).ap()
        h2d = nc.dram_tensor("h2d", [NS * T, D], BF16, kind="Internal").ap()
        hTd = nc.dram_tensor("hTd", [128, 8, T], BF16, kind="Internal").ap()
        B_hTd = P.buf("hTd")
        if self.dbg:
            self.dbgR = nc.dram_tensor("dbgR", [128, 4, T], BF16, kind="ExternalOutput").ap()
            self.dbgA = nc.dram_tensor("dbgA", [128, 4, T], BF16, kind="ExternalOutput").ap()
        B_uS = [P.buf(f"uS{c}") for c in range(15)]
        B_acc = [P.buf(f"acc{s}") for s in range(NS)]
        B_h2d = [P.buf(f"h2d{s}") for s in range(NS)]

        self.ps = []
        for i in range(8):
            t = st0.enter_context(nc.psum_tensor(f"ps{i}", [128, 512], F32))
            self.ps.append(Tl(t, P.buf(f"ps{i}")))

        sb = self.sb
        idf = sb(st0, "idf", [128, 128], F32)
        idb = sb(st0, "idb", [128, 128], BF16)
        bo = sb(st0, "bo", [128, 128], F32)
        ba = sb(st0, "ba", [128, 128], F32)
        onesb = sb(st0, "onesb", [128, 128], BF16)
        bob = sb(st0, "bob", [128, 128], BF16)
        bab = sb(st0, "bab", [128, 128], BF16)
        mu = sb(st0, "mu", [128, 15, 2], F32)
        mu0 = sb(st0, "mu0", [128, 15], F32)
        pp = sb(st0, "pp", [128, 4, 9], F32)
        omka = sb(st0, "omka", [128, 4], F32)
        g2 = sb(st0, "g2", [128, 8], F32)
        wrt = sb(st0, "wrt", [128, 8, NEXP], F32)
        affT = sb(st0, "affT", [128, T], F32)
        self.wst = [sb(st0, f"wst{i}", [128, 8 * NEXP], F32) for i in range(1)]
        self.wsi = 0
        PW0F, PW0B, PA0F, PA0B, PKK, PKA, PRK, PLW, PLB = range(9)

        for (dst, src) in ((idf, c_idf), (bo, c_bo), (ba, c_ba), (mu, mu_d), (pp, pp_d), (g2, g2_d)):
            self.dma(dst.t[:], src, w=[dst.b])
        self.cp("pool", idb[:], idf[:], r=[idf.b], w=[idb.b])
        self.ms("pool", onesb[:], 1.0, w=[onesb.b])
        self.cp("pool", bob[:], bo[:], r=[bo.b], w=[bob.b])
        self.cp("pool", bab[:], ba[:], r=[ba.b], w=[bab.b])
        self.tt("dve", mu0[:], mu[:, :, 0], mu[:, :, 1], ALU.add, r=[mu.b], w=[mu0.b])
        self.ts("dve", mu0[:], mu0[:], -1.0, 1.0, ALU.mult, ALU.add, r=[mu0.b], w=[mu0.b])
        self.ts("dve", omka[:], pp[:, :, PKA], -1.0, 1.0, ALU.mult, ALU.add, r=[pp.b], w=[omka.b])
        self.dma(self.wst[0].t[:, 0:8 * NEXP].rearrange("p (k n) -> p k n", k=8), wr_d, w=[self.wst[0].b])
        self.tt("dve", wrt[:], self.wst[0].t[:, 0:8 * NEXP].rearrange("p (k n) -> p k n", k=8),
                g2.t[:].unsqueeze(2).to_broadcast([128, 8, NEXP]),
                ALU.mult, r=[self.wst[0].b, g2.b], w=[wrt.b])
        self.ms("pool", affT[:], 0.0, w=[affT.b])
        self.yRk = sb(st0, "yRk", [128, 4, T], BF16)
        P.stage_end()

        env = locals()
        try:
            self.chk(0)
            for s in range(NS):
                self.phase_a(s, env)
            self.phase_b(env)
            self.chk(6)
            self.phase_c(env)
        except _Stop:
            return nc
        self.P.flush()
        print("signals", self.P.sigcount, "dmas", self.P.ndma)
        self.stack.close()
        return nc

    def norm_hT(self, st, s, env, hT):
        T, TT = self.T, self.TT
        x = env["x"]
        idb = env["idb"]
        gbc = self.sb(st, "gbc", [128, D], F32)
        self.dma(gbc.t[:], env["gmixr_d"].partition_broadcast(128), w=[gbc.b])
        xts = [self.sb(st, f"xt{i}", [128, D], F32) for i in range(2)]
        hns = [self.sb(st, f"hn{i}", [128, D], BF16) for i in range(2)]
        junk = self.sb(st, "junk", [128, D], F32)
        sst = [self.sb(st, f"ss{i}", [128, 4], F32) for i in range(2)]
        for tt_ in range(TT):
            xt, hn, ss = xts[tt_ % 2], hns[tt_ % 2], sst[tt_ % 2]
            r0 = s * T + tt_ * 128
            self.dma(xt.t[:], x[r0:r0 + 128, :], w=[xt.b])
            self.chk(20)
            self.act(junk[:], xt[:], AF.Square, accum=ss[:, 0:1], r=[xt.b], w=[junk.b, ss.b])
            self.chk(21)
            self.ts("dve", ss[:, 1:2], ss[:, 0:1], 1.0 / D, NORM_EPS, ALU.mult, ALU.add, r=[ss.b], w=[ss.b])
            self.act(ss[:, 2:3], ss[:, 1:2], AF.Sqrt, r=[ss.b], w=[ss.b])
            self.chk(22)
            self.P.emit("dve", lambda e, o=ss[:, 3:4], i=ss[:, 2:3]: e.reciprocal(out=o, in_=i), [ss.b], [ss.b])
            self.stt(hn[:], xt[:], ss[:, 3:4], gbc[:], ALU.mult, ALU.mult, r=[xt.b, ss.b, gbc.b], w=[hn.b])
            self.chk(23)
            ps = self.nps()
            psv = ps.t.bitcast(BF16)
            for kc in range(8):
                self.tr(psv[:, kc * 128:(kc + 1) * 128], hn[:, kc * 128:(kc + 1) * 128], idb[:], r=[hn.b, idb.b], w=[ps.b])
            self.chk(24)
            import os
            if os.environ.get("VARX") == "1":
                self.cp("dve", junk.t[:].bitcast(BF16)[:, 0:1024], psv[:, :], r=[ps.b], w=[junk.b])
            elif os.environ.get("VARX") == "2":
                self.cp("dve", hT[:, 0, 0:128], psv[:, 0:128], r=[ps.b], w=[hT.b])
            else:
                self.cp("dve", hT[:, :, tt_ * 128:(tt_ + 1) * 128], psv[:, :].rearrange("p (k n) -> p k n", k=8), r=[ps.b], w=[hT.b])
            self.chk(25)

    def inproj(self, hT, wb, col0, tq, reads, ps):
        QS = self.QS
        for kc in range(8):
            self.mm(ps[:, 0:QS], wb[:, kc, col0:col0 + 128], hT[:, kc, tq * QS:(tq + 1) * QS],
                    start=(kc == 0), stop=(kc == 7), r=reads, w=[ps.b])

    def phase_a(self, s, env):
        P, T, QS, NQ, TT, NCH, CQ = self.P, self.T, self.QS, self.NQ, self.TT, self.NCH, self.CQ
        w_in, mu, mu0, pp, omka = env["w_in"], env["mu"], env["mu0"], env["pp"], env["omka"]
        uS, B_uS = env["uS"], env["B_uS"]
        PW0F, PW0B, PA0F, PA0B, PKK, PKA, PRK, PLW, PLB = range(9)
        sb = self.sb

        with ExitStack() as st:
            hT = sb(st, "hT", [128, 8, T], BF16)
            self.norm_hT(st, s, env, hT)
            self.dma(env["hTd"], hT[:], r=[hT.b], w=[env["B_hTd"]])
            self.chk(10)
            wbs = [sb(st, f"wb{i}", [128, 8, 512], BF16) for i in range(2)]
            upad = sb(st, "upad", [128, T + 2], BF16)
            tmp = sb(st, "tmpA", [128, T], BF16)
            uss = [sb(st, f"us{i}", [128, T], BF16) for i in range(2)]
            self.ms("pool", upad[:, 0:1], 0.0, w=[upad.b])
            self.ms("pool", upad[:, T + 1:T + 2], 0.0, w=[upad.b])
            for grp in range(4):
                c0 = grp * 4
                ncol = min(4, 15 - c0)
                wb = wbs[grp % 2]
                self.load_w(wb[:, :, 0:ncol * 128], wb.b, w_in[:, :, c0 * 128:(c0 + ncol) * 128], 8, ncol * 128)
                self.chk(11)
                for ci in range(ncol):
                    c = c0 + ci
                    us = uss[c % 2]
                    for tq in range(NQ):
                        ps = self.nps()
                        self.inproj(hT, wb, ci * 128, tq, [wb.b, hT.b], ps)
                        self.cp("act", upad[:, 1 + tq * QS:1 + (tq + 1) * QS], ps[:, 0:QS], r=[ps.b], w=[upad.b])
                    self.ts("dve", tmp[:], upad[:, 1:T + 1], mu0[:, c:c + 1], None, ALU.mult, r=[upad.b, mu0.b], w=[tmp.b])
                    self.stt(tmp[:], upad[:, 0:T], mu[:, c, 0:1], tmp[:], ALU.mult, ALU.add, r=[upad.b, mu.b, tmp.b], w=[tmp.b])
                    self.stt(us[:], upad[:, 2:T + 2], mu[:, c, 1:2], tmp[:], ALU.mult, ALU.add, r=[upad.b, mu.b, tmp.b], w=[us.b])
                    if c == 12:
                        self.act(us[:], us[:], AF.Sigmoid, r=[us.b], w=[us.b])
                    elif c == 13:
                        self.act(us[:], us[:], AF.Tanh, r=[us.b], w=[us.b])
                    self.dma(uS[c], us[:], r=[us.b], w=[B_uS[c]])
                    self.chk(12)
            P.stage_end()
        self.chk(1)

        with ExitStack() as st:
            self.rwkv(st, s, env, self.yRk)
            if self.dbg and s == 0:
                self.dma(self.dbgR, self.yRk[:], r=[self.yRk.b])
            P.stage_end()
        self.chk(2)

        with ExitStack() as st:
            yA = sb(st, "yA", [128, 4, T], BF16)
            mrg = sb(st, "mrg", [128, 8, T], BF16)
            with ExitStack() as st1:
                hT = sb(st1, "hT", [128, 8, T], BF16)
                self.dma(hT.t[:], env["hTd"], r=[env["B_hTd"]], w=[hT.b])
                with ExitStack() as st2:
                    self.attention(st2, s, env, hT, yA)
                    if self.dbg and s == 0:
                        self.dma(self.dbgA, yA[:], r=[yA.b])
                    P.stage_end()
                self.chk(3)
                self.merge_gates(st1, s, env, hT, yA, mrg)
                P.stage_end()
                self.chk(4)
            with ExitStack() as st1:
                self.out_proj(st1, s, env, mrg)
                P.stage_end()
            self.chk(5)

    def rwkv(self, st, s, env, yR):
        P, T, NCH = self.P, self.T, self.NCH
        QS = min(512, T)
        NQ = T // QS
        CQ = QS // L
        pp, omka = env["pp"], env["omka"]
        uS, B_uS = env["uS"], env["B_uS"]
        bo, ba, idb, bob, bab = env["bo"], env["ba"], env["idb"], env["bob"], env["bab"]
        PW0F, PW0B, PA0F, PA0B, PKK, PKA, PRK, PLW, PLB = range(9)
        sb = self.sb
        mT = sb(st, "mT", [128, 2, 512], F32)
        mN = sb(st, "mN", [128, 2, 512], F32)
        idr = sb(st, "idr", [128, 512], F32)
        self.dma(mT.t[:], env["c_mT"], w=[mT.b])
        self.dma(mN.t[:], env["c_mN"], w=[mN.b])
        self.dma(idr.t[:], env["c_idr"], w=[idr.b])
        wup = sb(st, "wup", [128, 512], BF16)
        aup = sb(st, "aup", [128, 512], BF16)
        gup = sb(st, "gup", [128, 512], BF16)
        for dst, src in ((wup, env["wup_d"]), (aup, env["aup_d"]), (gup, env["gup_d"])):
            self.load_w(dst[:], dst.b, src)
        NQ_ = NQ
        bonus = [sb(st, f"bonus{i}", [128, T], BF16) for i in range(2)]
        wkv = [sb(st, f"wkv{i}", [128, T], BF16) for i in range(2)]
        vtok = [sb(st, f"vtok{i}", [128, NCH, L], BF16) for i in range(2)]
        for t_ in vtok:
            t_.bq = [P.buf("vtokq") for _ in range(NQ_)]
        R = []
        for d in range(2):
            Rd = dict(
                ar=sb(st, f"ar{d}", [128, NCH, 2, L], BF16),
                SB=sb(st, f"SB{d}", [128, NCH, L], BF16),
                SK=sb(st, f"SK{d}", [128, NCH, 2, L], BF16),
                TTm=sb(st, f"TT{d}", [128, NCH, L], BF16),
                BB=sb(st, f"BB{d}", [128, NCH, L], BF16),
                KB=sb(st, f"KB{d}", [128, NCH, L], BF16),
                Wtot=sb(st, f"Wtot{d}", [128, NCH], F32),
                ST=sb(st, f"ST{d}", [128, L], BF16),
                Xs=sb(st, f"Xs{d}", [128, L], BF16),
                Us=sb(st, f"Us{d}", [128, L], BF16),
            )
            for k_ in ("ar", "SB", "SK", "TTm", "BB", "KB", "Wtot"):
                Rd[k_].bq = [P.buf(k_ + "q") for _ in range(NQ_)]
            R.append(Rd)

        def q(name, dt=F32):
            return sb(st, name, [128, QS], dt)
        def mk_temps(sfx):
            names_f = ["t2", "sgw", "cs"] + (["X1"] if sfx == "A" else ["X2", "X3"])
            names_b = ["rq", "kq", "vb", "twb", "alb", "kkn", "sq", "t1", "t2b", "t3", "kft", "aqt", "E", "akk", "p1", "p2",
                       "bT", "kT", "Pm", "PTm", "Pm2", "PTm2"]
            d_ = {n: q(n + sfx) for n in names_f}
            d_.update({n: q(n + sfx, BF16) for n in names_b})
            for n in ("X1", "X2", "X3"):
                d_.setdefault(n, None)
            return d_
        TA, TB = mk_temps("A"), mk_temps("B")
        TA["grp"], TB["grp"] = "A", "B"
        sgb, t1, t2, sq = q("sgbP", BF16), q("t1P"), q("t2P"), q("sqP")
        scm = sb(st, "scm", [128, QS], F32)
        self.ms("pool", scm[:], 1.0, w=[scm.b])
        self.ms("pool", scm.t[:].rearrange("p (c l) -> p c l", l=L)[:, :, 0:1], 0.0, w=[scm.b])

        def v3(ap):
            return ap.rearrange("p (c l) -> p c l", l=L)

        def pre_pass(p, d, tq, first, tmp):
            (rq, kq, vb, twb, alb, kkn, sq, t1, t2, t2b, t3, kft, aqt, sgw, cs, X1, X2, X3, E, akk, p1, p2,
             bT, kT, Pm, PTm, Pm2, PTm2) = (tmp[n] for n in (
                "rq", "kq", "vb", "twb", "alb", "kkn", "sq", "t1", "t2", "t2b", "t3", "kft", "aqt", "sgw", "cs", "X1", "X2", "X3",
                "E", "akk", "p1", "p2", "bT", "kT", "Pm", "PTm", "Pm2", "PTm2"))
            par = p % 2
            pc = slice(p * 128, (p + 1) * 128)
            ts_ = slice(tq * QS, (tq + 1) * QS)
            cq0 = tq * CQ
            csl = slice(cq0, cq0 + CQ)
            Rd = R[d]
            hs_lo = slice(64 * d, 64 * d + 64)
            for dst, c in ((rq, p), (kq, 4 + p), (vb, 8 + p), (twb, 13), (alb, 14)):
                self.dma(dst.t[:], uS[c][:, ts_], r=[B_uS[c]], w=[dst.b])
            yield
            self.ts("dve", t1[:], kq[:], pp[:, p, PKK:PKK + 1], None, ALU.mult, r=[kq.b, pp.b], w=[t1.b])
            self.tt("pool", sq[:], t1[:], t1[:], ALU.mult, r=[t1.b], w=[sq.b])
            ps = self.nps(tmp["grp"])
            self.mm(ps[:, 0:QS], bob[:], sq[:], r=[bob.b, sq.b], w=[ps.b])
            self.ts("dve", t2[:], ps[:, 0:QS], 1e-24, None, ALU.max, r=[ps.b], w=[t2.b])
            self.act(t2[:], t2[:], AF.Ln, r=[t2.b], w=[t2.b])
            self.act(t2b[:], t2[:], AF.Exp, scale=-0.5, r=[t2.b], w=[t2b.b])
            self.tt("dve", kkn[:], t1[:], t2b[:], ALU.mult, r=[t1.b, t2b.b], w=[kkn.b])
            yield
            ps = self.nps(tmp["grp"])
            self.mm(ps[:, 0:QS], aup[hs_lo, pc], alb[hs_lo, :], r=[aup.b, alb.b], w=[ps.b])
            self.act(aqt[:], ps[:, 0:QS], AF.Sigmoid, bias=pp[:, p, PA0F + d:PA0F + d + 1], r=[ps.b, pp.b], w=[aqt.b])
            self.ts("dve", t3[:], aqt[:], pp[:, p, PKA:PKA + 1], omka[:, p:p + 1], ALU.mult, ALU.add,
                    r=[aqt.b, pp.b, omka.b], w=[t3.b])
            self.tt("dve", kft[:], kq[:], t3[:], ALU.mult, r=[kq.b, t3.b], w=[kft.b])
            yield
            self.stt(t3[:], kft[:], pp[:, p, PRK:PRK + 1], rq[:], ALU.mult, ALU.mult, r=[kft.b, pp.b, rq.b], w=[t3.b])
            ps = self.nps(tmp["grp"])
            self.mm(ps[:, 0:QS], bob[:], t3[:], r=[bob.b, t3.b], w=[ps.b])
            if first:
                self.tt("dve", bonus[par][:, ts_], ps[:, 0:QS], vb[:], ALU.mult, r=[ps.b, vb.b], w=[bonus[par].b])
            else:
                self.tt("dve", t3[:], ps[:, 0:QS], vb[:], ALU.mult, r=[ps.b, vb.b], w=[t3.b])
                self.tt("pool", bonus[par][:, ts_], bonus[par][:, ts_], t3[:], ALU.add, r=[t3.b, bonus[par].b], w=[bonus[par].b])
            yield
            if first:
                ps = self.nps(tmp["grp"])
                psv = ps.t.bitcast(BF16)
                for c in range(CQ):
                    for h in range(2):
                        hs = slice(64 * h, 64 * h + 64)
                        self.tr(psv[hs, c * L:(c + 1) * L], vb[hs, c * L:(c + 1) * L], idb[hs, hs], r=[vb.b, idb.b], w=[ps.b])
                self.cp("act", vtok[par][:, csl, :], v3(psv[:, 0:QS]), r=[ps.b], w=[vtok[par].bq[tq]])
                yield
            ps = self.nps(tmp["grp"])
            self.mm(ps[:, 0:QS], wup[hs_lo, pc], twb[hs_lo, :], r=[wup.b, twb.b], w=[ps.b])
            self.act(sgw[:], ps[:, 0:QS], AF.Sigmoid, bias=pp[:, p, PW0F + d:PW0F + d + 1], r=[ps.b, pp.b], w=[sgw.b])
            self.P.emit("dve", lambda e, o=cs[:], a=scm[:], b=sgw[:]: e.tensor_tensor_scan(
                out=o, data0=a, data1=b, initial=0.0, op0=ALU.mult, op1=ALU.add), [scm.b, sgw.b], [cs.b], est=(100 + 2 * QS) / 960.0)
            tot = v3(cs[:])[:, :, L - 1:L]
            self.act(Rd["Wtot"][:, csl], tot.rearrange("p c l -> p (c l)"), AF.Exp, scale=-DS, r=[cs.b], w=[Rd["Wtot"].bq[tq]])
            if d == 0:
                self.tt("pool", X1[:], cs[:], sgw[:], ALU.subtract, r=[cs.b, sgw.b], w=[X1.b])
                ce, ci = X1, cs
            else:
                self.tt("dve", v3(X2[:]), tot.to_broadcast([128, CQ, L]), v3(cs[:]), ALU.subtract, r=[cs.b], w=[X2.b])
                self.tt("pool", X3[:], X2[:], sgw[:], ALU.add, r=[X2.b, sgw.b], w=[X3.b])
                ce, ci = X2, X3
            yield
            ar = Rd["ar"]
            arb = ar.bq[tq]
            self.act(E[:], ce[:], AF.Exp, scale=-DS, r=[ce.b], w=[E.b])
            self.stt(ar[:, csl, 0, :], v3(kkn[:]), -1.0, v3(E[:]), ALU.mult, ALU.mult, r=[kkn.b, E.b], w=[arb])
            self.act(p1[:], ci[:], AF.Exp, scale=-DS, r=[ci.b], w=[p1.b])
            self.tt("dve", ar[:, csl, 1, :], v3(rq[:]), v3(p1[:]), ALU.mult, r=[rq.b, p1.b], w=[arb])
            yield
            self.tt("pool", akk[:], aqt[:], kkn[:], ALU.mult, r=[aqt.b, kkn.b], w=[akk.b])
            self.act(p2[:], ci[:], AF.Exp, scale=DS, r=[ci.b], w=[p2.b])
            self.tt("dve", bT[:], akk[:], p2[:], ALU.mult, r=[akk.b, p2.b], w=[bT.b])
            self.tt("dve", kT[:], kft[:], p2[:], ALU.mult, r=[kft.b, p2.b], w=[kT.b])
            yield
            for src, dstk in ((bT, "BB"), (kT, "KB")):
                ps = self.nps(tmp["grp"])
                psv = ps.t.bitcast(BF16)
                for c in range(CQ):
                    for h in range(2):
                        hs = slice(64 * h, 64 * h + 64)
                        self.tr(psv[hs, c * L:(c + 1) * L], src[hs, c * L:(c + 1) * L], idb[hs, hs],
                                r=[src.b, idb.b], w=[ps.b])
                self.cp("act", Rd[dstk][:, csl, :], v3(psv[:, 0:QS]), r=[ps.b], w=[Rd[dstk].bq[tq]])
                yield
            for lhs, dstk in ((bT, "SB"), (kT, "SK")):
                for c4 in range(0, CQ, 4):
                    ps = self.nps(tmp["grp"])
                    for cc in range(4):
                        c = c4 + cc
                        for h in range(2):
                            hs = slice(64 * h, 64 * h + 64)
                            self.mm(ps[hs, cc * 128:(cc + 1) * 128], lhs[hs, c * L:(c + 1) * L],
                                    ar[hs, cq0 + c, :, :].rearrange("p a l -> p (a l)"),
                                    r=[lhs.b, arb], w=[ps.b])
                    if dstk == "SK":
                        self.tt("dve", Rd[dstk][:, cq0 + c4:cq0 + c4 + 4, :, :].rearrange("p c a l -> p (c a l)"),
                                ps[:, :], mT[:, d, :], ALU.mult, r=[ps.b, mT.b], w=[Rd[dstk].bq[tq]])
                    else:
                        ps4 = ps[:, :].rearrange("p (c a l) -> p c a l", c=4, a=2)
                        m4 = mT[:, d, :].rearrange("p (c a l) -> p c a l", c=4, a=2)
                        self.tt("dve", v3(PTm[:])[:, c4:c4 + 4, :], ps4[:, :, 0, :], m4[:, :, 0, :], ALU.mult,
                                r=[ps.b, mT.b], w=[PTm.b])
                        self.tt("dve", Rd["SB"][:, cq0 + c4:cq0 + c4 + 4, :], ps4[:, :, 1, :], m4[:, :, 1, :], ALU.mult,
                                r=[ps.b, mT.b], w=[Rd["SB"].bq[tq]])
                    yield
            ps = self.nps(tmp["grp"])
            for c in range(CQ):
                for h in range(2):
                    hs = slice(64 * h, 64 * h + 64)
                    self.mm(ps[hs, c * L:(c + 1) * L], ar[hs, cq0 + c, 0, :], bT[hs, c * L:(c + 1) * L],
                            r=[arb, bT.b], w=[ps.b])
            self.tt("dve", Pm[:], ps[:, 0:QS], mN[:, d, 0:QS], ALU.mult, r=[ps.b, mN.b], w=[Pm.b])
            TTq = Rd["TTm"][:, csl, :]
            TTb = Rd["TTm"].bq[tq]
            self.tt("pool", TTq, v3(PTm[:]), v3(idr[:, 0:QS]), ALU.add, r=[PTm.b, idr.b], w=[TTb])
            yield
            Pc, PTc, Pn, PTn = Pm, PTm, Pm2, PTm2
            for lvl in range(1, 6):
                psA = self.nps(tmp["grp"])
                for c in range(CQ):
                    for h in range(2):
                        hs = slice(64 * h, 64 * h + 64)
                        cl = slice(c * L, (c + 1) * L)
                        self.mm(psA[hs, cl], PTc[hs, cl], Pc[hs, cl], r=[PTc.b, Pc.b], w=[psA.b])
                self.cp("act", Pn[:], psA[:, 0:QS], r=[psA.b], w=[Pn.b])
                if lvl < 5:
                    psB = self.nps(tmp["grp"])
                    for c in range(CQ):
                        for h in range(2):
                            hs = slice(64 * h, 64 * h + 64)
                            cl = slice(c * L, (c + 1) * L)
                            self.mm(psB[hs, cl], Pc[hs, cl], PTc[hs, cl], r=[PTc.b, Pc.b], w=[psB.b])
                    self.cp("act", PTn[:], psB[:, 0:QS], r=[psB.b], w=[PTn.b])
                yield
                psC = self.nps(tmp["grp"])
                for c in range(CQ):
                    for h in range(2):
                        hs = slice(64 * h, 64 * h + 64)
                        cl = slice(c * L, (c + 1) * L)
                        self.mm(psC[hs, cl], Pn[hs, cl], Rd["TTm"][hs, cq0 + c, :], r=[Pn.b, TTb], w=[psC.b])
                self.tt("dve", TTq, v3(psC[:, 0:QS]), TTq, ALU.add, r=[psC.b, TTb], w=[TTb])
                Pc, PTc, Pn, PTn = Pn, PTn, Pc, PTc
                yield

        def pre_stage(p, k):
            gens = []
            for d, tq, tmp in ((0, k, TA), (1, NQ_ - 1 - k, TB)):
                fstage, bstage = tq, NQ_ - 1 - tq
                first = (fstage <= bstage) if d == 0 else (bstage < fstage)
                gens.append(pre_pass(p, d, tq, first, tmp))
            return gens

        def chain_group(p, k):
            par = p % 2
            if k == 0:
                self.ms("pool", wkv[par][:], 0.0, w=[wkv[par].b])
                for d in range(2):
                    self.ms("pool", R[d]["ST"][:], 0.0, w=[R[d]["ST"].b])
            for step in range(k * CQ, (k + 1) * CQ):
                for d in range(2):
                    Rd = R[d]
                    c = step if d == 0 else NCH - 1 - step
                    qi = c // CQ
                    ar, SBm, SKm, TTm, BB, KB, ST, Xs, Us = (Rd[k_] for k_ in ("ar", "SB", "SK", "TTm", "BB", "KB", "ST", "Xs", "Us"))
                    vt = vtok[par]
                    vtb = vt.bq[qi]
                    psX = self.nps("chain")
                    for h in range(2):
                        hs = slice(64 * h, 64 * h + 64)
                        self.mm(psX[hs, 0:L], ar[hs, c, 0, :], ST[hs, :], start=True, stop=False, r=[ar.bq[qi], ST.b], w=[psX.b])
                        self.mm(psX[hs, 0:L], SKm[hs, c, 0, :], vt[hs, c, :], start=False, stop=True, r=[SKm.bq[qi], vtb], w=[psX.b])
                    self.cp("act", Xs[:], psX[:, 0:L], r=[psX.b], w=[Xs.b])
                    yield
                    psU = self.nps("chain")
                    for h in range(2):
                        hs = slice(64 * h, 64 * h + 64)
                        self.mm(psU[hs, 0:L], TTm[hs, c, :], Xs[hs, :], r=[TTm.bq[qi], Xs.b], w=[psU.b])
                    self.cp("dve", Us[:], psU[:, 0:L], r=[psU.b], w=[Us.b])
                    yield
                    psY = self.nps("chain")
                    for h in range(2):
                        hs = slice(64 * h, 64 * h + 64)
                        self.mm(psY[hs, 0:L], ST[hs, :], ar[hs, c, 1, :], start=True, stop=False, r=[ar.bq[qi], ST.b], w=[psY.b])
                        self.mm(psY[hs, 0:L], Us[hs, :], SBm[hs, c, :], start=False, stop=False, r=[Us.b, SBm.bq[qi]], w=[psY.b])
                        self.mm(psY[hs, 0:L], vt[hs, c, :], SKm[hs, c, 1, :], start=False, stop=True, r=[vtb, SKm.bq[qi]], w=[psY.b])
                    psS = self.nps("chain")
                    for h in range(2):
                        hs = slice(64 * h, 64 * h + 64)
                        self.mm(psS[hs, 0:L], idb[hs, hs], ST[hs, :], start=True, stop=False, r=[idb.b, ST.b], w=[psS.b])
                        self.mm(psS[hs, 0:L], BB[hs, c, :], Us[hs, :], start=False, stop=False, r=[BB.bq[qi], Us.b], w=[psS.b])
                        self.mm(psS[hs, 0:L], KB[hs, c, :], vt[hs, c, :], start=False, stop=True, r=[KB.bq[qi], vtb], w=[psS.b])
                    self.ts("dve", ST[:], psS[:, 0:L], Rd["Wtot"][:, c:c + 1], None, ALU.mult,
                            r=[Rd["Wtot"].bq[qi], psS.b], w=[ST.b])
                    wsl = wkv[par][:, c * L:(c + 1) * L]
                    self.tt("dve", wsl, psY[:, 0:L], wsl, ALU.add, r=[psY.b, wkv[par].b], w=[wkv[par].b])
                    yield

        def post(p):
            par = p % 2
            pc = slice(p * 128, (p + 1) * 128)
            for tq in range(NQ):
                ts_ = slice(tq * QS, (tq + 1) * QS)
                self.dma(sgb.t[:], uS[12][:, ts_], r=[B_uS[12]], w=[sgb.b])
                ps = self.nps("post")
                self.mm(ps[:, 0:QS], bab[:], wkv[par][:, ts_], r=[bab.b, wkv[par].b], w=[ps.b])
                self.tt("dve", t1[:], wkv[par][:, ts_], ps[:, 0:QS], ALU.subtract, r=[wkv[par].b, ps.b], w=[t1.b])
                self.tt("pool", sq[:], t1[:], t1[:], ALU.mult, r=[t1.b], w=[sq.b])
                yield
                ps = self.nps("post")
                self.mm(ps[:, 0:QS], ba[:], sq[:], r=[ba.b, sq.b], w=[ps.b])
                self.ts("dve", t2[:], ps[:, 0:QS], LNX_EPS, None, ALU.add, r=[ps.b], w=[t2.b])
                self.act(t2[:], t2[:], AF.Ln, r=[t2.b], w=[t2.b])
                self.act(t2[:], t2[:], AF.Exp, scale=-0.5, r=[t2.b], w=[t2.b])
                self.tt("dve", t1[:], t1[:], t2[:], ALU.mult, r=[t1.b, t2.b], w=[t1.b])
                yield
                self.ts("dve", t1[:], t1[:], pp[:, p, PLW:PLW + 1], pp[:, p, PLB:PLB + 1], ALU.mult, ALU.add,
                        r=[t1.b, pp.b], w=[t1.b])
                self.tt("pool", t1[:], t1[:], bonus[par][:, ts_], ALU.add, r=[t1.b, bonus[par].b], w=[t1.b])
                ps = self.nps("post")
                self.mm(ps[:, 0:QS], gup[:, pc], sgb[:], r=[gup.b, sgb.b], w=[ps.b])
                self.tt("dve", yR[:, p, ts_], t1[:], ps[:, 0:QS], ALU.mult, r=[t1.b, ps.b], w=[yR.b])
                yield

        def run(g):
            for _ in g:
                pass

        interleave = self.interleave

        interleave(pre_stage(0, 0))
        pending_post = None
        for p in range(4):
            for k in range(NQ_):
                nxt = None
                if k + 1 < NQ_:
                    nxt = pre_stage(p, k + 1)
                elif p + 1 < 4:
                    nxt = pre_stage(p + 1, 0)
                if nxt is not None and NQ_ > 1:
                    gens = [chain_group(p, k)] + nxt
                    if k == 0 and pending_post is not None:
                        gens.append(pending_post)
                        pending_post = None
                    interleave(gens)
                else:
                    if pending_post is not None:
                        run(pending_post)
                        pending_post = None
                    run(chain_group(p, k))
                    if nxt is not None:
                        interleave(nxt)
            pending_post = post(p)
        run(pending_post)

    def attention(self, st, s, env, hT, yA):
        P, T, QS, NQ, TT = self.P, self.T, self.QS, self.NQ, self.TT
        w_in, idb, onesb = env["w_in"], env["idb"], env["onesb"]
        sb = self.sb
        bias = sb(st, "bias", [128, 6, 512], BF16)
        self.load_w(bias[:], bias.b, env["c_bias"])
        snk = sb(st, "snk", [128, 8], F32)
        esk = sb(st, "esk", [128, 2, 2, 128], F32)
        self.dma(snk.t[:], env["sink_d"].partition_broadcast(128), w=[snk.b])
        self.act(snk[:], snk[:], AF.Exp, r=[snk.b], w=[snk.b])
        for kv in range(2):
            for gh in range(2):
                for gl in range(2):
                    hs = slice(64 * gl, 64 * gl + 64)
                    col = kv * 4 + 2 * gh + gl
                    self.cp("dve", esk[hs, kv, gh, :], snk[hs, col:col + 1].to_broadcast([64, 128]), r=[snk.b], w=[esk.b])
        qT = sb(st, "qT", [128, TT, 4, 128], BF16)
        kTz = [sb(st, f"kTz{i}", [128, T], BF16) for i in range(2)]
        vtk = sb(st, "vtk", [128, TT, 128], BF16)
        wq = sb(st, "wq", [128, 8, 512], BF16)
        wkv_ = sb(st, "wkvw", [128, 8, 256], BF16)
        c0 = 1920
        wq5 = wq.t[:].rearrange("p k (g kv d) -> p k g kv d", g=4, kv=2)
        for i in range(2):
            for kc in range(8):
                self.load_w(wq5[:, kc, :, i, :], wq.b,
                            w_in[:, kc, c0 + i * 256:c0 + (i + 1) * 256].rearrange("p (g d) -> p g d", g=4))
        self.load_w(wkv_[:], wkv_.b, w_in[:, :, c0 + 512:c0 + 768], 8, 256)
        for i in range(2):
            self.ms("pool", kTz[i][:], 0.0, w=[kTz[i].b])
        for tq in range(NQ):
            ts_ = slice(tq * QS, (tq + 1) * QS)
            for g in range(4):
                ps = self.nps()
                for kc in range(8):
                    self.mm(ps[:, 0:QS], wq[:, kc, g * 128:(g + 1) * 128], hT[:, kc, ts_], start=(kc == 0), stop=(kc == 7),
                            r=[wq.b, hT.b], w=[ps.b])
                sg_ = (g % 2) * 2 + g // 2
                nb_ = QS // 128
                self.act(qT[:, tq * nb_:(tq + 1) * nb_, sg_, :], ps[:, 0:QS].rearrange("p (b q) -> p b q", q=128), AF.Copy, scale=0.125,
                         r=[ps.b], w=[qT.b])
            ps = self.nps()
            self.inproj(hT, wkv_, 0, tq, [wkv_.b, hT.b], ps)
            for kv in range(2):
                hs = slice(64 * kv, 64 * kv + 64)
                self.cp("act", kTz[kv][hs, ts_], ps[hs, 0:QS], r=[ps.b], w=[kTz[kv].b])
        for tt_ in range(TT):
            ps = self.nps()
            for kc in range(8):
                self.mm(ps[:, 0:128], hT[:, kc, tt_ * 128:(tt_ + 1) * 128], wkv_[:, kc, 128:256], start=(kc == 0), stop=(kc == 7),
                        r=[wkv_.b, hT.b], w=[ps.b])
            self.cp("act", vtk[:, tt_, :], ps[:, 0:128], r=[ps.b], w=[vtk.b])
        pTs = [[sb(st, f"pT{j}_{i}", [128, 512], BF16) for i in range(3)] for j in range(2)]
        dens = [sb(st, f"den{j}", [128, 256], F32) for j in range(2)]

        def att_iter(kv, qb, par):
            den = dens[par]
            kbs = [kb for kb in (qb - 1, qb, qb + 1) if 0 <= kb < TT]
            psO = self.nps(f"attO{par}")
            psD = self.nps(f"attD{par}")
            for ki, kb in enumerate(kbs):
                rel = kb - qb + 1
                psS = self.nps(f"attS{par}")
                pT = pTs[par][ki]
                self.mm(psS[:, :], kTz[kv][:, kb * 128:(kb + 1) * 128], qT[:, qb, :, :].rearrange("p g q -> p (g q)"),
                        start=True, stop=False, r=[kTz[kv].b, qT.b], w=[psS.b])
                self.mm(psS[:, :], idb[:], bias[:, rel * 2 + kv, :], start=False, stop=True, r=[idb.b, bias.b], w=[psS.b])
                self.act(pT[:], psS[:, :], AF.Exp, r=[psS.b], w=[pT.b])
                yield
                for gl in range(2):
                    hs = slice(64 * gl, 64 * gl + 64)
                    self.mm(psO[hs, 0:256], vtk[:, kb, 64 * kv:64 * kv + 64], pT[:, gl * 256:(gl + 1) * 256],
                            start=(ki == 0), stop=(ki == len(kbs) - 1), r=[vtk.b, pT.b], w=[psO.b], nohold=True)
                    self.mm(psD[hs, 0:256], onesb[:, 0:64], pT[:, gl * 256:(gl + 1) * 256],
                            start=(ki == 0), stop=(ki == len(kbs) - 1), r=[onesb.b, pT.b], w=[psD.b], nohold=True)
                yield
            self.tt("dve", den[:], psD[:, 0:256], esk[:, kv, :, :].rearrange("p a q -> p (a q)"), ALU.add,
                    r=[psD.b, esk.b], w=[den.b])
            self.act(den[:], den[:], AF.Ln, r=[den.b], w=[den.b])
            self.act(den[:], den[:], AF.Exp, scale=-1.0, r=[den.b], w=[den.b])
            yield
            self.tt("dve", yA[:, 2 * kv:2 * kv + 2, qb * 128:(qb + 1) * 128],
                    psO[:, 0:256].rearrange("p (a q) -> p a q", a=2), den[:].rearrange("p (a q) -> p a q", a=2),
                    ALU.mult, r=[psO.b, den.b], w=[yA.b])
            yield

        its = [(kv, qb) for kv in range(2) for qb in range(TT)]
        self.interleave([(lambda slot, kv=kv, qb=qb: att_iter(kv, qb, slot)) for (kv, qb) in its], window=2)

    def merge_gates(self, st, s, env, hT, yA, mrg):
        P, T, QS, NQ, TT = self.P, self.T, self.QS, self.NQ, self.TT
        NS = self.NS
        w_in, idf = env["w_in"], env["idf"]
        x, acc, h2d, B_acc, B_h2d = env["x"], env["acc"], env["h2d"], env["B_acc"], env["B_h2d"]
        wrt, affT = env["wrt"], env["affT"]
        yR = self.yRk
        sb = self.sb
        wpr = sb(st, "wpr", [128, 4, D], BF16)
        wpa = sb(st, "wpa", [128, 4, D], BF16)
        self.load_w(wpr[:], wpr.b, env["wpr_d"])
        self.load_w(wpa[:], wpa.b, env["wpa_d"])
        wgs = [sb(st, f"wg{i}", [128, 8, 1024], BF16) for i in range(2)]
        sg1s = [sb(st, f"sg1_{i}", [128, QS], F32) for i in range(2)]
        sg2s = [sb(st, f"sg2_{i}", [128, QS], F32) for i in range(2)]
        m1s = [sb(st, f"m1_{i}", [128, QS], F32) for i in range(2)]
        m2s = [sb(st, f"m2_{i}", [128, QS], F32) for i in range(2)]
        cg = 1920 + 768

        def gate_iter(oc, tq, par):
            sg1, sg2, m1, m2 = sg1s[par], sg2s[par], m1s[par], m2s[par]
            wg = wgs[oc // 4]
            ol = (oc % 4) * 128
            if oc % 4 == 0 and tq == 0:
                self.load_w(wg[:, :, 0:512], wg.b, w_in[:, :, cg + oc * 128:cg + oc * 128 + 512])
                self.load_w(wg[:, :, 512:1024], wg.b, w_in[:, :, cg + 1024 + oc * 128:cg + 1024 + oc * 128 + 512])
            ts_ = slice(tq * QS, (tq + 1) * QS)
            ps1 = self.nps(f"h{par}")
            self.inproj(hT, wg, ol, tq, [wg.b, hT.b], ps1)
            self.act(sg1[:], ps1[:, 0:QS], AF.Sigmoid, r=[ps1.b], w=[sg1.b])
            yield
            ps2 = self.nps(f"h{par}")
            self.inproj(hT, wg, 512 + ol, tq, [wg.b, hT.b], ps2)
            self.act(sg2[:], ps2[:, 0:QS], AF.Sigmoid, r=[ps2.b], w=[sg2.b])
            yield
            ps3 = self.nps(f"h{par}")
            for kc in range(4):
                self.mm(ps3[:, 0:QS], wpr[:, kc, oc * 128:(oc + 1) * 128], yR[:, kc, ts_], start=(kc == 0), stop=(kc == 3),
                        r=[wpr.b, yR.b], w=[ps3.b])
            self.tt("dve", m1[:], sg1[:], ps3[:, 0:QS], ALU.mult, r=[sg1.b, ps3.b], w=[m1.b])
            yield
            ps4 = self.nps(f"h{par}")
            for kc in range(4):
                self.mm(ps4[:, 0:QS], wpa[:, kc, oc * 128:(oc + 1) * 128], yA[:, kc, ts_], start=(kc == 0), stop=(kc == 3),
                        r=[wpa.b, yA.b], w=[ps4.b])
            self.tt("dve", m2[:], sg2[:], ps4[:, 0:QS], ALU.mult, r=[sg2.b, ps4.b], w=[m2.b])
            yield
            self.tt("pool", mrg[:, oc, ts_], m1[:], m2[:], ALU.add, r=[m1.b, m2.b], w=[mrg.b])
            yield

        its = [(oc, tq) for oc in range(8) for tq in range(NQ)]
        self.interleave([(lambda slot, oc=oc, tq=tq: gate_iter(oc, tq, slot)) for (oc, tq) in its], window=2)

    def out_proj(self, st, s, env, mrg):
        P, T, QS, NQ, TT = self.P, self.T, self.QS, self.NQ, self.TT
        idf = env["idf"]
        x, acc, h2d, B_acc, B_h2d = env["x"], env["acc"], env["h2d"], env["B_acc"], env["B_h2d"]
        wrt, affT = env["wrt"], env["affT"]
        sb = self.sb
        wout = sb(st, "wout", [128, 8, D], BF16)
        self.load_w(wout[:], wout.b, env["wout_d"])
        g2bc = sb(st, "g2bc", [128, D], F32)
        self.dma(g2bc.t[:], env["g2r_d"].partition_broadcast(128), w=[g2bc.b])
        xts = [sb(st, f"xm{i}", [128, D], F32) for i in range(2)]
        x1s = [sb(st, f"x1{i}", [128, D], F32) for i in range(2)]
        h2s = [sb(st, f"h2{i}", [128, D], F32) for i in range(2)]
        h2bs = [sb(st, f"h2b{i}", [128, D], BF16) for i in range(2)]
        h2Ts = [sb(st, f"h2T{i}", [128, 8, 128], F32) for i in range(2)]
        junk = sb(st, "junk2", [128, D], F32)
        sst = [sb(st, f"sm{i}", [128, 8], F32) for i in range(2)]
        lgs = [sb(st, f"lg{i}", [128, NEXP], F32) for i in range(2)]
        affts = [sb(st, f"afft{i}", [128, 128], F32) for i in range(2)]
        for a_ in affts:
            self.ms("pool", a_[:], 0.0, w=[a_.b])
        def tile_iter(tt_, slot):
                xt, x1, h2, h2b, ss = xts[slot], x1s[slot], h2s[slot], h2bs[slot], sst[slot]
                h2T, lg, afft = h2Ts[slot], lgs[slot], affts[slot]
                r0 = s * T + tt_ * 128
                tl = slice(tt_ * 128, (tt_ + 1) * 128)
                self.dma(xt.t[:], x[r0:r0 + 128, :], w=[xt.b])
                for half in range(2):
                    ps = self.nps(f"h{slot}")
                    for kc in range(8):
                        self.mm(ps[:, :], mrg[:, kc, tl], wout[:, kc, half * 512:(half + 1) * 512], start=(kc == 0), stop=(kc == 7),
                                r=[mrg.b, wout.b], w=[ps.b])
                    self.tt("dve", x1[:, half * 512:(half + 1) * 512], xt[:, half * 512:(half + 1) * 512], ps[:, :], ALU.add,
                            r=[xt.b, ps.b], w=[x1.b])
                yield
                self.dma(acc[r0:r0 + 128, :], x1[:], r=[x1.b], w=[B_acc[s]])
                self.act(junk[:], x1[:], AF.Square, accum=ss[:, 0:1], r=[x1.b], w=[junk.b, ss.b])
                self.ts("dve", ss[:, 1:2], ss[:, 0:1], 1.0 / D, NORM_EPS, ALU.mult, ALU.add, r=[ss.b], w=[ss.b])
                self.act(ss[:, 2:3], ss[:, 1:2], AF.Sqrt, r=[ss.b], w=[ss.b])
                self.P.emit("dve", lambda e, o=ss[:, 3:4], i=ss[:, 2:3]: e.reciprocal(out=o, in_=i), [ss.b], [ss.b])
                self.ts("dve", h2[:], x1[:], ss[:, 3:4], None, ALU.mult, r=[x1.b, ss.b], w=[h2.b])
                self.tt("pool", h2b[:], h2[:], g2bc[:], ALU.mult, r=[h2.b, g2bc.b], w=[h2b.b])
                self.dma(h2d[r0:r0 + 128, :], h2b[:], r=[h2b.b], w=[B_h2d[s]])
                yield
                for k2 in range(2):
                    ps = self.nps(f"h{slot}")
                    for kk in range(4):
                        kc = k2 * 4 + kk
                        self.tr(ps[:, kk * 128:(kk + 1) * 128], h2[:, kc * 128:(kc + 1) * 128], idf[:], r=[h2.b, idf.b], w=[ps.b])
                    self.cp("act", h2T[:, k2 * 4:(k2 + 1) * 4, :], ps[:, :].rearrange("p (k n) -> p k n", k=4), r=[ps.b], w=[h2T.b])
                yield
                ps = self.nps(f"h{slot}")
                for kc in range(8):
                    self.mm(ps[:, 0:NEXP], h2T[:, kc, :], wrt[:, kc, :], start=(kc == 0), stop=(kc == 7), r=[h2T.b, wrt.b], w=[ps.b])
                self.P.emit("dve", lambda e, o=ss[:, 4:5], i=ps[:, 0:NEXP]: e.reduce_max(out=o, in_=i, axis=AX.X), [ps.b], [ss.b])
                self.ts("dve", ss[:, 5:6], ss[:, 4:5], -1.0, None, ALU.mult, r=[ss.b], w=[ss.b])
                self.act(lg[:], ps[:, 0:NEXP], AF.Exp, bias=ss[:, 5:6], accum=ss[:, 6:7], r=[ps.b, ss.b], w=[lg.b, ss.b])
                self.P.emit("dve", lambda e, o=ss[:, 7:8], i=ss[:, 6:7]: e.reciprocal(out=o, in_=i), [ss.b], [ss.b])
                self.ts("dve", afft[:, 32 * s:32 * s + NEXP], lg[:], ss[:, 7:8], None, ALU.mult, r=[lg.b, ss.b], w=[afft.b])
                ps = self.nps(f"h{slot}")
                self.tr(ps[:, 0:128], afft[:], idf[:], r=[afft.b, idf.b], w=[ps.b])
                self.tt("dve", affT[:, tl], affT[:, tl], ps[:, 0:128], ALU.add, r=[ps.b, affT.b], w=[affT.b])


        self.interleave([(lambda slot, tt_=tt_: tile_iter(tt_, slot)) for tt_ in range(TT)], window=2)

    def phase_b(self, env):
        P, NS, T, CAP, SB_, NB = self.P, self.NS, self.T, self.CAP, self.SB, self.NB
        affT, idf, idb, g2 = env["affT"], env["idf"], env["idb"], env["g2"]
        acc, h2d, B_acc, B_h2d = env["acc"], env["h2d"], env["B_acc"], env["B_h2d"]
        sb = self.sb
        with ExitStack() as st:
            wk = sb(st, "wk", [128, T], F32)
            mv = sb(st, "mv", [128, CAP], F32)
            mi = sb(st, "mi", [128, CAP], U32)
            mif = sb(st, "mif", [128, CAP], F32)
            offs = sb(st, "offs", [128, 128], F32)
            idxT = sb(st, "idxT", [128, NB, 128], I32)
            valT = sb(st, "valT", [128, NB, 128], F32)
            self.dma(offs.t[:], env["c_offs"], w=[offs.b])
            self.cp("dve", wk[:], affT[:], r=[affT.b], w=[wk.b])
            for r_ in range(CAP // 8):
                sl = slice(r_ * 8, r_ * 8 + 8)
                self.P.emit("dve", lambda e, o=mv[:, sl], i=wk[:]: e.max(out=o, in_=i), [wk.b], [mv.b])
                self.P.emit("dve", lambda e, o=mi[:, sl], m=mv[:, sl], i=wk[:]: e.max_index(out=o, in_max=m, in_values=i),
                            [wk.b, mv.b], [mi.b])
                self.P.emit("dve", lambda e, o=wk[:], m=mv[:, sl], i=wk[:]: e.match_replace(
                    out=o, in_to_replace=m, in_values=i, imm_value=-1.0), [wk.b, mv.b], [wk.b])
            self.cp("dve", mif[:], mi[:], r=[mi.b], w=[mif.b])
            for blk in range(NB):
                bs = slice(blk * SB_, (blk + 1) * SB_)
                ps = self.nps()
                self.tr(ps[0:SB_, 0:128], mif[:, bs], idf[:], r=[mif.b, idf.b], w=[ps.b])
                self.tt("dve", idxT[0:SB_, blk, :], ps[0:SB_, 0:128], offs[0:SB_, :], ALU.add, r=[ps.b, offs.b], w=[idxT.b])
                ps = self.nps()
                self.tr(ps[0:SB_, 0:128], mv[:, bs], idf[:], r=[mv.b, idf.b], w=[ps.b])
                self.cp("act", valT[0:SB_, blk, :], ps[0:SB_, 0:128], r=[ps.b], w=[valT.b])
            wgs = [sb(st, f"ewg{i}", [128, 8, D], BF16) for i in range(2)]
            wus = [sb(st, f"ewu{i}", [128, 8, D], BF16) for i in range(2)]
            wds = [sb(st, f"ewd{i}", [128, 8, D], BF16) for i in range(2)]
            NPF = 2
            xss = [sb(st, f"xs{i}", [128, D], BF16) for i in range((NPF + 1) * NB)]
            xsT = sb(st, "xsT", [128, 8, CAP], BF16)
            hid = sb(st, "hid", [128, 8, CAP], BF16)
            sl_ = sb(st, "silu", [128, CAP], F32)
            yss = [sb(st, f"ys{i}", [128, D], F32) for i in range(2 * NB)]
            its = [(e, s) for e in range(NEXP) for s in range(NS)]
            last_sc = {s: [] for s in range(NS)}

            def load_expert(e):
                for dst, src in ((wgs[e % 2], env["eg_d"]), (wus[e % 2], env["eu_d"]), (wds[e % 2], env["ed_d"])):
                    self.load_w(dst[:], dst.b, src[e].rearrange("(k p) n -> p k n", p=128))

            def gather(i):
                e, s = its[i]
                pcol = 32 * s + e
                for blk in range(NB):
                    xs = xss[(i % (NPF + 1)) * NB + blk]
                    ia = idxT[0:SB_, blk, pcol:pcol + 1]
                    self.P.emit("pool", lambda en, o=xs[0:SB_, :], ia=ia: en.indirect_dma_start(
                        out=o, out_offset=None, in_=h2d, in_offset=bass.IndirectOffsetOnAxis(ap=ia, axis=0)),
                        [idxT.b, B_h2d[s]], [xs.b], dma=True)

            load_expert(0)
            for i in range(min(NPF, len(its))):
                gather(i)
            for i, (e, s) in enumerate(its):
                wg, wu, wd = wgs[e % 2], wus[e % 2], wds[e % 2]
                pcol = 32 * s + e
                if s == 0 and e + 1 < NEXP:
                    load_expert(e + 1)
                if i + NPF < len(its):
                    gather(i + NPF)
                for blk in range(NB):
                    xs = xss[(i % (NPF + 1)) * NB + blk]
                    ps = self.nps()
                    psv = ps.t.bitcast(BF16)
                    for kc in range(8):
                        self.tr(psv[:, kc * 128:kc * 128 + SB_], xs[0:SB_, kc * 128:(kc + 1) * 128], idb[0:SB_, 0:SB_],
                                r=[xs.b, idb.b], w=[ps.b])
                    self.cp("act", xsT[:, :, blk * SB_:(blk + 1) * SB_],
                            psv[:, :].rearrange("p (k n) -> p k n", k=8)[:, :, 0:SB_], r=[ps.b], w=[xsT.b])
                for fc in range(8):
                    psg = self.nps()
                    psu = self.nps()
                    for kc in range(8):
                        self.mm(psg[:, 0:CAP], wg[:, kc, fc * 128:(fc + 1) * 128], xsT[:, kc, :], start=(kc == 0), stop=(kc == 7),
                                r=[wg.b, xsT.b], w=[psg.b])
                    for kc in range(8):
                        self.mm(psu[:, 0:CAP], wu[:, kc, fc * 128:(fc + 1) * 128], xsT[:, kc, :], start=(kc == 0), stop=(kc == 7),
                                r=[wu.b, xsT.b], w=[psu.b])
                    self.act(sl_[:], psg[:, 0:CAP], AF.Silu, r=[psg.b], w=[sl_.b])
                    self.tt("dve", hid[:, fc, :], sl_[:], psu[:, 0:CAP], ALU.mult, r=[sl_.b, psu.b], w=[hid.b])
                new_sc = []
                for blk in range(NB):
                    ys = yss[(i % 2) * NB + blk]
                    for half in range(2):
                        ps = self.nps()
                        for fc in range(8):
                            self.mm(ps[0:SB_, :], hid[:, fc, blk * SB_:(blk + 1) * SB_], wd[:, fc, half * 512:(half + 1) * 512],
                                    start=(fc == 0), stop=(fc == 7), r=[hid.b, wd.b], w=[ps.b])
                        self.act(ys[0:SB_, half * 512:(half + 1) * 512], ps[0:SB_, :], AF.Copy,
                                 scale=valT[0:SB_, blk, pcol:pcol + 1], r=[ps.b, valT.b], w=[ys.b])
                    ia = idxT[0:SB_, blk, pcol:pcol + 1]
                    op = self.P.emit("pool", lambda en, i_=ys[0:SB_, :], ia=ia: en.indirect_dma_start(
                        out=acc, out_offset=bass.IndirectOffsetOnAxis(ap=ia, axis=0), in_=i_, in_offset=None,
                        compute_op=ALU.add), [idxT.b, ys.b, B_acc[s]], [], dma=True)
                    for d_ in last_sc[s]:
                        self.P._dep(op, d_)
                    new_sc.append(op)
                last_sc[s] = new_sc
            P.stage_end()

    def phase_c(self, env):
        P, NS, T, TT = self.P, self.NS, self.T, self.TT
        acc, y, B_acc = env["acc"], env["y"], env["B_acc"]
        sb = self.sb
        outs = []
        with ExitStack() as st:
            gF = sb(st, "gF", [128, D], F32)
            self.dma(gF.t[:], env["gF_d"].partition_broadcast(128), w=[gF.b])
            NW = 4
            ats = [sb(st, f"at{i}", [128, D], F32) for i in range(NW)]
            ots = [sb(st, f"ot{i}", [128, D], F32) for i in range(NW)]
            junks = [sb(st, f"junk3_{i}", [128, D], F32) for i in range(2)]
            sst = [sb(st, f"sc{i}", [128, 4], F32) for i in range(NW)]

            def c_iter(n, s, tt_):
                at, ot, ss, junk = ats[n], ots[n], sst[n], junks[n % 2]
                r0 = s * T + tt_ * 128
                self.dma(at.t[:], acc[r0:r0 + 128, :], r=[B_acc[s]], w=[at.b])
                yield
                self.act(junk[:], at[:], AF.Square, accum=ss[:, 0:1], r=[at.b], w=[junk.b, ss.b])
                self.ts("dve", ss[:, 1:2], ss[:, 0:1], 1.0 / D, NORM_EPS, ALU.mult, ALU.add, r=[ss.b], w=[ss.b])
                self.act(ss[:, 2:3], ss[:, 1:2], AF.Sqrt, r=[ss.b], w=[ss.b])
                self.P.emit("dve", lambda e, o=ss[:, 3:4], i=ss[:, 2:3]: e.reciprocal(out=o, in_=i), [ss.b], [ss.b])
                yield
                self.stt(ot[:], at[:], ss[:, 3:4], gF[:], ALU.mult, ALU.mult, r=[at.b, ss.b, gF.b], w=[ot.b])
                self.P.emit("sp", lambda e, o=y[r0:r0 + 128, :], i=ot[:]: e.dma_start(out=o, in_=i), [ot.b], [], dma=True)
                yield

            fin_b = P.buf("yout")
            gens = []
            n = 0
            for s in range(NS):
                for tt_ in range(TT):
                    gens.append(lambda slot, s=s, tt_=tt_: c_iter(slot, s, tt_))
                    n += 1
            self.interleave(gens, window=NW)
            outs = []
            fin = P.buf("fin")
            op = P.emit("sp", lambda en: en.nop(), writes=[fin])
            for d_ in outs:
                P._dep(op, d_)
            P.stage_end()


def host_consts(T):
    c = {}
    c["c_idf"] = np.eye(128, dtype=np.float32)
    blk = (np.arange(128)[:, None] // 64 == np.arange(128)[None, :] // 64).astype(np.float32)
    c["c_bo"] = blk
    c["c_ba"] = blk / 64.0
    s = (np.arange(128) % 64)[:, None]
    j = np.arange(128)[None, :]
    t = j % 64
    isr = j >= 64
    mT = np.zeros((128, 2, 4, 128), np.float32)
    mT[:, 0] = np.where(isr, s <= t, s < t)[:, None, :]
    mT[:, 1] = np.where(isr, s >= t, s > t)[:, None, :]
    c["c_mT"] = mT.reshape(128, 2, 512)
    tt = (np.arange(128) % 64)[:, None]
    ss = np.arange(64)[None, :]
    mN = np.zeros((128, 2, 8, 64), np.float32)
    mN[:, 0] = (ss < tt)[:, None, :]
    mN[:, 1] = (ss > tt)[:, None, :]
    c["c_mN"] = mN.reshape(128, 2, 512)
    idr = np.zeros((128, 8, 64), np.float32)
    idr[:] = (ss == tt)[:, None, :]
    c["c_idr"] = idr.reshape(128, 512)
    slopes = 2.0 ** (-8.0 * np.arange(1, 9) / 8)
    key = np.arange(128)[:, None]
    qq = np.arange(128)[None, :]
    bias = np.zeros((128, 3, 2, 4, 128), np.float32)
    for rel in (-1, 0, 1):
        dist = np.abs(rel * 128 + key - qq)
        for kv in range(2):
            for sg in range(4):
                g = 2 * (sg % 2) + sg // 2
                bias[:, rel + 1, kv, sg, :] = np.where(dist <= 128, -slopes[kv * 4 + g] * dist, -1e30)
    c["c_bias"] = bias.reshape(128, 6, 512)
    offs = np.zeros((128, 128), np.float32)
    offs[:] = ((np.arange(128) // 32) * T)[None, :]
    c["c_offs"] = offs
    return c


def host_params(inp):
    f = lambda a: np.ascontiguousarray(np.asarray(a, dtype=np.float32))
    m = {}
    m["w_in"] = f(inp["w_in"][0])
    m["gmixr"] = f(inp["norm_mix_g"][0].reshape(1, D))
    m["g2r"] = f(inp["norm_ffn_g"][0].reshape(1, D))
    m["mu"] = f(np.stack([inp["mu_prev"][0].reshape(15, 128).T, inp["mu_next"][0].reshape(15, 128).T], axis=-1))
    names = ["w0_f", "w0_b", "a0_f", "a0_b", "k_k", "k_a", "r_k", "ln_x_w", "ln_x_b"]
    m["pp"] = f(np.stack([np.asarray(inp[n][0]).reshape(4, 128).T for n in names], axis=-1))
    m["sink"] = f(inp["attn_sink"][0].reshape(1, 8))
    m["g2"] = f(inp["norm_ffn_g"][0].reshape(8, 128).T)
    m["gF"] = f(np.asarray(inp["norm_final_g"]).reshape(1, D))
    m["wup"] = f(np.concatenate([inp["w_up_f"][0], inp["w_up_b"][0]], axis=0))
    m["aup"] = f(np.concatenate([inp["a_up_f"][0], inp["a_up_b"][0]], axis=0))
    m["gup"] = f(inp["g_up"][0])
    m["wpr"] = f(inp["w_proj_rwkv"][0])
    m["wpa"] = f(inp["w_proj_attn"][0])
    m["wout"] = f(inp["w_out"][0])
    m["wr"] = f(inp["w_router"][0])
    m["eg"] = f(inp["exp_w_gate"][0])
    m["eu"] = f(inp["exp_w_up"][0])
    m["ed"] = f(inp["exp_w_down"][0])
    return m


_NC_CACHE = {}


def run(inp, n_cores=8, stop=None, dbg=False, raw=False):
    x = np.asarray(inp["x"], dtype=np.float32)
    B, T, _ = x.shape
    NS = B // n_cores
    key = (NS, T, stop, dbg)
    if key not in _NC_CACHE:
        _NC_CACHE[key] = Builder(NS, T, stop, dbg).build()
    nc = _NC_CACHE[key]
    shared = host_params(inp)
    shared.update(host_consts(T))
    in_maps = []
    for c in range(n_cores):
        m = dict(shared)
        m["x"] = np.ascontiguousarray(x[c * NS:(c + 1) * NS].reshape(NS * T, D))
        in_maps.append(m)
    res = run_bass_kernel_spmd(nc, in_maps, core_ids=list(range(n_cores)))
    if raw:
        return res.results
    out = np.concatenate([r["y"].reshape(NS, T, D) for r in res.results], axis=0)
    return out.astype(np.float32)


def kernel(**inputs):
    return run(inputs, 8)
```

```python
import math
from contextlib import ExitStack
import numpy as np
import concourse.bass as bass
import concourse.mybir as mybir
from concourse.bass_utils import run_bass_kernel_spmd

F32 = mybir.dt.float32
BF16 = mybir.dt.bfloat16
U32 = mybir.dt.uint32
I32 = mybir.dt.int32
AF = mybir.ActivationFunctionType
ALU = mybir.AluOpType
AX = mybir.AxisListType

ENGS = ("pe", "dve", "act", "pool", "sp")
SEM_WRAP = 4000
NDMA_SEM = 12
NBANK = {'pe': 24, 'dve': 12, 'act': 12, 'pool': 6, 'sp': 2}

D = 1024
DS = math.exp(-0.5)
LNX_EPS = 64e-5
NORM_EPS = 1e-6
NEXP = 16
L = 64


class Buf:
    __slots__ = ("name", "w", "r")

    def __init__(self, name):
        self.name = name
        self.w = None
        self.r = []


class Op:
    __slots__ = ("eng", "fn", "waits", "signal", "idx", "dma", "tk", "sigval", "gid", "ninc")

    def __init__(self, eng, fn, dma):
        self.eng = eng
        self.fn = fn
        self.waits = []
        self.signal = False
        self.dma = dma
        self.tk = None
        self.sigval = None


class Prog:
    def __init__(self, nc, stack):
        self.nc = nc
        self.bufs = []
        self.sigcount = {e: 0 for e in ENGS}
        self.ndma = {e: 0 for e in ENGS}
        self.sems = {e: [stack.enter_context(nc.semaphore(f"s_{e}_{i}")) for i in range(NBANK[e])] for e in ENGS}
        self.dsems = {e: [stack.enter_context(nc.semaphore(f"d_{e}_{i}")) for i in range(NDMA_SEM)]
                      for e in ("sp", "pool", "act")}
        self.gid = 0
        self._reset()

    def _reset(self):
        self.q = {e: [] for e in ENGS}
        self.seen = {e: {} for e in ENGS}
        self.seen_dma = {e: set() for e in ENGS}
        self.dma_hist = {e: {} for e in ENGS}
        self.pending_dma = []
        for b in self.bufs:
            b.w = None
            b.r = []

    def buf(self, name="b"):
        b = Buf(name)
        self.bufs.append(b)
        return b

    def _dep(self, op, d):
        eng = op.eng
        if d is op:
            return
        if d.dma:
            if d.gid in self.seen_dma[eng]:
                return
            self.seen_dma[eng].add(d.gid)
            op.waits.append(d)
        else:
            if d.eng == eng and eng == "pe":
                return
            if self.seen[eng].get(d.eng, -1) >= d.idx:
                return
            self.seen[eng][d.eng] = d.idx
            d.signal = True
            op.waits.append(d)

    capture = None

    def emit(self, eng, fn, reads=(), writes=(), dma=False, ninc=1, est=None, hold=False):
        if self.capture is not None:
            self.capture.append((eng, fn, tuple(reads), tuple(writes), dma, est, hold))
            return None
        op = Op(eng, fn, dma)
        op.ninc = ninc
        op.gid = self.gid
        self.gid += 1
        op.idx = len(self.q[eng])
        deps = []
        for b in reads:
            if b.w is not None:
                deps.append(b.w)
        for b in writes:
            if b.w is not None:
                deps.append(b.w)
            deps.extend(b.r)
        for d in deps:
            self._dep(op, d)
        if dma:
            j = self.ndma[eng]
            self.ndma[eng] += 1
            op.tk = (j % NDMA_SEM, 16 * (j // NDMA_SEM + 1))
            hist = self.dma_hist[eng]
            if (j - NDMA_SEM) in hist:
                self._dep(op, hist[j - NDMA_SEM])
            hist[j] = op
            self.pending_dma.append(op)
        for b in reads:
            if not dma:
                b.r = [x for x in b.r if x.dma or x.eng != eng]
            b.r.append(op)
        for b in writes:
            b.w = op
            b.r = []
        self.q[eng].append(op)
        return op

    def barrier(self):
        pend = self.pending_dma
        self.pending_dma = []
        last = []
        for e in ENGS:
            for op in reversed(self.q[e]):
                if not op.dma and not getattr(op.fn, "_is_nop", False):
                    last.append(op)
                    break
        for e in ENGS:
            fn = lambda en: en.nop()
            op = self.emit(e, fn)
            for d in last:
                self._dep(op, d)
            for d in pend:
                self._dep(op, d)

    def stage_end(self):
        self.barrier()

    def flush(self):
        self.barrier()
        nc = self.nc
        base = dict(self.sigcount)
        for e in ENGS:
            c = base[e]
            for op in self.q[e]:
                if op.signal:
                    assert (c % SEM_WRAP) + op.ninc <= SEM_WRAP
                    c += op.ninc
                    op.sigval = c
            self.sigcount[e] = c
        sems, dsems = self.sems, self.dsems

        def resolve(d):
            if d.dma:
                return dsems[d.eng][d.tk[0]], d.tk[1]
            v = d.sigval - 1
            assert v // SEM_WRAP < NBANK[d.eng], (d.eng, v)
            return sems[d.eng][v // SEM_WRAP], v % SEM_WRAP + 1

        def run(e, engobj):
            for op in self.q[e]:
                for d in op.waits:
                    s, v = resolve(d)
                    engobj.wait_ge(s, v)
                ins = op.fn(engobj)
                if op.dma:
                    ins.then_inc(dsems[e][op.tk[0]], 16)
                elif op.signal:
                    v = op.sigval - 1
                    assert v // SEM_WRAP < NBANK[e], (e, v)
                    ins.then_inc(sems[e][v // SEM_WRAP], 1)

        with nc.Block() as block:
            @block.tensor
            def _(t):
                run("pe", t)

            @block.vector
            def _(v):
                run("dve", v)

            @block.scalar
            def _(s):
                run("act", s)

            @block.gpsimd
            def _(g):
                run("pool", g)

            @block.sync
            def _(sp):
                run("sp", sp)
        self._reset()


class Tl:
    def __init__(self, t, b):
        self.t = t
        self.b = b

    def __getitem__(self, k):
        return self.t[k]


class _Stop(Exception):
    pass


class Builder:
    def __init__(self, NS, T, stop=None, dbg=False):
        self.stop, self.dbg = stop, dbg
        self.NS, self.T = NS, T
        self.QS = min(512, T)
        self.NQ = T // self.QS
        self.TT = T // 128
        self.NCH = T // L
        self.CQ = self.QS // L
        self.CAP = 2 * T // NEXP
        self.SB = min(128, self.CAP)
        self.NB = self.CAP // self.SB
        self.nc = bass.Bass("TRN2", target_bir_lowering=False)
        self.stack = ExitStack()
        self.P = Prog(self.nc, self.stack)
        self.psi = 0

    def sb(self, st, name, shape, dt):
        self.nsb = getattr(self, "nsb", 0) + 1
        name = f"{name}__{self.nsb}"
        t = st.enter_context(self.nc.sbuf_tensor(name, list(shape), dt))
        return Tl(t, self.P.buf(name))

    def chk(self, k):
        if self.stop == k:
            self.P.flush()
            raise _Stop()

    def dram_in(self, name, shape, dt=F32):
        return self.nc.dram_tensor(name, list(shape), dt, kind="ExternalInput").ap()

    def nps(self, grp=None):
        if grp is None:
            p = self.ps[self.psi % 8]
            self.psi += 1
            return p
        banks = {"chain": (0, 1, 2), "post": (3,), "A": (4, 5), "B": (6, 7), "attO0": (0,), "attD0": (1,), "attO1": (2,), "attD1": (3,),
                 "attS0": (4, 5), "attS1": (6, 7), "h0": (0, 1, 2, 3), "h1": (4, 5, 6, 7)}[grp]
        self.psg = getattr(self, "psg", {})
        i = self.psg.get(grp, 0)
        self.psg[grp] = i + 1
        return self.ps[banks[i % len(banks)]]

    def mm(self, out, lhsT, rhs, start=True, stop=True, r=(), w=(), nohold=False):
        self.P.emit("pe", lambda e: e.matmul(out, lhsT=lhsT, rhs=rhs, start=start, stop=stop), r, w,
                    est=max(64, rhs.free_size()) / 2400.0 + 0.004, hold=(not stop) and not nohold)

    def tr(self, out, in_, ident, r=(), w=()):
        self.P.emit("pe", lambda e: e.transpose(out=out, in_=in_, identity=ident), r, w, est=0.06)

    def act(self, out, in_, func, bias=None, scale=None, accum=None, r=(), w=()):
        kw = {}
        if bias is not None:
            kw["bias"] = bias
        if scale is not None:
            kw["scale"] = scale
        if accum is not None:
            kw["accum_out"] = accum
        self.P.emit("act", lambda e: e.activation(out=out, in_=in_, func=func, **kw), r, w, ninc=1,
                    est=(224 + out.free_size()) / 1200.0)

    def tt(self, eng, out, in0, in1, op, r=(), w=()):
        self.P.emit(eng, lambda e: e.tensor_tensor(out=out, in0=in0, in1=in1, op=op), r, w, est=self._est(eng, out))

    def ts(self, eng, out, in0, s1, s2, op0, op1=None, r=(), w=()):
        if op1 is None:
            self.P.emit(eng, lambda e: e.tensor_scalar(out=out, in0=in0, scalar1=s1, scalar2=None, op0=op0), r, w,
                        est=self._est(eng, out))
        else:
            self.P.emit(eng, lambda e: e.tensor_scalar(out=out, in0=in0, scalar1=s1, scalar2=s2, op0=op0, op1=op1), r, w,
                        est=self._est(eng, out))

    def stt(self, out, in0, scalar, in1, op0, op1, r=(), w=()):
        self.P.emit("dve", lambda e: e.scalar_tensor_tensor(out=out, in0=in0, scalar=scalar, in1=in1, op0=op0, op1=op1), r, w,
                    est=self._est("dve", out))

    def cp(self, eng, out, in_, r=(), w=()):
        if eng == "act":
            self.P.emit("act", lambda e: e.activation(out=out, in_=in_, func=AF.Copy), r, w, est=(224 + out.free_size()) / 1200.0)
        else:
            self.P.emit(eng, lambda e: e.tensor_copy(out=out, in_=in_), r, w, est=self._est(eng, out))

    def ms(self, eng, ap, val, w=()):
        self.P.emit(eng, lambda e: e.memset(ap, val), (), w, est=self._est(eng, ap))

    def _est(self, eng, out):
        n = out.free_size()
        if eng == "pool":
            return (150 + 1.6 * n) / 1200.0
        return (100 + n) / 960.0

    def interleave(self, gens, window=None):
        P = self.P
        allg = [g for g in gens if g is not None]
        if window is None:
            window = len(allg)
        pending = allg[window:]

        def mk(f, slot):
            return {"g": (f(slot) if callable(f) else f), "q": [], "done": False}
        streams = [mk(g, i_) for i_, g in enumerate(allg[:window])]
        eng_free = getattr(self, "_sim_eng", None)
        if eng_free is None:
            eng_free = self._sim_eng = {e: 0.0 for e in ENGS}
            self._sim_ready = {}
            self._sim_lastrd = {}
        ready, lastrd = self._sim_ready, self._sim_lastrd

        def fill(st_):
            while not st_["q"] and not st_["done"]:
                P.capture = st_["q"]
                try:
                    next(st_["g"])
                except StopIteration:
                    st_["done"] = True
                finally:
                    P.capture = None

        def start_time(o):
            eng, fn, rd, wr, dma, est, hold = o
            t = eng_free[eng]
            for b in rd:
                t = max(t, ready.get(b, (0.0, None))[0] + (0.15 if ready.get(b, (0.0, eng))[1] != eng else 0.05))
            for b in wr:
                t = max(t, ready.get(b, (0.0, None))[0] + 0.05, lastrd.get(b, 0.0) + 0.1)
            return t

        forced = None
        while True:
            for st_ in streams:
                fill(st_)
            for i_, st_ in enumerate(streams):
                if st_["done"] and not st_["q"] and pending:
                    streams[i_] = mk(pending.pop(0), i_)
                    fill(streams[i_])
            live = [st_ for st_ in streams if st_["q"]]
            if not live:
                break
            if forced is not None and forced["q"]:
                best = forced
            else:
                best = min(live, key=lambda st_: start_time(st_["q"][0]))
            o = best["q"].pop(0)
            eng, fn, rd, wr, dma, est, hold = o
            forced = best if hold else None
            t0 = start_time(o)
            dur = est if est is not None else 0.3
            if dma:
                eng_free[eng] = t0 + 0.06
                tend = t0 + 2.5
            else:
                eng_free[eng] = t0 + dur
                tend = t0 + dur
            for b in rd:
                lastrd[b] = max(lastrd.get(b, 0.0), tend)
            for b in wr:
                ready[b] = (tend, eng)
                lastrd[b] = 0.0
            P.emit(eng, fn, rd, wr, dma=dma)

    def dma(self, out, in_, r=(), w=(), eng="sp"):
        return self.P.emit(eng, lambda e: e.dma_start(out=out, in_=in_), r, w, dma=True)

    def load_w(self, dst, dst_b, src, kc=None, n=None, scale=None):
        assert scale is None
        self.dma(dst, src, w=[dst_b], eng="pool")

    def build(self):
        nc, P, NS, T = self.nc, self.P, self.NS, self.T
        QS, NQ, TT, NCH, CQ = self.QS, self.NQ, self.TT, self.NCH, self.CQ
        st0 = self.stack
        din = self.dram_in
        x = din("x", [NS * T, D])
        w_in = din("w_in", [D, 4736]).rearrange("(k p) n -> p k n", p=128)
        gmixr_d = din("gmixr", [1, D])
        g2r_d = din("g2r", [1, D])
        mu_d = din("mu", [128, 15, 2])
        pp_d = din("pp", [128, 4, 9])
        sink_d = din("sink", [1, 8])
        g2_d = din("g2", [128, 8])
        gF_d = din("gF", [1, D])
        wup_d = din("wup", [128, 512])
        aup_d = din("aup", [128, 512])
        gup_d = din("gup", [128, 512])
        wpr_d = din("wpr", [512, D]).rearrange("(k p) n -> p k n", p=128)
        wpa_d = din("wpa", [512, D]).rearrange("(k p) n -> p k n", p=128)
        wout_d = din("wout", [D, D]).rearrange("(k p) n -> p k n", p=128)
        wr_d = din("wr", [D, NEXP]).rearrange("(k p) n -> p k n", p=128)
        eg_d = din("eg", [NEXP, D, D])
        eu_d = din("eu", [NEXP, D, D])
        ed_d = din("ed", [NEXP, D, D])
        c_idf = din("c_idf", [128, 128])
        c_bo = din("c_bo", [128, 128])
        c_ba = din("c_ba", [128, 128])
        c_mT = din("c_mT", [128, 2, 512])
        c_mN = din("c_mN", [128, 2, 512])
        c_idr = din("c_idr", [128, 512])
        c_bias = din("c_bias", [128, 6, 512])
        c_offs = din("c_offs", [128, 128])
        y = nc.dram_tensor("y", [NS * T, D], F32, kind="ExternalOutput").ap()
        ik = "ExternalOutput" if self.dbg else "Internal"
        uS = nc.dram_tensor("uS", [15, 128, T], BF16, kind=ik).ap()
        acc = nc.dram_tensor("acc", [NS * T, D], F32, kind=ik).ap()
        h2d = nc.dram_tensor("h2d", [NS * T, D], BF16, kind="Internal").ap()
        hTd = nc.dram_tensor("hTd", [128, 8, T], BF16, kind="Internal").ap()
        B_hTd = P.buf("hTd")
        if self.dbg:
            self.dbgR = nc.dram_tensor("dbgR", [128, 4, T], BF16, kind="ExternalOutput").ap()
            self.dbgA = nc.dram_tensor("dbgA", [128, 4, T], BF16, kind="ExternalOutput").ap()
        B_uS = [P.buf(f"uS{c}") for c in range(15)]
        B_acc = [P.buf(f"acc{s}") for s in range(NS)]
        B_h2d = [P.buf(f"h2d{s}") for s in range(NS)]

        self.ps = []
        for i in range(8):
            t = st0.enter_context(nc.psum_tensor(f"ps{i}", [128, 512], F32))
            self.ps.append(Tl(t, P.buf(f"ps{i}")))

        sb = self.sb
        idf = sb(st0, "idf", [128, 128], F32)
        idb = sb(st0, "idb", [128, 128], BF16)
        bo = sb(st0, "bo", [128, 128], F32)
        ba = sb(st0, "ba", [128, 128], F32)
        onesb = sb(st0, "onesb", [128, 128], BF16)
        bob = sb(st0, "bob", [128, 128], BF16)
        bab = sb(st0, "bab", [128, 128], BF16)
        mu = sb(st0, "mu", [128, 15, 2], F32)
        mu0 = sb(st0, "mu0", [128, 15], F32)
        pp = sb(st0, "pp", [128, 4, 9], F32)
        omka = sb(st0, "omka", [128, 4], F32)
        npp = sb(st0, "npp", [128, 4, 9], F32)
        onesf = sb(st0, "onesf", [128, 1], F32)
        g2 = sb(st0, "g2", [128, 8], F32)
        wrt = sb(st0, "wrt", [128, 8, NEXP], F32)
        affT = sb(st0, "affT", [128, T], F32)
        self.wst = [sb(st0, f"wst{i}", [128, 8 * NEXP], F32) for i in range(1)]
        self.wsi = 0
        PW0F, PW0B, PA0F, PA0B, PKK, PKA, PRK, PLW, PLB = range(9)

        for (dst, src) in ((idf, c_idf), (bo, c_bo), (ba, c_ba), (mu, mu_d), (pp, pp_d), (g2, g2_d)):
            self.dma(dst.t[:], src, w=[dst.b])
        self.cp("pool", idb[:], idf[:], r=[idf.b], w=[idb.b])
        self.ms("pool", onesb[:], 1.0, w=[onesb.b])
        self.cp("pool", bob[:], bo[:], r=[bo.b], w=[bob.b])
        self.cp("pool", bab[:], ba[:], r=[ba.b], w=[bab.b])
        self.tt("dve", mu0[:], mu[:, :, 0], mu[:, :, 1], ALU.add, r=[mu.b], w=[mu0.b])
        self.ts("dve", mu0[:], mu0[:], -1.0, 1.0, ALU.mult, ALU.add, r=[mu0.b], w=[mu0.b])
        self.ts("dve", omka[:], pp[:, :, PKA], -1.0, 1.0, ALU.mult, ALU.add, r=[pp.b], w=[omka.b])
        self.ts("dve", npp[:], pp[:], -1.0, None, ALU.mult, r=[pp.b], w=[npp.b])
        self.ms("pool", onesf[:], 1.0, w=[onesf.b])
        self.dma(self.wst[0].t[:, 0:8 * NEXP].rearrange("p (k n) -> p k n", k=8), wr_d, w=[self.wst[0].b])
        self.tt("dve", wrt[:], self.wst[0].t[:, 0:8 * NEXP].rearrange("p (k n) -> p k n", k=8),
                g2.t[:].unsqueeze(2).to_broadcast([128, 8, NEXP]),
                ALU.mult, r=[self.wst[0].b, g2.b], w=[wrt.b])
        self.ms("pool", affT[:], 0.0, w=[affT.b])
        self.yRk = sb(st0, "yRk", [128, 4, T], BF16)
        P.stage_end()

        env = locals()
        try:
            self.chk(0)
            for s in range(NS):
                self.phase_a(s, env)
            self.phase_b(env)
            self.chk(6)
            self.phase_c(env)
        except _Stop:
            return nc
        self.P.flush()
        print("signals", self.P.sigcount, "dmas", self.P.ndma)
        self.stack.close()
        return nc

    def norm_hT(self, st, s, env, hT):
        T, TT = self.T, self.TT
        x = env["x"]
        idb = env["idb"]
        gbc = self.sb(st, "gbc", [128, D], F32)
        self.dma(gbc.t[:], env["gmixr_d"].partition_broadcast(128), w=[gbc.b])
        xts = [self.sb(st, f"xt{i}", [128, D], F32) for i in range(2)]
        hns = [self.sb(st, f"hn{i}", [128, D], BF16) for i in range(2)]
        junk = self.sb(st, "junk", [128, D], F32)
        sst = [self.sb(st, f"ss{i}", [128, 4], F32) for i in range(2)]
        for tt_ in range(TT):
            xt, hn, ss = xts[tt_ % 2], hns[tt_ % 2], sst[tt_ % 2]
            r0 = s * T + tt_ * 128
            self.dma(xt.t[:], x[r0:r0 + 128, :], w=[xt.b])
            self.chk(20)
            self.act(junk[:], xt[:], AF.Square, accum=ss[:, 0:1], r=[xt.b], w=[junk.b, ss.b])
            self.chk(21)
            self.ts("dve", ss[:, 1:2], ss[:, 0:1], 1.0 / D, NORM_EPS, ALU.mult, ALU.add, r=[ss.b], w=[ss.b])
            self.act(ss[:, 2:3], ss[:, 1:2], AF.Sqrt, r=[ss.b], w=[ss.b])
            self.chk(22)
            self.P.emit("dve", lambda e, o=ss[:, 3:4], i=ss[:, 2:3]: e.reciprocal(out=o, in_=i), [ss.b], [ss.b])
            self.stt(hn[:], xt[:], ss[:, 3:4], gbc[:], ALU.mult, ALU.mult, r=[xt.b, ss.b, gbc.b], w=[hn.b])
            self.chk(23)
            ps = self.nps()
            psv = ps.t.bitcast(BF16)
            for kc in range(8):
                self.tr(psv[:, kc * 128:(kc + 1) * 128], hn[:, kc * 128:(kc + 1) * 128], idb[:], r=[hn.b, idb.b], w=[ps.b])
            self.chk(24)
            import os
            if os.environ.get("VARX") == "1":
                self.cp("dve", junk.t[:].bitcast(BF16)[:, 0:1024], psv[:, :], r=[ps.b], w=[junk.b])
            elif os.environ.get("VARX") == "2":
                self.cp("dve", hT[:, 0, 0:128], psv[:, 0:128], r=[ps.b], w=[hT.b])
            else:
                self.cp("dve", hT[:, :, tt_ * 128:(tt_ + 1) * 128], psv[:, :].rearrange("p (k n) -> p k n", k=8), r=[ps.b], w=[hT.b])
            self.chk(25)

    def inproj(self, hT, wb, col0, tq, reads, ps):
        QS = self.QS
        for kc in range(8):
            self.mm(ps[:, 0:QS], wb[:, kc, col0:col0 + 128], hT[:, kc, tq * QS:(tq + 1) * QS],
                    start=(kc == 0), stop=(kc == 7), r=reads, w=[ps.b])

    def phase_a(self, s, env):
        P, T, QS, NQ, TT, NCH, CQ = self.P, self.T, self.QS, self.NQ, self.TT, self.NCH, self.CQ
        w_in, mu, mu0, pp, omka = env["w_in"], env["mu"], env["mu0"], env["pp"], env["omka"]
        uS, B_uS = env["uS"], env["B_uS"]
        PW0F, PW0B, PA0F, PA0B, PKK, PKA, PRK, PLW, PLB = range(9)
        sb = self.sb

        with ExitStack() as st:
            hT = sb(st, "hT", [128, 8, T], BF16)
            self.norm_hT(st, s, env, hT)
            self.dma(env["hTd"], hT[:], r=[hT.b], w=[env["B_hTd"]])
            self.chk(10)
            wbs = [sb(st, f"wb{i}", [128, 8, 512], BF16) for i in range(2)]
            upad = sb(st, "upad", [128, T + 2], BF16)
            tmp = sb(st, "tmpA", [128, T], BF16)
            uss = [sb(st, f"us{i}", [128, T], BF16) for i in range(2)]
            self.ms("pool", upad[:, 0:1], 0.0, w=[upad.b])
            self.ms("pool", upad[:, T + 1:T + 2], 0.0, w=[upad.b])
            for grp in range(4):
                c0 = grp * 4
                ncol = min(4, 15 - c0)
                wb = wbs[grp % 2]
                self.load_w(wb[:, :, 0:ncol * 128], wb.b, w_in[:, :, c0 * 128:(c0 + ncol) * 128], 8, ncol * 128)
                self.chk(11)
                for ci in range(ncol):
                    c = c0 + ci
                    us = uss[c % 2]
                    for tq in range(NQ):
                        ps = self.nps()
                        self.inproj(hT, wb, ci * 128, tq, [wb.b, hT.b], ps)
                        self.cp("act", upad[:, 1 + tq * QS:1 + (tq + 1) * QS], ps[:, 0:QS], r=[ps.b], w=[upad.b])
                    self.ts("dve", tmp[:], upad[:, 1:T + 1], mu0[:, c:c + 1], None, ALU.mult, r=[upad.b, mu0.b], w=[tmp.b])
                    self.stt(tmp[:], upad[:, 0:T], mu[:, c, 0:1], tmp[:], ALU.mult, ALU.add, r=[upad.b, mu.b, tmp.b], w=[tmp.b])
                    self.stt(us[:], upad[:, 2:T + 2], mu[:, c, 1:2], tmp[:], ALU.mult, ALU.add, r=[upad.b, mu.b, tmp.b], w=[us.b])
                    if c == 12:
                        self.act(us[:], us[:], AF.Sigmoid, r=[us.b], w=[us.b])
                    elif c == 13:
                        self.act(us[:], us[:], AF.Tanh, r=[us.b], w=[us.b])
                    self.dma(uS[c], us[:], r=[us.b], w=[B_uS[c]])
                    self.chk(12)
            P.stage_end()
        self.chk(1)

        with ExitStack() as st:
            self.rwkv(st, s, env, self.yRk)
            if self.dbg and s == 0:
                self.dma(self.dbgR, self.yRk[:], r=[self.yRk.b])
            P.stage_end()
        self.chk(2)

        with ExitStack() as st:
            yA = sb(st, "yA", [128, 4, T], BF16)
            mrg = sb(st, "mrg", [128, 8, T], BF16)
            with ExitStack() as st1:
                hT = sb(st1, "hT", [128, 8, T], BF16)
                self.dma(hT.t[:], env["hTd"], r=[env["B_hTd"]], w=[hT.b])
                with ExitStack() as st2:
                    self.attention(st2, s, env, hT, yA)
                    if self.dbg and s == 0:
                        self.dma(self.dbgA, yA[:], r=[yA.b])
                    P.stage_end()
                self.chk(3)
                self.merge_gates(st1, s, env, hT, yA, mrg)
                P.stage_end()
                self.chk(4)
            with ExitStack() as st1:
                self.out_proj(st1, s, env, mrg)
                P.stage_end()
            self.chk(5)

    def rwkv(self, st, s, env, yR):
        P, T, NCH = self.P, self.T, self.NCH
        QS = min(512, T)
        NQ = T // QS
        CQ = QS // L
        pp, omka, npp, onesf = env["pp"], env["omka"], env["npp"], env["onesf"]
        uS, B_uS = env["uS"], env["B_uS"]
        bo, ba, idb, bob, bab = env["bo"], env["ba"], env["idb"], env["bob"], env["bab"]
        PW0F, PW0B, PA0F, PA0B, PKK, PKA, PRK, PLW, PLB = range(9)
        sb = self.sb
        mT = sb(st, "mT", [128, 2, 512], F32)
        mN = sb(st, "mN", [128, 2, 512], F32)
        idr = sb(st, "idr", [128, 512], F32)
        self.dma(mT.t[:], env["c_mT"], w=[mT.b])
        self.dma(mN.t[:], env["c_mN"], w=[mN.b])
        self.dma(idr.t[:], env["c_idr"], w=[idr.b])
        wup = sb(st, "wup", [128, 512], BF16)
        aup = sb(st, "aup", [128, 512], BF16)
        gup = sb(st, "gup", [128, 512], BF16)
        for dst, src in ((wup, env["wup_d"]), (aup, env["aup_d"]), (gup, env["gup_d"])):
            self.load_w(dst[:], dst.b, src)
        NQ_ = NQ
        bonus = [sb(st, f"bonus{i}", [128, T], BF16) for i in range(2)]
        wkv = [sb(st, f"wkv{i}", [128, T], BF16) for i in range(2)]
        vtok = [sb(st, f"vtok{i}", [128, NCH, L], BF16) for i in range(2)]
        for t_ in vtok:
            t_.bq = [P.buf("vtokq") for _ in range(NQ_)]
        R = []
        for d in range(2):
            Rd = dict(
                ar=sb(st, f"ar{d}", [128, NCH, 2, L], BF16),
                SB=sb(st, f"SB{d}", [128, NCH, L], BF16),
                SK=sb(st, f"SK{d}", [128, NCH, 2, L], BF16),
                TTm=sb(st, f"TT{d}", [128, NCH, L], BF16),
                BB=sb(st, f"BB{d}", [128, NCH, L], BF16),
                KB=sb(st, f"KB{d}", [128, NCH, L], BF16),
                Wtot=sb(st, f"Wtot{d}", [128, NCH], F32),
                ST=sb(st, f"ST{d}", [128, L], BF16),
                Xs=sb(st, f"Xs{d}", [128, L], BF16),
                Us=sb(st, f"Us{d}", [128, L], BF16),
            )
            for k_ in ("ar", "SB", "SK", "TTm", "BB", "KB", "Wtot"):
                Rd[k_].bq = [P.buf(k_ + "q") for _ in range(NQ_)]
            R.append(Rd)

        def q(name, dt=F32):
            return sb(st, name, [128, QS], dt)
        def mk_temps(sfx):
            names_f = ["t2", "sgw", "cs"] + (["X1"] if sfx == "A" else ["X2", "X3"])
            names_b = ["rq", "kq", "vb", "twb", "alb", "kkn", "sq", "t1", "t2b", "t3", "kft", "aqt", "E", "akk", "p1", "p2",
                       "bT", "kT", "Pm", "PTm", "Pm2", "PTm2"]
            d_ = {n: q(n + sfx) for n in names_f}
            d_.update({n: q(n + sfx, BF16) for n in names_b})
            for n in ("X1", "X2", "X3"):
                d_.setdefault(n, None)
            return d_
        TA, TB = mk_temps("A"), mk_temps("B")
        TA["grp"], TB["grp"] = "A", "B"
        sgb, t1, t2, sq = q("sgbP", BF16), q("t1P"), q("t2P"), q("sqP")
        scm = sb(st, "scm", [128, QS], F32)
        self.ms("pool", scm[:], 1.0, w=[scm.b])
        self.ms("pool", scm.t[:].rearrange("p (c l) -> p c l", l=L)[:, :, 0:1], 0.0, w=[scm.b])

        def v3(ap):
            return ap.rearrange("p (c l) -> p c l", l=L)

        def pre_pass(p, d, tq, first, tmp):
            (rq, kq, vb, twb, alb, kkn, sq, t1, t2, t2b, t3, kft, aqt, sgw, cs, X1, X2, X3, E, akk, p1, p2,
             bT, kT, Pm, PTm, Pm2, PTm2) = (tmp[n] for n in (
                "rq", "kq", "vb", "twb", "alb", "kkn", "sq", "t1", "t2", "t2b", "t3", "kft", "aqt", "sgw", "cs", "X1", "X2", "X3",
                "E", "akk", "p1", "p2", "bT", "kT", "Pm", "PTm", "Pm2", "PTm2"))
            par = p % 2
            pc = slice(p * 128, (p + 1) * 128)
            ts_ = slice(tq * QS, (tq + 1) * QS)
            cq0 = tq * CQ
            csl = slice(cq0, cq0 + CQ)
            Rd = R[d]
            hs_lo = slice(64 * d, 64 * d + 64)
            for dst, c in ((rq, p), (kq, 4 + p), (vb, 8 + p), (twb, 13), (alb, 14)):
                self.dma(dst.t[:], uS[c][:, ts_], r=[B_uS[c]], w=[dst.b])
            yield
            self.ts("dve", t1[:], kq[:], pp[:, p, PKK:PKK + 1], None, ALU.mult, r=[kq.b, pp.b], w=[t1.b])
            self.tt("pool", sq[:], t1[:], t1[:], ALU.mult, r=[t1.b], w=[sq.b])
            ps = self.nps(tmp["grp"])
            self.mm(ps[:, 0:QS], bob[:], sq[:], r=[bob.b, sq.b], w=[ps.b])
            self.ts("dve", t2[:], ps[:, 0:QS], 1e-24, None, ALU.max, r=[ps.b], w=[t2.b])
            self.act(t2[:], t2[:], AF.Ln, r=[t2.b], w=[t2.b])
            self.act(t2b[:], t2[:], AF.Exp, scale=-0.5, r=[t2.b], w=[t2b.b])
            self.tt("dve", kkn[:], t1[:], t2b[:], ALU.mult, r=[t1.b, t2b.b], w=[kkn.b])
            yield
            ps = self.nps(tmp["grp"])
            self.mm(ps[:, 0:QS], aup[hs_lo, pc], alb[hs_lo, :], r=[aup.b, alb.b], w=[ps.b])
            self.act(t2[:], ps[:, 0:QS], AF.Exp, scale=-1.0, bias=npp[:, p, PA0F + d:PA0F + d + 1], r=[ps.b, npp.b], w=[t2.b])
            self.act(t2[:], t2[:], AF.Ln, bias=onesf[:, 0:1], r=[t2.b, onesf.b], w=[t2.b])
            self.act(aqt[:], t2[:], AF.Exp, scale=-1.0, r=[t2.b], w=[aqt.b])
            self.ts("dve", t3[:], aqt[:], pp[:, p, PKA:PKA + 1], omka[:, p:p + 1], ALU.mult, ALU.add,
                    r=[aqt.b, pp.b, omka.b], w=[t3.b])
            self.tt("dve", kft[:], kq[:], t3[:], ALU.mult, r=[kq.b, t3.b], w=[kft.b])
            yield
            self.stt(t3[:], kft[:], pp[:, p, PRK:PRK + 1], rq[:], ALU.mult, ALU.mult, r=[kft.b, pp.b, rq.b], w=[t3.b])
            ps = self.nps(tmp["grp"])
            self.mm(ps[:, 0:QS], bob[:], t3[:], r=[bob.b, t3.b], w=[ps.b])
            if first:
                self.tt("dve", bonus[par][:, ts_], ps[:, 0:QS], vb[:], ALU.mult, r=[ps.b, vb.b], w=[bonus[par].b])
            else:
                self.tt("dve", t3[:], ps[:, 0:QS], vb[:], ALU.mult, r=[ps.b, vb.b], w=[t3.b])
                self.tt("pool", bonus[par][:, ts_], bonus[par][:, ts_], t3[:], ALU.add, r=[t3.b, bonus[par].b], w=[bonus[par].b])
            yield
            if first:
                ps = self.nps(tmp["grp"])
                psv = ps.t.bitcast(BF16)
                for c in range(CQ):
                    for h in range(2):
                        hs = slice(64 * h, 64 * h + 64)
                        self.tr(psv[hs, c * L:(c + 1) * L], vb[hs, c * L:(c + 1) * L], idb[hs, hs], r=[vb.b, idb.b], w=[ps.b])
                self.cp("act", vtok[par][:, csl, :], v3(psv[:, 0:QS]), r=[ps.b], w=[vtok[par].bq[tq]])
                yield
            ps = self.nps(tmp["grp"])
            self.mm(ps[:, 0:QS], wup[hs_lo, pc], twb[hs_lo, :], r=[wup.b, twb.b], w=[ps.b])
            self.act(sgw[:], ps[:, 0:QS], AF.Exp, scale=-1.0, bias=npp[:, p, PW0F + d:PW0F + d + 1], r=[ps.b, npp.b], w=[sgw.b])
            self.act(sgw[:], sgw[:], AF.Ln, bias=onesf[:, 0:1], r=[sgw.b, onesf.b], w=[sgw.b])
            self.act(sgw[:], sgw[:], AF.Exp, scale=-1.0, r=[sgw.b], w=[sgw.b])
            self.P.emit("dve", lambda e, o=cs[:], a=scm[:], b=sgw[:]: e.tensor_tensor_scan(
                out=o, data0=a, data1=b, initial=0.0, op0=ALU.mult, op1=ALU.add), [scm.b, sgw.b], [cs.b], est=(100 + 2 * QS) / 960.0)
            tot = v3(cs[:])[:, :, L - 1:L]
            self.act(Rd["Wtot"][:, csl], tot.rearrange("p c l -> p (c l)"), AF.Exp, scale=-DS, r=[cs.b], w=[Rd["Wtot"].bq[tq]])
            if d == 0:
                self.tt("pool", X1[:], cs[:], sgw[:], ALU.subtract, r=[cs.b, sgw.b], w=[X1.b])
                ce, ci = X1, cs
            else:
                self.tt("dve", v3(X2[:]), tot.to_broadcast([128, CQ, L]), v3(cs[:]), ALU.subtract, r=[cs.b], w=[X2.b])
                self.tt("pool", X3[:], X2[:], sgw[:], ALU.add, r=[X2.b, sgw.b], w=[X3.b])
                ce, ci = X2, X3
            yield
            ar = Rd["ar"]
            arb = ar.bq[tq]
            self.act(E[:], ce[:], AF.Exp, scale=-DS, r=[ce.b], w=[E.b])
            self.stt(ar[:, csl, 0, :], v3(kkn[:]), -1.0, v3(E[:]), ALU.mult, ALU.mult, r=[kkn.b, E.b], w=[arb])
            self.act(p1[:], ci[:], AF.Exp, scale=-DS, r=[ci.b], w=[p1.b])
            self.tt("dve", ar[:, csl, 1, :], v3(rq[:]), v3(p1[:]), ALU.mult, r=[rq.b, p1.b], w=[arb])
            yield
            self.tt("pool", akk[:], aqt[:], kkn[:], ALU.mult, r=[aqt.b, kkn.b], w=[akk.b])
            self.act(p2[:], ci[:], AF.Exp, scale=DS, r=[ci.b], w=[p2.b])
            self.tt("dve", bT[:], akk[:], p2[:], ALU.mult, r=[akk.b, p2.b], w=[bT.b])
            self.tt("dve", kT[:], kft[:], p2[:], ALU.mult, r=[kft.b, p2.b], w=[kT.b])
            yield
            for src, dstk in ((bT, "BB"), (kT, "KB")):
                ps = self.nps(tmp["grp"])
                psv = ps.t.bitcast(BF16)
                for c in range(CQ):
                    for h in range(2):
                        hs = slice(64 * h, 64 * h + 64)
                        self.tr(psv[hs, c * L:(c + 1) * L], src[hs, c * L:(c + 1) * L], idb[hs, hs],
                                r=[src.b, idb.b], w=[ps.b])
                self.cp("act", Rd[dstk][:, csl, :], v3(psv[:, 0:QS]), r=[ps.b], w=[Rd[dstk].bq[tq]])
                yield
            for lhs, dstk in ((bT, "SB"), (kT, "SK")):
                for c4 in range(0, CQ, 4):
                    ps = self.nps(tmp["grp"])
                    for cc in range(4):
                        c = c4 + cc
                        for h in range(2):
                            hs = slice(64 * h, 64 * h + 64)
                            self.mm(ps[hs, cc * 128:(cc + 1) * 128], lhs[hs, c * L:(c + 1) * L],
                                    ar[hs, cq0 + c, :, :].rearrange("p a l -> p (a l)"),
                                    r=[lhs.b, arb], w=[ps.b])
                    if dstk == "SK":
                        self.tt("dve", Rd[dstk][:, cq0 + c4:cq0 + c4 + 4, :, :].rearrange("p c a l -> p (c a l)"),
                                ps[:, :], mT[:, d, :], ALU.mult, r=[ps.b, mT.b], w=[Rd[dstk].bq[tq]])
                    else:
                        ps4 = ps[:, :].rearrange("p (c a l) -> p c a l", c=4, a=2)
                        m4 = mT[:, d, :].rearrange("p (c a l) -> p c a l", c=4, a=2)
                        self.tt("dve", v3(PTm[:])[:, c4:c4 + 4, :], ps4[:, :, 0, :], m4[:, :, 0, :], ALU.mult,
                                r=[ps.b, mT.b], w=[PTm.b])
                        self.tt("dve", Rd["SB"][:, cq0 + c4:cq0 + c4 + 4, :], ps4[:, :, 1, :], m4[:, :, 1, :], ALU.mult,
                                r=[ps.b, mT.b], w=[Rd["SB"].bq[tq]])
                    yield
            ps = self.nps(tmp["grp"])
            for c in range(CQ):
                for h in range(2):
                    hs = slice(64 * h, 64 * h + 64)
                    self.mm(ps[hs, c * L:(c + 1) * L], ar[hs, cq0 + c, 0, :], bT[hs, c * L:(c + 1) * L],
                            r=[arb, bT.b], w=[ps.b])
            self.tt("dve", Pm[:], ps[:, 0:QS], mN[:, d, 0:QS], ALU.mult, r=[ps.b, mN.b], w=[Pm.b])
            TTq = Rd["TTm"][:, csl, :]
            TTb = Rd["TTm"].bq[tq]
            self.tt("pool", TTq, v3(PTm[:]), v3(idr[:, 0:QS]), ALU.add, r=[PTm.b, idr.b], w=[TTb])
            yield
            Pc, PTc, Pn, PTn = Pm, PTm, Pm2, PTm2
            for lvl in range(1, 6):
                psA = self.nps(tmp["grp"])
                for c in range(CQ):
                    for h in range(2):
                        hs = slice(64 * h, 64 * h + 64)
                        cl = slice(c * L, (c + 1) * L)
                        self.mm(psA[hs, cl], PTc[hs, cl], Pc[hs, cl], r=[PTc.b, Pc.b], w=[psA.b])
                self.cp("act", Pn[:], psA[:, 0:QS], r=[psA.b], w=[Pn.b])
                if lvl < 5:
                    psB = self.nps(tmp["grp"])
                    for c in range(CQ):
                        for h in range(2):
                            hs = slice(64 * h, 64 * h + 64)
                            cl = slice(c * L, (c + 1) * L)
                            self.mm(psB[hs, cl], Pc[hs, cl], PTc[hs, cl], r=[PTc.b, Pc.b], w=[psB.b])
                    self.cp("act", PTn[:], psB[:, 0:QS], r=[psB.b], w=[PTn.b])
                yield
                psC = self.nps(tmp["grp"])
                for c in range(CQ):
                    for h in range(2):
                        hs = slice(64 * h, 64 * h + 64)
                        cl = slice(c * L, (c + 1) * L)
                        self.mm(psC[hs, cl], Pn[hs, cl], Rd["TTm"][hs, cq0 + c, :], r=[Pn.b, TTb], w=[psC.b])
                self.tt("dve", TTq, v3(psC[:, 0:QS]), TTq, ALU.add, r=[psC.b, TTb], w=[TTb])
                Pc, PTc, Pn, PTn = Pn, PTn, Pc, PTc
                yield

        def pre_stage(p, k):
            gens = []
            for d, tq, tmp in ((0, k, TA), (1, NQ_ - 1 - k, TB)):
                fstage, bstage = tq, NQ_ - 1 - tq
                first = (fstage <= bstage) if d == 0 else (bstage < fstage)
                gens.append(pre_pass(p, d, tq, first, tmp))
            return gens

        def chain_group(p, k):
            par = p % 2
            if k == 0:
                self.ms("pool", wkv[par][:], 0.0, w=[wkv[par].b])
                for d in range(2):
                    self.ms("pool", R[d]["ST"][:], 0.0, w=[R[d]["ST"].b])
            for step in range(k * CQ, (k + 1) * CQ):
                for d in range(2):
                    Rd = R[d]
                    c = step if d == 0 else NCH - 1 - step
                    qi = c // CQ
                    ar, SBm, SKm, TTm, BB, KB, ST, Xs, Us = (Rd[k_] for k_ in ("ar", "SB", "SK", "TTm", "BB", "KB", "ST", "Xs", "Us"))
                    vt = vtok[par]
                    vtb = vt.bq[qi]
                    psX = self.nps("chain")
                    for h in range(2):
                        hs = slice(64 * h, 64 * h + 64)
                        self.mm(psX[hs, 0:L], ar[hs, c, 0, :], ST[hs, :], start=True, stop=False, r=[ar.bq[qi], ST.b], w=[psX.b])
                        self.mm(psX[hs, 0:L], SKm[hs, c, 0, :], vt[hs, c, :], start=False, stop=True, r=[SKm.bq[qi], vtb], w=[psX.b])
                    self.cp("act", Xs[:], psX[:, 0:L], r=[psX.b], w=[Xs.b])
                    yield
                    psU = self.nps("chain")
                    for h in range(2):
                        hs = slice(64 * h, 64 * h + 64)
                        self.mm(psU[hs, 0:L], TTm[hs, c, :], Xs[hs, :], r=[TTm.bq[qi], Xs.b], w=[psU.b])
                    self.cp("dve", Us[:], psU[:, 0:L], r=[psU.b], w=[Us.b])
                    yield
                    psY = self.nps("chain")
                    for h in range(2):
                        hs = slice(64 * h, 64 * h + 64)
                        self.mm(psY[hs, 0:L], ST[hs, :], ar[hs, c, 1, :], start=True, stop=False, r=[ar.bq[qi], ST.b], w=[psY.b])
                        self.mm(psY[hs, 0:L], Us[hs, :], SBm[hs, c, :], start=False, stop=False, r=[Us.b, SBm.bq[qi]], w=[psY.b])
                        self.mm(psY[hs, 0:L], vt[hs, c, :], SKm[hs, c, 1, :], start=False, stop=True, r=[vtb, SKm.bq[qi]], w=[psY.b])
                    psS = self.nps("chain")
                    for h in range(2):
                        hs = slice(64 * h, 64 * h + 64)
                        self.mm(psS[hs, 0:L], idb[hs, hs], ST[hs, :], start=True, stop=False, r=[idb.b, ST.b], w=[psS.b])
                        self.mm(psS[hs, 0:L], BB[hs, c, :], Us[hs, :], start=False, stop=False, r=[BB.bq[qi], Us.b], w=[psS.b])
                        self.mm(psS[hs, 0:L], KB[hs, c, :], vt[hs, c, :], start=False, stop=True, r=[KB.bq[qi], vtb], w=[psS.b])
                    self.ts("dve", ST[:], psS[:, 0:L], Rd["Wtot"][:, c:c + 1], None, ALU.mult,
                            r=[Rd["Wtot"].bq[qi], psS.b], w=[ST.b])
                    wsl = wkv[par][:, c * L:(c + 1) * L]
                    self.tt("dve", wsl, psY[:, 0:L], wsl, ALU.add, r=[psY.b, wkv[par].b], w=[wkv[par].b])
                    yield

        def post(p):
            par = p % 2
            pc = slice(p * 128, (p + 1) * 128)
            for tq in range(NQ):
                ts_ = slice(tq * QS, (tq + 1) * QS)
                self.dma(sgb.t[:], uS[12][:, ts_], r=[B_uS[12]], w=[sgb.b])
                ps = self.nps("post")
                self.mm(ps[:, 0:QS], bab[:], wkv[par][:, ts_], r=[bab.b, wkv[par].b], w=[ps.b])
                self.tt("dve", t1[:], wkv[par][:, ts_], ps[:, 0:QS], ALU.subtract, r=[wkv[par].b, ps.b], w=[t1.b])
                self.tt("pool", sq[:], t1[:], t1[:], ALU.mult, r=[t1.b], w=[sq.b])
                yield
                ps = self.nps("post")
                self.mm(ps[:, 0:QS], ba[:], sq[:], r=[ba.b, sq.b], w=[ps.b])
                self.ts("dve", t2[:], ps[:, 0:QS], LNX_EPS, None, ALU.add, r=[ps.b], w=[t2.b])
                self.act(t2[:], t2[:], AF.Ln, r=[t2.b], w=[t2.b])
                self.act(t2[:], t2[:], AF.Exp, scale=-0.5, r=[t2.b], w=[t2.b])
                self.tt("dve", t1[:], t1[:], t2[:], ALU.mult, r=[t1.b, t2.b], w=[t1.b])
                yield
                self.ts("dve", t1[:], t1[:], pp[:, p, PLW:PLW + 1], pp[:, p, PLB:PLB + 1], ALU.mult, ALU.add,
                        r=[t1.b, pp.b], w=[t1.b])
                self.tt("pool", t1[:], t1[:], bonus[par][:, ts_], ALU.add, r=[t1.b, bonus[par].b], w=[t1.b])
                ps = self.nps("post")
                self.mm(ps[:, 0:QS], gup[:, pc], sgb[:], r=[gup.b, sgb.b], w=[ps.b])
                self.tt("dve", yR[:, p, ts_], t1[:], ps[:, 0:QS], ALU.mult, r=[t1.b, ps.b], w=[yR.b])
                yield

        def run(g):
            for _ in g:
                pass

        interleave = self.interleave

        interleave(pre_stage(0, 0))
        pending_post = None
        for p in range(4):
            for k in range(NQ_):
                nxt = None
                if k + 1 < NQ_:
                    nxt = pre_stage(p, k + 1)
                elif p + 1 < 4:
                    nxt = pre_stage(p + 1, 0)
                if nxt is not None and NQ_ > 1:
                    gens = [chain_group(p, k)] + nxt
                    if k == 0 and pending_post is not None:
                        gens.append(pending_post)
                        pending_post = None
                    interleave(gens)
                else:
                    if pending_post is not None:
                        run(pending_post)
                        pending_post = None
                    run(chain_group(p, k))
                    if nxt is not None:
                        interleave(nxt)
            pending_post = post(p)
        run(pending_post)

    def attention(self, st, s, env, hT, yA):
        P, T, QS, NQ, TT = self.P, self.T, self.QS, self.NQ, self.TT
        w_in, idb, onesb = env["w_in"], env["idb"], env["onesb"]
        sb = self.sb
        bias = sb(st, "bias", [128, 6, 512], BF16)
        self.load_w(bias[:], bias.b, env["c_bias"])
        snk = sb(st, "snk", [128, 8], F32)
        esk = sb(st, "esk", [128, 2, 2, 128], F32)
        self.dma(snk.t[:], env["sink_d"].partition_broadcast(128), w=[snk.b])
        self.act(snk[:], snk[:], AF.Exp, r=[snk.b], w=[snk.b])
        for kv in range(2):
            for gh in range(2):
                for gl in range(2):
                    hs = slice(64 * gl, 64 * gl + 64)
                    col = kv * 4 + 2 * gh + gl
                    self.cp("dve", esk[hs, kv, gh, :], snk[hs, col:col + 1].to_broadcast([64, 128]), r=[snk.b], w=[esk.b])
        qT = sb(st, "qT", [128, TT, 4, 128], BF16)
        kTz = [sb(st, f"kTz{i}", [128, T], BF16) for i in range(2)]
        vtk = sb(st, "vtk", [128, TT, 128], BF16)
        wq = sb(st, "wq", [128, 8, 512], BF16)
        wkv_ = sb(st, "wkvw", [128, 8, 256], BF16)
        c0 = 1920
        wq5 = wq.t[:].rearrange("p k (g kv d) -> p k g kv d", g=4, kv=2)
        for i in range(2):
            for kc in range(8):
                self.load_w(wq5[:, kc, :, i, :], wq.b,
                            w_in[:, kc, c0 + i * 256:c0 + (i + 1) * 256].rearrange("p (g d) -> p g d", g=4))
        self.load_w(wkv_[:], wkv_.b, w_in[:, :, c0 + 512:c0 + 768], 8, 256)
        for i in range(2):
            self.ms("pool", kTz[i][:], 0.0, w=[kTz[i].b])
        for tq in range(NQ):
            ts_ = slice(tq * QS, (tq + 1) * QS)
            for g in range(4):
                ps = self.nps()
                for kc in range(8):
                    self.mm(ps[:, 0:QS], wq[:, kc, g * 128:(g + 1) * 128], hT[:, kc, ts_], start=(kc == 0), stop=(kc == 7),
                            r=[wq.b, hT.b], w=[ps.b])
                sg_ = (g % 2) * 2 + g // 2
                nb_ = QS // 128
                self.act(qT[:, tq * nb_:(tq + 1) * nb_, sg_, :], ps[:, 0:QS].rearrange("p (b q) -> p b q", q=128), AF.Copy, scale=0.125,
                         r=[ps.b], w=[qT.b])
            ps = self.nps()
            self.inproj(hT, wkv_, 0, tq, [wkv_.b, hT.b], ps)
            for kv in range(2):
                hs = slice(64 * kv, 64 * kv + 64)
                self.cp("act", kTz[kv][hs, ts_], ps[hs, 0:QS], r=[ps.b], w=[kTz[kv].b])
        for tt_ in range(TT):
            ps = self.nps()
            for kc in range(8):
                self.mm(ps[:, 0:128], hT[:, kc, tt_ * 128:(tt_ + 1) * 128], wkv_[:, kc, 128:256], start=(kc == 0), stop=(kc == 7),
                        r=[wkv_.b, hT.b], w=[ps.b])
            self.cp("act", vtk[:, tt_, :], ps[:, 0:128], r=[ps.b], w=[vtk.b])
        pTs = [[sb(st, f"pT{j}_{i}", [128, 512], BF16) for i in range(3)] for j in range(2)]
        dens = [sb(st, f"den{j}", [128, 256], F32) for j in range(2)]

        def att_iter(kv, qb, par):
            den = dens[par]
            kbs = [kb for kb in (qb - 1, qb, qb + 1) if 0 <= kb < TT]
            psO = self.nps(f"attO{par}")
            psD = self.nps(f"attD{par}")
            for ki, kb in enumerate(kbs):
                rel = kb - qb + 1
                psS = self.nps(f"attS{par}")
                pT = pTs[par][ki]
                self.mm(psS[:, :], kTz[kv][:, kb * 128:(kb + 1) * 128], qT[:, qb, :, :].rearrange("p g q -> p (g q)"),
                        start=True, stop=False, r=[kTz[kv].b, qT.b], w=[psS.b])
                self.mm(psS[:, :], idb[:], bias[:, rel * 2 + kv, :], start=False, stop=True, r=[idb.b, bias.b], w=[psS.b])
                self.act(pT[:], psS[:, :], AF.Exp, r=[psS.b], w=[pT.b])
                yield
                for gl in range(2):
                    hs = slice(64 * gl, 64 * gl + 64)
                    self.mm(psO[hs, 0:256], vtk[:, kb, 64 * kv:64 * kv + 64], pT[:, gl * 256:(gl + 1) * 256],
                            start=(ki == 0), stop=(ki == len(kbs) - 1), r=[vtk.b, pT.b], w=[psO.b], nohold=True)
                    self.mm(psD[hs, 0:256], onesb[:, 0:64], pT[:, gl * 256:(gl + 1) * 256],
                            start=(ki == 0), stop=(ki == len(kbs) - 1), r=[onesb.b, pT.b], w=[psD.b], nohold=True)
                yield
            self.tt("dve", den[:], psD[:, 0:256], esk[:, kv, :, :].rearrange("p a q -> p (a q)"), ALU.add,
                    r=[psD.b, esk.b], w=[den.b])
            self.act(den[:], den[:], AF.Ln, r=[den.b], w=[den.b])
            self.act(den[:], den[:], AF.Exp, scale=-1.0, r=[den.b], w=[den.b])
            yield
            self.tt("dve", yA[:, 2 * kv:2 * kv + 2, qb * 128:(qb + 1) * 128],
                    psO[:, 0:256].rearrange("p (a q) -> p a q", a=2), den[:].rearrange("p (a q) -> p a q", a=2),
                    ALU.mult, r=[psO.b, den.b], w=[yA.b])
            yield

        its = [(kv, qb) for kv in range(2) for qb in range(TT)]
        self.interleave([(lambda slot, kv=kv, qb=qb: att_iter(kv, qb, slot)) for (kv, qb) in its], window=2)

    def merge_gates(self, st, s, env, hT, yA, mrg):
        P, T, QS, NQ, TT = self.P, self.T, self.QS, self.NQ, self.TT
        NS = self.NS
        w_in, idf = env["w_in"], env["idf"]
        x, acc, h2d, B_acc, B_h2d = env["x"], env["acc"], env["h2d"], env["B_acc"], env["B_h2d"]
        wrt, affT = env["wrt"], env["affT"]
        yR = self.yRk
        sb = self.sb
        wpr = sb(st, "wpr", [128, 4, D], BF16)
        wpa = sb(st, "wpa", [128, 4, D], BF16)
        self.load_w(wpr[:], wpr.b, env["wpr_d"])
        self.load_w(wpa[:], wpa.b, env["wpa_d"])
        wgs = [sb(st, f"wg{i}", [128, 8, 1024], BF16) for i in range(2)]
        sg1s = [sb(st, f"sg1_{i}", [128, QS], F32) for i in range(2)]
        sg2s = [sb(st, f"sg2_{i}", [128, QS], F32) for i in range(2)]
        m1s = [sb(st, f"m1_{i}", [128, QS], F32) for i in range(2)]
        m2s = [sb(st, f"m2_{i}", [128, QS], F32) for i in range(2)]
        cg = 1920 + 768

        def gate_iter(oc, tq, par):
            sg1, sg2, m1, m2 = sg1s[par], sg2s[par], m1s[par], m2s[par]
            wg = wgs[oc // 4]
            ol = (oc % 4) * 128
            if oc % 4 == 0 and tq == 0:
                self.load_w(wg[:, :, 0:512], wg.b, w_in[:, :, cg + oc * 128:cg + oc * 128 + 512])
                self.load_w(wg[:, :, 512:1024], wg.b, w_in[:, :, cg + 1024 + oc * 128:cg + 1024 + oc * 128 + 512])
            ts_ = slice(tq * QS, (tq + 1) * QS)
            ps1 = self.nps(f"h{par}")
            self.inproj(hT, wg, ol, tq, [wg.b, hT.b], ps1)
            self.act(sg1[:], ps1[:, 0:QS], AF.Sigmoid, r=[ps1.b], w=[sg1.b])
            yield
            ps2 = self.nps(f"h{par}")
            self.inproj(hT, wg, 512 + ol, tq, [wg.b, hT.b], ps2)
            self.act(sg2[:], ps2[:, 0:QS], AF.Sigmoid, r=[ps2.b], w=[sg2.b])
            yield
            ps3 = self.nps(f"h{par}")
            for kc in range(4):
                self.mm(ps3[:, 0:QS], wpr[:, kc, oc * 128:(oc + 1) * 128], yR[:, kc, ts_], start=(kc == 0), stop=(kc == 3),
                        r=[wpr.b, yR.b], w=[ps3.b])
            self.tt("dve", m1[:], sg1[:], ps3[:, 0:QS], ALU.mult, r=[sg1.b, ps3.b], w=[m1.b])
            yield
            ps4 = self.nps(f"h{par}")
            for kc in range(4):
                self.mm(ps4[:, 0:QS], wpa[:, kc, oc * 128:(oc + 1) * 128], yA[:, kc, ts_], start=(kc == 0), stop=(kc == 3),
                        r=[wpa.b, yA.b], w=[ps4.b])
            self.tt("dve", m2[:], sg2[:], ps4[:, 0:QS], ALU.mult, r=[sg2.b, ps4.b], w=[m2.b])
            yield
            self.tt("pool", mrg[:, oc, ts_], m1[:], m2[:], ALU.add, r=[m1.b, m2.b], w=[mrg.b])
            yield

        its = [(oc, tq) for oc in range(8) for tq in range(NQ)]
        self.interleave([(lambda slot, oc=oc, tq=tq: gate_iter(oc, tq, slot)) for (oc, tq) in its], window=2)

    def out_proj(self, st, s, env, mrg):
        P, T, QS, NQ, TT = self.P, self.T, self.QS, self.NQ, self.TT
        idf = env["idf"]
        x, acc, h2d, B_acc, B_h2d = env["x"], env["acc"], env["h2d"], env["B_acc"], env["B_h2d"]
        wrt, affT = env["wrt"], env["affT"]
        sb = self.sb
        wout = sb(st, "wout", [128, 8, D], BF16)
        self.load_w(wout[:], wout.b, env["wout_d"])
        g2bc = sb(st, "g2bc", [128, D], F32)
        self.dma(g2bc.t[:], env["g2r_d"].partition_broadcast(128), w=[g2bc.b])
        xts = [sb(st, f"xm{i}", [128, D], F32) for i in range(2)]
        x1s = [sb(st, f"x1{i}", [128, D], F32) for i in range(2)]
        h2s = [sb(st, f"h2{i}", [128, D], F32) for i in range(2)]
        h2bs = [sb(st, f"h2b{i}", [128, D], BF16) for i in range(2)]
        h2Ts = [sb(st, f"h2T{i}", [128, 8, 128], F32) for i in range(2)]
        junk = sb(st, "junk2", [128, D], F32)
        sst = [sb(st, f"sm{i}", [128, 8], F32) for i in range(2)]
        lgs = [sb(st, f"lg{i}", [128, NEXP], F32) for i in range(2)]
        affts = [sb(st, f"afft{i}", [128, 128], F32) for i in range(2)]
        for a_ in affts:
            self.ms("pool", a_[:], 0.0, w=[a_.b])
        def tile_iter(tt_, slot):
                xt, x1, h2, h2b, ss = xts[slot], x1s[slot], h2s[slot], h2bs[slot], sst[slot]
                h2T, lg, afft = h2Ts[slot], lgs[slot], affts[slot]
                r0 = s * T + tt_ * 128
                tl = slice(tt_ * 128, (tt_ + 1) * 128)
                self.dma(xt.t[:], x[r0:r0 + 128, :], w=[xt.b])
                for half in range(2):
                    ps = self.nps(f"h{slot}")
                    for kc in range(8):
                        self.mm(ps[:, :], mrg[:, kc, tl], wout[:, kc, half * 512:(half + 1) * 512], start=(kc == 0), stop=(kc == 7),
                                r=[mrg.b, wout.b], w=[ps.b])
                    self.tt("dve", x1[:, half * 512:(half + 1) * 512], xt[:, half * 512:(half + 1) * 512], ps[:, :], ALU.add,
                            r=[xt.b, ps.b], w=[x1.b])
                yield
                self.dma(acc[r0:r0 + 128, :], x1[:], r=[x1.b], w=[B_acc[s]])
                self.act(junk[:], x1[:], AF.Square, accum=ss[:, 0:1], r=[x1.b], w=[junk.b, ss.b])
                self.ts("dve", ss[:, 1:2], ss[:, 0:1], 1.0 / D, NORM_EPS, ALU.mult, ALU.add, r=[ss.b], w=[ss.b])
                self.act(ss[:, 2:3], ss[:, 1:2], AF.Ln, r=[ss.b], w=[ss.b])
                self.act(ss[:, 3:4], ss[:, 2:3], AF.Exp, scale=-0.5, r=[ss.b], w=[ss.b])
                self.ts("dve", h2[:], x1[:], ss[:, 3:4], None, ALU.mult, r=[x1.b, ss.b], w=[h2.b])
                self.tt("pool", h2b[:], h2[:], g2bc[:], ALU.mult, r=[h2.b, g2bc.b], w=[h2b.b])
                self.dma(h2d[r0:r0 + 128, :], h2b[:], r=[h2b.b], w=[B_h2d[s]])
                yield
                for k2 in range(2):
                    ps = self.nps(f"h{slot}")
                    for kk in range(4):
                        kc = k2 * 4 + kk
                        self.tr(ps[:, kk * 128:(kk + 1) * 128], h2[:, kc * 128:(kc + 1) * 128], idf[:], r=[h2.b, idf.b], w=[ps.b])
                    self.cp("act", h2T[:, k2 * 4:(k2 + 1) * 4, :], ps[:, :].rearrange("p (k n) -> p k n", k=4), r=[ps.b], w=[h2T.b])
                yield
                ps = self.nps(f"h{slot}")
                for kc in range(8):
                    self.mm(ps[:, 0:NEXP], h2T[:, kc, :], wrt[:, kc, :], start=(kc == 0), stop=(kc == 7), r=[h2T.b, wrt.b], w=[ps.b])
                self.P.emit("dve", lambda e, o=ss[:, 4:5], i=ps[:, 0:NEXP]: e.reduce_max(out=o, in_=i, axis=AX.X), [ps.b], [ss.b])
                self.ts("dve", ss[:, 5:6], ss[:, 4:5], -1.0, None, ALU.mult, r=[ss.b], w=[ss.b])
                self.act(lg[:], ps[:, 0:NEXP], AF.Exp, bias=ss[:, 5:6], accum=ss[:, 6:7], r=[ps.b, ss.b], w=[lg.b, ss.b])
                self.P.emit("dve", lambda e, o=ss[:, 7:8], i=ss[:, 6:7]: e.reciprocal(out=o, in_=i), [ss.b], [ss.b])
                self.ts("dve", afft[:, 32 * s:32 * s + NEXP], lg[:], ss[:, 7:8], None, ALU.mult, r=[lg.b, ss.b], w=[afft.b])
                ps = self.nps(f"h{slot}")
                self.tr(ps[:, 0:128], afft[:], idf[:], r=[afft.b, idf.b], w=[ps.b])
                self.tt("dve", affT[:, tl], affT[:, tl], ps[:, 0:128], ALU.add, r=[ps.b, affT.b], w=[affT.b])


        self.interleave([(lambda slot, tt_=tt_: tile_iter(tt_, slot)) for tt_ in range(TT)], window=2)

    def phase_b(self, env):
        P, NS, T, CAP, SB_, NB = self.P, self.NS, self.T, self.CAP, self.SB, self.NB
        affT, idf, idb, g2 = env["affT"], env["idf"], env["idb"], env["g2"]
        acc, h2d, B_acc, B_h2d = env["acc"], env["h2d"], env["B_acc"], env["B_h2d"]
        sb = self.sb
        with ExitStack() as st:
            wk = sb(st, "wk", [128, T], F32)
            mv = sb(st, "mv", [128, CAP], F32)
            mi = sb(st, "mi", [128, CAP], U32)
            mif = sb(st, "mif", [128, CAP], F32)
            offs = sb(st, "offs", [128, 128], F32)
            idxT = sb(st, "idxT", [128, NB, 128], I32)
            valT = sb(st, "valT", [128, NB, 128], F32)
            self.dma(offs.t[:], env["c_offs"], w=[offs.b])
            self.cp("dve", wk[:], affT[:], r=[affT.b], w=[wk.b])
            for r_ in range(CAP // 8):
                sl = slice(r_ * 8, r_ * 8 + 8)
                self.P.emit("dve", lambda e, o=mv[:, sl], i=wk[:]: e.max(out=o, in_=i), [wk.b], [mv.b])
                self.P.emit("dve", lambda e, o=mi[:, sl], m=mv[:, sl], i=wk[:]: e.max_index(out=o, in_max=m, in_values=i),
                            [wk.b, mv.b], [mi.b])
                self.P.emit("dve", lambda e, o=wk[:], m=mv[:, sl], i=wk[:]: e.match_replace(
                    out=o, in_to_replace=m, in_values=i, imm_value=-1.0), [wk.b, mv.b], [wk.b])
            self.cp("dve", mif[:], mi[:], r=[mi.b], w=[mif.b])
            for blk in range(NB):
                bs = slice(blk * SB_, (blk + 1) * SB_)
                ps = self.nps()
                self.tr(ps[0:SB_, 0:128], mif[:, bs], idf[:], r=[mif.b, idf.b], w=[ps.b])
                self.tt("dve", idxT[0:SB_, blk, :], ps[0:SB_, 0:128], offs[0:SB_, :], ALU.add, r=[ps.b, offs.b], w=[idxT.b])
                ps = self.nps()
                self.tr(ps[0:SB_, 0:128], mv[:, bs], idf[:], r=[mv.b, idf.b], w=[ps.b])
                self.cp("act", valT[0:SB_, blk, :], ps[0:SB_, 0:128], r=[ps.b], w=[valT.b])
            wgs = [sb(st, f"ewg{i}", [128, 8, D], BF16) for i in range(2)]
            wus = [sb(st, f"ewu{i}", [128, 8, D], BF16) for i in range(2)]
            wds = [sb(st, f"ewd{i}", [128, 8, D], BF16) for i in range(2)]
            NPF = 2
            xss = [sb(st, f"xs{i}", [128, D], BF16) for i in range((NPF + 1) * NB)]
            xsT = sb(st, "xsT", [128, 8, CAP], BF16)
            hid = sb(st, "hid", [128, 8, CAP], BF16)
            sl_ = sb(st, "silu", [128, CAP], F32)
            yss = [sb(st, f"ys{i}", [128, D], F32) for i in range(2 * NB)]
            its = [(e, s) for e in range(NEXP) for s in range(NS)]
            last_sc = {s: [] for s in range(NS)}

            def load_expert(e):
                for dst, src in ((wgs[e % 2], env["eg_d"]), (wus[e % 2], env["eu_d"]), (wds[e % 2], env["ed_d"])):
                    self.load_w(dst[:], dst.b, src[e].rearrange("(k p) n -> p k n", p=128))

            def gather(i):
                e, s = its[i]
                pcol = 32 * s + e
                for blk in range(NB):
                    xs = xss[(i % (NPF + 1)) * NB + blk]
                    ia = idxT[0:SB_, blk, pcol:pcol + 1]
                    self.P.emit("pool", lambda en, o=xs[0:SB_, :], ia=ia: en.indirect_dma_start(
                        out=o, out_offset=None, in_=h2d, in_offset=bass.IndirectOffsetOnAxis(ap=ia, axis=0)),
                        [idxT.b, B_h2d[s]], [xs.b], dma=True)

            load_expert(0)
            for i in range(min(NPF, len(its))):
                gather(i)
            for i, (e, s) in enumerate(its):
                wg, wu, wd = wgs[e % 2], wus[e % 2], wds[e % 2]
                pcol = 32 * s + e
                if s == 0 and e + 1 < NEXP:
                    load_expert(e + 1)
                if i + NPF < len(its):
                    gather(i + NPF)
                for blk in range(NB):
                    xs = xss[(i % (NPF + 1)) * NB + blk]
                    ps = self.nps()
                    psv = ps.t.bitcast(BF16)
                    for kc in range(8):
                        self.tr(psv[:, kc * 128:kc * 128 + SB_], xs[0:SB_, kc * 128:(kc + 1) * 128], idb[0:SB_, 0:SB_],
                                r=[xs.b, idb.b], w=[ps.b])
                    self.cp("act", xsT[:, :, blk * SB_:(blk + 1) * SB_],
                            psv[:, :].rearrange("p (k n) -> p k n", k=8)[:, :, 0:SB_], r=[ps.b], w=[xsT.b])
                for fc in range(8):
                    psg = self.nps()
                    psu = self.nps()
                    for kc in range(8):
                        self.mm(psg[:, 0:CAP], wg[:, kc, fc * 128:(fc + 1) * 128], xsT[:, kc, :], start=(kc == 0), stop=(kc == 7),
                                r=[wg.b, xsT.b], w=[psg.b])
                    for kc in range(8):
                        self.mm(psu[:, 0:CAP], wu[:, kc, fc * 128:(fc + 1) * 128], xsT[:, kc, :], start=(kc == 0), stop=(kc == 7),
                                r=[wu.b, xsT.b], w=[psu.b])
                    self.act(sl_[:], psg[:, 0:CAP], AF.Silu, r=[psg.b], w=[sl_.b])
                    self.tt("dve", hid[:, fc, :], sl_[:], psu[:, 0:CAP], ALU.mult, r=[sl_.b, psu.b], w=[hid.b])
                new_sc = []
                for blk in range(NB):
                    ys = yss[(i % 2) * NB + blk]
                    for half in range(2):
                        ps = self.nps()
                        for fc in range(8):
                            self.mm(ps[0:SB_, :], hid[:, fc, blk * SB_:(blk + 1) * SB_], wd[:, fc, half * 512:(half + 1) * 512],
                                    start=(fc == 0), stop=(fc == 7), r=[hid.b, wd.b], w=[ps.b])
                        self.act(ys[0:SB_, half * 512:(half + 1) * 512], ps[0:SB_, :], AF.Copy,
                                 scale=valT[0:SB_, blk, pcol:pcol + 1], r=[ps.b, valT.b], w=[ys.b])
                    ia = idxT[0:SB_, blk, pcol:pcol + 1]
                    op = self.P.emit("pool", lambda en, i_=ys[0:SB_, :], ia=ia: en.indirect_dma_start(
                        out=acc, out_offset=bass.IndirectOffsetOnAxis(ap=ia, axis=0), in_=i_, in_offset=None,
                        compute_op=ALU.add), [idxT.b, ys.b, B_acc[s]], [], dma=True)
                    for d_ in last_sc[s]:
                        self.P._dep(op, d_)
                    new_sc.append(op)
                last_sc[s] = new_sc
            P.stage_end()

    def phase_c(self, env):
        P, NS, T, TT = self.P, self.NS, self.T, self.TT
        acc, y, B_acc = env["acc"], env["y"], env["B_acc"]
        sb = self.sb
        outs = []
        with ExitStack() as st:
            gF = sb(st, "gF", [128, D], F32)
            self.dma(gF.t[:], env["gF_d"].partition_broadcast(128), w=[gF.b])
            NW = 4
            ats = [sb(st, f"at{i}", [128, D], F32) for i in range(NW)]
            ots = [sb(st, f"ot{i}", [128, D], F32) for i in range(NW)]
            junks = [sb(st, f"junk3_{i}", [128, D], F32) for i in range(2)]
            sst = [sb(st, f"sc{i}", [128, 4], F32) for i in range(NW)]

            def c_iter(n, s, tt_):
                at, ot, ss, junk = ats[n], ots[n], sst[n], junks[n % 2]
                r0 = s * T + tt_ * 128
                self.dma(at.t[:], acc[r0:r0 + 128, :], r=[B_acc[s]], w=[at.b])
                yield
                self.act(junk[:], at[:], AF.Square, accum=ss[:, 0:1], r=[at.b], w=[junk.b, ss.b])
                self.ts("dve", ss[:, 1:2], ss[:, 0:1], 1.0 / D, NORM_EPS, ALU.mult, ALU.add, r=[ss.b], w=[ss.b])
                self.act(ss[:, 2:3], ss[:, 1:2], AF.Sqrt, r=[ss.b], w=[ss.b])
                self.P.emit("dve", lambda e, o=ss[:, 3:4], i=ss[:, 2:3]: e.reciprocal(out=o, in_=i), [ss.b], [ss.b])
                yield
                self.stt(ot[:], at[:], ss[:, 3:4], gF[:], ALU.mult, ALU.mult, r=[at.b, ss.b, gF.b], w=[ot.b])
                self.P.emit("sp", lambda e, o=y[r0:r0 + 128, :], i=ot[:]: e.dma_start(out=o, in_=i), [ot.b], [], dma=True)
                yield

            fin_b = P.buf("yout")
            gens = []
            n = 0
            for s in range(NS):
                for tt_ in range(TT):
                    gens.append(lambda slot, s=s, tt_=tt_: c_iter(slot, s, tt_))
                    n += 1
            self.interleave(gens, window=NW)
            outs = []
            fin = P.buf("fin")
            op = P.emit("sp", lambda en: en.nop(), writes=[fin])
            for d_ in outs:
                P._dep(op, d_)
            P.stage_end()


def host_consts(T):
    c = {}
    c["c_idf"] = np.eye(128, dtype=np.float32)
    blk = (np.arange(128)[:, None] // 64 == np.arange(128)[None, :] // 64).astype(np.float32)
    c["c_bo"] = blk
    c["c_ba"] = blk / 64.0
    s = (np.arange(128) % 64)[:, None]
    j = np.arange(128)[None, :]
    t = j % 64
    isr = j >= 64
    mT = np.zeros((128, 2, 4, 128), np.float32)
    mT[:, 0] = np.where(isr, s <= t, s < t)[:, None, :]
    mT[:, 1] = np.where(isr, s >= t, s > t)[:, None, :]
    c["c_mT"] = mT.reshape(128, 2, 512)
    tt = (np.arange(128) % 64)[:, None]
    ss = np.arange(64)[None, :]
    mN = np.zeros((128, 2, 8, 64), np.float32)
    mN[:, 0] = (ss < tt)[:, None, :]
    mN[:, 1] = (ss > tt)[:, None, :]
    c["c_mN"] = mN.reshape(128, 2, 512)
    idr = np.zeros((128, 8, 64), np.float32)
    idr[:] = (ss == tt)[:, None, :]
    c["c_idr"] = idr.reshape(128, 512)
    slopes = 2.0 ** (-8.0 * np.arange(1, 9) / 8)
    key = np.arange(128)[:, None]
    qq = np.arange(128)[None, :]
    bias = np.zeros((128, 3, 2, 4, 128), np.float32)
    for rel in (-1, 0, 1):
        dist = np.abs(rel * 128 + key - qq)
        for kv in range(2):
            for sg in range(4):
                g = 2 * (sg % 2) + sg // 2
                bias[:, rel + 1, kv, sg, :] = np.where(dist <= 128, -slopes[kv * 4 + g] * dist, -1e30)
    c["c_bias"] = bias.reshape(128, 6, 512)
    offs = np.zeros((128, 128), np.float32)
    offs[:] = ((np.arange(128) // 32) * T)[None, :]
    c["c_offs"] = offs
    return c


def host_params(inp):
    f = lambda a: np.ascontiguousarray(np.asarray(a, dtype=np.float32))
    m = {}
    m["w_in"] = f(inp["w_in"][0])
    m["gmixr"] = f(inp["norm_mix_g"][0].reshape(1, D))
    m["g2r"] = f(inp["norm_ffn_g"][0].reshape(1, D))
    m["mu"] = f(np.stack([inp["mu_prev"][0].reshape(15, 128).T, inp["mu_next"][0].reshape(15, 128).T], axis=-1))
    names = ["w0_f", "w0_b", "a0_f", "a0_b", "k_k", "k_a", "r_k", "ln_x_w", "ln_x_b"]
    m["pp"] = f(np.stack([np.asarray(inp[n][0]).reshape(4, 128).T for n in names], axis=-1))
    m["sink"] = f(inp["attn_sink"][0].reshape(1, 8))
    m["g2"] = f(inp["norm_ffn_g"][0].reshape(8, 128).T)
    m["gF"] = f(np.asarray(inp["norm_final_g"]).reshape(1, D))
    m["wup"] = f(np.concatenate([inp["w_up_f"][0], inp["w_up_b"][0]], axis=0))
    m["aup"] = f(np.concatenate([inp["a_up_f"][0], inp["a_up_b"][0]], axis=0))
    m["gup"] = f(inp["g_up"][0])
    m["wpr"] = f(inp["w_proj_rwkv"][0])
    m["wpa"] = f(inp["w_proj_attn"][0])
    m["wout"] = f(inp["w_out"][0])
    m["wr"] = f(inp["w_router"][0])
    m["eg"] = f(inp["exp_w_gate"][0])
    m["eu"] = f(inp["exp_w_up"][0])
    m["ed"] = f(inp["exp_w_down"][0])
    return m


_NC_CACHE = {}


def run(inp, n_cores=8, stop=None, dbg=False, raw=False):
    x = np.asarray(inp["x"], dtype=np.float32)
    B, T, _ = x.shape
    NS = B // n_cores
    key = (NS, T, stop, dbg)
    if key not in _NC_CACHE:
        _NC_CACHE[key] = Builder(NS, T, stop, dbg).build()
    nc = _NC_CACHE[key]
    shared = host_params(inp)
    shared.update(host_consts(T))
    in_maps = []
    for c in range(n_cores):
        m = dict(shared)
        m["x"] = np.ascontiguousarray(x[c * NS:(c + 1) * NS].reshape(NS * T, D))
        in_maps.append(m)
    res = run_bass_kernel_spmd(nc, in_maps, core_ids=list(range(n_cores)))
    if raw:
        return res.results
    out = np.concatenate([r["y"].reshape(NS, T, D) for r in res.results], axis=0)
    return out.astype(np.float32)


def kernel(**inputs):
    return run(inputs, 8)
```

```python
import math
from contextlib import ExitStack
import numpy as np
import concourse.bass as bass
import concourse.mybir as mybir
from concourse.bass_utils import run_bass_kernel_spmd

F32 = mybir.dt.float32
BF16 = mybir.dt.bfloat16
U32 = mybir.dt.uint32
I32 = mybir.dt.int32
AF = mybir.ActivationFunctionType
ALU = mybir.AluOpType
AX = mybir.AxisListType

ENGS = ("pe", "dve", "act", "pool", "sp")
SEM_WRAP = 4000
NDMA_SEM = 12
NBANK = {'pe': 24, 'dve': 12, 'act': 12, 'pool': 6, 'sp': 2}

D = 1024
DS = math.exp(-0.5)
LNX_EPS = 64e-5
NORM_EPS = 1e-6
NEXP = 16
L = 64


class Buf:
    __slots__ = ("name", "w", "r")

    def __init__(self, name):
        self.name = name
        self.w = None
        self.r = []


class Op:
    __slots__ = ("eng", "fn", "waits", "signal", "idx", "dma", "tk", "sigval", "gid", "ninc")

    def __init__(self, eng, fn, dma):
        self.eng = eng
        self.fn = fn
        self.waits = []
        self.signal = False
        self.dma = dma
        self.tk = None
        self.sigval = None


class Prog:
    def __init__(self, nc, stack):
        self.nc = nc
        self.bufs = []
        self.sigcount = {e: 0 for e in ENGS}
        self.ndma = {e: 0 for e in ENGS}
        self.sems = {e: [stack.enter_context(nc.semaphore(f"s_{e}_{i}")) for i in range(NBANK[e])] for e in ENGS}
        self.dsems = {e: [stack.enter_context(nc.semaphore(f"d_{e}_{i}")) for i in range(NDMA_SEM)]
                      for e in ("sp", "pool", "act")}
        self.gid = 0
        self._reset()

    def _reset(self):
        self.q = {e: [] for e in ENGS}
        self.seen = {e: {} for e in ENGS}
        self.seen_dma = {e: set() for e in ENGS}
        self.dma_hist = {e: {} for e in ENGS}
        self.pending_dma = []
        for b in self.bufs:
            b.w = None
            b.r = []

    def buf(self, name="b"):
        b = Buf(name)
        self.bufs.append(b)
        return b

    def _dep(self, op, d):
        eng = op.eng
        if d is op:
            return
        if d.dma:
            if d.gid in self.seen_dma[eng]:
                return
            self.seen_dma[eng].add(d.gid)
            op.waits.append(d)
        else:
            if d.eng == eng and eng == "pe":
                return
            if self.seen[eng].get(d.eng, -1) >= d.idx:
                return
            self.seen[eng][d.eng] = d.idx
            d.signal = True
            op.waits.append(d)

    capture = None

    def emit(self, eng, fn, reads=(), writes=(), dma=False, ninc=1, est=None, hold=False):
        if self.capture is not None:
            self.capture.append((eng, fn, tuple(reads), tuple(writes), dma, est, hold))
            return None
        op = Op(eng, fn, dma)
        op.ninc = ninc
        op.gid = self.gid
        self.gid += 1
        op.idx = len(self.q[eng])
        deps = []
        for b in reads:
            if b.w is not None:
                deps.append(b.w)
        for b in writes:
            if b.w is not None:
                deps.append(b.w)
            deps.extend(b.r)
        for d in deps:
            self._dep(op, d)
        if dma:
            j = self.ndma[eng]
            self.ndma[eng] += 1
            op.tk = (j % NDMA_SEM, 16 * (j // NDMA_SEM + 1))
            hist = self.dma_hist[eng]
            if (j - NDMA_SEM) in hist:
                self._dep(op, hist[j - NDMA_SEM])
            hist[j] = op
            self.pending_dma.append(op)
        for b in reads:
            if not dma:
                b.r = [x for x in b.r if x.dma or x.eng != eng]
            b.r.append(op)
        for b in writes:
            b.w = op
            b.r = []
        self.q[eng].append(op)
        return op

    def barrier(self):
        pend = self.pending_dma
        self.pending_dma = []
        last = []
        for e in ENGS:
            for op in reversed(self.q[e]):
                if not op.dma and not getattr(op.fn, "_is_nop", False):
                    last.append(op)
                    break
        for e in ENGS:
            fn = lambda en: en.nop()
            op = self.emit(e, fn)
            for d in last:
                self._dep(op, d)
            for d in pend:
                self._dep(op, d)

    def stage_end(self):
        self.barrier()

    def flush(self):
        self.barrier()
        nc = self.nc
        base = dict(self.sigcount)
        for e in ENGS:
            c = base[e]
            for op in self.q[e]:
                if op.signal:
                    assert (c % SEM_WRAP) + op.ninc <= SEM_WRAP
                    c += op.ninc
                    op.sigval = c
            self.sigcount[e] = c
        sems, dsems = self.sems, self.dsems

        def resolve(d):
            if d.dma:
                return dsems[d.eng][d.tk[0]], d.tk[1]
            v = d.sigval - 1
            assert v // SEM_WRAP < NBANK[d.eng], (d.eng, v)
            return sems[d.eng][v // SEM_WRAP], v % SEM_WRAP + 1

        def run(e, engobj):
            for op in self.q[e]:
                for d in op.waits:
                    s, v = resolve(d)
                    engobj.wait_ge(s, v)
                ins = op.fn(engobj)
                if op.dma:
                    ins.then_inc(dsems[e][op.tk[0]], 16)
                elif op.signal:
                    v = op.sigval - 1
                    assert v // SEM_WRAP < NBANK[e], (e, v)
                    ins.then_inc(sems[e][v // SEM_WRAP], 1)

        with nc.Block() as block:
            @block.tensor
            def _(t):
                run("pe", t)

            @block.vector
            def _(v):
                run("dve", v)

            @block.scalar
            def _(s):
                run("act", s)

            @block.gpsimd
            def _(g):
                run("pool", g)

            @block.sync
            def _(sp):
                run("sp", sp)
        self._reset()


class Tl:
    def __init__(self, t, b):
        self.t = t
        self.b = b

    def __getitem__(self, k):
        return self.t[k]


class _Stop(Exception):
    pass


class Builder:
    def __init__(self, NS, T, stop=None, dbg=False):
        self.stop, self.dbg = stop, dbg
        self.NS, self.T = NS, T
        self.QS = min(512, T)
        self.NQ = T // self.QS
        self.TT = T // 128
        self.NCH = T // L
        self.CQ = self.QS // L
        self.CAP = 2 * T // NEXP
        self.SB = min(128, self.CAP)
        self.NB = self.CAP // self.SB
        self.nc = bass.Bass("TRN2", target_bir_lowering=False)
        self.stack = ExitStack()
        self.P = Prog(self.nc, self.stack)
        self.psi = 0

    def sb(self, st, name, shape, dt):
        self.nsb = getattr(self, "nsb", 0) + 1
        name = f"{name}__{self.nsb}"
        t = st.enter_context(self.nc.sbuf_tensor(name, list(shape), dt))
        return Tl(t, self.P.buf(name))

    def chk(self, k):
        if self.stop == k:
            self.P.flush()
            raise _Stop()

    def dram_in(self, name, shape, dt=F32):
        return self.nc.dram_tensor(name, list(shape), dt, kind="ExternalInput").ap()

    def nps(self, grp=None):
        if grp is None:
            p = self.ps[self.psi % 8]
            self.psi += 1
            return p
        banks = {"chain": (0, 1, 2), "post": (3,), "A": (4, 5), "B": (6, 7), "attO0": (0,), "attD0": (1,), "attO1": (2,), "attD1": (3,),
                 "attS0": (4, 5), "attS1": (6, 7), "h0": (0, 1, 2, 3), "h1": (4, 5, 6, 7)}[grp]
        self.psg = getattr(self, "psg", {})
        i = self.psg.get(grp, 0)
        self.psg[grp] = i + 1
        return self.ps[banks[i % len(banks)]]

    def mm(self, out, lhsT, rhs, start=True, stop=True, r=(), w=(), nohold=False):
        self.P.emit("pe", lambda e: e.matmul(out, lhsT=lhsT, rhs=rhs, start=start, stop=stop), r, w,
                    est=max(64, rhs.free_size()) / 2400.0 + 0.004, hold=(not stop) and not nohold)

    def tr(self, out, in_, ident, r=(), w=()):
        self.P.emit("pe", lambda e: e.transpose(out=out, in_=in_, identity=ident), r, w, est=0.06)

    def act(self, out, in_, func, bias=None, scale=None, accum=None, r=(), w=()):
        kw = {}
        if bias is not None:
            kw["bias"] = bias
        if scale is not None:
            kw["scale"] = scale
        if accum is not None:
            kw["accum_out"] = accum
        self.P.emit("act", lambda e: e.activation(out=out, in_=in_, func=func, **kw), r, w, ninc=1,
                    est=(224 + out.free_size()) / 1200.0)

    def tt(self, eng, out, in0, in1, op, r=(), w=()):
        self.P.emit(eng, lambda e: e.tensor_tensor(out=out, in0=in0, in1=in1, op=op), r, w, est=self._est(eng, out))

    def ts(self, eng, out, in0, s1, s2, op0, op1=None, r=(), w=()):
        if op1 is None:
            self.P.emit(eng, lambda e: e.tensor_scalar(out=out, in0=in0, scalar1=s1, scalar2=None, op0=op0), r, w,
                        est=self._est(eng, out))
        else:
            self.P.emit(eng, lambda e: e.tensor_scalar(out=out, in0=in0, scalar1=s1, scalar2=s2, op0=op0, op1=op1), r, w,
                        est=self._est(eng, out))

    def stt(self, out, in0, scalar, in1, op0, op1, r=(), w=()):
        self.P.emit("dve", lambda e: e.scalar_tensor_tensor(out=out, in0=in0, scalar=scalar, in1=in1, op0=op0, op1=op1), r, w,
                    est=self._est("dve", out))

    def cp(self, eng, out, in_, r=(), w=()):
        if eng == "act":
            self.P.emit("act", lambda e: e.activation(out=out, in_=in_, func=AF.Copy), r, w, est=(224 + out.free_size()) / 1200.0)
        else:
            self.P.emit(eng, lambda e: e.tensor_copy(out=out, in_=in_), r, w, est=self._est(eng, out))

    def ms(self, eng, ap, val, w=()):
        self.P.emit(eng, lambda e: e.memset(ap, val), (), w, est=self._est(eng, ap))

    def _est(self, eng, out):
        n = out.free_size()
        if eng == "pool":
            return (150 + 1.6 * n) / 1200.0
        return (100 + n) / 960.0

    def interleave(self, gens, window=None):
        P = self.P
        allg = [g for g in gens if g is not None]
        if window is None:
            window = len(allg)
        pending = allg[window:]

        def mk(f, slot):
            return {"g": (f(slot) if callable(f) else f), "q": [], "done": False}
        streams = [mk(g, i_) for i_, g in enumerate(allg[:window])]
        eng_free = getattr(self, "_sim_eng", None)
        if eng_free is None:
            eng_free = self._sim_eng = {e: 0.0 for e in ENGS}
            self._sim_ready = {}
            self._sim_lastrd = {}
        ready, lastrd = self._sim_ready, self._sim_lastrd

        def fill(st_):
            while not st_["q"] and not st_["done"]:
                P.capture = st_["q"]
                try:
                    next(st_["g"])
                except StopIteration:
                    st_["done"] = True
                finally:
                    P.capture = None

        def start_time(o):
            eng, fn, rd, wr, dma, est, hold = o
            t = eng_free[eng]
            for b in rd:
                t = max(t, ready.get(b, (0.0, None))[0] + (0.15 if ready.get(b, (0.0, eng))[1] != eng else 0.05))
            for b in wr:
                t = max(t, ready.get(b, (0.0, None))[0] + 0.05, lastrd.get(b, 0.0) + 0.1)
            return t

        forced = None
        while True:
            for st_ in streams:
                fill(st_)
            for i_, st_ in enumerate(streams):
                if st_["done"] and not st_["q"] and pending:
                    streams[i_] = mk(pending.pop(0), i_)
                    fill(streams[i_])
            live = [st_ for st_ in streams if st_["q"]]
            if not live:
                break
            if forced is not None and forced["q"]:
                best = forced
            else:
                best = min(live, key=lambda st_: start_time(st_["q"][0]))
            o = best["q"].pop(0)
            eng, fn, rd, wr, dma, est, hold = o
            forced = best if hold else None
            t0 = start_time(o)
            dur = est if est is not None else 0.3
            if dma:
                eng_free[eng] = t0 + 0.06
                tend = t0 + 2.5
            else:
                eng_free[eng] = t0 + dur
                tend = t0 + dur
            for b in rd:
                lastrd[b] = max(lastrd.get(b, 0.0), tend)
            for b in wr:
                ready[b] = (tend, eng)
                lastrd[b] = 0.0
            P.emit(eng, fn, rd, wr, dma=dma)

    def dma(self, out, in_, r=(), w=(), eng="sp"):
        return self.P.emit(eng, lambda e: e.dma_start(out=out, in_=in_), r, w, dma=True)

    def load_w(self, dst, dst_b, src, kc=None, n=None, scale=None):
        assert scale is None
        self.dma(dst, src, w=[dst_b], eng="pool")

    def build(self):
        nc, P, NS, T = self.nc, self.P, self.NS, self.T
        QS, NQ, TT, NCH, CQ = self.QS, self.NQ, self.TT, self.NCH, self.CQ
        st0 = self.stack
        din = self.dram_in
        x = din("x", [NS * T, D])
        w_in = din("w_in", [D, 4736]).rearrange("(k p) n -> p k n", p=128)
        gmixr_d = din("gmixr", [1, D])
        g2r_d = din("g2r", [1, D])
        mu_d = din("mu", [128, 15, 2])
        pp_d = din("pp", [128, 4, 9])
        sink_d = din("sink", [1, 8])
        g2_d = din("g2", [128, 8])
        gF_d = din("gF", [1, D])
        wup_d = din("wup", [128, 512])
        aup_d = din("aup", [128, 512])
        gup_d = din("gup", [128, 512])
        wpr_d = din("wpr", [512, D]).rearrange("(k p) n -> p k n", p=128)
        wpa_d = din("wpa", [512, D]).rearrange("(k p) n -> p k n", p=128)
        wout_d = din("wout", [D, D]).rearrange("(k p) n -> p k n", p=128)
        wr_d = din("wr", [D, NEXP]).rearrange("(k p) n -> p k n", p=128)
        eg_d = din("eg", [NEXP, D, D])
        eu_d = din("eu", [NEXP, D, D])
        ed_d = din("ed", [NEXP, D, D])
        c_idf = din("c_idf", [128, 128])
        c_bo = din("c_bo", [128, 128])
        c_ba = din("c_ba", [128, 128])
        c_mT = din("c_mT", [128, 2, 512])
        c_mN = din("c_mN", [128, 2, 512])
        c_idr = din("c_idr", [128, 512])
        c_bias = din("c_bias", [128, 6, 512])
        c_offs = din("c_offs", [128, 128])
        y = nc.dram_tensor("y", [NS * T, D], F32, kind="ExternalOutput").ap()
        ik = "ExternalOutput" if self.dbg else "Internal"
        uS = nc.dram_tensor("uS", [15, 128, T], BF16, kind=ik).ap()
        acc = nc.dram_tensor("acc", [NS * T, D], F32, kind=ik).ap()
        h2d = nc.dram_tensor("h2d", [NS * T, D], BF16, kind="Internal").ap()
        hTd = nc.dram_tensor("hTd", [128, 8, T], BF16, kind="Internal").ap()
        B_hTd = P.buf("hTd")
        if self.dbg:
            self.dbgR = nc.dram_tensor("dbgR", [128, 4, T], BF16, kind="ExternalOutput").ap()
            self.dbgA = nc.dram_tensor("dbgA", [128, 4, T], BF16, kind="ExternalOutput").ap()
        B_uS = [P.buf(f"uS{c}") for c in range(15)]
        B_acc = [P.buf(f"acc{s}") for s in range(NS)]
        B_h2d = [P.buf(f"h2d{s}") for s in range(NS)]

        self.ps = []
        for i in range(8):
            t = st0.enter_context(nc.psum_tensor(f"ps{i}", [128, 512], F32))
            self.ps.append(Tl(t, P.buf(f"ps{i}")))

        sb = self.sb
        idf = sb(st0, "idf", [128, 128], F32)
        idb = sb(st0, "idb", [128, 128], BF16)
        bo = sb(st0, "bo", [128, 128], F32)
        ba = sb(st0, "ba", [128, 128], F32)
        onesb = sb(st0, "onesb", [128, 128], BF16)
        bob = sb(st0, "bob", [128, 128], BF16)
        bab = sb(st0, "bab", [128, 128], BF16)
        mu = sb(st0, "mu", [128, 15, 2], F32)
        mu0 = sb(st0, "mu0", [128, 15], F32)
        pp = sb(st0, "pp", [128, 4, 9], F32)
        omka = sb(st0, "omka", [128, 4], F32)
        npp = sb(st0, "npp", [128, 4, 9], F32)
        onesf = sb(st0, "onesf", [128, 1], F32)
        g2 = sb(st0, "g2", [128, 8], F32)
        wrt = sb(st0, "wrt", [128, 8, NEXP], F32)
        affT = sb(st0, "affT", [128, T], F32)
        self.wst = [sb(st0, f"wst{i}", [128, 8 * NEXP], F32) for i in range(1)]
        self.wsi = 0
        PW0F, PW0B, PA0F, PA0B, PKK, PKA, PRK, PLW, PLB = range(9)

        for (dst, src) in ((idf, c_idf), (bo, c_bo), (ba, c_ba), (mu, mu_d), (pp, pp_d), (g2, g2_d)):
            self.dma(dst.t[:], src, w=[dst.b])
        self.cp("pool", idb[:], idf[:], r=[idf.b], w=[idb.b])
        self.ms("pool", onesb[:], 1.0, w=[onesb.b])
        self.cp("pool", bob[:], bo[:], r=[bo.b], w=[bob.b])
        self.cp("pool", bab[:], ba[:], r=[ba.b], w=[bab.b])
        self.tt("dve", mu0[:], mu[:, :, 0], mu[:, :, 1], ALU.add, r=[mu.b], w=[mu0.b])
        self.ts("dve", mu0[:], mu0[:], -1.0, 1.0, ALU.mult, ALU.add, r=[mu0.b], w=[mu0.b])
        self.ts("dve", omka[:], pp[:, :, PKA], -1.0, 1.0, ALU.mult, ALU.add, r=[pp.b], w=[omka.b])
        self.ts("dve", npp[:], pp[:], -1.0, None, ALU.mult, r=[pp.b], w=[npp.b])
        self.ms("pool", onesf[:], 1.0, w=[onesf.b])
        self.dma(self.wst[0].t[:, 0:8 * NEXP].rearrange("p (k n) -> p k n", k=8), wr_d, w=[self.wst[0].b])
        self.tt("dve", wrt[:], self.wst[0].t[:, 0:8 * NEXP].rearrange("p (k n) -> p k n", k=8),
                g2.t[:].unsqueeze(2).to_broadcast([128, 8, NEXP]),
                ALU.mult, r=[self.wst[0].b, g2.b], w=[wrt.b])
        self.ms("pool", affT[:], 0.0, w=[affT.b])
        self.yRk = sb(st0, "yRk", [128, 4, T], BF16)
        P.stage_end()

        env = locals()
        try:
            self.chk(0)
            for s in range(NS):
                self.phase_a(s, env)
            self.phase_b(env)
            self.chk(6)
            self.phase_c(env)
        except _Stop:
            return nc
        self.P.flush()
        print("signals", self.P.sigcount, "dmas", self.P.ndma)
        self.stack.close()
        return nc

    def norm_hT(self, st, s, env, hT):
        T, TT = self.T, self.TT
        x = env["x"]
        idb = env["idb"]
        gbc = self.sb(st, "gbc", [128, D], F32)
        self.dma(gbc.t[:], env["gmixr_d"].partition_broadcast(128), w=[gbc.b])
        xts = [self.sb(st, f"xt{i}", [128, D], F32) for i in range(2)]
        hns = [self.sb(st, f"hn{i}", [128, D], BF16) for i in range(2)]
        junk = self.sb(st, "junk", [128, D], F32)
        sst = [self.sb(st, f"ss{i}", [128, 4], F32) for i in range(2)]
        for tt_ in range(TT):
            xt, hn, ss = xts[tt_ % 2], hns[tt_ % 2], sst[tt_ % 2]
            r0 = s * T + tt_ * 128
            self.dma(xt.t[:], x[r0:r0 + 128, :], w=[xt.b])
            self.chk(20)
            self.act(junk[:], xt[:], AF.Square, accum=ss[:, 0:1], r=[xt.b], w=[junk.b, ss.b])
            self.chk(21)
            self.ts("dve", ss[:, 1:2], ss[:, 0:1], 1.0 / D, NORM_EPS, ALU.mult, ALU.add, r=[ss.b], w=[ss.b])
            self.act(ss[:, 2:3], ss[:, 1:2], AF.Sqrt, r=[ss.b], w=[ss.b])
            self.chk(22)
            self.P.emit("dve", lambda e, o=ss[:, 3:4], i=ss[:, 2:3]: e.reciprocal(out=o, in_=i), [ss.b], [ss.b])
            self.stt(hn[:], xt[:], ss[:, 3:4], gbc[:], ALU.mult, ALU.mult, r=[xt.b, ss.b, gbc.b], w=[hn.b])
            self.chk(23)
            ps = self.nps()
            psv = ps.t.bitcast(BF16)
            for kc in range(8):
                self.tr(psv[:, kc * 128:(kc + 1) * 128], hn[:, kc * 128:(kc + 1) * 128], idb[:], r=[hn.b, idb.b], w=[ps.b])
            self.chk(24)
            import os
            if os.environ.get("VARX") == "1":
                self.cp("dve", junk.t[:].bitcast(BF16)[:, 0:1024], psv[:, :], r=[ps.b], w=[junk.b])
            elif os.environ.get("VARX") == "2":
                self.cp("dve", hT[:, 0, 0:128], psv[:, 0:128], r=[ps.b], w=[hT.b])
            else:
                self.cp("dve", hT[:, :, tt_ * 128:(tt_ + 1) * 128], psv[:, :].rearrange("p (k n) -> p k n", k=8), r=[ps.b], w=[hT.b])
            self.chk(25)

    def inproj(self, hT, wb, col0, tq, reads, ps):
        QS = self.QS
        for kc in range(8):
            self.mm(ps[:, 0:QS], wb[:, kc, col0:col0 + 128], hT[:, kc, tq * QS:(tq + 1) * QS],
                    start=(kc == 0), stop=(kc == 7), r=reads, w=[ps.b])

    def phase_a(self, s, env):
        P, T, QS, NQ, TT, NCH, CQ = self.P, self.T, self.QS, self.NQ, self.TT, self.NCH, self.CQ
        w_in, mu, mu0, pp, omka = env["w_in"], env["mu"], env["mu0"], env["pp"], env["omka"]
        uS, B_uS = env["uS"], env["B_uS"]
        PW0F, PW0B, PA0F, PA0B, PKK, PKA, PRK, PLW, PLB = range(9)
        sb = self.sb

        with ExitStack() as st:
            hT = sb(st, "hT", [128, 8, T], BF16)
            self.norm_hT(st, s, env, hT)
            self.dma(env["hTd"], hT[:], r=[hT.b], w=[env["B_hTd"]])
            self.chk(10)
            wbs = [sb(st, f"wb{i}", [128, 8, 512], BF16) for i in range(2)]
            upad = sb(st, "upad", [128, T + 2], BF16)
            tmp = sb(st, "tmpA", [128, T], BF16)
            uss = [sb(st, f"us{i}", [128, T], BF16) for i in range(2)]
            self.ms("pool", upad[:, 0:1], 0.0, w=[upad.b])
            self.ms("pool", upad[:, T + 1:T + 2], 0.0, w=[upad.b])
            for grp in range(4):
                c0 = grp * 4
                ncol = min(4, 15 - c0)
                wb = wbs[grp % 2]
                self.load_w(wb[:, :, 0:ncol * 128], wb.b, w_in[:, :, c0 * 128:(c0 + ncol) * 128], 8, ncol * 128)
                self.chk(11)
                for ci in range(ncol):
                    c = c0 + ci
                    us = uss[c % 2]
                    for tq in range(NQ):
                        ps = self.nps()
                        self.inproj(hT, wb, ci * 128, tq, [wb.b, hT.b], ps)
                        self.cp("act", upad[:, 1 + tq * QS:1 + (tq + 1) * QS], ps[:, 0:QS], r=[ps.b], w=[upad.b])
                    self.ts("dve", tmp[:], upad[:, 1:T + 1], mu0[:, c:c + 1], None, ALU.mult, r=[upad.b, mu0.b], w=[tmp.b])
                    self.stt(tmp[:], upad[:, 0:T], mu[:, c, 0:1], tmp[:], ALU.mult, ALU.add, r=[upad.b, mu.b, tmp.b], w=[tmp.b])
                    self.stt(us[:], upad[:, 2:T + 2], mu[:, c, 1:2], tmp[:], ALU.mult, ALU.add, r=[upad.b, mu.b, tmp.b], w=[us.b])
                    if c == 12:
                        self.act(us[:], us[:], AF.Sigmoid, r=[us.b], w=[us.b])
                    elif c == 13:
                        self.act(us[:], us[:], AF.Tanh, r=[us.b], w=[us.b])
                    self.dma(uS[c], us[:], r=[us.b], w=[B_uS[c]])
                    self.chk(12)
            P.stage_end()
        self.chk(1)

        with ExitStack() as st:
            self.rwkv(st, s, env, self.yRk)
            if self.dbg and s == 0:
                self.dma(self.dbgR, self.yRk[:], r=[self.yRk.b])
            P.stage_end()
        self.chk(2)

        with ExitStack() as st:
            yA = sb(st, "yA", [128, 4, T], BF16)
            mrg = sb(st, "mrg", [128, 8, T], BF16)
            with ExitStack() as st1:
                hT = sb(st1, "hT", [128, 8, T], BF16)
                self.dma(hT.t[:], env["hTd"], r=[env["B_hTd"]], w=[hT.b])
                with ExitStack() as st2:
                    self.attention(st2, s, env, hT, yA)
                    if self.dbg and s == 0:
                        self.dma(self.dbgA, yA[:], r=[yA.b])
                    P.stage_end()
                self.chk(3)
                self.merge_gates(st1, s, env, hT, yA, mrg)
                P.stage_end()
                self.chk(4)
            with ExitStack() as st1:
                self.out_proj(st1, s, env, mrg)
                P.stage_end()
            self.chk(5)

    def rwkv(self, st, s, env, yR):
        P, T, NCH = self.P, self.T, self.NCH
        QS = min(512, T)
        NQ = T // QS
        CQ = QS // L
        pp, omka, npp, onesf = env["pp"], env["omka"], env["npp"], env["onesf"]
        uS, B_uS = env["uS"], env["B_uS"]
        bo, ba, idb, bob, bab = env["bo"], env["ba"], env["idb"], env["bob"], env["bab"]
        PW0F, PW0B, PA0F, PA0B, PKK, PKA, PRK, PLW, PLB = range(9)
        sb = self.sb
        mT = sb(st, "mT", [128, 2, 512], F32)
        mN = sb(st, "mN", [128, 2, 512], F32)
        idr = sb(st, "idr", [128, 512], F32)
        self.dma(mT.t[:], env["c_mT"], w=[mT.b])
        self.dma(mN.t[:], env["c_mN"], w=[mN.b])
        self.dma(idr.t[:], env["c_idr"], w=[idr.b])
        wup = sb(st, "wup", [128, 512], BF16)
        aup = sb(st, "aup", [128, 512], BF16)
        gup = sb(st, "gup", [128, 512], BF16)
        for dst, src in ((wup, env["wup_d"]), (aup, env["aup_d"]), (gup, env["gup_d"])):
            self.load_w(dst[:], dst.b, src)
        NQ_ = NQ
        bonus = [sb(st, f"bonus{i}", [128, T], BF16) for i in range(2)]
        wkv = [sb(st, f"wkv{i}", [128, T], BF16) for i in range(2)]
        vtok = [sb(st, f"vtok{i}", [128, NCH, L], BF16) for i in range(2)]
        for t_ in vtok:
            t_.bq = [P.buf("vtokq") for _ in range(NQ_)]
        R = []
        for d in range(2):
            Rd = dict(
                ar=sb(st, f"ar{d}", [128, NCH, 2, L], BF16),
                SB=sb(st, f"SB{d}", [128, NCH, L], BF16),
                SK=sb(st, f"SK{d}", [128, NCH, 2, L], BF16),
                TTm=sb(st, f"TT{d}", [128, NCH, L], BF16),
                BB=sb(st, f"BB{d}", [128, NCH, L], BF16),
                KB=sb(st, f"KB{d}", [128, NCH, L], BF16),
                Wtot=sb(st, f"Wtot{d}", [128, NCH], F32),
                ST=sb(st, f"ST{d}", [128, L], BF16),
                Xs=sb(st, f"Xs{d}", [128, L], BF16),
                Us=sb(st, f"Us{d}", [128, L], BF16),
            )
            for k_ in ("ar", "SB", "SK", "TTm", "BB", "KB", "Wtot"):
                Rd[k_].bq = [P.buf(k_ + "q") for _ in range(NQ_)]
            R.append(Rd)

        def q(name, dt=F32):
            return sb(st, name, [128, QS], dt)
        def mk_temps(sfx):
            names_f = ["t2", "sgw", "cs"] + (["X1"] if sfx == "A" else ["X2", "X3"])
            names_b = ["rq", "kq", "vb", "twb", "alb", "kkn", "sq", "t1", "t2b", "t3", "kft", "aqt", "E", "akk", "p1", "p2",
                       "bT", "kT", "Pm", "PTm", "Pm2", "PTm2"]
            d_ = {n: q(n + sfx) for n in names_f}
            d_.update({n: q(n + sfx, BF16) for n in names_b})
            for n in ("X1", "X2", "X3"):
                d_.setdefault(n, None)
            return d_
        TA, TB = mk_temps("A"), mk_temps("B")
        TA["grp"], TB["grp"] = "A", "B"
        sgb, t1, t2, sq = q("sgbP", BF16), q("t1P"), q("t2P"), q("sqP")
        scm = sb(st, "scm", [128, QS], F32)
        self.ms("pool", scm[:], 1.0, w=[scm.b])
        self.ms("pool", scm.t[:].rearrange("p (c l) -> p c l", l=L)[:, :, 0:1], 0.0, w=[scm.b])

        def v3(ap):
            return ap.rearrange("p (c l) -> p c l", l=L)

        def pre_pass(p, d, tq, first, tmp):
            (rq, kq, vb, twb, alb, kkn, sq, t1, t2, t2b, t3, kft, aqt, sgw, cs, X1, X2, X3, E, akk, p1, p2,
             bT, kT, Pm, PTm, Pm2, PTm2) = (tmp[n] for n in (
                "rq", "kq", "vb", "twb", "alb", "kkn", "sq", "t1", "t2", "t2b", "t3", "kft", "aqt", "sgw", "cs", "X1", "X2", "X3",
                "E", "akk", "p1", "p2", "bT", "kT", "Pm", "PTm", "Pm2", "PTm2"))
            par = p % 2
            pc = slice(p * 128, (p + 1) * 128)
            ts_ = slice(tq * QS, (tq + 1) * QS)
            cq0 = tq * CQ
            csl = slice(cq0, cq0 + CQ)
            Rd = R[d]
            hs_lo = slice(64 * d, 64 * d + 64)
            for dst, c in ((rq, p), (kq, 4 + p), (vb, 8 + p), (twb, 13), (alb, 14)):
                self.dma(dst.t[:], uS[c][:, ts_], r=[B_uS[c]], w=[dst.b])
            yield
            self.ts("dve", t1[:], kq[:], pp[:, p, PKK:PKK + 1], None, ALU.mult, r=[kq.b, pp.b], w=[t1.b])
            self.tt("pool", sq[:], t1[:], t1[:], ALU.mult, r=[t1.b], w=[sq.b])
            ps = self.nps(tmp["grp"])
            self.mm(ps[:, 0:QS], bob[:], sq[:], r=[bob.b, sq.b], w=[ps.b])
            self.ts("dve", t2[:], ps[:, 0:QS], 1e-24, None, ALU.max, r=[ps.b], w=[t2.b])
            self.act(t2[:], t2[:], AF.Ln, r=[t2.b], w=[t2.b])
            self.act(t2b[:], t2[:], AF.Exp, scale=-0.5, r=[t2.b], w=[t2b.b])
            self.tt("dve", kkn[:], t1[:], t2b[:], ALU.mult, r=[t1.b, t2b.b], w=[kkn.b])
            yield
            ps = self.nps(tmp["grp"])
            self.mm(ps[:, 0:QS], aup[hs_lo, pc], alb[hs_lo, :], r=[aup.b, alb.b], w=[ps.b])
            self.act(t2[:], ps[:, 0:QS], AF.Exp, scale=-1.0, bias=npp[:, p, PA0F + d:PA0F + d + 1], r=[ps.b, npp.b], w=[t2.b])
            self.act(t2[:], t2[:], AF.Ln, bias=onesf[:, 0:1], r=[t2.b, onesf.b], w=[t2.b])
            self.act(aqt[:], t2[:], AF.Exp, scale=-1.0, r=[t2.b], w=[aqt.b])
            self.ts("dve", t3[:], aqt[:], pp[:, p, PKA:PKA + 1], omka[:, p:p + 1], ALU.mult, ALU.add,
                    r=[aqt.b, pp.b, omka.b], w=[t3.b])
            self.tt("dve", kft[:], kq[:], t3[:], ALU.mult, r=[kq.b, t3.b], w=[kft.b])
            yield
            self.stt(t3[:], kft[:], pp[:, p, PRK:PRK + 1], rq[:], ALU.mult, ALU.mult, r=[kft.b, pp.b, rq.b], w=[t3.b])
            ps = self.nps(tmp["grp"])
            self.mm(ps[:, 0:QS], bob[:], t3[:], r=[bob.b, t3.b], w=[ps.b])
            if first:
                self.tt("dve", bonus[par][:, ts_], ps[:, 0:QS], vb[:], ALU.mult, r=[ps.b, vb.b], w=[bonus[par].b])
            else:
                self.tt("dve", t3[:], ps[:, 0:QS], vb[:], ALU.mult, r=[ps.b, vb.b], w=[t3.b])
                self.tt("pool", bonus[par][:, ts_], bonus[par][:, ts_], t3[:], ALU.add, r=[t3.b, bonus[par].b], w=[bonus[par].b])
            yield
            if first:
                ps = self.nps(tmp["grp"])
                psv = ps.t.bitcast(BF16)
                for c in range(CQ):
                    for h in range(2):
                        hs = slice(64 * h, 64 * h + 64)
                        self.tr(psv[hs, c * L:(c + 1) * L], vb[hs, c * L:(c + 1) * L], idb[hs, hs], r=[vb.b, idb.b], w=[ps.b])
                self.cp("act", vtok[par][:, csl, :], v3(psv[:, 0:QS]), r=[ps.b], w=[vtok[par].bq[tq]])
                yield
            ps = self.nps(tmp["grp"])
            self.mm(ps[:, 0:QS], wup[hs_lo, pc], twb[hs_lo, :], r=[wup.b, twb.b], w=[ps.b])
            self.act(sgw[:], ps[:, 0:QS], AF.Exp, scale=-1.0, bias=npp[:, p, PW0F + d:PW0F + d + 1], r=[ps.b, npp.b], w=[sgw.b])
            self.act(sgw[:], sgw[:], AF.Ln, bias=onesf[:, 0:1], r=[sgw.b, onesf.b], w=[sgw.b])
            self.act(sgw[:], sgw[:], AF.Exp, scale=-1.0, r=[sgw.b], w=[sgw.b])
            self.P.emit("dve", lambda e, o=cs[:], a=scm[:], b=sgw[:]: e.tensor_tensor_scan(
                out=o, data0=a, data1=b, initial=0.0, op0=ALU.mult, op1=ALU.add), [scm.b, sgw.b], [cs.b], est=(100 + 2 * QS) / 960.0)
            tot = v3(cs[:])[:, :, L - 1:L]
            self.act(Rd["Wtot"][:, csl], tot.rearrange("p c l -> p (c l)"), AF.Exp, scale=-DS, r=[cs.b], w=[Rd["Wtot"].bq[tq]])
            if d == 0:
                self.tt("pool", X1[:], cs[:], sgw[:], ALU.subtract, r=[cs.b, sgw.b], w=[X1.b])
                ce, ci = X1, cs
            else:
                self.tt("dve", v3(X2[:]), tot.to_broadcast([128, CQ, L]), v3(cs[:]), ALU.subtract, r=[cs.b], w=[X2.b])
                self.tt("pool", X3[:], X2[:], sgw[:], ALU.add, r=[X2.b, sgw.b], w=[X3.b])
                ce, ci = X2, X3
            yield
            ar = Rd["ar"]
            arb = ar.bq[tq]
            self.act(E[:], ce[:], AF.Exp, scale=-DS, r=[ce.b], w=[E.b])
            self.stt(ar[:, csl, 0, :], v3(kkn[:]), -1.0, v3(E[:]), ALU.mult, ALU.mult, r=[kkn.b, E.b], w=[arb])
            self.act(p1[:], ci[:], AF.Exp, scale=-DS, r=[ci.b], w=[p1.b])
            self.tt("dve", ar[:, csl, 1, :], v3(rq[:]), v3(p1[:]), ALU.mult, r=[rq.b, p1.b], w=[arb])
            yield
            self.tt("pool", akk[:], aqt[:], kkn[:], ALU.mult, r=[aqt.b, kkn.b], w=[akk.b])
            self.act(p2[:], ci[:], AF.Exp, scale=DS, r=[ci.b], w=[p2.b])
            self.tt("dve", bT[:], akk[:], p2[:], ALU.mult, r=[akk.b, p2.b], w=[bT.b])
            self.tt("dve", kT[:], kft[:], p2[:], ALU.mult, r=[kft.b, p2.b], w=[kT.b])
            yield
            for src, dstk in ((bT, "BB"), (kT, "KB")):
                ps = self.nps(tmp["grp"])
                psv = ps.t.bitcast(BF16)
                for c in range(CQ):
                    for h in range(2):
                        hs = slice(64 * h, 64 * h + 64)
                        self.tr(psv[hs, c * L:(c + 1) * L], src[hs, c * L:(c + 1) * L], idb[hs, hs],
                                r=[src.b, idb.b], w=[ps.b])
                self.cp("act", Rd[dstk][:, csl, :], v3(psv[:, 0:QS]), r=[ps.b], w=[Rd[dstk].bq[tq]])
                yield
            for lhs, dstk in ((bT, "SB"), (kT, "SK")):
                for c4 in range(0, CQ, 4):
                    ps = self.nps(tmp["grp"])
                    for cc in range(4):
                        c = c4 + cc
                        for h in range(2):
                            hs = slice(64 * h, 64 * h + 64)
                            self.mm(ps[hs, cc * 128:(cc + 1) * 128], lhs[hs, c * L:(c + 1) * L],
                                    ar[hs, cq0 + c, :, :].rearrange("p a l -> p (a l)"),
                                    r=[lhs.b, arb], w=[ps.b])
                    if dstk == "SK":
                        self.tt("dve", Rd[dstk][:, cq0 + c4:cq0 + c4 + 4, :, :].rearrange("p c a l -> p (c a l)"),
                                ps[:, :], mT[:, d, :], ALU.mult, r=[ps.b, mT.b], w=[Rd[dstk].bq[tq]])
                    else:
                        ps4 = ps[:, :].rearrange("p (c a l) -> p c a l", c=4, a=2)
                        m4 = mT[:, d, :].rearrange("p (c a l) -> p c a l", c=4, a=2)
                        self.tt("dve", v3(PTm[:])[:, c4:c4 + 4, :], ps4[:, :, 0, :], m4[:, :, 0, :], ALU.mult,
                                r=[ps.b, mT.b], w=[PTm.b])
                        self.tt("dve", Rd["SB"][:, cq0 + c4:cq0 + c4 + 4, :], ps4[:, :, 1, :], m4[:, :, 1, :], ALU.mult,
                                r=[ps.b, mT.b], w=[Rd["SB"].bq[tq]])
                    yield
            ps = self.nps(tmp["grp"])
            for c in range(CQ):
                for h in range(2):
                    hs = slice(64 * h, 64 * h + 64)
                    self.mm(ps[hs, c * L:(c + 1) * L], ar[hs, cq0 + c, 0, :], bT[hs, c * L:(c + 1) * L],
                            r=[arb, bT.b], w=[ps.b])
            self.tt("dve", Pm[:], ps[:, 0:QS], mN[:, d, 0:QS], ALU.mult, r=[ps.b, mN.b], w=[Pm.b])
            TTq = Rd["TTm"][:, csl, :]
            TTb = Rd["TTm"].bq[tq]
            self.tt("pool", TTq, v3(PTm[:]), v3(idr[:, 0:QS]), ALU.add, r=[PTm.b, idr.b], w=[TTb])
            yield
            Pc, PTc, Pn, PTn = Pm, PTm, Pm2, PTm2
            for lvl in range(1, 6):
                psA = self.nps(tmp["grp"])
                for c in range(CQ):
                    for h in range(2):
                        hs = slice(64 * h, 64 * h + 64)
                        cl = slice(c * L, (c + 1) * L)
                        self.mm(psA[hs, cl], PTc[hs, cl], Pc[hs, cl], r=[PTc.b, Pc.b], w=[psA.b])
                self.cp("act", Pn[:], psA[:, 0:QS], r=[psA.b], w=[Pn.b])
                if lvl < 5:
                    psB = self.nps(tmp["grp"])
                    for c in range(CQ):
                        for h in range(2):
                            hs = slice(64 * h, 64 * h + 64)
                            cl = slice(c * L, (c + 1) * L)
                            self.mm(psB[hs, cl], Pc[hs, cl], PTc[hs, cl], r=[PTc.b, Pc.b], w=[psB.b])
                    self.cp("act", PTn[:], psB[:, 0:QS], r=[psB.b], w=[PTn.b])
                yield
                psC = self.nps(tmp["grp"])
                for c in range(CQ):
                    for h in range(2):
                        hs = slice(64 * h, 64 * h + 64)
                        cl = slice(c * L, (c + 1) * L)
                        self.mm(psC[hs, cl], Pn[hs, cl], Rd["TTm"][hs, cq0 + c, :], r=[Pn.b, TTb], w=[psC.b])
                self.tt("dve", TTq, v3(psC[:, 0:QS]), TTq, ALU.add, r=[psC.b, TTb], w=[TTb])
                Pc, PTc, Pn, PTn = Pn, PTn, Pc, PTc
                yield

        def pre_stage(p, k):
            gens = []
            for d, tq, tmp in ((0, k, TA), (1, NQ_ - 1 - k, TB)):
                fstage, bstage = tq, NQ_ - 1 - tq
                first = (fstage <= bstage) if d == 0 else (bstage < fstage)
                gens.append(pre_pass(p, d, tq, first, tmp))
            return gens

        def chain_group(p, k):
            par = p % 2
            if k == 0:
                self.ms("pool", wkv[par][:], 0.0, w=[wkv[par].b])
                for d in range(2):
                    self.ms("pool", R[d]["ST"][:], 0.0, w=[R[d]["ST"].b])
            for step in range(k * CQ, (k + 1) * CQ):
                for d in range(2):
                    Rd = R[d]
                    c = step if d == 0 else NCH - 1 - step
                    qi = c // CQ
                    ar, SBm, SKm, TTm, BB, KB, ST, Xs, Us = (Rd[k_] for k_ in ("ar", "SB", "SK", "TTm", "BB", "KB", "ST", "Xs", "Us"))
                    vt = vtok[par]
                    vtb = vt.bq[qi]
                    psX = self.nps("chain")
                    for h in range(2):
                        hs = slice(64 * h, 64 * h + 64)
                        self.mm(psX[hs, 0:L], ar[hs, c, 0, :], ST[hs, :], start=True, stop=False, r=[ar.bq[qi], ST.b], w=[psX.b])
                        self.mm(psX[hs, 0:L], SKm[hs, c, 0, :], vt[hs, c, :], start=False, stop=True, r=[SKm.bq[qi], vtb], w=[psX.b])
                    self.cp("act", Xs[:], psX[:, 0:L], r=[psX.b], w=[Xs.b])
                    yield
                    psU = self.nps("chain")
                    for h in range(2):
                        hs = slice(64 * h, 64 * h + 64)
                        self.mm(psU[hs, 0:L], TTm[hs, c, :], Xs[hs, :], r=[TTm.bq[qi], Xs.b], w=[psU.b])
                    self.cp("dve", Us[:], psU[:, 0:L], r=[psU.b], w=[Us.b])
                    yield
                    psY = self.nps("chain")
                    for h in range(2):
                        hs = slice(64 * h, 64 * h + 64)
                        self.mm(psY[hs, 0:L], ST[hs, :], ar[hs, c, 1, :], start=True, stop=False, r=[ar.bq[qi], ST.b], w=[psY.b])
                        self.mm(psY[hs, 0:L], Us[hs, :], SBm[hs, c, :], start=False, stop=False, r=[Us.b, SBm.bq[qi]], w=[psY.b])
                        self.mm(psY[hs, 0:L], vt[hs, c, :], SKm[hs, c, 1, :], start=False, stop=True, r=[vtb, SKm.bq[qi]], w=[psY.b])
                    psS = self.nps("chain")
                    for h in range(2):
                        hs = slice(64 * h, 64 * h + 64)
                        self.mm(psS[hs, 0:L], idb[hs, hs], ST[hs, :], start=True, stop=False, r=[idb.b, ST.b], w=[psS.b])
                        self.mm(psS[hs, 0:L], BB[hs, c, :], Us[hs, :], start=False, stop=False, r=[BB.bq[qi], Us.b], w=[psS.b])
                        self.mm(psS[hs, 0:L], KB[hs, c, :], vt[hs, c, :], start=False, stop=True, r=[KB.bq[qi], vtb], w=[psS.b])
                    self.ts("dve", ST[:], psS[:, 0:L], Rd["Wtot"][:, c:c + 1], None, ALU.mult,
                            r=[Rd["Wtot"].bq[qi], psS.b], w=[ST.b])
                    wsl = wkv[par][:, c * L:(c + 1) * L]
                    self.tt("dve", wsl, psY[:, 0:L], wsl, ALU.add, r=[psY.b, wkv[par].b], w=[wkv[par].b])
                    yield

        def post(p):
            par = p % 2
            pc = slice(p * 128, (p + 1) * 128)
            for tq in range(NQ):
                ts_ = slice(tq * QS, (tq + 1) * QS)
                self.dma(sgb.t[:], uS[12][:, ts_], r=[B_uS[12]], w=[sgb.b])
                ps = self.nps("post")
                self.mm(ps[:, 0:QS], bab[:], wkv[par][:, ts_], r=[bab.b, wkv[par].b], w=[ps.b])
                self.tt("dve", t1[:], wkv[par][:, ts_], ps[:, 0:QS], ALU.subtract, r=[wkv[par].b, ps.b], w=[t1.b])
                self.tt("pool", sq[:], t1[:], t1[:], ALU.mult, r=[t1.b], w=[sq.b])
                yield
                ps = self.nps("post")
                self.mm(ps[:, 0:QS], ba[:], sq[:], r=[ba.b, sq.b], w=[ps.b])
                self.ts("dve", t2[:], ps[:, 0:QS], LNX_EPS, None, ALU.add, r=[ps.b], w=[t2.b])
                self.act(t2[:], t2[:], AF.Ln, r=[t2.b], w=[t2.b])
                self.act(t2[:], t2[:], AF.Exp, scale=-0.5, r=[t2.b], w=[t2.b])
                self.tt("dve", t1[:], t1[:], t2[:], ALU.mult, r=[t1.b, t2.b], w=[t1.b])
                yield
                self.ts("dve", t1[:], t1[:], pp[:, p, PLW:PLW + 1], pp[:, p, PLB:PLB + 1], ALU.mult, ALU.add,
                        r=[t1.b, pp.b], w=[t1.b])
                self.tt("pool", t1[:], t1[:], bonus[par][:, ts_], ALU.add, r=[t1.b, bonus[par].b], w=[t1.b])
                ps = self.nps("post")
                self.mm(ps[:, 0:QS], gup[:, pc], sgb[:], r=[gup.b, sgb.b], w=[ps.b])
                self.tt("dve", yR[:, p, ts_], t1[:], ps[:, 0:QS], ALU.mult, r=[t1.b, ps.b], w=[yR.b])
                yield

        def run(g):
            for _ in g:
                pass

        interleave = self.interleave

        interleave(pre_stage(0, 0))
        pending_post = None
        for p in range(4):
            for k in range(NQ_):
                nxt = None
                if k + 1 < NQ_:
                    nxt = pre_stage(p, k + 1)
                elif p + 1 < 4:
                    nxt = pre_stage(p + 1, 0)
                if nxt is not None and NQ_ > 1:
                    gens = [chain_group(p, k)] + nxt
                    if k == 0 and pending_post is not None:
                        gens.append(pending_post)
                        pending_post = None
                    interleave(gens)
                else:
                    if pending_post is not None:
                        run(pending_post)
                        pending_post = None
                    run(chain_group(p, k))
                    if nxt is not None:
                        interleave(nxt)
            pending_post = post(p)
        run(pending_post)

    def attention(self, st, s, env, hT, yA):
        P, T, QS, NQ, TT = self.P, self.T, self.QS, self.NQ, self.TT
        w_in, idb, onesb = env["w_in"], env["idb"], env["onesb"]
        sb = self.sb
        bias = sb(st, "bias", [128, 6, 512], BF16)
        self.load_w(bias[:], bias.b, env["c_bias"])
        snk = sb(st, "snk", [128, 8], F32)
        esk = sb(st, "esk", [128, 2, 2, 128], F32)
        self.dma(snk.t[:], env["sink_d"].partition_broadcast(128), w=[snk.b])
        self.act(snk[:], snk[:], AF.Exp, r=[snk.b], w=[snk.b])
        for kv in range(2):
            for gh in range(2):
                for gl in range(2):
                    hs = slice(64 * gl, 64 * gl + 64)
                    col = kv * 4 + 2 * gh + gl
                    self.cp("dve", esk[hs, kv, gh, :], snk[hs, col:col + 1].to_broadcast([64, 128]), r=[snk.b], w=[esk.b])
        qT = sb(st, "qT", [128, TT, 4, 128], BF16)
        kTz = [sb(st, f"kTz{i}", [128, T], BF16) for i in range(2)]
        vtk = sb(st, "vtk", [128, TT, 128], BF16)
        wq = sb(st, "wq", [128, 8, 512], BF16)
        wkv_ = sb(st, "wkvw", [128, 8, 256], BF16)
        c0 = 1920
        wq5 = wq.t[:].rearrange("p k (g kv d) -> p k g kv d", g=4, kv=2)
        for i in range(2):
            for kc in range(8):
                self.load_w(wq5[:, kc, :, i, :], wq.b,
                            w_in[:, kc, c0 + i * 256:c0 + (i + 1) * 256].rearrange("p (g d) -> p g d", g=4))
        self.load_w(wkv_[:], wkv_.b, w_in[:, :, c0 + 512:c0 + 768], 8, 256)
        for i in range(2):
            self.ms("pool", kTz[i][:], 0.0, w=[kTz[i].b])
        for tq in range(NQ):
            ts_ = slice(tq * QS, (tq + 1) * QS)
            for g in range(4):
                ps = self.nps()
                for kc in range(8):
                    self.mm(ps[:, 0:QS], wq[:, kc, g * 128:(g + 1) * 128], hT[:, kc, ts_], start=(kc == 0), stop=(kc == 7),
                            r=[wq.b, hT.b], w=[ps.b])
                sg_ = (g % 2) * 2 + g // 2
                nb_ = QS // 128
                self.act(qT[:, tq * nb_:(tq + 1) * nb_, sg_, :], ps[:, 0:QS].rearrange("p (b q) -> p b q", q=128), AF.Copy, scale=0.125,
                         r=[ps.b], w=[qT.b])
            ps = self.nps()
            self.inproj(hT, wkv_, 0, tq, [wkv_.b, hT.b], ps)
            for kv in range(2):
                hs = slice(64 * kv, 64 * kv + 64)
                self.cp("act", kTz[kv][hs, ts_], ps[hs, 0:QS], r=[ps.b], w=[kTz[kv].b])
        for tt_ in range(TT):
            ps = self.nps()
            for kc in range(8):
                self.mm(ps[:, 0:128], hT[:, kc, tt_ * 128:(tt_ + 1) * 128], wkv_[:, kc, 128:256], start=(kc == 0), stop=(kc == 7),
                        r=[wkv_.b, hT.b], w=[ps.b])
            self.cp("act", vtk[:, tt_, :], ps[:, 0:128], r=[ps.b], w=[vtk.b])
        pTs = [[sb(st, f"pT{j}_{i}", [128, 512], BF16) for i in range(3)] for j in range(2)]
        dens = [sb(st, f"den{j}", [128, 256], F32) for j in range(2)]

        def att_iter(kv, qb, par):
            den = dens[par]
            kbs = [kb for kb in (qb - 1, qb, qb + 1) if 0 <= kb < TT]
            psO = self.nps(f"attO{par}")
            psD = self.nps(f"attD{par}")
            for ki, kb in enumerate(kbs):
                rel = kb - qb + 1
                psS = self.nps(f"attS{par}")
                pT = pTs[par][ki]
                self.mm(psS[:, :], kTz[kv][:, kb * 128:(kb + 1) * 128], qT[:, qb, :, :].rearrange("p g q -> p (g q)"),
                        start=True, stop=False, r=[kTz[kv].b, qT.b], w=[psS.b])
                self.mm(psS[:, :], idb[:], bias[:, rel * 2 + kv, :], start=False, stop=True, r=[idb.b, bias.b], w=[psS.b])
                self.act(pT[:], psS[:, :], AF.Exp, r=[psS.b], w=[pT.b])
                yield
                for gl in range(2):
                    hs = slice(64 * gl, 64 * gl + 64)
                    self.mm(psO[hs, 0:256], vtk[:, kb, 64 * kv:64 * kv + 64], pT[:, gl * 256:(gl + 1) * 256],
                            start=(ki == 0), stop=(ki == len(kbs) - 1), r=[vtk.b, pT.b], w=[psO.b], nohold=True)
                    self.mm(psD[hs, 0:256], onesb[:, 0:64], pT[:, gl * 256:(gl + 1) * 256],
                            start=(ki == 0), stop=(ki == len(kbs) - 1), r=[onesb.b, pT.b], w=[psD.b], nohold=True)
                yield
            self.tt("dve", den[:], psD[:, 0:256], esk[:, kv, :, :].rearrange("p a q -> p (a q)"), ALU.add,
                    r=[psD.b, esk.b], w=[den.b])
            self.act(den[:], den[:], AF.Ln, r=[den.b], w=[den.b])
            self.act(den[:], den[:], AF.Exp, scale=-1.0, r=[den.b], w=[den.b])
            yield
            self.tt("dve", yA[:, 2 * kv:2 * kv + 2, qb * 128:(qb + 1) * 128],
                    psO[:, 0:256].rearrange("p (a q) -> p a q", a=2), den[:].rearrange("p (a q) -> p a q", a=2),
                    ALU.mult, r=[psO.b, den.b], w=[yA.b])
            yield

        its = [(kv, qb) for kv in range(2) for qb in range(TT)]
        self.interleave([(lambda slot, kv=kv, qb=qb: att_iter(kv, qb, slot)) for (kv, qb) in its], window=2)

    def merge_gates(self, st, s, env, hT, yA, mrg):
        P, T, QS, NQ, TT = self.P, self.T, self.QS, self.NQ, self.TT
        NS = self.NS
        w_in, idf = env["w_in"], env["idf"]
        x, acc, h2d, B_acc, B_h2d = env["x"], env["acc"], env["h2d"], env["B_acc"], env["B_h2d"]
        wrt, affT = env["wrt"], env["affT"]
        yR = self.yRk
        sb = self.sb
        wpr = sb(st, "wpr", [128, 4, D], BF16)
        wpa = sb(st, "wpa", [128, 4, D], BF16)
        self.load_w(wpr[:], wpr.b, env["wpr_d"])
        self.load_w(wpa[:], wpa.b, env["wpa_d"])
        wgs = [sb(st, f"wg{i}", [128, 8, 1024], BF16) for i in range(2)]
        sg1s = [sb(st, f"sg1_{i}", [128, QS], F32) for i in range(2)]
        sg2s = [sb(st, f"sg2_{i}", [128, QS], F32) for i in range(2)]
        m1s = [sb(st, f"m1_{i}", [128, QS], F32) for i in range(2)]
        m2s = [sb(st, f"m2_{i}", [128, QS], F32) for i in range(2)]
        cg = 1920 + 768

        def gate_iter(oc, tq, par):
            sg1, sg2, m1, m2 = sg1s[par], sg2s[par], m1s[par], m2s[par]
            wg = wgs[oc // 4]
            ol = (oc % 4) * 128
            if oc % 4 == 0 and tq == 0:
                self.load_w(wg[:, :, 0:512], wg.b, w_in[:, :, cg + oc * 128:cg + oc * 128 + 512])
                self.load_w(wg[:, :, 512:1024], wg.b, w_in[:, :, cg + 1024 + oc * 128:cg + 1024 + oc * 128 + 512])
            ts_ = slice(tq * QS, (tq + 1) * QS)
            ps1 = self.nps(f"h{par}")
            self.inproj(hT, wg, ol, tq, [wg.b, hT.b], ps1)
            self.act(sg1[:], ps1[:, 0:QS], AF.Sigmoid, r=[ps1.b], w=[sg1.b])
            yield
            ps2 = self.nps(f"h{par}")
            self.inproj(hT, wg, 512 + ol, tq, [wg.b, hT.b], ps2)
            self.act(sg2[:], ps2[:, 0:QS], AF.Sigmoid, r=[ps2.b], w=[sg2.b])
            yield
            ps3 = self.nps(f"h{par}")
            for kc in range(4):
                self.mm(ps3[:, 0:QS], wpr[:, kc, oc * 128:(oc + 1) * 128], yR[:, kc, ts_], start=(kc == 0), stop=(kc == 3),
                        r=[wpr.b, yR.b], w=[ps3.b])
            self.tt("dve", m1[:], sg1[:], ps3[:, 0:QS], ALU.mult, r=[sg1.b, ps3.b], w=[m1.b])
            yield
            ps4 = self.nps(f"h{par}")
            for kc in range(4):
                self.mm(ps4[:, 0:QS], wpa[:, kc, oc * 128:(oc + 1) * 128], yA[:, kc, ts_], start=(kc == 0), stop=(kc == 3),
                        r=[wpa.b, yA.b], w=[ps4.b])
            self.tt("dve", m2[:], sg2[:], ps4[:, 0:QS], ALU.mult, r=[sg2.b, ps4.b], w=[m2.b])
            yield
            self.tt("pool", mrg[:, oc, ts_], m1[:], m2[:], ALU.add, r=[m1.b, m2.b], w=[mrg.b])
            yield

        its = [(oc, tq) for oc in range(8) for tq in range(NQ)]
        self.interleave([(lambda slot, oc=oc, tq=tq: gate_iter(oc, tq, slot)) for (oc, tq) in its], window=2)

    def out_proj(self, st, s, env, mrg):
        P, T, QS, NQ, TT = self.P, self.T, self.QS, self.NQ, self.TT
        idf = env["idf"]
        x, acc, h2d, B_acc, B_h2d = env["x"], env["acc"], env["h2d"], env["B_acc"], env["B_h2d"]
        wrt, affT = env["wrt"], env["affT"]
        sb = self.sb
        wout = sb(st, "wout", [128, 8, D], BF16)
        self.load_w(wout[:], wout.b, env["wout_d"])
        g2bc = sb(st, "g2bc", [128, D], F32)
        self.dma(g2bc.t[:], env["g2r_d"].partition_broadcast(128), w=[g2bc.b])
        xts = [sb(st, f"xm{i}", [128, D], F32) for i in range(2)]
        x1s = [sb(st, f"x1{i}", [128, D], F32) for i in range(2)]
        h2s = [sb(st, f"h2{i}", [128, D], F32) for i in range(2)]
        h2bs = [sb(st, f"h2b{i}", [128, D], BF16) for i in range(2)]
        h2Ts = [sb(st, f"h2T{i}", [128, 8, 128], F32) for i in range(2)]
        junk = sb(st, "junk2", [128, D], F32)
        sst = [sb(st, f"sm{i}", [128, 8], F32) for i in range(2)]
        lgs = [sb(st, f"lg{i}", [128, NEXP], F32) for i in range(2)]
        affts = [sb(st, f"afft{i}", [128, 128], F32) for i in range(2)]
        for a_ in affts:
            self.ms("pool", a_[:], 0.0, w=[a_.b])
        def tile_iter(tt_, slot):
                xt, x1, h2, h2b, ss = xts[slot], x1s[slot], h2s[slot], h2bs[slot], sst[slot]
                h2T, lg, afft = h2Ts[slot], lgs[slot], affts[slot]
                r0 = s * T + tt_ * 128
                tl = slice(tt_ * 128, (tt_ + 1) * 128)
                self.dma(xt.t[:], x[r0:r0 + 128, :], w=[xt.b])
                for half in range(2):
                    ps = self.nps(f"h{slot}")
                    for kc in range(8):
                        self.mm(ps[:, :], mrg[:, kc, tl], wout[:, kc, half * 512:(half + 1) * 512], start=(kc == 0), stop=(kc == 7),
                                r=[mrg.b, wout.b], w=[ps.b])
                    self.tt("dve", x1[:, half * 512:(half + 1) * 512], xt[:, half * 512:(half + 1) * 512], ps[:, :], ALU.add,
                            r=[xt.b, ps.b], w=[x1.b])
                yield
                self.dma(acc[r0:r0 + 128, :], x1[:], r=[x1.b], w=[B_acc[s]])
                self.act(junk[:], x1[:], AF.Square, accum=ss[:, 0:1], r=[x1.b], w=[junk.b, ss.b])
                self.ts("dve", ss[:, 1:2], ss[:, 0:1], 1.0 / D, NORM_EPS, ALU.mult, ALU.add, r=[ss.b], w=[ss.b])
                self.act(ss[:, 2:3], ss[:, 1:2], AF.Ln, r=[ss.b], w=[ss.b])
                self.act(ss[:, 3:4], ss[:, 2:3], AF.Exp, scale=-0.5, r=[ss.b], w=[ss.b])
                self.ts("dve", h2[:], x1[:], ss[:, 3:4], None, ALU.mult, r=[x1.b, ss.b], w=[h2.b])
                self.tt("pool", h2b[:], h2[:], g2bc[:], ALU.mult, r=[h2.b, g2bc.b], w=[h2b.b])
                self.dma(h2d[r0:r0 + 128, :], h2b[:], r=[h2b.b], w=[B_h2d[s]])
                yield
                for k2 in range(2):
                    ps = self.nps(f"h{slot}")
                    for kk in range(4):
                        kc = k2 * 4 + kk
                        self.tr(ps[:, kk * 128:(kk + 1) * 128], h2[:, kc * 128:(kc + 1) * 128], idf[:], r=[h2.b, idf.b], w=[ps.b])
                    self.cp("act", h2T[:, k2 * 4:(k2 + 1) * 4, :], ps[:, :].rearrange("p (k n) -> p k n", k=4), r=[ps.b], w=[h2T.b])
                yield
                ps = self.nps(f"h{slot}")
                for kc in range(8):
                    self.mm(ps[:, 0:NEXP], h2T[:, kc, :], wrt[:, kc, :], start=(kc == 0), stop=(kc == 7), r=[h2T.b, wrt.b], w=[ps.b])
                self.P.emit("dve", lambda e, o=ss[:, 4:5], i=ps[:, 0:NEXP]: e.reduce_max(out=o, in_=i, axis=AX.X), [ps.b], [ss.b])
                self.ts("dve", ss[:, 5:6], ss[:, 4:5], -1.0, None, ALU.mult, r=[ss.b], w=[ss.b])
                self.act(lg[:], ps[:, 0:NEXP], AF.Exp, bias=ss[:, 5:6], accum=ss[:, 6:7], r=[ps.b, ss.b], w=[lg.b, ss.b])
                self.P.emit("dve", lambda e, o=ss[:, 7:8], i=ss[:, 6:7]: e.reciprocal(out=o, in_=i), [ss.b], [ss.b])
                self.ts("dve", afft[:, 32 * s:32 * s + NEXP], lg[:], ss[:, 7:8], None, ALU.mult, r=[lg.b, ss.b], w=[afft.b])
                ps = self.nps(f"h{slot}")
                self.tr(ps[:, 0:128], afft[:], idf[:], r=[afft.b, idf.b], w=[ps.b])
                self.tt("dve", affT[:, tl], affT[:, tl], ps[:, 0:128], ALU.add, r=[ps.b, affT.b], w=[affT.b])


        self.interleave([(lambda slot, tt_=tt_: tile_iter(tt_, slot)) for tt_ in range(TT)], window=2)

    def phase_b(self, env):
        P, NS, T, CAP, SB_, NB = self.P, self.NS, self.T, self.CAP, self.SB, self.NB
        affT, idf, idb, g2 = env["affT"], env["idf"], env["idb"], env["g2"]
        acc, h2d, B_acc, B_h2d = env["acc"], env["h2d"], env["B_acc"], env["B_h2d"]
        sb = self.sb
        with ExitStack() as st:
            wk = sb(st, "wk", [128, T], F32)
            mv = sb(st, "mv", [128, CAP], F32)
            mi = sb(st, "mi", [128, CAP], U32)
            mif = sb(st, "mif", [128, CAP], F32)
            offs = sb(st, "offs", [128, 128], F32)
            idxT = sb(st, "idxT", [128, NB, 128], I32)
            valT = sb(st, "valT", [128, NB, 128], F32)
            self.dma(offs.t[:], env["c_offs"], w=[offs.b])
            self.cp("dve", wk[:], affT[:], r=[affT.b], w=[wk.b])
            for r_ in range(CAP // 8):
                sl = slice(r_ * 8, r_ * 8 + 8)
                self.P.emit("dve", lambda e, o=mv[:, sl], i=wk[:]: e.max(out=o, in_=i), [wk.b], [mv.b])
                self.P.emit("dve", lambda e, o=mi[:, sl], m=mv[:, sl], i=wk[:]: e.max_index(out=o, in_max=m, in_values=i),
                            [wk.b, mv.b], [mi.b])
                self.P.emit("dve", lambda e, o=wk[:], m=mv[:, sl], i=wk[:]: e.match_replace(
                    out=o, in_to_replace=m, in_values=i, imm_value=-1.0), [wk.b, mv.b], [wk.b])
            self.cp("dve", mif[:], mi[:], r=[mi.b], w=[mif.b])
            for blk in range(NB):
                bs = slice(blk * SB_, (blk + 1) * SB_)
                ps = self.nps()
                self.tr(ps[0:SB_, 0:128], mif[:, bs], idf[:], r=[mif.b, idf.b], w=[ps.b])
                self.tt("dve", idxT[0:SB_, blk, :], ps[0:SB_, 0:128], offs[0:SB_, :], ALU.add, r=[ps.b, offs.b], w=[idxT.b])
                ps = self.nps()
                self.tr(ps[0:SB_, 0:128], mv[:, bs], idf[:], r=[mv.b, idf.b], w=[ps.b])
                self.cp("act", valT[0:SB_, blk, :], ps[0:SB_, 0:128], r=[ps.b], w=[valT.b])
            wgs = [sb(st, f"ewg{i}", [128, 8, D], BF16) for i in range(2)]
            wus = [sb(st, f"ewu{i}", [128, 8, D], BF16) for i in range(2)]
            wds = [sb(st, f"ewd{i}", [128, 8, D], BF16) for i in range(2)]
            NPF = 1
            G = 2 if NS % 2 == 0 else 1
            NBG = NB * G
            xss = [sb(st, f"xs{i}", [128, D], BF16) for i in range((NPF + 1) * NBG)]
            xsT = sb(st, "xsT", [128, 8, G * CAP], BF16)
            hid = sb(st, "hid", [128, 8, G * CAP], BF16)
            sl_ = sb(st, "silu", [128, G * CAP], F32)
            yss = [sb(st, f"ys{i}", [128, D], F32) for i in range(NBG)]
            its = [(e, g) for e in range(NEXP) for g in range(NS // G)]
            last_sc = {s: [] for s in range(NS)}

            def load_expert(e):
                for dst, src in ((wgs[e % 2], env["eg_d"]), (wus[e % 2], env["eu_d"]), (wds[e % 2], env["ed_d"])):
                    self.load_w(dst[:], dst.b, src[e].rearrange("(k p) n -> p k n", p=128))

            def gather(i):
                e, g = its[i]
                for j in range(G):
                    s = g * G + j
                    pcol = 32 * s + e
                    for blk in range(NB):
                        xs = xss[(i % (NPF + 1)) * NBG + j * NB + blk]
                        ia = idxT[0:SB_, blk, pcol:pcol + 1]
                        self.P.emit("pool", lambda en, o=xs[0:SB_, :], ia=ia: en.indirect_dma_start(
                            out=o, out_offset=None, in_=h2d, in_offset=bass.IndirectOffsetOnAxis(ap=ia, axis=0)),
                            [idxT.b, B_h2d[s]], [xs.b], dma=True)

            load_expert(0)
            for i in range(min(NPF, len(its))):
                gather(i)
            GC = G * CAP
            for i, (e, g) in enumerate(its):
                wg, wu, wd = wgs[e % 2], wus[e % 2], wds[e % 2]
                if g == 0 and e + 1 < NEXP:
                    load_expert(e + 1)
                if i + NPF < len(its):
                    gather(i + NPF)
                for jb in range(NBG):
                    xs = xss[(i % (NPF + 1)) * NBG + jb]
                    ps = self.nps()
                    psv = ps.t.bitcast(BF16)
                    for kc in range(8):
                        self.tr(psv[:, kc * 128:kc * 128 + SB_], xs[0:SB_, kc * 128:(kc + 1) * 128], idb[0:SB_, 0:SB_],
                                r=[xs.b, idb.b], w=[ps.b])
                    self.cp("act", xsT[:, :, jb * SB_:(jb + 1) * SB_],
                            psv[:, :].rearrange("p (k n) -> p k n", k=8)[:, :, 0:SB_], r=[ps.b], w=[xsT.b])
                for fc in range(8):
                    psg = self.nps()
                    psu = self.nps()
                    for kc in range(8):
                        self.mm(psg[:, 0:GC], wg[:, kc, fc * 128:(fc + 1) * 128], xsT[:, kc, :], start=(kc == 0), stop=(kc == 7),
                                r=[wg.b, xsT.b], w=[psg.b])
                    for kc in range(8):
                        self.mm(psu[:, 0:GC], wu[:, kc, fc * 128:(fc + 1) * 128], xsT[:, kc, :], start=(kc == 0), stop=(kc == 7),
                                r=[wu.b, xsT.b], w=[psu.b])
                    self.act(sl_[:], psg[:, 0:GC], AF.Silu, r=[psg.b], w=[sl_.b])
                    self.tt("dve", hid[:, fc, :], sl_[:], psu[:, 0:GC], ALU.mult, r=[sl_.b, psu.b], w=[hid.b])
                for j in range(G):
                    s = g * G + j
                    pcol = 32 * s + e
                    new_sc = []
                    for blk in range(NB):
                        jb = j * NB + blk
                        ys = yss[jb]
                        for half in range(2):
                            ps = self.nps()
                            for fc in range(8):
                                self.mm(ps[0:SB_, :], hid[:, fc, jb * SB_:(jb + 1) * SB_], wd[:, fc, half * 512:(half + 1) * 512],
                                        start=(fc == 0), stop=(fc == 7), r=[hid.b, wd.b], w=[ps.b])
                            self.act(ys[0:SB_, half * 512:(half + 1) * 512], ps[0:SB_, :], AF.Copy,
                                     scale=valT[0:SB_, blk, pcol:pcol + 1], r=[ps.b, valT.b], w=[ys.b])
                        ia = idxT[0:SB_, blk, pcol:pcol + 1]
                        op = self.P.emit("pool", lambda en, i_=ys[0:SB_, :], ia=ia: en.indirect_dma_start(
                            out=acc, out_offset=bass.IndirectOffsetOnAxis(ap=ia, axis=0), in_=i_, in_offset=None,
                            compute_op=ALU.add), [idxT.b, ys.b, B_acc[s]], [], dma=True)
                        for d_ in last_sc[s]:
                            self.P._dep(op, d_)
                        new_sc.append(op)
                    last_sc[s] = new_sc
            P.stage_end()

    def phase_c(self, env):
        P, NS, T, TT = self.P, self.NS, self.T, self.TT
        acc, y, B_acc = env["acc"], env["y"], env["B_acc"]
        sb = self.sb
        outs = []
        with ExitStack() as st:
            gF = sb(st, "gF", [128, D], F32)
            self.dma(gF.t[:], env["gF_d"].partition_broadcast(128), w=[gF.b])
            NW = 4
            ats = [sb(st, f"at{i}", [128, D], F32) for i in range(NW)]
            ots = [sb(st, f"ot{i}", [128, D], F32) for i in range(NW)]
            junks = [sb(st, f"junk3_{i}", [128, D], F32) for i in range(2)]
            sst = [sb(st, f"sc{i}", [128, 4], F32) for i in range(NW)]

            def c_iter(n, s, tt_):
                at, ot, ss, junk = ats[n], ots[n], sst[n], junks[n % 2]
                r0 = s * T + tt_ * 128
                self.dma(at.t[:], acc[r0:r0 + 128, :], r=[B_acc[s]], w=[at.b])
                yield
                self.act(junk[:], at[:], AF.Square, accum=ss[:, 0:1], r=[at.b], w=[junk.b, ss.b])
                self.ts("dve", ss[:, 1:2], ss[:, 0:1], 1.0 / D, NORM_EPS, ALU.mult, ALU.add, r=[ss.b], w=[ss.b])
                self.act(ss[:, 2:3], ss[:, 1:2], AF.Sqrt, r=[ss.b], w=[ss.b])
                self.P.emit("dve", lambda e, o=ss[:, 3:4], i=ss[:, 2:3]: e.reciprocal(out=o, in_=i), [ss.b], [ss.b])
                yield
                self.stt(ot[:], at[:], ss[:, 3:4], gF[:], ALU.mult, ALU.mult, r=[at.b, ss.b, gF.b], w=[ot.b])
                self.P.emit("sp", lambda e, o=y[r0:r0 + 128, :], i=ot[:]: e.dma_start(out=o, in_=i), [ot.b], [], dma=True)
                yield

            fin_b = P.buf("yout")
            gens = []
            n = 0
            for s in range(NS):
                for tt_ in range(TT):
                    gens.append(lambda slot, s=s, tt_=tt_: c_iter(slot, s, tt_))
                    n += 1
            self.interleave(gens, window=NW)
            outs = []
            fin = P.buf("fin")
            op = P.emit("sp", lambda en: en.nop(), writes=[fin])
            for d_ in outs:
                P._dep(op, d_)
            P.stage_end()


def host_consts(T):
    c = {}
    c["c_idf"] = np.eye(128, dtype=np.float32)
    blk = (np.arange(128)[:, None] // 64 == np.arange(128)[None, :] // 64).astype(np.float32)
    c["c_bo"] = blk
    c["c_ba"] = blk / 64.0
    s = (np.arange(128) % 64)[:, None]
    j = np.arange(128)[None, :]
    t = j % 64
    isr = j >= 64
    mT = np.zeros((128, 2, 4, 128), np.float32)
    mT[:, 0] = np.where(isr, s <= t, s < t)[:, None, :]
    mT[:, 1] = np.where(isr, s >= t, s > t)[:, None, :]
    c["c_mT"] = mT.reshape(128, 2, 512)
    tt = (np.arange(128) % 64)[:, None]
    ss = np.arange(64)[None, :]
    mN = np.zeros((128, 2, 8, 64), np.float32)
    mN[:, 0] = (ss < tt)[:, None, :]
    mN[:, 1] = (ss > tt)[:, None, :]
    c["c_mN"] = mN.reshape(128, 2, 512)
    idr = np.zeros((128, 8, 64), np.float32)
    idr[:] = (ss == tt)[:, None, :]
    c["c_idr"] = idr.reshape(128, 512)
    slopes = 2.0 ** (-8.0 * np.arange(1, 9) / 8)
    key = np.arange(128)[:, None]
    qq = np.arange(128)[None, :]
    bias = np.zeros((128, 3, 2, 4, 128), np.float32)
    for rel in (-1, 0, 1):
        dist = np.abs(rel * 128 + key - qq)
        for kv in range(2):
            for sg in range(4):
                g = 2 * (sg % 2) + sg // 2
                bias[:, rel + 1, kv, sg, :] = np.where(dist <= 128, -slopes[kv * 4 + g] * dist, -1e30)
    c["c_bias"] = bias.reshape(128, 6, 512)
    offs = np.zeros((128, 128), np.float32)
    offs[:] = ((np.arange(128) // 32) * T)[None, :]
    c["c_offs"] = offs
    return c


def host_params(inp):
    f = lambda a: np.ascontiguousarray(np.asarray(a, dtype=np.float32))
    m = {}
    m["w_in"] = f(inp["w_in"][0])
    m["gmixr"] = f(inp["norm_mix_g"][0].reshape(1, D))
    m["g2r"] = f(inp["norm_ffn_g"][0].reshape(1, D))
    m["mu"] = f(np.stack([inp["mu_prev"][0].reshape(15, 128).T, inp["mu_next"][0].reshape(15, 128).T], axis=-1))
    names = ["w0_f", "w0_b", "a0_f", "a0_b", "k_k", "k_a", "r_k", "ln_x_w", "ln_x_b"]
    m["pp"] = f(np.stack([np.asarray(inp[n][0]).reshape(4, 128).T for n in names], axis=-1))
    m["sink"] = f(inp["attn_sink"][0].reshape(1, 8))
    m["g2"] = f(inp["norm_ffn_g"][0].reshape(8, 128).T)
    m["gF"] = f(np.asarray(inp["norm_final_g"]).reshape(1, D))
    m["wup"] = f(np.concatenate([inp["w_up_f"][0], inp["w_up_b"][0]], axis=0))
    m["aup"] = f(np.concatenate([inp["a_up_f"][0], inp["a_up_b"][0]], axis=0))
    m["gup"] = f(inp["g_up"][0])
    m["wpr"] = f(inp["w_proj_rwkv"][0])
    m["wpa"] = f(inp["w_proj_attn"][0])
    m["wout"] = f(inp["w_out"][0])
    m["wr"] = f(inp["w_router"][0])
    m["eg"] = f(inp["exp_w_gate"][0])
    m["eu"] = f(inp["exp_w_up"][0])
    m["ed"] = f(inp["exp_w_down"][0])
    return m


_NC_CACHE = {}


def run(inp, n_cores=8, stop=None, dbg=False, raw=False):
    x = np.asarray(inp["x"], dtype=np.float32)
    B, T, _ = x.shape
    NS = B // n_cores
    key = (NS, T, stop, dbg)
    if key not in _NC_CACHE:
        _NC_CACHE[key] = Builder(NS, T, stop, dbg).build()
    nc = _NC_CACHE[key]
    shared = host_params(inp)
    shared.update(host_consts(T))
    in_maps = []
    for c in range(n_cores):
        m = dict(shared)
        m["x"] = np.ascontiguousarray(x[c * NS:(c + 1) * NS].reshape(NS * T, D))
        in_maps.append(m)
    res = run_bass_kernel_spmd(nc, in_maps, core_ids=list(range(n_cores)))
    if raw:
        return res.results
    out = np.concatenate([r["y"].reshape(NS, T, D) for r in res.results], axis=0)
    return out.astype(np.float32)


def kernel(**inputs):
    return run(inputs, 8)
```

```python
import math
from contextlib import ExitStack
import numpy as np
import concourse.bass as bass
import concourse.mybir as mybir
from concourse.bass_utils import run_bass_kernel_spmd

F32 = mybir.dt.float32
BF16 = mybir.dt.bfloat16
U32 = mybir.dt.uint32
I32 = mybir.dt.int32
AF = mybir.ActivationFunctionType
ALU = mybir.AluOpType
AX = mybir.AxisListType

ENGS = ("pe", "dve", "act", "pool", "sp")
SEM_WRAP = 4000
NDMA_SEM = 12
NBANK = {'pe': 24, 'dve': 12, 'act': 12, 'pool': 6, 'sp': 2}

D = 1024
DS = math.exp(-0.5)
LNX_EPS = 64e-5
NORM_EPS = 1e-6
NEXP = 16
L = 64


class Buf:
    __slots__ = ("name", "w", "r")

    def __init__(self, name):
        self.name = name
        self.w = None
        self.r = []


class Op:
    __slots__ = ("eng", "fn", "waits", "signal", "idx", "dma", "tk", "sigval", "gid", "ninc")

    def __init__(self, eng, fn, dma):
        self.eng = eng
        self.fn = fn
        self.waits = []
        self.signal = False
        self.dma = dma
        self.tk = None
        self.sigval = None


class Prog:
    def __init__(self, nc, stack):
        self.nc = nc
        self.bufs = []
        self.sigcount = {e: 0 for e in ENGS}
        self.ndma = {e: 0 for e in ENGS}
        self.sems = {e: [stack.enter_context(nc.semaphore(f"s_{e}_{i}")) for i in range(NBANK[e])] for e in ENGS}
        self.dsems = {e: [stack.enter_context(nc.semaphore(f"d_{e}_{i}")) for i in range(NDMA_SEM)]
                      for e in ("sp", "pool", "act")}
        self.gid = 0
        self._reset()

    def _reset(self):
        self.q = {e: [] for e in ENGS}
        self.seen = {e: {} for e in ENGS}
        self.seen_dma = {e: set() for e in ENGS}
        self.dma_hist = {e: {} for e in ENGS}
        self.pending_dma = []
        for b in self.bufs:
            b.w = None
            b.r = []

    def buf(self, name="b"):
        b = Buf(name)
        self.bufs.append(b)
        return b

    def _dep(self, op, d):
        eng = op.eng
        if d is op:
            return
        if d.dma:
            if d.gid in self.seen_dma[eng]:
                return
            self.seen_dma[eng].add(d.gid)
            op.waits.append(d)
        else:
            if d.eng == eng and eng == "pe":
                return
            if self.seen[eng].get(d.eng, -1) >= d.idx:
                return
            self.seen[eng][d.eng] = d.idx
            d.signal = True
            op.waits.append(d)

    capture = None

    def emit(self, eng, fn, reads=(), writes=(), dma=False, ninc=1, est=None, hold=False):
        if self.capture is not None:
            self.capture.append((eng, fn, tuple(reads), tuple(writes), dma, est, hold))
            return None
        op = Op(eng, fn, dma)
        op.ninc = ninc
        op.gid = self.gid
        self.gid += 1
        op.idx = len(self.q[eng])
        deps = []
        for b in reads:
            if b.w is not None:
                deps.append(b.w)
        for b in writes:
            if b.w is not None:
                deps.append(b.w)
            deps.extend(b.r)
        for d in deps:
            self._dep(op, d)
        if dma:
            j = self.ndma[eng]
            self.ndma[eng] += 1
            op.tk = (j % NDMA_SEM, 16 * (j // NDMA_SEM + 1))
            hist = self.dma_hist[eng]
            if (j - NDMA_SEM) in hist:
                self._dep(op, hist[j - NDMA_SEM])
            hist[j] = op
            self.pending_dma.append(op)
        for b in reads:
            if not dma:
                b.r = [x for x in b.r if x.dma or x.eng != eng]
            b.r.append(op)
        for b in writes:
            b.w = op
            b.r = []
        self.q[eng].append(op)
        return op

    def barrier(self):
        pend = self.pending_dma
        self.pending_dma = []
        last = []
        for e in ENGS:
            for op in reversed(self.q[e]):
                if not op.dma and not getattr(op.fn, "_is_nop", False):
                    last.append(op)
                    break
        for e in ENGS:
            fn = lambda en: en.nop()
            op = self.emit(e, fn)
            for d in last:
                self._dep(op, d)
            for d in pend:
                self._dep(op, d)

    def stage_end(self):
        self.barrier()

    def flush(self):
        self.barrier()
        nc = self.nc
        base = dict(self.sigcount)
        for e in ENGS:
            c = base[e]
            for op in self.q[e]:
                if op.signal:
                    assert (c % SEM_WRAP) + op.ninc <= SEM_WRAP
                    c += op.ninc
                    op.sigval = c
            self.sigcount[e] = c
        sems, dsems = self.sems, self.dsems

        def resolve(d):
            if d.dma:
                return dsems[d.eng][d.tk[0]], d.tk[1]
            v = d.sigval - 1
            assert v // SEM_WRAP < NBANK[d.eng], (d.eng, v)
            return sems[d.eng][v // SEM_WRAP], v % SEM_WRAP + 1

        def run(e, engobj):
            for op in self.q[e]:
                for d in op.waits:
                    s, v = resolve(d)
                    engobj.wait_ge(s, v)
                ins = op.fn(engobj)
                if op.dma:
                    ins.then_inc(dsems[e][op.tk[0]], 16)
                elif op.signal:
                    v = op.sigval - 1
                    assert v // SEM_WRAP < NBANK[e], (e, v)
                    ins.then_inc(sems[e][v // SEM_WRAP], 1)

        with nc.Block() as block:
            @block.tensor
            def _(t):
                run("pe", t)

            @block.vector
            def _(v):
                run("dve", v)

            @block.scalar
            def _(s):
                run("act", s)

            @block.gpsimd
            def _(g):
                run("pool", g)

            @block.sync
            def _(sp):
                run("sp", sp)
        self._reset()


class Tl:
    def __init__(self, t, b):
        self.t = t
        self.b = b

    def __getitem__(self, k):
        return self.t[k]


class _Stop(Exception):
    pass


class Builder:
    def __init__(self, NS, T, stop=None, dbg=False):
        self.stop, self.dbg = stop, dbg
        self.NS, self.T = NS, T
        self.QS = min(512, T)
        self.NQ = T // self.QS
        self.TT = T // 128
        self.NCH = T // L
        self.CQ = self.QS // L
        self.CAP = 2 * T // NEXP
        self.SB = min(128, self.CAP)
        self.NB = self.CAP // self.SB
        self.nc = bass.Bass("TRN2", target_bir_lowering=False)
        self.stack = ExitStack()
        self.P = Prog(self.nc, self.stack)
        self.psi = 0

    def sb(self, st, name, shape, dt):
        self.nsb = getattr(self, "nsb", 0) + 1
        name = f"{name}__{self.nsb}"
        t = st.enter_context(self.nc.sbuf_tensor(name, list(shape), dt))
        return Tl(t, self.P.buf(name))

    def chk(self, k):
        if self.stop == k:
            self.P.flush()
            raise _Stop()

    def dram_in(self, name, shape, dt=F32):
        return self.nc.dram_tensor(name, list(shape), dt, kind="ExternalInput").ap()

    def nps(self, grp=None):
        if grp is None:
            p = self.ps[self.psi % 8]
            self.psi += 1
            return p
        banks = {"chain": (0, 1, 2), "post": (3,), "A": (4, 5), "B": (6, 7), "attO0": (0,), "attD0": (1,), "attO1": (2,), "attD1": (3,),
                 "attS0": (4, 5), "attS1": (6, 7), "h0": (0, 1, 2, 3), "h1": (4, 5, 6, 7),
                 "t0": (0, 1, 2), "t1": (3, 4, 5), "t2": (6, 7)}[grp]
        self.psg = getattr(self, "psg", {})
        i = self.psg.get(grp, 0)
        self.psg[grp] = i + 1
        return self.ps[banks[i % len(banks)]]

    def mm(self, out, lhsT, rhs, start=True, stop=True, r=(), w=(), nohold=False):
        self.P.emit("pe", lambda e: e.matmul(out, lhsT=lhsT, rhs=rhs, start=start, stop=stop), r, w,
                    est=max(64, rhs.free_size()) / 2400.0 + 0.004, hold=(not stop) and not nohold)

    def tr(self, out, in_, ident, r=(), w=()):
        self.P.emit("pe", lambda e: e.transpose(out=out, in_=in_, identity=ident), r, w, est=0.06)

    def act(self, out, in_, func, bias=None, scale=None, accum=None, r=(), w=()):
        kw = {}
        if bias is not None:
            kw["bias"] = bias
        if scale is not None:
            kw["scale"] = scale
        if accum is not None:
            kw["accum_out"] = accum
        self.P.emit("act", lambda e: e.activation(out=out, in_=in_, func=func, **kw), r, w, ninc=1,
                    est=(224 + out.free_size()) / 1200.0)

    def tt(self, eng, out, in0, in1, op, r=(), w=()):
        self.P.emit(eng, lambda e: e.tensor_tensor(out=out, in0=in0, in1=in1, op=op), r, w, est=self._est(eng, out))

    def ts(self, eng, out, in0, s1, s2, op0, op1=None, r=(), w=()):
        if op1 is None:
            self.P.emit(eng, lambda e: e.tensor_scalar(out=out, in0=in0, scalar1=s1, scalar2=None, op0=op0), r, w,
                        est=self._est(eng, out))
        else:
            self.P.emit(eng, lambda e: e.tensor_scalar(out=out, in0=in0, scalar1=s1, scalar2=s2, op0=op0, op1=op1), r, w,
                        est=self._est(eng, out))

    def stt(self, out, in0, scalar, in1, op0, op1, r=(), w=()):
        self.P.emit("dve", lambda e: e.scalar_tensor_tensor(out=out, in0=in0, scalar=scalar, in1=in1, op0=op0, op1=op1), r, w,
                    est=self._est("dve", out))

    def cp(self, eng, out, in_, r=(), w=()):
        if eng == "act":
            self.P.emit("act", lambda e: e.activation(out=out, in_=in_, func=AF.Copy), r, w, est=(224 + out.free_size()) / 1200.0)
        else:
            self.P.emit(eng, lambda e: e.tensor_copy(out=out, in_=in_), r, w, est=self._est(eng, out))

    def ms(self, eng, ap, val, w=()):
        self.P.emit(eng, lambda e: e.memset(ap, val), (), w, est=self._est(eng, ap))

    def _est(self, eng, out):
        n = out.free_size()
        if eng == "pool":
            return (150 + 1.6 * n) / 1200.0
        return (100 + n) / 960.0

    def interleave(self, gens, window=None):
        P = self.P
        allg = [g for g in gens if g is not None]
        if window is None:
            window = len(allg)
        pending = allg[window:]

        def mk(f, slot):
            return {"g": (f(slot) if callable(f) else f), "q": [], "done": False}
        streams = [mk(g, i_) for i_, g in enumerate(allg[:window])]
        eng_free = getattr(self, "_sim_eng", None)
        if eng_free is None:
            eng_free = self._sim_eng = {e: 0.0 for e in ENGS}
            self._sim_ready = {}
            self._sim_lastrd = {}
        ready, lastrd = self._sim_ready, self._sim_lastrd

        def fill(st_):
            while not st_["q"] and not st_["done"]:
                P.capture = st_["q"]
                try:
                    next(st_["g"])
                except StopIteration:
                    st_["done"] = True
                finally:
                    P.capture = None

        def start_time(o):
            eng, fn, rd, wr, dma, est, hold = o
            t = eng_free[eng]
            for b in rd:
                t = max(t, ready.get(b, (0.0, None))[0] + (0.15 if ready.get(b, (0.0, eng))[1] != eng else 0.05))
            for b in wr:
                t = max(t, ready.get(b, (0.0, None))[0] + 0.05, lastrd.get(b, 0.0) + 0.1)
            return t

        forced = None
        while True:
            for st_ in streams:
                fill(st_)
            for i_, st_ in enumerate(streams):
                if st_["done"] and not st_["q"] and pending:
                    streams[i_] = mk(pending.pop(0), i_)
                    fill(streams[i_])
            live = [st_ for st_ in streams if st_["q"]]
            if not live:
                break
            if forced is not None and forced["q"]:
                best = forced
            else:
                best = min(live, key=lambda st_: start_time(st_["q"][0]))
            o = best["q"].pop(0)
            eng, fn, rd, wr, dma, est, hold = o
            forced = best if hold else None
            t0 = start_time(o)
            dur = est if est is not None else 0.3
            if dma:
                eng_free[eng] = t0 + 0.06
                tend = t0 + 2.5
            else:
                eng_free[eng] = t0 + dur
                tend = t0 + dur
            for b in rd:
                lastrd[b] = max(lastrd.get(b, 0.0), tend)
            for b in wr:
                ready[b] = (tend, eng)
                lastrd[b] = 0.0
            P.emit(eng, fn, rd, wr, dma=dma)

    def dma(self, out, in_, r=(), w=(), eng="sp"):
        return self.P.emit(eng, lambda e: e.dma_start(out=out, in_=in_), r, w, dma=True)

    def load_w(self, dst, dst_b, src, kc=None, n=None, scale=None):
        assert scale is None
        self.dma(dst, src, w=[dst_b], eng="pool")

    def build(self):
        nc, P, NS, T = self.nc, self.P, self.NS, self.T
        QS, NQ, TT, NCH, CQ = self.QS, self.NQ, self.TT, self.NCH, self.CQ
        st0 = self.stack
        din = self.dram_in
        x = din("x", [NS * T, D])
        w_in = din("w_in", [D, 4736]).rearrange("(k p) n -> p k n", p=128)
        gmixr_d = din("gmixr", [1, D])
        g2r_d = din("g2r", [1, D])
        mu_d = din("mu", [128, 15, 2])
        pp_d = din("pp", [128, 4, 9])
        sink_d = din("sink", [1, 8])
        g2_d = din("g2", [128, 8])
        gF_d = din("gF", [1, D])
        wup_d = din("wup", [128, 512])
        aup_d = din("aup", [128, 512])
        gup_d = din("gup", [128, 512])
        wpr_d = din("wpr", [512, D]).rearrange("(k p) n -> p k n", p=128)
        wpa_d = din("wpa", [512, D]).rearrange("(k p) n -> p k n", p=128)
        wout_d = din("wout", [D, D]).rearrange("(k p) n -> p k n", p=128)
        wr_d = din("wr", [D, NEXP]).rearrange("(k p) n -> p k n", p=128)
        eg_d = din("eg", [NEXP, D, D])
        eu_d = din("eu", [NEXP, D, D])
        ed_d = din("ed", [NEXP, D, D])
        c_idf = din("c_idf", [128, 128])
        c_bo = din("c_bo", [128, 128])
        c_ba = din("c_ba", [128, 128])
        c_mT = din("c_mT", [128, 2, 512])
        c_mN = din("c_mN", [128, 2, 512])
        c_idr = din("c_idr", [128, 512])
        c_bias = din("c_bias", [128, 6, 512])
        c_offs = din("c_offs", [128, 128])
        y = nc.dram_tensor("y", [NS * T, D], F32, kind="ExternalOutput").ap()
        ik = "ExternalOutput" if self.dbg else "Internal"
        uS = nc.dram_tensor("uS", [15, 128, T], BF16, kind=ik).ap()
        acc = nc.dram_tensor("acc", [NS * T, D], F32, kind=ik).ap()
        h2d = nc.dram_tensor("h2d", [NS * T, D], BF16, kind="Internal").ap()
        hTd = nc.dram_tensor("hTd", [128, 8, T], BF16, kind="Internal").ap()
        B_hTd = P.buf("hTd")
        if self.dbg:
            self.dbgR = nc.dram_tensor("dbgR", [128, 4, T], BF16, kind="ExternalOutput").ap()
            self.dbgA = nc.dram_tensor("dbgA", [128, 4, T], BF16, kind="ExternalOutput").ap()
        B_uS = [P.buf(f"uS{c}") for c in range(15)]
        B_acc = [P.buf(f"acc{s}") for s in range(NS)]
        B_h2d = [P.buf(f"h2d{s}") for s in range(NS)]

        self.ps = []
        for i in range(8):
            t = st0.enter_context(nc.psum_tensor(f"ps{i}", [128, 512], F32))
            self.ps.append(Tl(t, P.buf(f"ps{i}")))

        sb = self.sb
        idf = sb(st0, "idf", [128, 128], F32)
        idb = sb(st0, "idb", [128, 128], BF16)
        bo = sb(st0, "bo", [128, 128], F32)
        ba = sb(st0, "ba", [128, 128], F32)
        onesb = sb(st0, "onesb", [128, 128], BF16)
        bob = sb(st0, "bob", [128, 128], BF16)
        bab = sb(st0, "bab", [128, 128], BF16)
        mu = sb(st0, "mu", [128, 15, 2], F32)
        mu0 = sb(st0, "mu0", [128, 15], F32)
        pp = sb(st0, "pp", [128, 4, 9], F32)
        omka = sb(st0, "omka", [128, 4], F32)
        npp = sb(st0, "npp", [128, 4, 9], F32)
        onesf = sb(st0, "onesf", [128, 1], F32)
        g2 = sb(st0, "g2", [128, 8], F32)
        wrt = sb(st0, "wrt", [128, 8, NEXP], F32)
        affT = sb(st0, "affT", [128, T], F32)
        self.wst = [sb(st0, f"wst{i}", [128, 8 * NEXP], F32) for i in range(1)]
        self.wsi = 0
        PW0F, PW0B, PA0F, PA0B, PKK, PKA, PRK, PLW, PLB = range(9)

        for (dst, src) in ((idf, c_idf), (bo, c_bo), (ba, c_ba), (mu, mu_d), (pp, pp_d), (g2, g2_d)):
            self.dma(dst.t[:], src, w=[dst.b])
        self.cp("pool", idb[:], idf[:], r=[idf.b], w=[idb.b])
        self.ms("pool", onesb[:], 1.0, w=[onesb.b])
        self.cp("pool", bob[:], bo[:], r=[bo.b], w=[bob.b])
        self.cp("pool", bab[:], ba[:], r=[ba.b], w=[bab.b])
        self.tt("dve", mu0[:], mu[:, :, 0], mu[:, :, 1], ALU.add, r=[mu.b], w=[mu0.b])
        self.ts("dve", mu0[:], mu0[:], -1.0, 1.0, ALU.mult, ALU.add, r=[mu0.b], w=[mu0.b])
        self.ts("dve", omka[:], pp[:, :, PKA], -1.0, 1.0, ALU.mult, ALU.add, r=[pp.b], w=[omka.b])
        self.ts("dve", npp[:], pp[:], -1.0, None, ALU.mult, r=[pp.b], w=[npp.b])
        self.ms("pool", onesf[:], 1.0, w=[onesf.b])
        self.dma(self.wst[0].t[:, 0:8 * NEXP].rearrange("p (k n) -> p k n", k=8), wr_d, w=[self.wst[0].b])
        self.tt("dve", wrt[:], self.wst[0].t[:, 0:8 * NEXP].rearrange("p (k n) -> p k n", k=8),
                g2.t[:].unsqueeze(2).to_broadcast([128, 8, NEXP]),
                ALU.mult, r=[self.wst[0].b, g2.b], w=[wrt.b])
        self.ms("pool", affT[:], 0.0, w=[affT.b])
        self.yRk = sb(st0, "yRk", [128, 4, T], BF16)
        P.stage_end()

        env = locals()
        try:
            self.chk(0)
            for s in range(NS):
                self.phase_a(s, env)
            self.phase_b(env)
            self.chk(6)
            self.phase_c(env)
        except _Stop:
            return nc
        self.P.flush()
        print("signals", self.P.sigcount, "dmas", self.P.ndma)
        self.stack.close()
        return nc

    def norm_hT(self, st, s, env, hT):
        T, TT = self.T, self.TT
        x = env["x"]
        idb = env["idb"]
        gbc = self.sb(st, "gbc", [128, D], F32)
        self.dma(gbc.t[:], env["gmixr_d"].partition_broadcast(128), w=[gbc.b])
        xts = [self.sb(st, f"xt{i}", [128, D], F32) for i in range(2)]
        hns = [self.sb(st, f"hn{i}", [128, D], BF16) for i in range(2)]
        junk = self.sb(st, "junk", [128, D], F32)
        sst = [self.sb(st, f"ss{i}", [128, 4], F32) for i in range(2)]
        for tt_ in range(TT):
            xt, hn, ss = xts[tt_ % 2], hns[tt_ % 2], sst[tt_ % 2]
            r0 = s * T + tt_ * 128
            self.dma(xt.t[:], x[r0:r0 + 128, :], w=[xt.b])
            self.chk(20)
            self.act(junk[:], xt[:], AF.Square, accum=ss[:, 0:1], r=[xt.b], w=[junk.b, ss.b])
            self.chk(21)
            self.ts("dve", ss[:, 1:2], ss[:, 0:1], 1.0 / D, NORM_EPS, ALU.mult, ALU.add, r=[ss.b], w=[ss.b])
            self.act(ss[:, 2:3], ss[:, 1:2], AF.Sqrt, r=[ss.b], w=[ss.b])
            self.chk(22)
            self.P.emit("dve", lambda e, o=ss[:, 3:4], i=ss[:, 2:3]: e.reciprocal(out=o, in_=i), [ss.b], [ss.b])
            self.stt(hn[:], xt[:], ss[:, 3:4], gbc[:], ALU.mult, ALU.mult, r=[xt.b, ss.b, gbc.b], w=[hn.b])
            self.chk(23)
            ps = self.nps()
            psv = ps.t.bitcast(BF16)
            for kc in range(8):
                self.tr(psv[:, kc * 128:(kc + 1) * 128], hn[:, kc * 128:(kc + 1) * 128], idb[:], r=[hn.b, idb.b], w=[ps.b])
            self.chk(24)
            import os
            if os.environ.get("VARX") == "1":
                self.cp("dve", junk.t[:].bitcast(BF16)[:, 0:1024], psv[:, :], r=[ps.b], w=[junk.b])
            elif os.environ.get("VARX") == "2":
                self.cp("dve", hT[:, 0, 0:128], psv[:, 0:128], r=[ps.b], w=[hT.b])
            else:
                self.cp("dve", hT[:, :, tt_ * 128:(tt_ + 1) * 128], psv[:, :].rearrange("p (k n) -> p k n", k=8), r=[ps.b], w=[hT.b])
            self.chk(25)

    def inproj(self, hT, wb, col0, tq, reads, ps):
        QS = self.QS
        for kc in range(8):
            self.mm(ps[:, 0:QS], wb[:, kc, col0:col0 + 128], hT[:, kc, tq * QS:(tq + 1) * QS],
                    start=(kc == 0), stop=(kc == 7), r=reads, w=[ps.b])

    def phase_a(self, s, env):
        P, T, QS, NQ, TT, NCH, CQ = self.P, self.T, self.QS, self.NQ, self.TT, self.NCH, self.CQ
        w_in, mu, mu0, pp, omka = env["w_in"], env["mu"], env["mu0"], env["pp"], env["omka"]
        uS, B_uS = env["uS"], env["B_uS"]
        PW0F, PW0B, PA0F, PA0B, PKK, PKA, PRK, PLW, PLB = range(9)
        sb = self.sb

        with ExitStack() as st:
            hT = sb(st, "hT", [128, 8, T], BF16)
            self.norm_hT(st, s, env, hT)
            self.dma(env["hTd"], hT[:], r=[hT.b], w=[env["B_hTd"]])
            self.chk(10)
            wbs = [sb(st, f"wb{i}", [128, 8, 512], BF16) for i in range(2)]
            upads = [sb(st, f"upad{i}", [128, T + 2], BF16) for i in range(2)]
            tmps = [sb(st, f"tmpA{i}", [128, T], BF16) for i in range(2)]
            uss = [sb(st, f"us{i}", [128, T], BF16) for i in range(2)]
            for upad in upads:
                self.ms("pool", upad[:, 0:1], 0.0, w=[upad.b])
                self.ms("pool", upad[:, T + 1:T + 2], 0.0, w=[upad.b])
            for grp in range(4):
                c0 = grp * 4
                ncol = min(4, 15 - c0)
                wb = wbs[grp % 2]
                self.load_w(wb[:, :, 0:ncol * 128], wb.b, w_in[:, :, c0 * 128:(c0 + ncol) * 128], 8, ncol * 128)
                self.chk(11)
                for ci in range(ncol):
                    c = c0 + ci
                    us = uss[c % 2]
                    upad, tmp = upads[c % 2], tmps[c % 2]
                    for tq in range(NQ):
                        ps = self.nps()
                        self.inproj(hT, wb, ci * 128, tq, [wb.b, hT.b], ps)
                        self.cp("act", upad[:, 1 + tq * QS:1 + (tq + 1) * QS], ps[:, 0:QS], r=[ps.b], w=[upad.b])
                    self.ts("dve", tmp[:], upad[:, 1:T + 1], mu0[:, c:c + 1], None, ALU.mult, r=[upad.b, mu0.b], w=[tmp.b])
                    self.stt(tmp[:], upad[:, 0:T], mu[:, c, 0:1], tmp[:], ALU.mult, ALU.add, r=[upad.b, mu.b, tmp.b], w=[tmp.b])
                    self.stt(us[:], upad[:, 2:T + 2], mu[:, c, 1:2], tmp[:], ALU.mult, ALU.add, r=[upad.b, mu.b, tmp.b], w=[us.b])
                    if c == 12:
                        self.act(us[:], us[:], AF.Sigmoid, r=[us.b], w=[us.b])
                    elif c == 13:
                        self.act(us[:], us[:], AF.Tanh, r=[us.b], w=[us.b])
                    self.dma(uS[c], us[:], r=[us.b], w=[B_uS[c]])
                    self.chk(12)
            P.stage_end()
        self.chk(1)

        with ExitStack() as st:
            self.rwkv(st, s, env, self.yRk)
            if self.dbg and s == 0:
                self.dma(self.dbgR, self.yRk[:], r=[self.yRk.b])
            P.stage_end()
        self.chk(2)

        with ExitStack() as st:
            yA = sb(st, "yA", [128, 4, T], BF16)
            mrg = sb(st, "mrg", [128, 8, T], BF16)
            with ExitStack() as st1:
                hT = sb(st1, "hT", [128, 8, T], BF16)
                self.dma(hT.t[:], env["hTd"], r=[env["B_hTd"]], w=[hT.b])
                with ExitStack() as st2:
                    self.attention(st2, s, env, hT, yA)
                    if self.dbg and s == 0:
                        self.dma(self.dbgA, yA[:], r=[yA.b])
                    P.stage_end()
                self.chk(3)
                self.merge_gates(st1, s, env, hT, yA, mrg)
                P.stage_end()
                self.chk(4)
            with ExitStack() as st1:
                self.out_proj(st1, s, env, mrg)
                P.stage_end()
            self.chk(5)

    def rwkv(self, st, s, env, yR):
        P, T, NCH = self.P, self.T, self.NCH
        QS = min(512, T)
        NQ = T // QS
        CQ = QS // L
        pp, omka, npp, onesf = env["pp"], env["omka"], env["npp"], env["onesf"]
        uS, B_uS = env["uS"], env["B_uS"]
        bo, ba, idb, bob, bab = env["bo"], env["ba"], env["idb"], env["bob"], env["bab"]
        PW0F, PW0B, PA0F, PA0B, PKK, PKA, PRK, PLW, PLB = range(9)
        sb = self.sb
        mT = sb(st, "mT", [128, 2, 512], F32)
        mN = sb(st, "mN", [128, 2, 512], F32)
        idr = sb(st, "idr", [128, 512], F32)
        self.dma(mT.t[:], env["c_mT"], w=[mT.b])
        self.dma(mN.t[:], env["c_mN"], w=[mN.b])
        self.dma(idr.t[:], env["c_idr"], w=[idr.b])
        wup = sb(st, "wup", [128, 512], BF16)
        aup = sb(st, "aup", [128, 512], BF16)
        gup = sb(st, "gup", [128, 512], BF16)
        for dst, src in ((wup, env["wup_d"]), (aup, env["aup_d"]), (gup, env["gup_d"])):
            self.load_w(dst[:], dst.b, src)
        NQ_ = NQ
        bonus = [sb(st, f"bonus{i}", [128, T], BF16) for i in range(2)]
        wkv = [sb(st, f"wkv{i}", [128, T], BF16) for i in range(2)]
        vtok = [sb(st, f"vtok{i}", [128, NCH, L], BF16) for i in range(2)]
        for t_ in vtok:
            t_.bq = [P.buf("vtokq") for _ in range(NQ_)]
        R = []
        for d in range(2):
            Rd = dict(
                ar=sb(st, f"ar{d}", [128, NCH, 2, L], BF16),
                SB=sb(st, f"SB{d}", [128, NCH, L], BF16),
                SK=sb(st, f"SK{d}", [128, NCH, 2, L], BF16),
                TTm=sb(st, f"TT{d}", [128, NCH, L], BF16),
                BB=sb(st, f"BB{d}", [128, NCH, L], BF16),
                KB=sb(st, f"KB{d}", [128, NCH, L], BF16),
                Wtot=sb(st, f"Wtot{d}", [128, NCH], F32),
                ST=sb(st, f"ST{d}", [128, L], BF16),
                Xs=sb(st, f"Xs{d}", [128, L], BF16),
                Us=sb(st, f"Us{d}", [128, L], BF16),
            )
            for k_ in ("ar", "SB", "SK", "TTm", "BB", "KB", "Wtot"):
                Rd[k_].bq = [P.buf(k_ + "q") for _ in range(NQ_)]
            R.append(Rd)

        def q(name, dt=F32):
            return sb(st, name, [128, QS], dt)
        def mk_temps(sfx):
            names_f = ["t2", "sgw", "cs"] + (["X1"] if sfx == "A" else ["X2", "X3"])
            names_b = ["rq", "kq", "vb", "twb", "alb", "kkn", "sq", "t1", "t2b", "t3", "kft", "aqt", "E", "akk", "p1", "p2",
                       "bT", "kT", "Pm", "PTm", "Pm2", "PTm2"]
            d_ = {n: q(n + sfx) for n in names_f}
            d_.update({n: q(n + sfx, BF16) for n in names_b})
            for n in ("X1", "X2", "X3"):
                d_.setdefault(n, None)
            return d_
        TA, TB = mk_temps("A"), mk_temps("B")
        TA["grp"], TB["grp"] = "A", "B"
        sgb, t1, t2, sq = q("sgbP", BF16), q("t1P"), q("t2P"), q("sqP")
        scm = sb(st, "scm", [128, QS], F32)
        self.ms("pool", scm[:], 1.0, w=[scm.b])
        self.ms("pool", scm.t[:].rearrange("p (c l) -> p c l", l=L)[:, :, 0:1], 0.0, w=[scm.b])

        def v3(ap):
            return ap.rearrange("p (c l) -> p c l", l=L)

        def pre_pass(p, d, tq, first, tmp):
            (rq, kq, vb, twb, alb, kkn, sq, t1, t2, t2b, t3, kft, aqt, sgw, cs, X1, X2, X3, E, akk, p1, p2,
             bT, kT, Pm, PTm, Pm2, PTm2) = (tmp[n] for n in (
                "rq", "kq", "vb", "twb", "alb", "kkn", "sq", "t1", "t2", "t2b", "t3", "kft", "aqt", "sgw", "cs", "X1", "X2", "X3",
                "E", "akk", "p1", "p2", "bT", "kT", "Pm", "PTm", "Pm2", "PTm2"))
            par = p % 2
            pc = slice(p * 128, (p + 1) * 128)
            ts_ = slice(tq * QS, (tq + 1) * QS)
            cq0 = tq * CQ
            csl = slice(cq0, cq0 + CQ)
            Rd = R[d]
            hs_lo = slice(64 * d, 64 * d + 64)
            for dst, c in ((rq, p), (kq, 4 + p), (vb, 8 + p), (twb, 13), (alb, 14)):
                self.dma(dst.t[:], uS[c][:, ts_], r=[B_uS[c]], w=[dst.b])
            yield
            self.ts("dve", t1[:], kq[:], pp[:, p, PKK:PKK + 1], None, ALU.mult, r=[kq.b, pp.b], w=[t1.b])
            self.tt("pool", sq[:], t1[:], t1[:], ALU.mult, r=[t1.b], w=[sq.b])
            ps = self.nps(tmp["grp"])
            self.mm(ps[:, 0:QS], bob[:], sq[:], r=[bob.b, sq.b], w=[ps.b])
            self.ts("dve", t2[:], ps[:, 0:QS], 1e-24, None, ALU.max, r=[ps.b], w=[t2.b])
            self.act(t2[:], t2[:], AF.Ln, r=[t2.b], w=[t2.b])
            self.act(t2b[:], t2[:], AF.Exp, scale=-0.5, r=[t2.b], w=[t2b.b])
            self.tt("dve", kkn[:], t1[:], t2b[:], ALU.mult, r=[t1.b, t2b.b], w=[kkn.b])
            yield
            ps = self.nps(tmp["grp"])
            self.mm(ps[:, 0:QS], aup[hs_lo, pc], alb[hs_lo, :], r=[aup.b, alb.b], w=[ps.b])
            self.act(t2[:], ps[:, 0:QS], AF.Exp, scale=-1.0, bias=npp[:, p, PA0F + d:PA0F + d + 1], r=[ps.b, npp.b], w=[t2.b])
            self.act(t2[:], t2[:], AF.Ln, bias=onesf[:, 0:1], r=[t2.b, onesf.b], w=[t2.b])
            self.act(aqt[:], t2[:], AF.Exp, scale=-1.0, r=[t2.b], w=[aqt.b])
            self.ts("dve", t3[:], aqt[:], pp[:, p, PKA:PKA + 1], omka[:, p:p + 1], ALU.mult, ALU.add,
                    r=[aqt.b, pp.b, omka.b], w=[t3.b])
            self.tt("dve", kft[:], kq[:], t3[:], ALU.mult, r=[kq.b, t3.b], w=[kft.b])
            yield
            self.stt(t3[:], kft[:], pp[:, p, PRK:PRK + 1], rq[:], ALU.mult, ALU.mult, r=[kft.b, pp.b, rq.b], w=[t3.b])
            ps = self.nps(tmp["grp"])
            self.mm(ps[:, 0:QS], bob[:], t3[:], r=[bob.b, t3.b], w=[ps.b])
            if first:
                self.tt("dve", bonus[par][:, ts_], ps[:, 0:QS], vb[:], ALU.mult, r=[ps.b, vb.b], w=[bonus[par].b])
            else:
                self.tt("dve", t3[:], ps[:, 0:QS], vb[:], ALU.mult, r=[ps.b, vb.b], w=[t3.b])
                self.tt("pool", bonus[par][:, ts_], bonus[par][:, ts_], t3[:], ALU.add, r=[t3.b, bonus[par].b], w=[bonus[par].b])
            yield
            if first:
                ps = self.nps(tmp["grp"])
                psv = ps.t.bitcast(BF16)
                for c in range(CQ):
                    for h in range(2):
                        hs = slice(64 * h, 64 * h + 64)
                        self.tr(psv[hs, c * L:(c + 1) * L], vb[hs, c * L:(c + 1) * L], idb[hs, hs], r=[vb.b, idb.b], w=[ps.b])
                self.cp("act", vtok[par][:, csl, :], v3(psv[:, 0:QS]), r=[ps.b], w=[vtok[par].bq[tq]])
                yield
            ps = self.nps(tmp["grp"])
            self.mm(ps[:, 0:QS], wup[hs_lo, pc], twb[hs_lo, :], r=[wup.b, twb.b], w=[ps.b])
            self.act(sgw[:], ps[:, 0:QS], AF.Exp, scale=-1.0, bias=npp[:, p, PW0F + d:PW0F + d + 1], r=[ps.b, npp.b], w=[sgw.b])
            self.act(sgw[:], sgw[:], AF.Ln, bias=onesf[:, 0:1], r=[sgw.b, onesf.b], w=[sgw.b])
            self.act(sgw[:], sgw[:], AF.Exp, scale=-1.0, r=[sgw.b], w=[sgw.b])
            self.P.emit("dve", lambda e, o=cs[:], a=scm[:], b=sgw[:]: e.tensor_tensor_scan(
                out=o, data0=a, data1=b, initial=0.0, op0=ALU.mult, op1=ALU.add), [scm.b, sgw.b], [cs.b], est=(100 + 2 * QS) / 960.0)
            tot = v3(cs[:])[:, :, L - 1:L]
            self.act(Rd["Wtot"][:, csl], tot.rearrange("p c l -> p (c l)"), AF.Exp, scale=-DS, r=[cs.b], w=[Rd["Wtot"].bq[tq]])
            if d == 0:
                self.tt("pool", X1[:], cs[:], sgw[:], ALU.subtract, r=[cs.b, sgw.b], w=[X1.b])
                ce, ci = X1, cs
            else:
                self.tt("dve", v3(X2[:]), tot.to_broadcast([128, CQ, L]), v3(cs[:]), ALU.subtract, r=[cs.b], w=[X2.b])
                self.tt("pool", X3[:], X2[:], sgw[:], ALU.add, r=[X2.b, sgw.b], w=[X3.b])
                ce, ci = X2, X3
            yield
            ar = Rd["ar"]
            arb = ar.bq[tq]
            self.act(E[:], ce[:], AF.Exp, scale=-DS, r=[ce.b], w=[E.b])
            self.stt(ar[:, csl, 0, :], v3(kkn[:]), -1.0, v3(E[:]), ALU.mult, ALU.mult, r=[kkn.b, E.b], w=[arb])
            self.act(p1[:], ci[:], AF.Exp, scale=-DS, r=[ci.b], w=[p1.b])
            self.tt("dve", ar[:, csl, 1, :], v3(rq[:]), v3(p1[:]), ALU.mult, r=[rq.b, p1.b], w=[arb])
            yield
            self.tt("pool", akk[:], aqt[:], kkn[:], ALU.mult, r=[aqt.b, kkn.b], w=[akk.b])
            self.act(p2[:], ci[:], AF.Exp, scale=DS, r=[ci.b], w=[p2.b])
            self.tt("dve", bT[:], akk[:], p2[:], ALU.mult, r=[akk.b, p2.b], w=[bT.b])
            self.tt("dve", kT[:], kft[:], p2[:], ALU.mult, r=[kft.b, p2.b], w=[kT.b])
            yield
            for src, dstk in ((bT, "BB"), (kT, "KB")):
                ps = self.nps(tmp["grp"])
                psv = ps.t.bitcast(BF16)
                for c in range(CQ):
                    for h in range(2):
                        hs = slice(64 * h, 64 * h + 64)
                        self.tr(psv[hs, c * L:(c + 1) * L], src[hs, c * L:(c + 1) * L], idb[hs, hs],
                                r=[src.b, idb.b], w=[ps.b])
                self.cp("act", Rd[dstk][:, csl, :], v3(psv[:, 0:QS]), r=[ps.b], w=[Rd[dstk].bq[tq]])
                yield
            for lhs, dstk in ((bT, "SB"), (kT, "SK")):
                for c4 in range(0, CQ, 4):
                    ps = self.nps(tmp["grp"])
                    for cc in range(4):
                        c = c4 + cc
                        for h in range(2):
                            hs = slice(64 * h, 64 * h + 64)
                            self.mm(ps[hs, cc * 128:(cc + 1) * 128], lhs[hs, c * L:(c + 1) * L],
                                    ar[hs, cq0 + c, :, :].rearrange("p a l -> p (a l)"),
                                    r=[lhs.b, arb], w=[ps.b])
                    if dstk == "SK":
                        self.tt("dve", Rd[dstk][:, cq0 + c4:cq0 + c4 + 4, :, :].rearrange("p c a l -> p (c a l)"),
                                ps[:, :], mT[:, d, :], ALU.mult, r=[ps.b, mT.b], w=[Rd[dstk].bq[tq]])
                    else:
                        ps4 = ps[:, :].rearrange("p (c a l) -> p c a l", c=4, a=2)
                        m4 = mT[:, d, :].rearrange("p (c a l) -> p c a l", c=4, a=2)
                        self.tt("dve", v3(PTm[:])[:, c4:c4 + 4, :], ps4[:, :, 0, :], m4[:, :, 0, :], ALU.mult,
                                r=[ps.b, mT.b], w=[PTm.b])
                        self.tt("dve", Rd["SB"][:, cq0 + c4:cq0 + c4 + 4, :], ps4[:, :, 1, :], m4[:, :, 1, :], ALU.mult,
                                r=[ps.b, mT.b], w=[Rd["SB"].bq[tq]])
                    yield
            ps = self.nps(tmp["grp"])
            for c in range(CQ):
                for h in range(2):
                    hs = slice(64 * h, 64 * h + 64)
                    self.mm(ps[hs, c * L:(c + 1) * L], ar[hs, cq0 + c, 0, :], bT[hs, c * L:(c + 1) * L],
                            r=[arb, bT.b], w=[ps.b])
            self.tt("dve", Pm[:], ps[:, 0:QS], mN[:, d, 0:QS], ALU.mult, r=[ps.b, mN.b], w=[Pm.b])
            TTq = Rd["TTm"][:, csl, :]
            TTb = Rd["TTm"].bq[tq]
            self.tt("pool", TTq, v3(PTm[:]), v3(idr[:, 0:QS]), ALU.add, r=[PTm.b, idr.b], w=[TTb])
            yield
            Pc, PTc, Pn, PTn = Pm, PTm, Pm2, PTm2
            for lvl in range(1, 6):
                psA = self.nps(tmp["grp"])
                for c in range(CQ):
                    for h in range(2):
                        hs = slice(64 * h, 64 * h + 64)
                        cl = slice(c * L, (c + 1) * L)
                        self.mm(psA[hs, cl], PTc[hs, cl], Pc[hs, cl], r=[PTc.b, Pc.b], w=[psA.b])
                self.cp("act", Pn[:], psA[:, 0:QS], r=[psA.b], w=[Pn.b])
                if lvl < 5:
                    psB = self.nps(tmp["grp"])
                    for c in range(CQ):
                        for h in range(2):
                            hs = slice(64 * h, 64 * h + 64)
                            cl = slice(c * L, (c + 1) * L)
                            self.mm(psB[hs, cl], Pc[hs, cl], PTc[hs, cl], r=[PTc.b, Pc.b], w=[psB.b])
                    self.cp("act", PTn[:], psB[:, 0:QS], r=[psB.b], w=[PTn.b])
                yield
                psC = self.nps(tmp["grp"])
                for c in range(CQ):
                    for h in range(2):
                        hs = slice(64 * h, 64 * h + 64)
                        cl = slice(c * L, (c + 1) * L)
                        self.mm(psC[hs, cl], Pn[hs, cl], Rd["TTm"][hs, cq0 + c, :], r=[Pn.b, TTb], w=[psC.b])
                self.tt("dve", TTq, v3(psC[:, 0:QS]), TTq, ALU.add, r=[psC.b, TTb], w=[TTb])
                Pc, PTc, Pn, PTn = Pn, PTn, Pc, PTc
                yield

        def pre_stage(p, k):
            gens = []
            for d, tq, tmp in ((0, k, TA), (1, NQ_ - 1 - k, TB)):
                fstage, bstage = tq, NQ_ - 1 - tq
                first = (fstage <= bstage) if d == 0 else (bstage < fstage)
                gens.append(pre_pass(p, d, tq, first, tmp))
            return gens

        def chain_group(p, k):
            par = p % 2
            if k == 0:
                self.ms("pool", wkv[par][:], 0.0, w=[wkv[par].b])
                for d in range(2):
                    self.ms("pool", R[d]["ST"][:], 0.0, w=[R[d]["ST"].b])
            for step in range(k * CQ, (k + 1) * CQ):
                for d in range(2):
                    Rd = R[d]
                    c = step if d == 0 else NCH - 1 - step
                    qi = c // CQ
                    ar, SBm, SKm, TTm, BB, KB, ST, Xs, Us = (Rd[k_] for k_ in ("ar", "SB", "SK", "TTm", "BB", "KB", "ST", "Xs", "Us"))
                    vt = vtok[par]
                    vtb = vt.bq[qi]
                    psX = self.nps("chain")
                    for h in range(2):
                        hs = slice(64 * h, 64 * h + 64)
                        self.mm(psX[hs, 0:L], ar[hs, c, 0, :], ST[hs, :], start=True, stop=False, r=[ar.bq[qi], ST.b], w=[psX.b])
                        self.mm(psX[hs, 0:L], SKm[hs, c, 0, :], vt[hs, c, :], start=False, stop=True, r=[SKm.bq[qi], vtb], w=[psX.b])
                    self.cp("act", Xs[:], psX[:, 0:L], r=[psX.b], w=[Xs.b])
                    yield
                    psU = self.nps("chain")
                    for h in range(2):
                        hs = slice(64 * h, 64 * h + 64)
                        self.mm(psU[hs, 0:L], TTm[hs, c, :], Xs[hs, :], r=[TTm.bq[qi], Xs.b], w=[psU.b])
                    self.cp("dve", Us[:], psU[:, 0:L], r=[psU.b], w=[Us.b])
                    yield
                    psY = self.nps("chain")
                    for h in range(2):
                        hs = slice(64 * h, 64 * h + 64)
                        self.mm(psY[hs, 0:L], ST[hs, :], ar[hs, c, 1, :], start=True, stop=False, r=[ar.bq[qi], ST.b], w=[psY.b])
                        self.mm(psY[hs, 0:L], Us[hs, :], SBm[hs, c, :], start=False, stop=False, r=[Us.b, SBm.bq[qi]], w=[psY.b])
                        self.mm(psY[hs, 0:L], vt[hs, c, :], SKm[hs, c, 1, :], start=False, stop=True, r=[vtb, SKm.bq[qi]], w=[psY.b])
                    psS = self.nps("chain")
                    for h in range(2):
                        hs = slice(64 * h, 64 * h + 64)
                        self.mm(psS[hs, 0:L], idb[hs, hs], ST[hs, :], start=True, stop=False, r=[idb.b, ST.b], w=[psS.b])
                        self.mm(psS[hs, 0:L], BB[hs, c, :], Us[hs, :], start=False, stop=False, r=[BB.bq[qi], Us.b], w=[psS.b])
                        self.mm(psS[hs, 0:L], KB[hs, c, :], vt[hs, c, :], start=False, stop=True, r=[KB.bq[qi], vtb], w=[psS.b])
                    self.ts("dve", ST[:], psS[:, 0:L], Rd["Wtot"][:, c:c + 1], None, ALU.mult,
                            r=[Rd["Wtot"].bq[qi], psS.b], w=[ST.b])
                    wsl = wkv[par][:, c * L:(c + 1) * L]
                    self.tt("dve", wsl, psY[:, 0:L], wsl, ALU.add, r=[psY.b, wkv[par].b], w=[wkv[par].b])
                    yield

        def post(p):
            par = p % 2
            pc = slice(p * 128, (p + 1) * 128)
            for tq in range(NQ):
                ts_ = slice(tq * QS, (tq + 1) * QS)
                self.dma(sgb.t[:], uS[12][:, ts_], r=[B_uS[12]], w=[sgb.b])
                ps = self.nps("post")
                self.mm(ps[:, 0:QS], bab[:], wkv[par][:, ts_], r=[bab.b, wkv[par].b], w=[ps.b])
                self.tt("dve", t1[:], wkv[par][:, ts_], ps[:, 0:QS], ALU.subtract, r=[wkv[par].b, ps.b], w=[t1.b])
                self.tt("pool", sq[:], t1[:], t1[:], ALU.mult, r=[t1.b], w=[sq.b])
                yield
                ps = self.nps("post")
                self.mm(ps[:, 0:QS], ba[:], sq[:], r=[ba.b, sq.b], w=[ps.b])
                self.ts("dve", t2[:], ps[:, 0:QS], LNX_EPS, None, ALU.add, r=[ps.b], w=[t2.b])
                self.act(t2[:], t2[:], AF.Ln, r=[t2.b], w=[t2.b])
                self.act(t2[:], t2[:], AF.Exp, scale=-0.5, r=[t2.b], w=[t2.b])
                self.tt("dve", t1[:], t1[:], t2[:], ALU.mult, r=[t1.b, t2.b], w=[t1.b])
                yield
                self.ts("dve", t1[:], t1[:], pp[:, p, PLW:PLW + 1], pp[:, p, PLB:PLB + 1], ALU.mult, ALU.add,
                        r=[t1.b, pp.b], w=[t1.b])
                self.tt("pool", t1[:], t1[:], bonus[par][:, ts_], ALU.add, r=[t1.b, bonus[par].b], w=[t1.b])
                ps = self.nps("post")
                self.mm(ps[:, 0:QS], gup[:, pc], sgb[:], r=[gup.b, sgb.b], w=[ps.b])
                self.tt("dve", yR[:, p, ts_], t1[:], ps[:, 0:QS], ALU.mult, r=[t1.b, ps.b], w=[yR.b])
                yield

        def run(g):
            for _ in g:
                pass

        interleave = self.interleave

        interleave(pre_stage(0, 0))
        pending_post = None
        for p in range(4):
            for k in range(NQ_):
                nxt = None
                if k + 1 < NQ_:
                    nxt = pre_stage(p, k + 1)
                elif p + 1 < 4:
                    nxt = pre_stage(p + 1, 0)
                if nxt is not None and NQ_ > 1:
                    gens = [chain_group(p, k)] + nxt
                    if k == 0 and pending_post is not None:
                        gens.append(pending_post)
                        pending_post = None
                    interleave(gens)
                else:
                    if pending_post is not None:
                        run(pending_post)
                        pending_post = None
                    run(chain_group(p, k))
                    if nxt is not None:
                        interleave(nxt)
            pending_post = post(p)
        run(pending_post)

    def attention(self, st, s, env, hT, yA):
        P, T, QS, NQ, TT = self.P, self.T, self.QS, self.NQ, self.TT
        w_in, idb, onesb = env["w_in"], env["idb"], env["onesb"]
        sb = self.sb
        bias = sb(st, "bias", [128, 6, 512], BF16)
        self.load_w(bias[:], bias.b, env["c_bias"])
        snk = sb(st, "snk", [128, 8], F32)
        esk = sb(st, "esk", [128, 2, 2, 128], F32)
        self.dma(snk.t[:], env["sink_d"].partition_broadcast(128), w=[snk.b])
        self.act(snk[:], snk[:], AF.Exp, r=[snk.b], w=[snk.b])
        for kv in range(2):
            for gh in range(2):
                for gl in range(2):
                    hs = slice(64 * gl, 64 * gl + 64)
                    col = kv * 4 + 2 * gh + gl
                    self.cp("dve", esk[hs, kv, gh, :], snk[hs, col:col + 1].to_broadcast([64, 128]), r=[snk.b], w=[esk.b])
        qT = sb(st, "qT", [128, TT, 4, 128], BF16)
        kTz = [sb(st, f"kTz{i}", [128, T], BF16) for i in range(2)]
        vtk = sb(st, "vtk", [128, TT, 128], BF16)
        wq = sb(st, "wq", [128, 8, 512], BF16)
        wkv_ = sb(st, "wkvw", [128, 8, 256], BF16)
        c0 = 1920
        wq5 = wq.t[:].rearrange("p k (g kv d) -> p k g kv d", g=4, kv=2)
        for i in range(2):
            for kc in range(8):
                self.load_w(wq5[:, kc, :, i, :], wq.b,
                            w_in[:, kc, c0 + i * 256:c0 + (i + 1) * 256].rearrange("p (g d) -> p g d", g=4))
        self.load_w(wkv_[:], wkv_.b, w_in[:, :, c0 + 512:c0 + 768], 8, 256)
        for i in range(2):
            self.ms("pool", kTz[i][:], 0.0, w=[kTz[i].b])
        for tq in range(NQ):
            ts_ = slice(tq * QS, (tq + 1) * QS)
            for g in range(4):
                ps = self.nps()
                for kc in range(8):
                    self.mm(ps[:, 0:QS], wq[:, kc, g * 128:(g + 1) * 128], hT[:, kc, ts_], start=(kc == 0), stop=(kc == 7),
                            r=[wq.b, hT.b], w=[ps.b])
                sg_ = (g % 2) * 2 + g // 2
                nb_ = QS // 128
                self.act(qT[:, tq * nb_:(tq + 1) * nb_, sg_, :], ps[:, 0:QS].rearrange("p (b q) -> p b q", q=128), AF.Copy, scale=0.125,
                         r=[ps.b], w=[qT.b])
            ps = self.nps()
            self.inproj(hT, wkv_, 0, tq, [wkv_.b, hT.b], ps)
            for kv in range(2):
                hs = slice(64 * kv, 64 * kv + 64)
                self.cp("act", kTz[kv][hs, ts_], ps[hs, 0:QS], r=[ps.b], w=[kTz[kv].b])
        for tt_ in range(TT):
            ps = self.nps()
            for kc in range(8):
                self.mm(ps[:, 0:128], hT[:, kc, tt_ * 128:(tt_ + 1) * 128], wkv_[:, kc, 128:256], start=(kc == 0), stop=(kc == 7),
                        r=[wkv_.b, hT.b], w=[ps.b])
            self.cp("act", vtk[:, tt_, :], ps[:, 0:128], r=[ps.b], w=[vtk.b])
        pTs = [[sb(st, f"pT{j}_{i}", [128, 512], BF16) for i in range(3)] for j in range(2)]
        dens = [sb(st, f"den{j}", [128, 256], F32) for j in range(2)]

        def att_iter(kv, qb, par):
            den = dens[par]
            kbs = [kb for kb in (qb - 1, qb, qb + 1) if 0 <= kb < TT]
            psO = self.nps(f"attO{par}")
            psD = self.nps(f"attD{par}")
            for ki, kb in enumerate(kbs):
                rel = kb - qb + 1
                psS = self.nps(f"attS{par}")
                pT = pTs[par][ki]
                self.mm(psS[:, :], kTz[kv][:, kb * 128:(kb + 1) * 128], qT[:, qb, :, :].rearrange("p g q -> p (g q)"),
                        start=True, stop=False, r=[kTz[kv].b, qT.b], w=[psS.b])
                self.mm(psS[:, :], idb[:], bias[:, rel * 2 + kv, :], start=False, stop=True, r=[idb.b, bias.b], w=[psS.b])
                self.act(pT[:], psS[:, :], AF.Exp, r=[psS.b], w=[pT.b])
                yield
                for gl in range(2):
                    hs = slice(64 * gl, 64 * gl + 64)
                    self.mm(psO[hs, 0:256], vtk[:, kb, 64 * kv:64 * kv + 64], pT[:, gl * 256:(gl + 1) * 256],
                            start=(ki == 0), stop=(ki == len(kbs) - 1), r=[vtk.b, pT.b], w=[psO.b], nohold=True)
                    self.mm(psD[hs, 0:256], onesb[:, 0:64], pT[:, gl * 256:(gl + 1) * 256],
                            start=(ki == 0), stop=(ki == len(kbs) - 1), r=[onesb.b, pT.b], w=[psD.b], nohold=True)
                yield
            self.tt("dve", den[:], psD[:, 0:256], esk[:, kv, :, :].rearrange("p a q -> p (a q)"), ALU.add,
                    r=[psD.b, esk.b], w=[den.b])
            self.act(den[:], den[:], AF.Ln, r=[den.b], w=[den.b])
            self.act(den[:], den[:], AF.Exp, scale=-1.0, r=[den.b], w=[den.b])
            yield
            self.tt("dve", yA[:, 2 * kv:2 * kv + 2, qb * 128:(qb + 1) * 128],
                    psO[:, 0:256].rearrange("p (a q) -> p a q", a=2), den[:].rearrange("p (a q) -> p a q", a=2),
                    ALU.mult, r=[psO.b, den.b], w=[yA.b])
            yield

        its = [(kv, qb) for kv in range(2) for qb in range(TT)]
        self.interleave([(lambda slot, kv=kv, qb=qb: att_iter(kv, qb, slot)) for (kv, qb) in its], window=2)

    def merge_gates(self, st, s, env, hT, yA, mrg):
        P, T, QS, NQ, TT = self.P, self.T, self.QS, self.NQ, self.TT
        NS = self.NS
        w_in, idf = env["w_in"], env["idf"]
        x, acc, h2d, B_acc, B_h2d = env["x"], env["acc"], env["h2d"], env["B_acc"], env["B_h2d"]
        wrt, affT = env["wrt"], env["affT"]
        yR = self.yRk
        sb = self.sb
        wpr = sb(st, "wpr", [128, 4, D], BF16)
        wpa = sb(st, "wpa", [128, 4, D], BF16)
        self.load_w(wpr[:], wpr.b, env["wpr_d"])
        self.load_w(wpa[:], wpa.b, env["wpa_d"])
        wgs = [sb(st, f"wg{i}", [128, 8, 1024], BF16) for i in range(2)]
        sg1s = [sb(st, f"sg1_{i}", [128, QS], F32) for i in range(2)]
        sg2s = [sb(st, f"sg2_{i}", [128, QS], F32) for i in range(2)]
        m1s = [sb(st, f"m1_{i}", [128, QS], F32) for i in range(2)]
        m2s = [sb(st, f"m2_{i}", [128, QS], F32) for i in range(2)]
        cg = 1920 + 768

        def gate_iter(oc, tq, par):
            sg1, sg2, m1, m2 = sg1s[par], sg2s[par], m1s[par], m2s[par]
            wg = wgs[oc // 4]
            ol = (oc % 4) * 128
            if oc % 4 == 0 and tq == 0:
                self.load_w(wg[:, :, 0:512], wg.b, w_in[:, :, cg + oc * 128:cg + oc * 128 + 512])
                self.load_w(wg[:, :, 512:1024], wg.b, w_in[:, :, cg + 1024 + oc * 128:cg + 1024 + oc * 128 + 512])
            ts_ = slice(tq * QS, (tq + 1) * QS)
            ps1 = self.nps(f"h{par}")
            self.inproj(hT, wg, ol, tq, [wg.b, hT.b], ps1)
            self.act(sg1[:], ps1[:, 0:QS], AF.Sigmoid, r=[ps1.b], w=[sg1.b])
            yield
            ps2 = self.nps(f"h{par}")
            self.inproj(hT, wg, 512 + ol, tq, [wg.b, hT.b], ps2)
            self.act(sg2[:], ps2[:, 0:QS], AF.Sigmoid, r=[ps2.b], w=[sg2.b])
            yield
            ps3 = self.nps(f"h{par}")
            for kc in range(4):
                self.mm(ps3[:, 0:QS], wpr[:, kc, oc * 128:(oc + 1) * 128], yR[:, kc, ts_], start=(kc == 0), stop=(kc == 3),
                        r=[wpr.b, yR.b], w=[ps3.b])
            self.tt("dve", m1[:], sg1[:], ps3[:, 0:QS], ALU.mult, r=[sg1.b, ps3.b], w=[m1.b])
            yield
            ps4 = self.nps(f"h{par}")
            for kc in range(4):
                self.mm(ps4[:, 0:QS], wpa[:, kc, oc * 128:(oc + 1) * 128], yA[:, kc, ts_], start=(kc == 0), stop=(kc == 3),
                        r=[wpa.b, yA.b], w=[ps4.b])
            self.tt("dve", m2[:], sg2[:], ps4[:, 0:QS], ALU.mult, r=[sg2.b, ps4.b], w=[m2.b])
            yield
            self.tt("pool", mrg[:, oc, ts_], m1[:], m2[:], ALU.add, r=[m1.b, m2.b], w=[mrg.b])
            yield

        its = [(oc, tq) for oc in range(8) for tq in range(NQ)]
        self.interleave([(lambda slot, oc=oc, tq=tq: gate_iter(oc, tq, slot)) for (oc, tq) in its], window=2)

    def out_proj(self, st, s, env, mrg):
        P, T, QS, NQ, TT = self.P, self.T, self.QS, self.NQ, self.TT
        idf = env["idf"]
        x, acc, h2d, B_acc, B_h2d = env["x"], env["acc"], env["h2d"], env["B_acc"], env["B_h2d"]
        wrt, affT = env["wrt"], env["affT"]
        sb = self.sb
        wout = sb(st, "wout", [128, 8, D], BF16)
        self.load_w(wout[:], wout.b, env["wout_d"])
        g2bc = sb(st, "g2bc", [128, D], F32)
        self.dma(g2bc.t[:], env["g2r_d"].partition_broadcast(128), w=[g2bc.b])
        xts = [sb(st, f"xm{i}", [128, D], F32) for i in range(3)]
        x1s = [sb(st, f"x1{i}", [128, D], F32) for i in range(3)]
        h2s = [sb(st, f"h2{i}", [128, D], F32) for i in range(3)]
        h2bs = [sb(st, f"h2b{i}", [128, D], BF16) for i in range(3)]
        h2Ts = [sb(st, f"h2T{i}", [128, 8, 128], F32) for i in range(3)]
        junk = sb(st, "junk2", [128, D], F32)
        sst = [sb(st, f"sm{i}", [128, 8], F32) for i in range(3)]
        lgs = [sb(st, f"lg{i}", [128, NEXP], F32) for i in range(3)]
        affts = [sb(st, f"afft{i}", [128, 128], F32) for i in range(3)]
        for a_ in affts:
            self.ms("pool", a_[:], 0.0, w=[a_.b])
        def tile_iter(tt_, slot):
                xt, x1, h2, h2b, ss = xts[slot], x1s[slot], h2s[slot], h2bs[slot], sst[slot]
                h2T, lg, afft = h2Ts[slot], lgs[slot], affts[slot]
                r0 = s * T + tt_ * 128
                tl = slice(tt_ * 128, (tt_ + 1) * 128)
                self.dma(xt.t[:], x[r0:r0 + 128, :], w=[xt.b])
                for half in range(2):
                    ps = self.nps(f"t{slot}")
                    for kc in range(8):
                        self.mm(ps[:, :], mrg[:, kc, tl], wout[:, kc, half * 512:(half + 1) * 512], start=(kc == 0), stop=(kc == 7),
                                r=[mrg.b, wout.b], w=[ps.b])
                    self.tt("dve", x1[:, half * 512:(half + 1) * 512], xt[:, half * 512:(half + 1) * 512], ps[:, :], ALU.add,
                            r=[xt.b, ps.b], w=[x1.b])
                yield
                self.dma(acc[r0:r0 + 128, :], x1[:], r=[x1.b], w=[B_acc[s]])
                self.act(junk[:], x1[:], AF.Square, accum=ss[:, 0:1], r=[x1.b], w=[junk.b, ss.b])
                self.ts("dve", ss[:, 1:2], ss[:, 0:1], 1.0 / D, NORM_EPS, ALU.mult, ALU.add, r=[ss.b], w=[ss.b])
                self.act(ss[:, 2:3], ss[:, 1:2], AF.Ln, r=[ss.b], w=[ss.b])
                self.act(ss[:, 3:4], ss[:, 2:3], AF.Exp, scale=-0.5, r=[ss.b], w=[ss.b])
                self.ts("dve", h2[:], x1[:], ss[:, 3:4], None, ALU.mult, r=[x1.b, ss.b], w=[h2.b])
                self.tt("pool", h2b[:], h2[:], g2bc[:], ALU.mult, r=[h2.b, g2bc.b], w=[h2b.b])
                self.dma(h2d[r0:r0 + 128, :], h2b[:], r=[h2b.b], w=[B_h2d[s]])
                yield
                for k2 in range(2):
                    ps = self.nps(f"t{slot}")
                    for kk in range(4):
                        kc = k2 * 4 + kk
                        self.tr(ps[:, kk * 128:(kk + 1) * 128], h2[:, kc * 128:(kc + 1) * 128], idf[:], r=[h2.b, idf.b], w=[ps.b])
                    self.cp("act", h2T[:, k2 * 4:(k2 + 1) * 4, :], ps[:, :].rearrange("p (k n) -> p k n", k=4), r=[ps.b], w=[h2T.b])
                yield
                ps = self.nps(f"t{slot}")
                for kc in range(8):
                    self.mm(ps[:, 0:NEXP], h2T[:, kc, :], wrt[:, kc, :], start=(kc == 0), stop=(kc == 7), r=[h2T.b, wrt.b], w=[ps.b])
                self.P.emit("dve", lambda e, o=ss[:, 4:5], i=ps[:, 0:NEXP]: e.reduce_max(out=o, in_=i, axis=AX.X), [ps.b], [ss.b])
                self.ts("dve", ss[:, 5:6], ss[:, 4:5], -1.0, None, ALU.mult, r=[ss.b], w=[ss.b])
                self.act(lg[:], ps[:, 0:NEXP], AF.Exp, bias=ss[:, 5:6], accum=ss[:, 6:7], r=[ps.b, ss.b], w=[lg.b, ss.b])
                self.P.emit("dve", lambda e, o=ss[:, 7:8], i=ss[:, 6:7]: e.reciprocal(out=o, in_=i), [ss.b], [ss.b])
                self.ts("dve", afft[:, 32 * s:32 * s + NEXP], lg[:], ss[:, 7:8], None, ALU.mult, r=[lg.b, ss.b], w=[afft.b])
                ps = self.nps(f"t{slot}")
                self.tr(ps[:, 0:128], afft[:], idf[:], r=[afft.b, idf.b], w=[ps.b])
                self.tt("dve", affT[:, tl], affT[:, tl], ps[:, 0:128], ALU.add, r=[ps.b, affT.b], w=[affT.b])


        self.interleave([(lambda slot, tt_=tt_: tile_iter(tt_, slot)) for tt_ in range(TT)], window=3)

    def phase_b(self, env):
        P, NS, T, CAP, SB_, NB = self.P, self.NS, self.T, self.CAP, self.SB, self.NB
        affT, idf, idb, g2 = env["affT"], env["idf"], env["idb"], env["g2"]
        acc, h2d, B_acc, B_h2d = env["acc"], env["h2d"], env["B_acc"], env["B_h2d"]
        sb = self.sb
        with ExitStack() as st:
            wk = sb(st, "wk", [128, T], F32)
            mv = sb(st, "mv", [128, CAP], F32)
            mi = sb(st, "mi", [128, CAP], U32)
            mif = sb(st, "mif", [128, CAP], F32)
            offs = sb(st, "offs", [128, 128], F32)
            idxT = sb(st, "idxT", [128, NB, 128], I32)
            valT = sb(st, "valT", [128, NB, 128], F32)
            self.dma(offs.t[:], env["c_offs"], w=[offs.b])
            self.cp("dve", wk[:], affT[:], r=[affT.b], w=[wk.b])
            for r_ in range(CAP // 8):
                sl = slice(r_ * 8, r_ * 8 + 8)
                self.P.emit("dve", lambda e, o=mv[:, sl], i=wk[:]: e.max(out=o, in_=i), [wk.b], [mv.b])
                self.P.emit("dve", lambda e, o=mi[:, sl], m=mv[:, sl], i=wk[:]: e.max_index(out=o, in_max=m, in_values=i),
                            [wk.b, mv.b], [mi.b])
                self.P.emit("dve", lambda e, o=wk[:], m=mv[:, sl], i=wk[:]: e.match_replace(
                    out=o, in_to_replace=m, in_values=i, imm_value=-1.0), [wk.b, mv.b], [wk.b])
            self.cp("dve", mif[:], mi[:], r=[mi.b], w=[mif.b])
            for blk in range(NB):
                bs = slice(blk * SB_, (blk + 1) * SB_)
                ps = self.nps()
                self.tr(ps[0:SB_, 0:128], mif[:, bs], idf[:], r=[mif.b, idf.b], w=[ps.b])
                self.tt("dve", idxT[0:SB_, blk, :], ps[0:SB_, 0:128], offs[0:SB_, :], ALU.add, r=[ps.b, offs.b], w=[idxT.b])
                ps = self.nps()
                self.tr(ps[0:SB_, 0:128], mv[:, bs], idf[:], r=[mv.b, idf.b], w=[ps.b])
                self.cp("act", valT[0:SB_, blk, :], ps[0:SB_, 0:128], r=[ps.b], w=[valT.b])
            wgs = [sb(st, f"ewg{i}", [128, 8, D], BF16) for i in range(2)]
            wus = [sb(st, f"ewu{i}", [128, 8, D], BF16) for i in range(2)]
            wds = [sb(st, f"ewd{i}", [128, 8, D], BF16) for i in range(2)]
            NPF = 1
            G = 2 if NS % 2 == 0 else 1
            NBG = NB * G
            xss = [sb(st, f"xs{i}", [128, D], BF16) for i in range((NPF + 1) * NBG)]
            xsT = sb(st, "xsT", [128, 8, G * CAP], BF16)
            hid = sb(st, "hid", [128, 8, G * CAP], BF16)
            sl_ = sb(st, "silu", [128, G * CAP], F32)
            yss = [sb(st, f"ys{i}", [128, D], F32) for i in range(NBG)]
            its = [(e, g) for e in range(NEXP) for g in range(NS // G)]
            last_sc = {s: [] for s in range(NS)}

            def load_expert(e):
                for dst, src in ((wgs[e % 2], env["eg_d"]), (wus[e % 2], env["eu_d"]), (wds[e % 2], env["ed_d"])):
                    self.load_w(dst[:], dst.b, src[e].rearrange("(k p) n -> p k n", p=128))

            def gather(i):
                e, g = its[i]
                for j in range(G):
                    s = g * G + j
                    pcol = 32 * s + e
                    for blk in range(NB):
                        xs = xss[(i % (NPF + 1)) * NBG + j * NB + blk]
                        ia = idxT[0:SB_, blk, pcol:pcol + 1]
                        self.P.emit("pool", lambda en, o=xs[0:SB_, :], ia=ia: en.indirect_dma_start(
                            out=o, out_offset=None, in_=h2d, in_offset=bass.IndirectOffsetOnAxis(ap=ia, axis=0)),
                            [idxT.b, B_h2d[s]], [xs.b], dma=True)

            load_expert(0)
            for i in range(min(NPF, len(its))):
                gather(i)
            GC = G * CAP
            for i, (e, g) in enumerate(its):
                wg, wu, wd = wgs[e % 2], wus[e % 2], wds[e % 2]
                if g == 0 and e + 1 < NEXP:
                    load_expert(e + 1)
                if i + NPF < len(its):
                    gather(i + NPF)
                for jb in range(NBG):
                    xs = xss[(i % (NPF + 1)) * NBG + jb]
                    ps = self.nps()
                    psv = ps.t.bitcast(BF16)
                    for kc in range(8):
                        self.tr(psv[:, kc * 128:kc * 128 + SB_], xs[0:SB_, kc * 128:(kc + 1) * 128], idb[0:SB_, 0:SB_],
                                r=[xs.b, idb.b], w=[ps.b])
                    self.cp("act", xsT[:, :, jb * SB_:(jb + 1) * SB_],
                            psv[:, :].rearrange("p (k n) -> p k n", k=8)[:, :, 0:SB_], r=[ps.b], w=[xsT.b])
                for fc in range(8):
                    psg = self.nps()
                    psu = self.nps()
                    for kc in range(8):
                        self.mm(psg[:, 0:GC], wg[:, kc, fc * 128:(fc + 1) * 128], xsT[:, kc, :], start=(kc == 0), stop=(kc == 7),
                                r=[wg.b, xsT.b], w=[psg.b])
                    for kc in range(8):
                        self.mm(psu[:, 0:GC], wu[:, kc, fc * 128:(fc + 1) * 128], xsT[:, kc, :], start=(kc == 0), stop=(kc == 7),
                                r=[wu.b, xsT.b], w=[psu.b])
                    self.act(sl_[:], psg[:, 0:GC], AF.Silu, r=[psg.b], w=[sl_.b])
                    self.tt("dve", hid[:, fc, :], sl_[:], psu[:, 0:GC], ALU.mult, r=[sl_.b, psu.b], w=[hid.b])
                for j in range(G):
                    s = g * G + j
                    pcol = 32 * s + e
                    new_sc = []
                    for blk in range(NB):
                        jb = j * NB + blk
                        ys = yss[jb]
                        for half in range(2):
                            ps = self.nps()
                            for fc in range(8):
                                self.mm(ps[0:SB_, :], hid[:, fc, jb * SB_:(jb + 1) * SB_], wd[:, fc, half * 512:(half + 1) * 512],
                                        start=(fc == 0), stop=(fc == 7), r=[hid.b, wd.b], w=[ps.b])
                            self.act(ys[0:SB_, half * 512:(half + 1) * 512], ps[0:SB_, :], AF.Copy,
                                     scale=valT[0:SB_, blk, pcol:pcol + 1], r=[ps.b, valT.b], w=[ys.b])
                        ia = idxT[0:SB_, blk, pcol:pcol + 1]
                        op = self.P.emit("pool", lambda en, i_=ys[0:SB_, :], ia=ia: en.indirect_dma_start(
                            out=acc, out_offset=bass.IndirectOffsetOnAxis(ap=ia, axis=0), in_=i_, in_offset=None,
                            compute_op=ALU.add), [idxT.b, ys.b, B_acc[s]], [], dma=True)
                        for d_ in last_sc[s]:
                            self.P._dep(op, d_)
                        new_sc.append(op)
                    last_sc[s] = new_sc
            P.stage_end()

    def phase_c(self, env):
        P, NS, T, TT = self.P, self.NS, self.T, self.TT
        acc, y, B_acc = env["acc"], env["y"], env["B_acc"]
        sb = self.sb
        outs = []
        with ExitStack() as st:
            gF = sb(st, "gF", [128, D], F32)
            self.dma(gF.t[:], env["gF_d"].partition_broadcast(128), w=[gF.b])
            NW = 4
            ats = [sb(st, f"at{i}", [128, D], F32) for i in range(NW)]
            ots = [sb(st, f"ot{i}", [128, D], F32) for i in range(NW)]
            junks = [sb(st, f"junk3_{i}", [128, D], F32) for i in range(2)]
            sst = [sb(st, f"sc{i}", [128, 4], F32) for i in range(NW)]

            def c_iter(n, s, tt_):
                at, ot, ss, junk = ats[n], ots[n], sst[n], junks[n % 2]
                r0 = s * T + tt_ * 128
                self.dma(at.t[:], acc[r0:r0 + 128, :], r=[B_acc[s]], w=[at.b])
                yield
                self.act(junk[:], at[:], AF.Square, accum=ss[:, 0:1], r=[at.b], w=[junk.b, ss.b])
                self.ts("dve", ss[:, 1:2], ss[:, 0:1], 1.0 / D, NORM_EPS, ALU.mult, ALU.add, r=[ss.b], w=[ss.b])
                self.act(ss[:, 2:3], ss[:, 1:2], AF.Sqrt, r=[ss.b], w=[ss.b])
                self.P.emit("dve", lambda e, o=ss[:, 3:4], i=ss[:, 2:3]: e.reciprocal(out=o, in_=i), [ss.b], [ss.b])
                yield
                self.stt(ot[:], at[:], ss[:, 3:4], gF[:], ALU.mult, ALU.mult, r=[at.b, ss.b, gF.b], w=[ot.b])
                self.P.emit("sp", lambda e, o=y[r0:r0 + 128, :], i=ot[:]: e.dma_start(out=o, in_=i), [ot.b], [], dma=True)
                yield

            fin_b = P.buf("yout")
            gens = []
            n = 0
            for s in range(NS):
                for tt_ in range(TT):
                    gens.append(lambda slot, s=s, tt_=tt_: c_iter(slot, s, tt_))
                    n += 1
            self.interleave(gens, window=NW)
            outs = []
            fin = P.buf("fin")
            op = P.emit("sp", lambda en: en.nop(), writes=[fin])
            for d_ in outs:
                P._dep(op, d_)
            P.stage_end()


def host_consts(T):
    c = {}
    c["c_idf"] = np.eye(128, dtype=np.float32)
    blk = (np.arange(128)[:, None] // 64 == np.arange(128)[None, :] // 64).astype(np.float32)
    c["c_bo"] = blk
    c["c_ba"] = blk / 64.0
    s = (np.arange(128) % 64)[:, None]
    j = np.arange(128)[None, :]
    t = j % 64
    isr = j >= 64
    mT = np.zeros((128, 2, 4, 128), np.float32)
    mT[:, 0] = np.where(isr, s <= t, s < t)[:, None, :]
    mT[:, 1] = np.where(isr, s >= t, s > t)[:, None, :]
    c["c_mT"] = mT.reshape(128, 2, 512)
    tt = (np.arange(128) % 64)[:, None]
    ss = np.arange(64)[None, :]
    mN = np.zeros((128, 2, 8, 64), np.float32)
    mN[:, 0] = (ss < tt)[:, None, :]
    mN[:, 1] = (ss > tt)[:, None, :]
    c["c_mN"] = mN.reshape(128, 2, 512)
    idr = np.zeros((128, 8, 64), np.float32)
    idr[:] = (ss == tt)[:, None, :]
    c["c_idr"] = idr.reshape(128, 512)
    slopes = 2.0 ** (-8.0 * np.arange(1, 9) / 8)
    key = np.arange(128)[:, None]
    qq = np.arange(128)[None, :]
    bias = np.zeros((128, 3, 2, 4, 128), np.float32)
    for rel in (-1, 0, 1):
        dist = np.abs(rel * 128 + key - qq)
        for kv in range(2):
            for sg in range(4):
                g = 2 * (sg % 2) + sg // 2
                bias[:, rel + 1, kv, sg, :] = np.where(dist <= 128, -slopes[kv * 4 + g] * dist, -1e30)
    c["c_bias"] = bias.reshape(128, 6, 512)
    offs = np.zeros((128, 128), np.float32)
    offs[:] = ((np.arange(128) // 32) * T)[None, :]
    c["c_offs"] = offs
    return c


def host_params(inp):
    f = lambda a: np.ascontiguousarray(np.asarray(a, dtype=np.float32))
    m = {}
    m["w_in"] = f(inp["w_in"][0])
    m["gmixr"] = f(inp["norm_mix_g"][0].reshape(1, D))
    m["g2r"] = f(inp["norm_ffn_g"][0].reshape(1, D))
    m["mu"] = f(np.stack([inp["mu_prev"][0].reshape(15, 128).T, inp["mu_next"][0].reshape(15, 128).T], axis=-1))
    names = ["w0_f", "w0_b", "a0_f", "a0_b", "k_k", "k_a", "r_k", "ln_x_w", "ln_x_b"]
    m["pp"] = f(np.stack([np.asarray(inp[n][0]).reshape(4, 128).T for n in names], axis=-1))
    m["sink"] = f(inp["attn_sink"][0].reshape(1, 8))
    m["g2"] = f(inp["norm_ffn_g"][0].reshape(8, 128).T)
    m["gF"] = f(np.asarray(inp["norm_final_g"]).reshape(1, D))
    m["wup"] = f(np.concatenate([inp["w_up_f"][0], inp["w_up_b"][0]], axis=0))
    m["aup"] = f(np.concatenate([inp["a_up_f"][0], inp["a_up_b"][0]], axis=0))
    m["gup"] = f(inp["g_up"][0])
    m["wpr"] = f(inp["w_proj_rwkv"][0])
    m["wpa"] = f(inp["w_proj_attn"][0])
    m["wout"] = f(inp["w_out"][0])
    m["wr"] = f(inp["w_router"][0])
    m["eg"] = f(inp["exp_w_gate"][0])
    m["eu"] = f(inp["exp_w_up"][0])
    m["ed"] = f(inp["exp_w_down"][0])
    return m


_NC_CACHE = {}


def run(inp, n_cores=8, stop=None, dbg=False, raw=False):
    x = np.asarray(inp["x"], dtype=np.float32)
    B, T, _ = x.shape
    NS = B // n_cores
    key = (NS, T, stop, dbg)
    if key not in _NC_CACHE:
        _NC_CACHE[key] = Builder(NS, T, stop, dbg).build()
    nc = _NC_CACHE[key]
    shared = host_params(inp)
    shared.update(host_consts(T))
    in_maps = []
    for c in range(n_cores):
        m = dict(shared)
        m["x"] = np.ascontiguousarray(x[c * NS:(c + 1) * NS].reshape(NS * T, D))
        in_maps.append(m)
    res = run_bass_kernel_spmd(nc, in_maps, core_ids=list(range(n_cores)))
    if raw:
        return res.results
    out = np.concatenate([r["y"].reshape(NS, T, D) for r in res.results], axis=0)
    return out.astype(np.float32)


def kernel(**inputs):
    return run(inputs, 8)
```
